# Optimizing a Trainium2 kernel written in Bass

```python
import math
import jax
import jax.numpy as jnp
from jax import lax
import numpy as np

D_MODEL = 1024
BATCH = 8
SEQ = 4096
DEPTH = 2

BRANCH_W = D_MODEL // 2
N_BRANCH = 3
NSA_HEADS = 8
NSA_GROUPS = 2
NSA_HPG = NSA_HEADS // NSA_GROUPS
NSA_DH = BRANCH_W // NSA_HEADS
NSA_KV = NSA_GROUPS * NSA_DH
CMP_BLOCK = 32
CMP_STRIDE = 16
CMP_HIDDEN = 2 * NSA_DH
SEL_BLOCK = 64
SEL_TOPK = 8
WINDOW = 512
Q_BLOCK = 128
RET_HEADS = 4
RET_DV = BRANCH_W // RET_HEADS
RET_DK = RET_DV // 2
RET_CHUNK = 128
ROPE_BASE = 10000.0
CONV_CH = BRANCH_W
CONV_WIDTH = 31
REL_BUCKETS = 32
REL_MAX_DIST = 128
D_FF = 4 * D_MODEL
EPS = 1e-6
IN_WIDTHS = (NSA_HEADS * NSA_DH, NSA_KV, NSA_KV, NSA_KV, NSA_KV, NSA_KV, NSA_KV, 3 * NSA_HEADS,
             RET_HEADS * RET_DK, RET_HEADS * RET_DK, RET_HEADS * RET_DV, RET_HEADS * RET_DV,
             CONV_CH, CONV_CH, N_BRANCH * D_MODEL)
IN_TOTAL = sum(IN_WIDTHS)

kernel_name = 'hybrid_nsa_retention_conformer_block'


def _rmsnorm(x, g):
    x32 = x.astype(jnp.float32)
    y = x32 * lax.rsqrt(jnp.mean(x32 * x32, axis=-1, keepdims=True) + EPS)
    return (y * g.astype(jnp.float32)).astype(x.dtype)


def _layernorm(x, g, b=None):
    x32 = x.astype(jnp.float32)
    mu = jnp.mean(x32, axis=-1, keepdims=True)
    var = jnp.mean(jnp.square(x32 - mu), axis=-1, keepdims=True)
    y = (x32 - mu) * lax.rsqrt(var + EPS) * g.astype(jnp.float32)
    if b is not None:
        y = y + b.astype(jnp.float32)
    return y


def _split_cols(z):
    parts = []
    start = 0
    for w in IN_WIDTHS:
        parts.append(z[..., start:start + w])
        start += w
    return parts


def _rel_bucket(dist):
    n = jnp.maximum(dist, 0)
    max_exact = REL_BUCKETS // 2
    nf = jnp.maximum(n, 1).astype(jnp.float32)
    large = max_exact + (jnp.log(nf / max_exact) / math.log(REL_MAX_DIST / max_exact)
                         * (REL_BUCKETS - max_exact)).astype(jnp.int32)
    large = jnp.minimum(large, REL_BUCKETS - 1)
    return jnp.where(n < max_exact, n, large)


def _masked_softmax(s, mask):
    s = jnp.where(mask, s, -1e30)
    m = jnp.max(s, axis=-1, keepdims=True)
    p = jnp.exp(s - m) * mask
    den = jnp.sum(p, axis=-1, keepdims=True)
    return p / jnp.maximum(den, 1e-30)


def _rotary(x, pos):
    half = x.shape[-1] // 2
    inv = ROPE_BASE ** (-jnp.arange(half, dtype=jnp.float32) / half)
    ang = pos[:, None] * inv[None, :]
    cos = jnp.cos(ang)[:, None, :]
    sin = jnp.sin(ang)[:, None, :]
    x32 = x.astype(jnp.float32)
    x1, x2 = x32[..., :half], x32[..., half:]
    return jnp.concatenate([x1 * cos - x2 * sin, x1 * sin + x2 * cos], axis=-1).astype(x.dtype)


def _compress(x, pe, w1, w2):
    B, S, G, DH = x.shape
    n_ch = S // CMP_STRIDE
    r = CMP_BLOCK // CMP_STRIDE
    n_c = n_ch - r + 1
    ch = x.reshape(B, n_ch, CMP_STRIDE, G, DH)
    blocks = jnp.concatenate([ch[:, i:i + n_c] for i in range(r)], axis=2) + pe[None, None, :, None, :]
    hid = jax.nn.gelu(jnp.einsum('bnlgd,ldf->bngf', blocks, w1))
    return jnp.einsum('bngf,fd->bngd', hid, w2)


def _nsa(q, kc, vc, ks, vs, kw, vw, gl, pe_k, w1_k, w2_k, pe_v, w1_v, w2_v, rel_table):
    B, S, _ = q.shape
    G, HPG, DH = NSA_GROUPS, NSA_HPG, NSA_DH
    dt = q.dtype
    q = q.reshape(B, S, G, HPG, DH) * (DH ** -0.5)
    kc, vc, ks, vs, kw, vw = [a.reshape(B, S, G, DH) for a in (kc, vc, ks, vs, kw, vw)]
    k_cmp = _compress(kc, pe_k, w1_k, w2_k)
    v_cmp = _compress(vc, pe_v, w1_v, w2_v)
    n_c = k_cmp.shape[1]
    cmp_start = jnp.arange(n_c) * CMP_STRIDE
    cmp_end = cmp_start + (CMP_BLOCK - 1)
    n_s = S // SEL_BLOCK
    k_eff = min(SEL_TOPK, n_s)
    sel_start = jnp.arange(n_s) * SEL_BLOCK
    overlap = ((cmp_start[:, None] <= sel_start[None, :] + SEL_BLOCK - 1)
               & (cmp_end[:, None] >= sel_start[None, :])).astype(jnp.float32)
    ks_blk = ks.reshape(B, n_s, SEL_BLOCK, G, DH).transpose(0, 3, 1, 2, 4)
    vs_blk = vs.reshape(B, n_s, SEL_BLOCK, G, DH).transpose(0, 3, 1, 2, 4)
    kw_pad = jnp.pad(kw, ((0, 0), (WINDOW, 0), (0, 0), (0, 0)))
    vw_pad = jnp.pad(vw, ((0, 0), (WINDOW, 0), (0, 0), (0, 0)))
    tb = rel_table.reshape(REL_BUCKETS, G, HPG).transpose(1, 0, 2)
    b_idx = jnp.arange(B)[:, None, None, None]
    g_idx = jnp.arange(G)[None, :, None, None]
    sb_ar = jnp.arange(SEL_BLOCK)

    def head_bias(dist):
        bias = rel_table[_rel_bucket(dist)]
        return jnp.transpose(bias, (2, 0, 1)).reshape(G, HPG, dist.shape[0], dist.shape[1]).astype(jnp.float32)

    def block_fn(args):
        c, qb, gb = args
        t = c * Q_BLOCK + jnp.arange(Q_BLOCK)
        dist_c = t[:, None] - cmp_end[None, :]
        s_c = jnp.einsum('bqghd,bngd->bghqn', qb, k_cmp).astype(jnp.float32) + head_bias(dist_c)
        p_c = _masked_softmax(s_c, dist_c >= 0)
        o_c = jnp.einsum('bghqn,bngd->bqghd', p_c, v_cmp)
        imp = jnp.einsum('bghqn,ns->bgqs', p_c, overlap)
        jb = jnp.arange(n_s)[None, :]
        cur = (t // SEL_BLOCK)[:, None]
        valid = jb <= cur
        forced = (jb == 0) | (jb == cur) | (jb == cur - 1)
        score = jnp.where(forced, imp + 1e4, jnp.where(valid, imp, -1e4))
        _, idx = lax.top_k(score, k_eff)
        kg = ks_blk[b_idx, g_idx, idx]
        vg = vs_blk[b_idx, g_idx, idx]
        pos_s = idx[..., None] * SEL_BLOCK + sb_ar
        dist_s = t[None, None, :, None, None] - pos_s
        bias_s = tb[g_idx[..., None], _rel_bucket(dist_s)]
        s_s = jnp.einsum('bqghd,bgqkld->bghqkl', qb, kg).astype(jnp.float32) + jnp.moveaxis(bias_s, -1, 2).astype(jnp.float32)
        mask_s = (dist_s >= 0)[:, :, None]
        p_s = _masked_softmax(s_s.reshape(B, G, HPG, Q_BLOCK, k_eff * SEL_BLOCK),
                              mask_s.reshape(B, G, 1, Q_BLOCK, k_eff * SEL_BLOCK))
        p_s = p_s.reshape(B, G, HPG, Q_BLOCK, k_eff, SEL_BLOCK)
        o_s = jnp.einsum('bghqkl,bgqkld->bqghd', p_s, vg)
        start = c * Q_BLOCK
        kwb = lax.dynamic_slice_in_dim(kw_pad, start, WINDOW + Q_BLOCK, axis=1)
        vwb = lax.dynamic_slice_in_dim(vw_pad, start, WINDOW + Q_BLOCK, axis=1)
        pos_w = start - WINDOW + jnp.arange(WINDOW + Q_BLOCK)
        dist_w = t[:, None] - pos_w[None, :]
        mask_w = (dist_w >= 0) & (dist_w < WINDOW) & (pos_w[None, :] >= 0)
        s_w = jnp.einsum('bqghd,bkgd->bghqk', qb, kwb).astype(jnp.float32) + head_bias(dist_w)
        p_w = _masked_softmax(s_w, mask_w)
        o_w = jnp.einsum('bghqk,bkgd->bqghd', p_w, vwb)
        gs = jax.nn.sigmoid(gb.astype(jnp.float32)).reshape(B, Q_BLOCK, G, HPG, 3)
        o = gs[..., 0:1] * o_c + gs[..., 1:2] * o_s + gs[..., 2:3] * o_w
        return o.astype(dt)

    n_qb = S // Q_BLOCK
    q_blocks = q.reshape(B, n_qb, Q_BLOCK, G, HPG, DH).transpose(1, 0, 2, 3, 4, 5)
    g_blocks = gl.reshape(B, n_qb, Q_BLOCK, NSA_HEADS, 3).transpose(1, 0, 2, 3, 4)
    out = lax.map(block_fn, (jnp.arange(n_qb), q_blocks, g_blocks))
    return out.transpose(1, 0, 2, 3, 4, 5).reshape(B, S, NSA_HEADS * DH)


def _retention(q, k, v, g, gn_gain):
    B, S, _ = q.shape
    H, DK, DV, C = RET_HEADS, RET_DK, RET_DV, RET_CHUNK
    dt = q.dtype
    pos = jnp.arange(S, dtype=jnp.float32)
    q = _rotary(q.reshape(B, S, H, DK), pos)
    k = _rotary(k.reshape(B, S, H, DK), pos) * (DK ** -0.5)
    v = v.reshape(B, S, H, DV)
    log_g = jnp.log1p(-jnp.exp2(-5.0 - jnp.arange(H, dtype=jnp.float32)))
    N = S // C
    qc = q.reshape(B, N, C, H, DK)
    kc = k.reshape(B, N, C, H, DK)
    vc = v.reshape(B, N, C, H, DV)
    ar = jnp.arange(C)
    diff = ar[:, None] - ar[None, :]
    decay = jnp.where(diff >= 0, jnp.exp(log_g[:, None, None] * jnp.maximum(diff, 0).astype(jnp.float32)), 0.0)
    inner = jnp.einsum('bnchd,bnehd->bnhce', qc, kc).astype(jnp.float32) * decay
    o_inner = jnp.einsum('bnhce,bnehv->bnchv', inner, vc)
    zeta = jnp.exp(log_g[:, None] * (C - 1 - ar).astype(jnp.float32))
    xi = jnp.exp(log_g[:, None] * (ar + 1).astype(jnp.float32))
    kv = jnp.einsum('bnchd,hc,bnchv->bnhdv', kc, zeta, vc)
    g_chunk = jnp.exp(log_g * C)[None, :, None, None]

    def step(state, kv_n):
        return g_chunk * state + kv_n, state

    init = jnp.zeros((B, H, DK, DV), kv.dtype)
    _, prev = lax.scan(step, init, kv.transpose(1, 0, 2, 3, 4))
    prev = prev.transpose(1, 0, 2, 3, 4)
    o_cross = jnp.einsum('bnchd,hc,bnhdv->bnchv', qc, xi, prev)
    o = (o_inner + o_cross).reshape(B, S, H, DV)
    o = _layernorm(o, jnp.ones((DV,), jnp.float32)).reshape(B, S, H * DV) * gn_gain.astype(jnp.float32)
    return (jax.nn.silu(g.astype(jnp.float32)) * o).astype(dt)


def _conv_module(a, b, dw_w, dw_b, ln_g, ln_b):
    u = a * jax.nn.sigmoid(b)
    kern = dw_w[:, None, :].astype(u.dtype)
    y = lax.conv_general_dilated(u, kern, window_strides=(1,), padding=[(CONV_WIDTH - 1, 0)],
                                 dimension_numbers=('NWC', 'WIO', 'NWC'), feature_group_count=CONV_CH)
    y = y + dw_b
    y = _layernorm(y, ln_g, ln_b)
    return jax.nn.silu(y).astype(a.dtype)


def setup_inputs(seed: int = 0) -> dict:
    key = jax.random.key(seed)
    k = jax.random.split(key, 21)
    f32 = jnp.float32

    def nrm(kk, shape, scale):
        return jax.random.normal(kk, shape, f32) * scale

    return {
        'x': nrm(k[0], (BATCH, SEQ, D_MODEL), 1.0),
        'rel_table': nrm(k[1], (REL_BUCKETS, NSA_HEADS), 0.5),
        'norm_mix': 1.0 + nrm(k[2], (DEPTH, D_MODEL), 0.05),
        'w_in': nrm(k[3], (DEPTH, D_MODEL, IN_TOTAL), D_MODEL ** -0.5),
        'cmp_pe_k': nrm(k[4], (DEPTH, CMP_BLOCK, NSA_DH), 0.5),
        'cmp_w1_k': nrm(k[5], (DEPTH, CMP_BLOCK, NSA_DH, CMP_HIDDEN), (CMP_BLOCK * NSA_DH) ** -0.5),
        'cmp_w2_k': nrm(k[6], (DEPTH, CMP_HIDDEN, NSA_DH), CMP_HIDDEN ** -0.5),
        'cmp_pe_v': nrm(k[7], (DEPTH, CMP_BLOCK, NSA_DH), 0.5),
        'cmp_w1_v': nrm(k[8], (DEPTH, CMP_BLOCK, NSA_DH, CMP_HIDDEN), (CMP_BLOCK * NSA_DH) ** -0.5),
        'cmp_w2_v': nrm(k[9], (DEPTH, CMP_HIDDEN, NSA_DH), CMP_HIDDEN ** -0.5),
        'ret_gn': 1.0 + nrm(k[10], (DEPTH, RET_HEADS * RET_DV), 0.05),
        'conv_w': nrm(k[11], (DEPTH, CONV_WIDTH, CONV_CH), CONV_WIDTH ** -0.5),
        'conv_b': nrm(k[12], (DEPTH, CONV_CH), 0.02),
        'conv_ln_g': 1.0 + nrm(k[13], (DEPTH, CONV_CH), 0.05),
        'conv_ln_b': nrm(k[14], (DEPTH, CONV_CH), 0.02),
        'w_branch': nrm(k[15], (DEPTH, N_BRANCH, BRANCH_W, D_MODEL), BRANCH_W ** -0.5),
        'w_out': nrm(k[16], (DEPTH, D_MODEL, D_MODEL), D_MODEL ** -0.5),
        'norm_mlp': 1.0 + nrm(k[17], (DEPTH, D_MODEL), 0.05),
        'w_ff1': nrm(k[18], (DEPTH, D_MODEL, D_FF), D_MODEL ** -0.5),
        'w_ff2': nrm(k[19], (DEPTH, D_FF, D_MODEL), D_FF ** -0.5),
        'norm_final': 1.0 + nrm(k[20], (D_MODEL,), 0.05),
    }


def reference(x, rel_table, norm_mix, w_in, cmp_pe_k, cmp_w1_k, cmp_w2_k, cmp_pe_v, cmp_w1_v, cmp_w2_v,
              ret_gn, conv_w, conv_b, conv_ln_g, conv_ln_b, w_branch, w_out, norm_mlp, w_ff1, w_ff2,
              norm_final):
    B, S, _ = x.shape
    for l in range(DEPTH):
        h = _rmsnorm(x, norm_mix[l])
        z = h @ w_in[l]
        (q_n, kc, vc, ks, vs, kw, vw, g_n, q_r, k_r, v_r, g_r, c_a, c_b, m_g) = _split_cols(z)
        o_nsa = _nsa(q_n, kc, vc, ks, vs, kw, vw, g_n, cmp_pe_k[l], cmp_w1_k[l], cmp_w2_k[l],
                     cmp_pe_v[l], cmp_w1_v[l], cmp_w2_v[l], rel_table)
        o_ret = _retention(q_r, k_r, v_r, g_r, ret_gn[l])
        o_conv = _conv_module(c_a, c_b, conv_w[l], conv_b[l], conv_ln_g[l], conv_ln_b[l])
        br = jnp.stack([o_nsa.astype(x.dtype), o_ret.astype(x.dtype), o_conv.astype(x.dtype)], axis=2)
        proj = jnp.einsum('bsnc,ncd->bsnd', br, w_branch[l])
        gate = jax.nn.sigmoid(m_g.astype(jnp.float32)).reshape(B, S, N_BRANCH, D_MODEL)
        merged = jnp.sum(gate * proj, axis=2).astype(x.dtype)
        x = x + merged @ w_out[l]
        h2 = _rmsnorm(x, norm_mlp[l])
        x = x + jnp.square(jax.nn.relu(h2 @ w_ff1[l])) @ w_ff2[l]
    return _rmsnorm(x, norm_final)
```

```python
import numpy as np
from contextlib import ExitStack
import concourse.bass as bass
import concourse.mybir as mybir
from concourse.bass_utils import run_bass_kernel_spmd

F32 = mybir.dt.float32
BF16 = mybir.dt.bfloat16
AF = mybir.ActivationFunctionType
ALU = mybir.AluOpType
AX = mybir.AxisListType

S = 4096
D = 1024
NT = 32
DEPTH = 2
IN_TOTAL = 6936
EPS = 1e-6
NEG = -30000.0
C_QN, C_KC, C_VC, C_KS, C_VS, C_KW, C_VW, C_GN = 0, 512, 640, 768, 896, 1024, 1152, 1280
C_QR, C_KR, C_VR, C_GR, C_CA, C_CB, C_MG = 1304, 1560, 1816, 2328, 2840, 3352, 3864

SAME_ENGINE_SYNC = True


class Buf:
    __slots__ = ("w", "r", "name", "wd")

    def __init__(self, name=""):
        self.w = None
        self.r = {}
        self.name = name
        self.wd = False


class _Sem:
    def __init__(self, sem, name):
        self.sem = sem
        self.count = 0
        self.name = name


class Eng(_Sem):
    def __init__(self, name, handle, sem):
        super().__init__(sem, name)
        self.h = handle
        self.waited = {}


class K:
    def __init__(self, nc, stack, n_dma_sems=40):
        self.nc = nc
        self.engs = {}
        for name, h in (("pe", nc.tensor), ("act", nc.scalar), ("dve", nc.vector),
                        ("pool", nc.gpsimd), ("sp", nc.sync)):
            sem = stack.enter_context(nc.semaphore("sem_" + name))
            self.engs[name] = Eng(name, h, sem)
        self.dsems = [_Sem(stack.enter_context(nc.semaphore("dsem%d" % i)), "d%d" % i)
                      for i in range(n_dma_sems)]
        self.dnext = 0
        self.n_ops = 0
        self.muted = False

    def _wait_deps(self, E, reads, writes):
        deps = {}

        def need(tok):
            if tok is None:
                return
            s, v = tok
            if deps.get(s, 0) < v:
                deps[s] = v
        for b in reads:
            for t in b.w or ():
                need(t)
        for b in writes:
            for t in b.w or ():
                need(t)
            for t in b.r.values():
                need(t)
        for s, v in deps.items():
            if s is E and (E.name in ("pe", "sp") or not SAME_ENGINE_SYNC):
                continue
            if E.waited.get(s, 0) < v:
                E.h.wait_ge(s.sem, v)
                E.waited[s] = v

    def _mark(self, tok, reads, writes, is_dma=False):
        for b in reads:
            b.r[tok[0]] = tok
        for b in writes:
            if is_dma and b.w and getattr(b, "wd", False) and len(b.w) < 24:
                b.w = b.w + [tok]
            else:
                b.w = [tok]
            b.wd = is_dma
            b.r = {}

    def op(self, en, fn, reads=(), writes=()):
        if self.muted:
            return None
        E = self.engs[en]
        self._wait_deps(E, reads, writes)
        ins = fn(E.h)
        E.count += 1
        ins.then_inc(E.sem, 1)
        self._mark((E, E.count), reads, writes)
        self.n_ops += 1
        return ins

    def dma(self, en, out, in_, reads=(), writes=(), **kw):
        if self.muted:
            return None
        E = self.engs[en]
        self._wait_deps(E, reads, writes)
        d = self.dsems[self.dnext]
        self.dnext = (self.dnext + 1) % len(self.dsems)
        if d.count and E.waited.get(d, 0) < d.count:
            E.h.wait_ge(d.sem, d.count)
            E.waited[d] = d.count
        ins = E.h.dma_start(out=out, in_=in_, **kw)
        d.count += 16
        ins.then_inc(d.sem, 16)
        self._mark((d, d.count), reads, writes, is_dma=True)
        self.n_ops += 1
        return ins

    def barrier(self):
        allsems = list(self.engs.values()) + self.dsems
        for E in self.engs.values():
            for s in allsems:
                if s is E or s.count == 0:
                    continue
                if E.waited.get(s, 0) < s.count:
                    E.h.wait_ge(s.sem, s.count)
                    E.waited[s] = s.count


def _rel_bucket_np(dist):
    n = np.maximum(dist, 0)
    nf = np.maximum(n, 1).astype(np.float32)
    large = 16 + (np.log(nf / np.float32(16)) / np.float32(np.log(8.0)) * np.float32(16)).astype(np.int32)
    large = np.minimum(large, 31)
    return np.where(n < 16, n, large).astype(np.int64)


_CONST_CACHE = {}


def _host_consts():
    if _CONST_CACHE:
        return _CONST_CACHE
    c = {}
    i = np.arange(128)
    c["ident"] = np.eye(128, dtype=np.float32)
    c["i4"] = np.tile(np.eye(128, dtype=np.float32), (1, 4))
    mw = np.zeros((128, 5, 128), np.float32)
    jj, ii = np.meshgrid(i, i, indexing="ij")
    mw[:, 0, :] = (ii >= jj)
    mw[:, 1:4, :] = 1.0
    mw[:, 4, :] = (ii < jj)
    c["maskw"] = mw
    dist_w = 128 * np.arange(5)[None, :, None] + ii[:, None, :] - jj[:, None, :]
    c["_bucket_w"] = _rel_bucket_np(dist_w)
    m = np.arange(504)
    dist_c = i[None, :] - 16 * (m[:, None] - 248) - 31
    c["maskc"] = (dist_c >= 0).astype(np.float32)
    c["_bucket_c"] = _rel_bucket_np(dist_c)
    n = np.arange(256)
    s = np.arange(64)
    ov = ((16 * n[:, None] <= 64 * s[None, :] + 63) & (16 * n[:, None] + 31 >= 64 * s[None, :])).astype(np.float32)
    ov[255] = 0.0
    c["overlap"] = ov.reshape(2, 128, 64).transpose(1, 0, 2).copy()
    j = np.arange(126)
    sp = j[None, :] - 62
    cur = (i[:, None] >= 64).astype(np.int64)
    valid = sp <= cur
    forced = (sp == cur) | (sp == cur - 1)
    c["selvalid"] = valid.astype(np.float32)
    c["seladd"] = np.where(forced, 1e4, np.where(valid, 0.0, -1e4)).astype(np.float32)
    half = 32
    inv = (10000.0 ** (-np.arange(half, dtype=np.float32) / half)).astype(np.float32)
    pos = np.arange(S, dtype=np.float32)
    ang = (pos[:, None] * inv[None, :]).astype(np.float32)
    c["cos"] = np.cos(ang).astype(np.float32).reshape(NT, 128, 32).transpose(1, 0, 2).copy()
    c["sin"] = np.sin(ang).astype(np.float32).reshape(NT, 128, 32).transpose(1, 0, 2).copy()
    log_g = np.log1p(-np.exp2(-5.0 - np.arange(4, dtype=np.float32))).astype(np.float32)
    diff = i[None, :] - i[:, None]
    dec = np.where(diff[None] >= 0, np.exp(log_g[:, None, None] * np.maximum(diff[None], 0)), 0.0)
    c["decayT"] = dec.transpose(1, 0, 2).astype(np.float32).copy()
    xi = np.exp(log_g[:, None] * (i[None, :] + 1)).astype(np.float32)
    c["xi"] = np.ascontiguousarray(np.broadcast_to(xi[None, :, :], (64, 4, 128))).astype(np.float32)
    c["zeta"] = np.exp(log_g[None, :] * (127 - i[:, None])).astype(np.float32)
    gch = np.exp(log_g * 128).astype(np.float32)
    c["gch"] = np.ascontiguousarray(np.broadcast_to(gch[None, :], (64, 4))).astype(np.float32)
    c["ones"] = np.ones((128, 128), np.float32)
    _CONST_CACHE.update(c)
    return c


CONST_SHAPES = {
    "ident": [128, 128], "i4": [128, 512], "maskw": [128, 5, 128], "maskc": [504, 128],
    "overlap": [128, 2, 64], "selvalid": [128, 126], "seladd": [128, 126],
    "cos": [128, NT, 32], "sin": [128, NT, 32], "decayT": [128, 4, 128], "xi": [64, 4, 128],
    "zeta": [128, 4], "gch": [64, 4], "ones": [128, 128],
    "bias_w": [128, 5, 8, 128], "bias_c": [504, 8, 128], "b31": [128, 8],
}

W_SHAPES = {
    "w_in": [DEPTH, D, IN_TOTAL], "w_branch": [DEPTH, 3, 512, D], "w_out": [DEPTH, D, D],
    "w_ff1": [DEPTH, D, 4 * D], "w_ff2": [DEPTH, 4 * D, D],
    "cmp_w1_k": [DEPTH, 32, 64, 128], "cmp_w1_v": [DEPTH, 32, 64, 128],
    "cmp_w2_k": [DEPTH, 128, 64], "cmp_w2_v": [DEPTH, 128, 64],
    "cmp_pe_kT": [DEPTH, 64, 32], "cmp_pe_vT": [DEPTH, 64, 32],
    "nmix": [DEPTH, 128, D], "nmlp": [DEPTH, 128, D], "nfin": [128, D],
    "retgn": [DEPTH, 128, 512],
    "convw": [DEPTH, 128, 4, 31], "convb": [DEPTH, 128, 4], "convg": [DEPTH, 128, 4], "convbb": [DEPTH, 128, 4],
}


def _host_inputs(inp):
    c = _host_consts()
    f = lambda a: np.ascontiguousarray(a, dtype=np.float32)
    out = {k: f(v) for k, v in c.items() if not k.startswith("_")}
    rt = f(inp["rel_table"])
    out["bias_w"] = f(rt[c["_bucket_w"]].transpose(0, 1, 3, 2))
    out["bias_c"] = f(rt[c["_bucket_c"]].transpose(0, 2, 1))
    out["b31"] = f(np.broadcast_to(rt[31][None, :], (128, 8)))
    for kname in ("w_in", "w_branch", "w_out", "w_ff1", "w_ff2", "cmp_w1_k", "cmp_w1_v", "cmp_w2_k", "cmp_w2_v"):
        out[kname] = f(inp[kname])
    out["cmp_pe_kT"] = f(np.transpose(inp["cmp_pe_k"], (0, 2, 1)))
    out["cmp_pe_vT"] = f(np.transpose(inp["cmp_pe_v"], (0, 2, 1)))
    out["nmix"] = f(np.broadcast_to(inp["norm_mix"][:, None, :], (DEPTH, 128, D)))
    out["nmlp"] = f(np.broadcast_to(inp["norm_mlp"][:, None, :], (DEPTH, 128, D)))
    out["nfin"] = f(np.broadcast_to(inp["norm_final"][None, :], (128, D)))
    out["retgn"] = f(np.broadcast_to(inp["ret_gn"][:, None, :], (DEPTH, 128, 512)))
    out["convw"] = f(np.transpose(inp["conv_w"].reshape(DEPTH, 31, 4, 128), (0, 3, 2, 1)))
    for a, b in (("convb", "conv_b"), ("convg", "conv_ln_g"), ("convbb", "conv_ln_b")):
        out[a] = f(np.transpose(inp[b].reshape(DEPTH, 4, 128), (0, 2, 1)))
    return out


class _Stop(Exception):
    pass


class Prog:
    def __init__(self, n_layers=DEPTH, phases=("nsa", "ret", "conv", "merge", "ffn"), dbg=False):
        self.n_layers = n_layers
        self.phases = phases
        self.dbg = dbg
        nc = self.nc = bass.Bass("TRN2", target_bir_lowering=False)
        self.din = {}
        self.din["x"] = nc.dram_tensor("x", [S, D], F32, kind="ExternalInput").ap()
        for name, shp in list(CONST_SHAPES.items()) + list(W_SHAPES.items()):
            self.din[name] = nc.dram_tensor(name, shp, F32, kind="ExternalInput").ap()
        self.out = nc.dram_tensor("out", [S, D], F32, kind="ExternalOutput").ap()
        skind = "ExternalOutput" if dbg else "Internal"
        self.xres = nc.dram_tensor("xres", [S, D], F32, kind=skind).ap()
        self.brT = nc.dram_tensor("brT", [3, 4, 128, S], BF16, kind=skind).ap()
        self.ebc_d = nc.dram_tensor("ebc_d", [504, 8, 128], BF16, kind=skind).ap()
        self.dbg_d = nc.dram_tensor("dbg_d", [128, 2048], F32, kind=skind).ap()
        self.xres_b = [Buf("xres%d" % t) for t in range(NT)]
        self.brT_b = [[Buf("brT%d_%d" % (b, t)) for t in range(NT)] for b in range(3)]
        self.ebc_db = Buf("ebc_d")
        self.out_b = Buf("out")
        with ExitStack() as st:
            self.st = st
            self.k = K(nc, st)
            self.build()

    def sb(self, st, name, shape, dt):
        self._uid = getattr(self, "_uid", 0) + 1
        return st.enter_context(self.nc.sbuf_tensor("s%d_%s" % (self._uid, name), shape, dt))

    def ps(self, st, name, shape, dt):
        self._uid = getattr(self, "_uid", 0) + 1
        return st.enter_context(self.nc.psum_tensor("p%d_%s" % (self._uid, name), shape, dt))

    def chk(self, n):
        import os
        v = os.environ.get("RSTOP")
        if v is not None and int(v) == n:
            self.k.muted = True

    def load_const(self, dst, src, buf, eng="sp"):
        self.k.dma(eng, dst, src, writes=[buf])

    def build(self):
        k, nc, st = self.k, self.nc, self.st
        self.hT = self.sb(st, "hT_all", [128, 8, S], BF16)
        self.hT_b = [Buf("hT%d" % t) for t in range(NT)]
        self.ident = self.sb(st, "ident", [128, 128], BF16)
        self.i4 = self.sb(st, "i4", [128, 512], BF16)
        self.identf = self.sb(st, "identf", [128, 128], F32)
        self.cst = self.sb(st, "cst", [128, 4], F32)
        self.EB = self.sb(st, "EB", [128, 5, 8, 128], BF16)
        self.cb = Buf("consts")
        k.dma("pool", self.ident[:], self.din["ident"][:, :], writes=[self.cb])
        k.dma("pool", self.i4[:], self.din["i4"][:, :], writes=[self.cb])
        k.dma("sp", self.identf[:], self.din["ident"][:, :], writes=[self.cb])
        k.op("dve", lambda e: e.memset(self.cst[:, 0:1], EPS), writes=[self.cb])
        k.op("dve", lambda e: e.memset(self.cst[:, 1:2], 0.0), writes=[self.cb])
        k.op("dve", lambda e: e.memset(self.cst[:, 2:3], 1.0), writes=[self.cb])
        self.phase_bias_tables()
        k.barrier()
        self.phase0()
        k.barrier()
        for l in range(self.n_layers):
            last = (l == DEPTH - 1)
            if "nsa" in self.phases:
                self.phase_nsa(l)
                k.barrier()
            if "ret" in self.phases:
                self.phase_ret(l)
                k.muted = False
                k.barrier()
            if "conv" in self.phases:
                self.phase_conv(l)
                k.barrier()
            if "merge" in self.phases:
                self.phase_merge(l)
                k.barrier()
            if "ffn" in self.phases:
                self.phase_ffn(l, last)
                k.barrier()
        k.barrier()

    def rms_alloc(self, st, tag):
        r = {}
        r["junk"] = self.sb(st, "rjunk" + tag, [128, D], BF16)
        r["ss"] = self.sb(st, "rss" + tag, [128, 4], F32)
        r["hb"] = self.sb(st, "rhb" + tag, [128, D], BF16)
        r["b"] = Buf("rms" + tag)
        r["hbb"] = Buf("rmshb" + tag)
        return r

    def rms_stats(self, r, src, src_bufs):
        k = self.k
        ss = r["ss"]
        k.op("dve", lambda e: e.memset(ss[:, 0:1], 0.0), writes=[r["b"]])
        k.op("act", lambda e: e.activation(out=r["junk"][:], in_=src, func=AF.Square, accum_out=ss[:, 0:1]),
             reads=list(src_bufs) + [r["b"]], writes=[r["b"]])
        k.op("act", lambda e: e.activation(out=ss[:, 1:2], in_=ss[:, 0:1], func=AF.Sqrt, bias=self.cst[:, 0:1], scale=1.0 / D),
             reads=[r["b"], self.cb], writes=[r["b"]])
        k.op("dve", lambda e: e.reciprocal(out=ss[:, 2:3], in_=ss[:, 1:2]), reads=[r["b"]], writes=[r["b"]])
        return ss[:, 2:3]

    def rms_to_hT(self, r, src, src_bufs, gain, gain_buf, t, ptr, ptr_b):
        k = self.k
        rstd = self.rms_stats(r, src, src_bufs)
        hb = r["hb"]
        k.op("dve", lambda e: e.scalar_tensor_tensor(out=hb[:], in0=src, scalar=rstd, in1=gain, op0=ALU.mult, op1=ALU.mult),
             reads=list(src_bufs) + [r["b"], gain_buf], writes=[r["hbb"]])
        for c in range(8):
            k.op("pe", lambda e, c=c: e.transpose(ptr[:, c * 128:(c + 1) * 128], hb[:, c * 128:(c + 1) * 128], self.ident[:]),
                 reads=[r["hbb"], self.cb], writes=[ptr_b])
        k.op("act", lambda e: e.activation(out=self.hT[:, :, t * 128:(t + 1) * 128],
                                           in_=ptr[:, :].rearrange("p (c q) -> p c q", c=8), func=AF.Copy),
             reads=[ptr_b], writes=[self.hT_b[t]])

    def phase_bias_tables(self):
        k = self.k
        with ExitStack() as st:
            bw = self.sb(st, "bw", [128, 5, 8, 128], F32)
            mw = self.sb(st, "mw", [128, 5, 128], F32)
            b31 = self.sb(st, "b31", [128, 8], F32)
            bb = Buf("bw")
            k.dma("sp", bw[:], self.din["bias_w"][:, :, :, :], writes=[bb])
            k.dma("sp", mw[:], self.din["maskw"][:, :, :], writes=[bb])
            k.dma("sp", b31[:], self.din["b31"][:, :], writes=[bb])
            for off in range(5):
                k.op("dve", lambda e, off=off: e.tensor_tensor(out=bw[:, off], in0=bw[:, off],
                                                               in1=b31[:, :].unsqueeze(2).to_broadcast([128, 8, 128]), op=ALU.subtract),
                     reads=[bb], writes=[bb])
                k.op("act", lambda e, off=off: e.activation(out=bw[:, off], in_=bw[:, off], func=AF.Exp), reads=[bb], writes=[bb])
                k.op("dve", lambda e, off=off: e.tensor_tensor(out=self.EB[:, off], in0=bw[:, off],
                                                               in1=mw[:, off:off + 1, :].to_broadcast([128, 8, 128]), op=ALU.mult),
                     reads=[bb], writes=[self.cb])
            bc = self.sb(st, "bc", [126, 8, 128], F32)
            mc = self.sb(st, "mc", [126, 128], F32)
            bcb = self.sb(st, "bcb", [126, 8, 128], BF16)
            cbuf = Buf("bc")
            for r4 in range(4):
                rows = slice(r4 * 126, (r4 + 1) * 126)
                k.dma("sp", bc[:], self.din["bias_c"][rows, :, :], writes=[cbuf])
                k.dma("sp", mc[:], self.din["maskc"][rows, :], writes=[cbuf])
                k.op("dve", lambda e: e.tensor_tensor(out=bc[:], in0=bc[:], in1=b31[0:126, :].unsqueeze(2).to_broadcast([126, 8, 128]),
                                                      op=ALU.subtract), reads=[cbuf, bb], writes=[cbuf])
                k.op("act", lambda e: e.activation(out=bc[:], in_=bc[:], func=AF.Exp), reads=[cbuf], writes=[cbuf])
                k.op("dve", lambda e: e.tensor_tensor(out=bcb[:], in0=bc[:], in1=mc[:, :].unsqueeze(1).to_broadcast([126, 8, 128]),
                                                      op=ALU.mult), reads=[cbuf], writes=[cbuf])
                k.dma("sp", self.ebc_d[rows, :, :], bcb[:], reads=[cbuf], writes=[self.ebc_db])
            k.barrier()

    def phase0(self):
        k = self.k
        with ExitStack() as st:
            r = self.rms_alloc(st, "p0")
            gain = self.sb(st, "gain0", [128, D], F32)
            gb = Buf("gain0")
            k.dma("sp", gain[:], self.din["nmix"][0], writes=[gb])
            xt = [self.sb(st, "p0x%d" % i, [128, D], F32) for i in range(2)]
            xb = [Buf("p0x%d" % i) for i in range(2)]
            ptr = self.ps(st, "p0tr", [128, 1024], BF16)
            ptr_b = Buf("p0tr")
            for t in range(NT):
                i = t % 2
                k.dma("sp", xt[i][:], self.din["x"][t * 128:(t + 1) * 128, :], writes=[xb[i]])
                k.dma("pool", self.xres[t * 128:(t + 1) * 128, :], xt[i][:], reads=[xb[i]], writes=[self.xres_b[t]])
                self.rms_to_hT(r, xt[i][:], [xb[i]], gain[:], gb, t, ptr, ptr_b)

    def load_w(self, dst, src, buf, kc):
        for c in range(kc):
            self.k.dma("pool", dst[:, c, :], src[c * 128:(c + 1) * 128, :], writes=[buf])

    def x_update(self, ctx, t, psum_halves, psum_bufs, hook):
        k = self.k
        i = ctx["i"] = (ctx.get("i", 0) + 1) % 2
        xt, xb = ctx["xt"][i], ctx["xb"][i]
        k.dma("sp", xt[:], self.xres[t * 128:(t + 1) * 128, :], reads=[self.xres_b[t]], writes=[xb])
        for h in range(2):
            k.op("dve", lambda e, h=h: e.tensor_tensor(out=xt[:, h * 512:(h + 1) * 512], in0=psum_halves[h],
                                                       in1=xt[:, h * 512:(h + 1) * 512], op=ALU.add),
                 reads=[psum_bufs[h], xb], writes=[xb])
        if hook != "final":
            k.dma("pool", self.xres[t * 128:(t + 1) * 128, :], xt[:], reads=[xb], writes=[self.xres_b[t]])
        if hook is None:
            return
        r = ctx["rms"]
        if hook == "final":
            rstd = self.rms_stats(r, xt[:], [xb])
            ot = ctx["ot"]
            k.op("dve", lambda e: e.scalar_tensor_tensor(out=ot[:], in0=xt[:], scalar=rstd, in1=ctx["gain"][:], op0=ALU.mult, op1=ALU.mult),
                 reads=[xb, r["b"], ctx["gain_b"]], writes=[ctx["ot_b"]])
            k.dma("pool", self.out[t * 128:(t + 1) * 128, :], ot[:], reads=[ctx["ot_b"]], writes=[self.out_b])
        else:
            self.rms_to_hT(r, xt[:], [xb], ctx["gain"][:], ctx["gain_b"], t, ctx["ptr"], ctx["ptr_b"])

    def upd_alloc(self, st, tag, gain_src, final=False):
        ctx = {}
        ctx["xt"] = [self.sb(st, "ux%s%d" % (tag, i), [128, D], F32) for i in range(2)]
        ctx["xb"] = [Buf("ux%d" % i) for i in range(2)]
        if gain_src is not None:
            ctx["rms"] = self.rms_alloc(st, "u" + tag)
            ctx["gain"] = self.sb(st, "ug" + tag, [128, D], F32)
            ctx["gain_b"] = Buf("ug")
            self.k.dma("sp", ctx["gain"][:], gain_src, writes=[ctx["gain_b"]])
            if final:
                ctx["ot"] = self.sb(st, "uo" + tag, [128, D], F32)
                ctx["ot_b"] = Buf("uo")
        return ctx

    def phase_ffn(self, l, last):
        k = self.k
        for fh in range(2):
            with ExitStack() as st:
                W1 = self.sb(st, "W1", [128, 8, 2048], BF16)
                W2 = self.sb(st, "W2", [128, 16, 1024], BF16)
                w1b, w2b = Buf("W1"), Buf("W2")
                self.load_w(W1, self.din["w_ff1"][l][:, fh * 2048:(fh + 1) * 2048], w1b, 8)
                self.load_w(W2, self.din["w_ff2"][l][fh * 2048:(fh + 1) * 2048, :], w2b, 16)
                actT = self.sb(st, "actT", [128, 16, 512], BF16)
                act_b = [Buf("act%d" % i) for i in range(16)]
                rl = [self.sb(st, "rl%d" % i, [128, 512], F32) for i in range(2)]
                rl_b = [Buf("rl%d" % i) for i in range(2)]
                hook = None
                gain_src = None
                if fh == 1:
                    hook = "final" if last else "norm"
                    gain_src = self.din["nfin"][:, :] if last else self.din["nmix"][l + 1]
                ctx = self.upd_alloc(st, "f", gain_src, final=(fh == 1 and last))
                pb = [self.ps(st, "fpb%d" % i, [128, 512], F32) for i in range(6)]
                pbb = [Buf("fpb%d" % i) for i in range(6)]
                if hook == "norm":
                    ctx["ptr"] = self.ps(st, "fptr", [128, 1024], BF16)
                    ctx["ptr_b"] = Buf("fptr")
                for G in range(8):
                    toks = slice(G * 512, (G + 1) * 512)
                    hb = self.hT_b[G * 4:(G + 1) * 4]
                    for fc in range(16):
                        p = fc % 2
                        for kc in range(8):
                            k.op("pe", lambda e, kc=kc, fc=fc, p=p: e.matmul(pb[p][:, :], lhsT=W1[:, kc, fc * 128:(fc + 1) * 128],
                                                                               rhs=self.hT[:, kc, toks], start=(kc == 0), stop=(kc == 7)),
                                 reads=[w1b] + hb, writes=[pbb[p]])
                        k.op("act", lambda e, p=p: e.activation(out=rl[p][:], in_=pb[p][:, :], func=AF.Relu), reads=[pbb[p]], writes=[rl_b[p]])
                        k.op("dve", lambda e, p=p, fc=fc: e.tensor_tensor(out=actT[:, fc, :], in0=rl[p][:], in1=rl[p][:], op=ALU.mult),
                             reads=[rl_b[p]], writes=[act_b[fc]])
                    for tt in range(4):
                        t = G * 4 + tt
                        pp = [2 + 2 * (tt % 2), 3 + 2 * (tt % 2)]
                        for nh in range(2):
                            for fc in range(16):
                                k.op("pe", lambda e, nh=nh, fc=fc, tt=tt: e.matmul(pb[pp[nh]][:, :], lhsT=actT[:, fc, tt * 128:(tt + 1) * 128],
                                                                                  rhs=W2[:, fc, nh * 512:(nh + 1) * 512], start=(fc == 0), stop=(fc == 15)),
                                     reads=[w2b, act_b[fc]], writes=[pbb[pp[nh]]])
                        self.x_update(ctx, t, [pb[pp[0]][:, :], pb[pp[1]][:, :]], [pbb[pp[0]], pbb[pp[1]]], hook)
                k.barrier()

    def phase_nsa(self, l):
        k = self.k
        win = self.din["w_in"][l]
        with ExitStack() as st:
            Wq = self.sb(st, "nWq", [128, 8, 512], BF16); wqb = Buf("nWq")
            self.load_w(Wq, win[:, C_QN:C_QN + 512], wqb, 8)
            ksT = self.sb(st, "nksT", [64, 2, S], BF16); ksTb = Buf("ksT")
            kwT = self.sb(st, "nkwT", [64, 2, S], BF16); kwTb = Buf("kwT")
            vsa = self.sb(st, "nvsa", [128, NT, 2, 65], BF16); vsab = Buf("vsa")
            vwa = self.sb(st, "nvwa", [128, NT, 2, 65], BF16); vwab = Buf("vwa")
            gn = self.sb(st, "ngn", [128, NT, 24], F32); gnb = Buf("gn")
            kcmpT = self.sb(st, "nkcmpT", [64, 2, 256], BF16); kcmpb = Buf("kcmpT")
            VC = self.sb(st, "nVC", [128, 2, 2, 129], BF16); VCb = Buf("VC")
            selv = self.sb(st, "nselv", [128, 126], F32)
            sela = self.sb(st, "nsela", [128, 126], F32)
            ovl = self.sb(st, "novl", [128, 2, 64], F32)
            ncb = Buf("nconst")
            k.dma("sp", selv[:], self.din["selvalid"], writes=[ncb])
            k.dma("sp", sela[:], self.din["seladd"], writes=[ncb])
            k.dma("sp", ovl[:], self.din["overlap"], writes=[ncb])
            A = [self.ps(st, "nA%d" % i, [128, 512], F32) for i in range(2)]
            Ab = [Buf("nA%d" % i) for i in range(2)]
            OC = [self.ps(st, "nOC%d" % i, [128, 512], F32) for i in range(2)]
            OCb = [Buf("nOC%d" % i) for i in range(2)]
            OS = self.ps(st, "nOS", [128, 512], F32); OSb = Buf("OS")
            OW = self.ps(st, "nOW", [128, 512], F32); OWb = Buf("OW")
            ptr = self.ps(st, "nptr", [128, 1024], BF16); ptrb = Buf("nptr")
            Q = self.ps(st, "nQ", [128, 512], F32); Qb = Buf("nQ")
            k.op("dve", lambda e: e.memset(vsa[:], 1.0), writes=[vsab])
            k.op("dve", lambda e: e.memset(vwa[:], 1.0), writes=[vwab])
            k.op("dve", lambda e: e.memset(kcmpT[:], 0.0), writes=[kcmpb])
            k.op("dve", lambda e: e.memset(VC[:], 0.0), writes=[VCb])
            for j in range(2):
                for g in range(2):
                    k.op("dve", lambda e, j=j, g=g: e.memset(VC[:, j, g, 64:65], 1.0), writes=[VCb])
                    k.op("dve", lambda e, j=j, g=g: e.tensor_copy(out=VC[:, j, g, 65:129], in_=ovl[:, j, :]), reads=[ncb], writes=[VCb])
            with ExitStack() as st2:
                Wk = self.sb(st2, "nWk", [128, 8, 512], BF16); wkb = Buf("nWk")
                for i, c0 in enumerate((C_KC, C_VC, C_KS, C_KW)):
                    for kc in range(8):
                        k.dma("pool", Wk[:, kc, i * 128:(i + 1) * 128], win[kc * 128:(kc + 1) * 128, c0:c0 + 128], writes=[wkb])
                Wtm = self.sb(st2, "nWtm", [128, 8, 280], BF16); wtb = Buf("nWtm")
                for (o, c0, n) in ((0, C_VS, 128), (128, C_VW, 128), (256, C_GN, 24)):
                    for kc in range(8):
                        k.dma("pool", Wtm[:, kc, o:o + n], win[kc * 128:(kc + 1) * 128, c0:c0 + n], writes=[wtb])
                w1 = [self.sb(st2, "nw1%d" % i, [64, 32, 128], BF16) for i in range(2)]
                w2k = self.sb(st2, "nw2k", [128, 64], BF16)
                w2v = self.sb(st2, "nw2v", [128, 64], BF16)
                peT = [self.sb(st2, "npeT%d" % i, [64, 32], BF16) for i in range(2)]
                cwb = Buf("cmpw")
                for i, nm in enumerate(("cmp_w1_k", "cmp_w1_v")):
                    k.dma("pool", w1[i][:], self.din[nm][l].rearrange("l d f -> d l f"), writes=[cwb])
                k.dma("pool", w2k[:], self.din["cmp_w2_k"][l], writes=[cwb])
                k.dma("pool", w2v[:], self.din["cmp_w2_v"][l], writes=[cwb])
                k.dma("pool", peT[0][:], self.din["cmp_pe_kT"][l], writes=[cwb])
                k.dma("pool", peT[1][:], self.din["cmp_pe_vT"][l], writes=[cwb])
                for t in range(NT):
                    tk = slice(t * 128, (t + 1) * 128)
                    P = A[t % 2]; PB = Ab[t % 2]
                    for kc in range(8):
                        k.op("pe", lambda e, kc=kc, P=P: e.matmul(P[:, 0:280], lhsT=self.hT[:, kc, tk], rhs=Wtm[:, kc, :], start=(kc == 0), stop=(kc == 7)),
                             reads=[wtb, self.hT_b[t]], writes=[PB])
                    k.op("act", lambda e, P=P, t=t: e.activation(out=vsa[:, t, :, 0:64], in_=P[:, 0:128].rearrange("p (g d) -> p g d", g=2), func=AF.Copy),
                         reads=[PB], writes=[vsab])
                    k.op("act", lambda e, P=P, t=t: e.activation(out=vwa[:, t, :, 0:64], in_=P[:, 128:256].rearrange("p (g d) -> p g d", g=2), func=AF.Copy),
                         reads=[PB], writes=[vwab])
                    k.op("act", lambda e, P=P, t=t: e.activation(out=gn[:, t, :], in_=P[:, 256:280], func=AF.Sigmoid), reads=[PB], writes=[gnb])
                it = 0
                for (dst, dstb, wi) in ((ksT, ksTb, 2), (kwT, kwTb, 3)):
                    for g in range(2):
                        for G in range(8):
                            toks = slice(G * 512, (G + 1) * 512)
                            P = OC[it % 2]; PB = OCb[it % 2]; it += 1
                            for kc in range(8):
                                k.op("pe", lambda e, kc=kc, P=P, wi=wi, g=g: e.matmul(P[0:64, :], lhsT=Wk[:, kc, wi * 128 + g * 64:wi * 128 + g * 64 + 64],
                                                                                     rhs=self.hT[:, kc, toks], start=(kc == 0), stop=(kc == 7)),
                                     reads=[wkb] + self.hT_b[G * 4:(G + 1) * 4], writes=[PB])
                            k.op("act", lambda e, P=P, dst=dst, g=g: e.activation(out=dst[:, g, toks], in_=P[0:64, :], func=AF.Copy), reads=[PB], writes=[dstb])
                cT = [self.sb(st2, "ncT%d" % i, [64, S], BF16) for i in range(2)]
                cTb = [Buf("ncT%d" % i) for i in range(2)]
                hx = self.sb(st2, "nhx", [128, 4, 256], F32); hxb = Buf("nhx")
                hidb = self.sb(st2, "nhidb", [128, 256], BF16); hidbb = Buf("nhidb")
                cbias = self.sb(st2, "ncbias", [128, 2], F32); cbb = Buf("ncbias")
                for kv in range(2):
                    for lidx in range(32):
                        k.op("pe", lambda e, kv=kv, lidx=lidx: e.matmul(Q[:, kv:kv + 1], lhsT=w1[kv][:, lidx, :], rhs=peT[kv][:, lidx:lidx + 1],
                                                                       start=(lidx == 0), stop=(lidx == 31)), reads=[cwb], writes=[Qb])
                k.op("act", lambda e: e.activation(out=cbias[:], in_=Q[:, 0:2], func=AF.Copy), reads=[Qb], writes=[cbb])
                for g in range(2):
                    for kv in range(2):
                        for G in range(8):
                            toks = slice(G * 512, (G + 1) * 512)
                            P = OC[it % 2]; PB = OCb[it % 2]; it += 1
                            for kc in range(8):
                                k.op("pe", lambda e, kc=kc, P=P, kv=kv, g=g: e.matmul(P[0:64, :], lhsT=Wk[:, kc, kv * 128 + g * 64:kv * 128 + g * 64 + 64],
                                                                                     rhs=self.hT[:, kc, toks], start=(kc == 0), stop=(kc == 7)),
                                     reads=[wkb] + self.hT_b[G * 4:(G + 1) * 4], writes=[PB])
                            k.op("act", lambda e, P=P, kv=kv: e.activation(out=cT[kv][:, toks], in_=P[0:64, :], func=AF.Copy), reads=[PB], writes=[cTb[kv]])
                    for kv in range(2):
                        P = A[kv]; PB = Ab[kv]
                        for lidx in range(32):
                            k.op("pe", lambda e, kv=kv, lidx=lidx, P=P: e.matmul(P[:, 0:255], lhsT=w1[kv][:, lidx, :], rhs=cT[kv][:, lidx:lidx + 16 * 254 + 1:16],
                                                                                start=(lidx == 0), stop=(lidx == 31)), reads=[cwb, cTb[kv]], writes=[PB])
                        x_ = hx[:, 0, 0:255]
                        k.op("act", lambda e, P=P, kv=kv: e.activation(out=x_, in_=P[:, 0:255], func=AF.Identity, bias=cbias[:, kv:kv + 1], scale=1.0),
                             reads=[PB, cbb], writes=[hxb])
                        k.op("dve", lambda e: e.tensor_tensor(out=hx[:, 1, 0:255], in0=x_, in1=x_, op=ALU.mult), reads=[hxb], writes=[hxb])
                        k.op("dve", lambda e: e.tensor_scalar(out=hx[:, 1, 0:255], in0=hx[:, 1, 0:255], scalar1=0.044715, scalar2=1.0, op0=ALU.mult, op1=ALU.add),
                             reads=[hxb], writes=[hxb])
                        k.op("dve", lambda e: e.tensor_tensor(out=hx[:, 2, 0:255], in0=hx[:, 1, 0:255], in1=x_, op=ALU.mult), reads=[hxb], writes=[hxb])
                        k.op("act", lambda e: e.activation(out=hx[:, 3, 0:255], in_=hx[:, 2, 0:255], func=AF.Sigmoid, scale=1.5957691216057308),
                             reads=[hxb], writes=[hxb])
                        k.op("dve", lambda e: e.tensor_tensor(out=hidb[:, 0:255], in0=hx[:, 3, 0:255], in1=x_, op=ALU.mult), reads=[hxb], writes=[hidbb])
                        if kv == 0:
                            k.op("pe", lambda e: e.matmul(Q[0:64, 0:255], lhsT=w2k[:], rhs=hidb[:, 0:255], start=True, stop=True), reads=[cwb, hidbb], writes=[Qb])
                            k.op("act", lambda e, g=g: e.activation(out=kcmpT[:, g, 0:255], in_=Q[0:64, 0:255], func=AF.Copy), reads=[Qb], writes=[kcmpb])
                        else:
                            for j in range(2):
                                nn = 128 if j == 0 else 127
                                k.op("pe", lambda e, j=j, nn=nn: e.matmul(Q[0:nn, j * 64:(j + 1) * 64], lhsT=hidb[:, j * 128:j * 128 + nn], rhs=w2v[:],
                                                                         start=True, stop=True), reads=[cwb, hidbb], writes=[Qb])
                                k.op("act", lambda e, j=j, nn=nn, g=g: e.activation(out=VC[0:nn, j, g, 0:64], in_=Q[0:nn, j * 64:(j + 1) * 64], func=AF.Copy),
                                     reads=[Qb], writes=[VCb])
            k.barrier()
            qT = self.sb(st, "nqT", [64, 4, 8, 128], BF16); qTb = Buf("nqT")
            ebc = [self.sb(st, "nebc%d" % i, [128, 2, 8, 128], BF16) for i in range(2)]
            ebcb = [Buf("nebc%d" % i) for i in range(2)]
            pT = [self.sb(st, "npT%d" % i, [128, 512], BF16) for i in range(3)]
            pTb = [Buf("npT%d" % i) for i in range(3)]
            snX = self.sb(st, "nsnX", [128, S], BF16); snXb = Buf("snX")
            sm = self.sb(st, "nsm", [128, 3, 4], F32); smb = Buf("nsm")
            imp = self.sb(st, "nimp", [128, 64], F32); impb = Buf("nimp")
            m8 = self.sb(st, "nm8", [128, 8], F32)
            sneg = self.sb(st, "nsneg", [128, 64], BF16); snegb = Buf("nsneg")
            coef = self.sb(st, "ncoef", [128, 4, 3], F32); coefb = Buf("ncoef")
            oacc = self.sb(st, "noacc", [128, 4, 64], F32); oaccb = Buf("noacc")
            onsa = self.sb(st, "nonsa", [128, 512], BF16); onsab = Buf("nonsa")
            onT = [self.sb(st, "nonT%d" % i, [128, 4, 128], BF16) for i in range(2)]
            onTb = [Buf("nonT%d" % i) for i in range(2)]
            sT = [self.sb(st, "nsT%d" % i, [65, 512], F32) for i in range(2)]
            sTb = [Buf("nsT%d" % i) for i in range(2)]
            T = OC; Tb = OCb
            R = [OS, OW]; Rb = [OSb, OWb]
            pti = 0

            def finish(ti, ri, rows):
                k.op("act", lambda e: e.activation(out=sT[ti][0:rows, :], in_=T[ti][0:rows, :], func=AF.Copy), reads=[Tb[ti]], writes=[sTb[ti]])
                for h4 in range(4):
                    k.op("pe", lambda e, h4=h4: e.transpose(R[ri][:, h4 * 65:h4 * 65 + rows], sT[ti][0:rows, h4 * 128:(h4 + 1) * 128], self.identf[0:rows, 0:rows]),
                         reads=[sTb[ti], self.cb], writes=[Rb[ri]])

            for c in range(NT):
                if c % 4 == 0:
                    toks = slice(c * 128, (c + 4) * 128)
                    for h in range(8):
                        for kc in range(8):
                            k.op("pe", lambda e, kc=kc, h=h: e.matmul(Q[0:64, :], lhsT=Wq[:, kc, h * 64:(h + 1) * 64], rhs=self.hT[:, kc, toks],
                                                                     start=(kc == 0), stop=(kc == 7)), reads=[wqb] + self.hT_b[c:c + 4], writes=[Qb])
                        k.op("act", lambda e, h=h: e.activation(out=qT[:, :, h, :], in_=Q[0:64, :].rearrange("p (b q) -> p b q", b=4), func=AF.Copy, scale=0.125),
                             reads=[Qb], writes=[qTb])
                e_ = ebc[c % 2]; e_b = ebcb[c % 2]
                njt = 2 if c >= 16 else 1
                for j in range(njt):
                    r0 = 248 - 8 * c + 128 * j
                    k.dma("sp", e_[:, j], self.ebc_d[r0:r0 + 128, :, :], reads=[self.ebc_db], writes=[e_b])
                for g in range(2):
                    qg = qT[:, c % 4, g * 4:(g + 1) * 4, :].rearrange("p h q -> p (h q)")
                    for j in range(njt):
                        ai = pti % 2; pi = pti % 3; pti += 1
                        k.op("pe", lambda e, j=j, ai=ai, g=g: e.matmul(A[ai][:, :], lhsT=kcmpT[:, g, j * 128:(j + 1) * 128], rhs=qg, start=True, stop=True),
                             reads=[kcmpb, qTb], writes=[Ab[ai]])
                        k.op("act", lambda e, ai=ai, pi=pi: e.activation(out=pT[pi][:], in_=A[ai][:, :], func=AF.Exp), reads=[Ab[ai]], writes=[pTb[pi]])
                        k.op("dve", lambda e, pi=pi, j=j, g=g: e.tensor_tensor(out=pT[pi][:], in0=pT[pi][:],
                                                                               in1=e_[:, j, g * 4:(g + 1) * 4, :].rearrange("p h q -> p (h q)"), op=ALU.mult),
                             reads=[pTb[pi], e_b], writes=[pTb[pi]])
                        k.op("pe", lambda e, pi=pi, j=j, g=g: e.matmul(T[0][0:65, :], lhsT=VC[:, j, g, 0:65], rhs=pT[pi][:], start=(j == 0), stop=(j == njt - 1)),
                             reads=[pTb[pi], VCb], writes=[Tb[0]])
                        k.op("pe", lambda e, pi=pi, j=j, g=g: e.matmul(T[1][0:64, :], lhsT=VC[:, j, g, 65:129], rhs=pT[pi][:], start=(j == 0), stop=(j == njt - 1)),
                             reads=[pTb[pi], VCb], writes=[Tb[1]])
                    finish(0, 0, 65)
                    finish(1, 1, 64)
                    k.op("dve", lambda e: e.tensor_scalar(out=sm[:, 0, :], in0=R[0][:, 64:64 + 260:65], scalar1=1e-30, scalar2=None, op0=ALU.max), reads=[Rb[0], smb], writes=[smb])
                    k.op("dve", lambda e: e.reciprocal(out=sm[:, 0, :], in_=sm[:, 0, :]), reads=[smb], writes=[smb])
                    for h4 in range(4):
                        src = R[1][:, h4 * 65:h4 * 65 + 64]
                        if h4 == 0:
                            k.op("dve", lambda e, src=src: e.tensor_scalar(out=imp[:], in0=src, scalar1=sm[:, 0, 0:1], scalar2=None, op0=ALU.mult),
                                 reads=[Rb[1], smb], writes=[impb])
                        else:
                            k.op("dve", lambda e, src=src, h4=h4: e.scalar_tensor_tensor(out=imp[:], in0=src, scalar=sm[:, 0, h4:h4 + 1], in1=imp[:], op0=ALU.mult, op1=ALU.add),
                                 reads=[Rb[1], smb, impb], writes=[impb])
                    gview = gn[:, c, g * 12:(g + 1) * 12].rearrange("p (h b) -> p h b", h=4)
                    k.op("dve", lambda e: e.tensor_tensor(out=coef[:, :, 0], in0=gview[:, :, 0], in1=sm[:, 0, :], op=ALU.mult), reads=[gnb, smb, coefb], writes=[coefb])
                    for h4 in range(4):
                        k.op("dve", lambda e, h4=h4: e.tensor_scalar(out=oacc[:, h4, :], in0=R[0][:, h4 * 65:h4 * 65 + 64], scalar1=coef[:, h4, 0:1], scalar2=None, op0=ALU.mult),
                             reads=[Rb[0], coefb, oaccb], writes=[oaccb])
                    sl = slice(62 - 2 * c, 62 - 2 * c + 64)
                    k.op("dve", lambda e: e.tensor_tensor(out=imp[:], in0=imp[:], in1=selv[:, sl], op=ALU.mult), reads=[impb, ncb], writes=[impb])
                    k.op("dve", lambda e: e.tensor_tensor(out=imp[:], in0=imp[:], in1=sela[:, sl], op=ALU.add), reads=[impb, ncb], writes=[impb])
                    if c >= 1:
                        k.op("dve", lambda e: e.tensor_scalar(out=imp[:, 0:1], in0=imp[:, 0:1], scalar1=1e4, scalar2=None, op0=ALU.add), reads=[impb], writes=[impb])
                    k.op("dve", lambda e: e.max(out=m8[:], in_=imp[:]), reads=[impb], writes=[impb])
                    k.op("dve", lambda e: e.tensor_scalar(out=sneg[:], in0=imp[:], scalar1=m8[:, 7:8], scalar2=NEG, op0=ALU.is_lt, op1=ALU.mult),
                         reads=[impb], writes=[snegb])
                    nb = 2 * (c + 1)
                    k.op("dve", lambda e, nb=nb: e.tensor_copy(out=snX[:, 0:nb * 64].rearrange("p (a b) -> p a b", a=nb),
                                                               in_=sneg[:, 0:nb].unsqueeze(2).to_broadcast([128, nb, 64])), reads=[snegb], writes=[snXb])
                    tiles = [("s", kb) for kb in range(c + 1)] + [("w", kb) for kb in range(max(0, c - 4), c + 1)]
                    nsel = c + 1
                    slots = []

                    def emit_qk(idx):
                        nonlocal pti
                        kind, kb = tiles[idx]
                        ai = pti % 2; pi = pti % 3; pti += 1
                        slots.append((ai, pi))
                        kt = slice(kb * 128, (kb + 1) * 128)
                        if kind == "s":
                            k.op("pe", lambda e: e.matmul(A[ai][:, :], lhsT=ksT[:, g, kt], rhs=qg, start=True, stop=False), reads=[ksTb, qTb], writes=[Ab[ai]])
                            k.op("pe", lambda e: e.matmul(A[ai][:, :], lhsT=snX[:, kt], rhs=self.i4[:], start=False, stop=True), reads=[snXb, self.cb], writes=[Ab[ai]])
                        else:
                            k.op("pe", lambda e: e.matmul(A[ai][:, :], lhsT=kwT[:, g, kt], rhs=qg, start=True, stop=True), reads=[kwTb, qTb], writes=[Ab[ai]])

                    def emit_pv(idx):
                        kind, kb = tiles[idx]
                        ai, pi = slots[idx]
                        off = c - kb
                        k.op("act", lambda e: e.activation(out=pT[pi][:], in_=A[ai][:, :], func=AF.Exp), reads=[Ab[ai]], writes=[pTb[pi]])
                        if kind == "w" or off <= 1:
                            k.op("dve", lambda e: e.tensor_tensor(out=pT[pi][:], in0=pT[pi][:],
                                                                  in1=self.EB[:, off, g * 4:(g + 1) * 4, :].rearrange("p h q -> p (h q)"), op=ALU.mult),
                                 reads=[pTb[pi], self.cb], writes=[pTb[pi]])
                        if kind == "s":
                            ti, V, Vb = 0, vsa, vsab
                            first, lastt = (idx == 0), (idx == nsel - 1)
                        else:
                            ti, V, Vb = 1, vwa, vwab
                            first, lastt = (idx == nsel), (idx == len(tiles) - 1)
                        k.op("pe", lambda e: e.matmul(T[ti][0:65, :], lhsT=V[:, kb, g, :], rhs=pT[pi][:], start=first, stop=lastt),
                             reads=[pTb[pi], Vb], writes=[Tb[ti]])
                    for idx in range(len(tiles) + 1):
                        if idx < len(tiles):
                            emit_qk(idx)
                        if idx >= 1:
                            emit_pv(idx - 1)
                    finish(0, 0, 65)
                    finish(1, 1, 65)
                    k.op("dve", lambda e: e.tensor_scalar(out=sm[:, 1, :], in0=R[0][:, 64:64 + 260:65], scalar1=1e-30, scalar2=None, op0=ALU.max), reads=[Rb[0], smb], writes=[smb])
                    k.op("dve", lambda e: e.tensor_scalar(out=sm[:, 2, :], in0=R[1][:, 64:64 + 260:65], scalar1=1e-30, scalar2=None, op0=ALU.max), reads=[Rb[1], smb], writes=[smb])
                    k.op("dve", lambda e: e.reciprocal(out=sm[:, 1:3, :], in_=sm[:, 1:3, :]), reads=[smb], writes=[smb])
                    k.op("dve", lambda e: e.tensor_tensor(out=coef[:, :, 1:3], in0=gview[:, :, 1:3],
                                                          in1=sm[:, 1:3, :].rearrange("p b h -> p h b"), op=ALU.mult), reads=[gnb, smb, coefb], writes=[coefb])
                    for h4 in range(4):
                        k.op("dve", lambda e, h4=h4: e.scalar_tensor_tensor(out=oacc[:, h4, :], in0=R[0][:, h4 * 65:h4 * 65 + 64], scalar=coef[:, h4, 1:2], in1=oacc[:, h4, :],
                                                                             op0=ALU.mult, op1=ALU.add), reads=[Rb[0], coefb, oaccb], writes=[oaccb])
                        hh = g * 4 + h4
                        k.op("dve", lambda e, h4=h4, hh=hh: e.scalar_tensor_tensor(out=onsa[:, hh * 64:(hh + 1) * 64], in0=R[1][:, h4 * 65:h4 * 65 + 64], scalar=coef[:, h4, 2:3],
                                                                                    in1=oacc[:, h4, :], op0=ALU.mult, op1=ALU.add), reads=[Rb[1], coefb, oaccb], writes=[onsab])
                import os
                if self.dbg and c == int(os.environ.get("DBG_C", "-1")):
                    dt_ = self.sb(st, "ndbg", [128, 2048], F32); dtb = Buf("ndbg")
                    k.op("dve", lambda e: e.memset(dt_[:], 0.0), writes=[dtb])
                    k.op("dve", lambda e: e.tensor_copy(out=dt_[:, 0:64], in_=imp[:]), reads=[impb], writes=[dtb])
                    k.op("dve", lambda e: e.tensor_copy(out=dt_[:, 64:128], in_=sneg[:]), reads=[snegb], writes=[dtb])
                    k.op("dve", lambda e: e.tensor_copy(out=dt_[:, 128:140], in_=sm[:, :, :].rearrange("p a b -> p (a b)")), reads=[smb], writes=[dtb])
                    k.op("dve", lambda e: e.tensor_copy(out=dt_[:, 140:152], in_=coef[:, :, :].rearrange("p a b -> p (a b)")), reads=[coefb], writes=[dtb])
                    k.op("dve", lambda e: e.tensor_copy(out=dt_[:, 152:160], in_=m8[:]), reads=[impb], writes=[dtb])
                    k.op("dve", lambda e: e.tensor_copy(out=dt_[:, 256:512], in_=OC[0][:, 0:256]), reads=[OCb[0]], writes=[dtb])
                    k.op("dve", lambda e: e.tensor_copy(out=dt_[:, 512:1024], in_=OS[:, :]), reads=[OSb], writes=[dtb])
                    k.op("dve", lambda e: e.tensor_copy(out=dt_[:, 1024:1536], in_=OW[:, :]), reads=[OWb], writes=[dtb])
                    k.op("dve", lambda e: e.tensor_copy(out=dt_[:, 1536:2048], in_=onsa[:]), reads=[onsab], writes=[dtb])
                    k.dma("sp", self.dbg_d[:, :], dt_[:], reads=[dtb], writes=[Buf("dbgd")])
                for j in range(4):
                    k.op("pe", lambda e, j=j: e.transpose(ptr[:, j * 128:(j + 1) * 128], onsa[:, j * 128:(j + 1) * 128], self.ident[:]),
                         reads=[onsab, self.cb], writes=[ptrb])
                i2 = c % 2
                k.op("act", lambda e, i2=i2: e.activation(out=onT[i2][:], in_=ptr[:, 0:512].rearrange("p (j c) -> p j c", j=4), func=AF.Copy),
                     reads=[ptrb], writes=[onTb[i2]])
                tk = slice(c * 128, (c + 1) * 128)
                k.dma("sp", self.brT[0, :, :, tk].rearrange("c p q -> p c q"), onT[i2][:], reads=[onTb[i2]], writes=[self.brT_b[0][c]])

    def phase_ret(self, l):
        k = self.k
        with ExitStack() as st:
            Wqk = self.sb(st, "rWqk", [128, 8, 512], BF16)
            Wv = self.sb(st, "rWv", [128, 8, 512], BF16)
            Wg = self.sb(st, "rWg", [128, 8, 512], BF16)
            wb = Buf("rW")
            self.load_w(Wqk, self.din["w_in"][l][:, C_QR:C_QR + 512], wb, 8)
            self.load_w(Wv, self.din["w_in"][l][:, C_VR:C_VR + 512], wb, 8)
            self.load_w(Wg, self.din["w_in"][l][:, C_GR:C_GR + 512], wb, 8)
            cos = self.sb(st, "rcos", [128, NT, 32], F32)
            sin = self.sb(st, "rsin", [128, NT, 32], F32)
            dec = self.sb(st, "rdec", [128, 4, 128], F32)
            xi = self.sb(st, "rxi", [64, 4, 128], F32)
            zeta = self.sb(st, "rzeta", [128, 4], F32)
            gch = self.sb(st, "rgch", [64, 4], F32)
            gn = self.sb(st, "rgn", [128, 512], F32)
            rc = Buf("rconst")
            for dst, src in ((cos, "cos"), (sin, "sin"), (dec, "decayT"), (xi, "xi"), (zeta, "zeta"), (gch, "gch")):
                k.dma("sp", dst[:], self.din[src], writes=[rc])
            k.dma("sp", gn[:], self.din["retgn"][l], writes=[rc])
            Sf = self.sb(st, "rSf", [64, 4, 128], F32)
            Sb = self.sb(st, "rSb", [64, 4, 128], BF16)
            Sfb, Sbb = Buf("Sf"), Buf("Sb")
            k.op("dve", lambda e: e.memset(Sf[:], 0.0), writes=[Sfb])
            k.op("dve", lambda e: e.memset(Sb[:], 0.0), writes=[Sbb])
            qk = self.sb(st, "rqk", [128, 512], F32); qkb = Buf("rqk")
            tm = self.sb(st, "rtm", [128, 4, 8, 32], F32); tmb = Buf("rtm")
            rot = self.sb(st, "rrot", [128, 8, 2, 32], F32); rotb = Buf("rrot")
            qkbf = self.sb(st, "rqkbf", [128, 512], BF16); qkbfb = Buf("rqkbf")
            khat = self.sb(st, "rkhat", [128, 4, 64], BF16); khatb = Buf("rkhat")
            qkT = self.sb(st, "rqkT", [64, 8, 128], BF16); qkTb = Buf("rqkT")
            qxiT = self.sb(st, "rqxiT", [64, 4, 128], BF16); qxiTb = Buf("rqxiT")
            qf32 = self.sb(st, "rqf32", [64, 4, 128], F32); qf32b = Buf("rqf32")
            inT = self.sb(st, "rinT", [128, 4, 128], BF16); inTb = Buf("rinT")
            vbf = self.sb(st, "rvbf", [128, 512], BF16); vbfb = Buf("rvbf")
            osb = self.sb(st, "rosb", [128, 512], F32); osbb = Buf("rosb")
            osq = self.sb(st, "rosq", [128, 512], F32); osqb = Buf("rosq")
            sm = self.sb(st, "rsm", [128, 6, 4], F32); smb = Buf("rsm")
            yn = self.sb(st, "ryn", [128, 512], F32); ynb = Buf("ryn")
            gs = self.sb(st, "rgs", [128, 512], F32); gsb = Buf("rgs")
            orb = self.sb(st, "rorb", [128, 512], BF16); orbb = Buf("rorb")
            orT = [self.sb(st, "rorT%d" % i, [128, 4, 128], BF16) for i in range(2)]
            orTb = [Buf("rorT%d" % i) for i in range(2)]
            pqk = self.ps(st, "rpqk", [128, 512], F32); pqkb = Buf("pqk")
            pv = self.ps(st, "rpv", [128, 512], F32); pvb = Buf("pv")
            pg = self.ps(st, "rpg", [128, 512], F32); pgb = Buf("pg")
            pin = self.ps(st, "rpin", [128, 512], F32); pinb = Buf("pin")
            po = self.ps(st, "rpo", [128, 512], F32); pob = Buf("po")
            pkv = self.ps(st, "rpkv", [128, 512], F32); pkvb = Buf("pkv")
            ptr = self.ps(st, "rptr", [128, 1024], BF16); ptrb = Buf("ptr")
            ptr2 = self.ps(st, "rptr2", [128, 1024], BF16); ptr2b = Buf("ptr2")
            for t in range(NT):
                tk = slice(t * 128, (t + 1) * 128)
                for (W, P, PB) in ((Wqk, pqk, pqkb), (Wv, pv, pvb), (Wg, pg, pgb)):
                    for kc in range(8):
                        k.op("pe", lambda e, kc=kc, W=W, P=P: e.matmul(P[:, :], lhsT=self.hT[:, kc, tk], rhs=W[:, kc, :], start=(kc == 0), stop=(kc == 7)),
                             reads=[wb, self.hT_b[t]], writes=[PB])
                self.chk(1)
                k.op("act", lambda e: e.activation(out=qk[:, 0:256], in_=pqk[:, 0:256], func=AF.Copy), reads=[pqkb], writes=[qkb])
                k.op("act", lambda e: e.activation(out=qk[:, 256:512], in_=pqk[:, 256:512], func=AF.Copy, scale=0.125), reads=[pqkb], writes=[qkb])
                xv = qk[:, :].rearrange("p (h two d) -> p h two d", h=8, two=2)
                x1, x2 = xv[:, :, 0, :], xv[:, :, 1, :]
                cb_ = cos[:, t, :].unsqueeze(1).to_broadcast([128, 8, 32])
                sb_ = sin[:, t, :].unsqueeze(1).to_broadcast([128, 8, 32])
                k.op("dve", lambda e: e.tensor_tensor(out=tm[:, 0], in0=x1, in1=cb_, op=ALU.mult), reads=[qkb, rc], writes=[tmb])
                k.op("dve", lambda e: e.tensor_tensor(out=tm[:, 1], in0=x2, in1=sb_, op=ALU.mult), reads=[qkb, rc], writes=[tmb])
                k.op("dve", lambda e: e.tensor_tensor(out=tm[:, 2], in0=x1, in1=sb_, op=ALU.mult), reads=[qkb, rc], writes=[tmb])
                k.op("dve", lambda e: e.tensor_tensor(out=tm[:, 3], in0=x2, in1=cb_, op=ALU.mult), reads=[qkb, rc], writes=[tmb])
                k.op("dve", lambda e: e.tensor_tensor(out=rot[:, :, 0, :], in0=tm[:, 0], in1=tm[:, 1], op=ALU.subtract), reads=[tmb], writes=[rotb])
                k.op("dve", lambda e: e.tensor_tensor(out=rot[:, :, 1, :], in0=tm[:, 2], in1=tm[:, 3], op=ALU.add), reads=[tmb], writes=[rotb])
                self.chk(2)
                rflat = rot[:, :, :, :].rearrange("p h two d -> p (h two d)")
                k.op("act", lambda e: e.activation(out=qkbf[:], in_=rflat, func=AF.Copy), reads=[rotb], writes=[qkbfb])
                k.op("dve", lambda e: e.tensor_tensor(out=khat[:], in0=rflat[:, 256:512].rearrange("p (h d) -> p h d", h=4),
                                                      in1=zeta[:, :].unsqueeze(2).to_broadcast([128, 4, 64]), op=ALU.mult),
                     reads=[rotb, rc], writes=[khatb])
                self.chk(3)
                for j in range(8):
                    k.op("pe", lambda e, j=j: e.transpose(ptr[0:64, j * 128:(j + 1) * 128], qkbf[:, j * 64:(j + 1) * 64], self.ident[:]),
                         reads=[qkbfb, self.cb], writes=[ptrb])
                k.op("act", lambda e: e.activation(out=qkT[:], in_=ptr[0:64, 0:1024].rearrange("p (j c) -> p j c", j=8), func=AF.Copy),
                     reads=[ptrb], writes=[qkTb])
                k.op("act", lambda e: e.activation(out=qf32[:], in_=ptr[0:64, 0:512].rearrange("p (j c) -> p j c", j=4), func=AF.Copy),
                     reads=[ptrb], writes=[qf32b])
                k.op("dve", lambda e: e.tensor_tensor(out=qxiT[:], in0=qf32[:], in1=xi[:], op=ALU.mult),
                     reads=[qf32b, rc], writes=[qxiTb])
                self.chk(4)
                for h in range(4):
                    k.op("pe", lambda e, h=h: e.matmul(pin[:, h * 128:(h + 1) * 128], lhsT=qkT[:, 4 + h, :], rhs=qkT[:, h, :],
                                                       start=True, stop=True), reads=[qkTb], writes=[pinb])
                self.chk(45)
                k.op("dve", lambda e: e.tensor_tensor(out=inT[:], in0=pin[:, :].rearrange("p (h c) -> p h c", h=4), in1=dec[:], op=ALU.mult),
                     reads=[pinb, rc], writes=[inTb])
                self.chk(5)
                k.op("act", lambda e: e.activation(out=vbf[:], in_=pv[:, :], func=AF.Copy), reads=[pvb], writes=[vbfb])
                for h in range(4):
                    rows = slice((h % 2) * 64, (h % 2) * 64 + 64)
                    hc = slice(h * 128, (h + 1) * 128)
                    k.op("pe", lambda e, h=h, hc=hc: e.matmul(po[:, hc], lhsT=inT[:, h, :], rhs=vbf[:, hc], start=True, stop=False),
                         reads=[inTb, vbfb], writes=[pob])
                    k.op("pe", lambda e, h=h, hc=hc: e.matmul(po[:, hc], lhsT=qxiT[:, h, :], rhs=Sb[:, h, :], start=False, stop=True),
                         reads=[qxiTb, Sbb], writes=[pob])
                self.chk(6)
                for h in range(4):
                    hc = slice(h * 128, (h + 1) * 128)
                    k.op("pe", lambda e, h=h, hc=hc: e.matmul(pkv[0:64, hc], lhsT=khat[:, h, :],
                                                             rhs=vbf[:, hc], start=True, stop=True), reads=[khatb, vbfb], writes=[pkvb])
                self.chk(7)
                for h in range(4):
                    rows = slice((h % 2) * 64, (h % 2) * 64 + 64)
                    hc = slice(h * 128, (h + 1) * 128)
                    k.op("dve", lambda e, h=h, hc=hc: e.scalar_tensor_tensor(out=Sf[:, h, :], in0=Sf[:, h, :],
                                                                              scalar=gch[:, h:h + 1], in1=pkv[0:64, hc],
                                                                              op0=ALU.mult, op1=ALU.add),
                         reads=[pkvb, rc, Sfb], writes=[Sfb])
                k.op("act", lambda e: e.activation(out=Sb[:], in_=Sf[:], func=AF.Copy), reads=[Sfb], writes=[Sbb])
                self.chk(8)
                k.op("act", lambda e: e.activation(out=osb[:], in_=po[:, :], func=AF.Copy), reads=[pob], writes=[osbb])
                k.op("act", lambda e: e.activation(out=osq[:], in_=po[:, :], func=AF.Square), reads=[pob], writes=[osqb])
                k.op("dve", lambda e: e.reduce_sum(out=sm[:, 0, :], in_=osb[:, :].rearrange("p (h v) -> p h v", h=4), axis=AX.X), reads=[osbb], writes=[smb])
                k.op("dve", lambda e: e.reduce_sum(out=sm[:, 1, :], in_=osq[:, :].rearrange("p (h v) -> p h v", h=4), axis=AX.X), reads=[osqb, smb], writes=[smb])
                k.op("dve", lambda e: e.tensor_scalar(out=sm[:, 2, :], in0=sm[:, 0, :], scalar1=1.0 / 128, scalar2=None, op0=ALU.mult), reads=[smb], writes=[smb])
                k.op("dve", lambda e: e.tensor_tensor(out=sm[:, 3, :], in0=sm[:, 2, :], in1=sm[:, 2, :], op=ALU.mult), reads=[smb], writes=[smb])
                k.op("dve", lambda e: e.scalar_tensor_tensor(out=sm[:, 3, :], in0=sm[:, 1, :], scalar=1.0 / 128, in1=sm[:, 3, :], op0=ALU.mult, op1=ALU.subtract),
                     reads=[smb], writes=[smb])
                k.op("act", lambda e: e.activation(out=sm[:, 4, :], in_=sm[:, 3, :], func=AF.Sqrt, bias=self.cst[:, 0:1], scale=1.0), reads=[smb, self.cb], writes=[smb])
                k.op("dve", lambda e: e.reciprocal(out=sm[:, 5, :], in_=sm[:, 4, :]), reads=[smb], writes=[smb])
                self.chk(9)
                for h in range(4):
                    hc = slice(h * 128, (h + 1) * 128)
                    k.op("dve", lambda e, h=h, hc=hc: e.tensor_scalar(out=yn[:, hc], in0=osb[:, hc], scalar1=sm[:, 2, h:h + 1], scalar2=sm[:, 5, h:h + 1],
                                                                      op0=ALU.subtract, op1=ALU.mult), reads=[osbb, smb], writes=[ynb])
                k.op("dve", lambda e: e.tensor_tensor(out=yn[:], in0=yn[:], in1=gn[:], op=ALU.mult), reads=[ynb, rc], writes=[ynb])
                self.chk(10)
                k.op("act", lambda e: e.activation(out=gs[:], in_=pg[:, :], func=AF.Silu), reads=[pgb], writes=[gsb])
                k.op("dve", lambda e: e.tensor_tensor(out=orb[:], in0=yn[:], in1=gs[:], op=ALU.mult), reads=[ynb, gsb], writes=[orbb])
                self.chk(11)
                for j in range(4):
                    k.op("pe", lambda e, j=j: e.transpose(ptr2[:, j * 128:(j + 1) * 128], orb[:, j * 128:(j + 1) * 128], self.ident[:]),
                         reads=[orbb, self.cb], writes=[ptr2b])
                i = t % 2
                k.op("act", lambda e, i=i: e.activation(out=orT[i][:], in_=ptr2[:, 0:512].rearrange("p (j c) -> p j c", j=4), func=AF.Copy),
                     reads=[ptr2b], writes=[orTb[i]])
                self.chk(12)
                k.dma("sp", self.brT[1, :, :, tk].rearrange("c p q -> p c q"), orT[i][:], reads=[orTb[i]], writes=[self.brT_b[1][t]])

    def phase_conv(self, l):
        k = self.k
        with ExitStack() as st:
            Wa = self.sb(st, "cWa", [128, 8, 512], BF16)
            Wb = self.sb(st, "cWb", [128, 8, 512], BF16)
            wab = Buf("cW")
            self.load_w(Wa, self.din["w_in"][l][:, C_CA:C_CA + 512], wab, 8)
            self.load_w(Wb, self.din["w_in"][l][:, C_CB:C_CB + 512], wab, 8)
            cw = self.sb(st, "ccw", [128, 4, 31], F32)
            cvec = self.sb(st, "cvec", [128, 3, 4], F32)
            ones = self.sb(st, "cones", [128, 128], F32)
            cc = Buf("cconst")
            k.dma("sp", cw[:], self.din["convw"][l], writes=[cc])
            k.dma("sp", cvec[:, 0, :], self.din["convb"][l], writes=[cc])
            k.dma("sp", cvec[:, 1, :], self.din["convg"][l], writes=[cc])
            k.dma("sp", cvec[:, 2, :], self.din["convbb"][l], writes=[cc])
            k.dma("sp", ones[:], self.din["ones"][:, :], writes=[cc])
            u = [self.sb(st, "cu%d" % i, [128, 4, 542], F32) for i in range(2)]
            ub = [[Buf("cu%d_%d" % (i, ct)) for ct in range(4)] for i in range(2)]
            acc = self.sb(st, "cacc", [128, 4, 512], F32)
            accb = [Buf("cacc%d" % ct) for ct in range(4)]
            sg = [self.sb(st, "csg%d" % i, [128, 512], F32) for i in range(2)]
            sgb = [Buf("csg%d" % i) for i in range(2)]
            ysq = [self.sb(st, "cysq%d" % i, [128, 512], F32) for i in range(2)]
            ysqb = [Buf("cysq%d" % i) for i in range(2)]
            stt = self.sb(st, "cstt", [128, 4, 512], F32)
            sttb = Buf("cstt")
            yn = [self.sb(st, "cyn%d" % i, [128, 512], F32) for i in range(2)]
            ynb = [Buf("cyn%d" % i) for i in range(2)]
            oc = [self.sb(st, "coc%d" % i, [128, 512], BF16) for i in range(2)]
            ocb = [Buf("coc%d" % i) for i in range(2)]
            pa = [self.ps(st, "cpa%d" % i, [128, 512], F32) for i in range(2)]
            pab = [Buf("cpa%d" % i) for i in range(2)]
            pbk = [self.ps(st, "cpb%d" % i, [128, 512], F32) for i in range(2)]
            pbb = [Buf("cpb%d" % i) for i in range(2)]
            s1 = self.ps(st, "cs1", [128, 512], F32)
            s2 = self.ps(st, "cs2", [128, 512], F32)
            s1b, s2b = Buf("cs1"), Buf("cs2")
            for ct in range(4):
                k.op("dve", lambda e, ct=ct: e.memset(u[0][:, ct, 0:30], 0.0), writes=[ub[0][ct]])
            for G in range(8):
                toks = slice(G * 512, (G + 1) * 512)
                hb = self.hT_b[G * 4:(G + 1) * 4]
                ug, ugb = u[G % 2], ub[G % 2]
                for ct in range(4):
                    p = ct % 2
                    cols = slice(ct * 128, (ct + 1) * 128)
                    for kc in range(8):
                        k.op("pe", lambda e, kc=kc, cols=cols, p=p: e.matmul(pa[p][:, :], lhsT=Wa[:, kc, cols], rhs=self.hT[:, kc, toks],
                                                                              start=(kc == 0), stop=(kc == 7)), reads=[wab] + hb, writes=[pab[p]])
                    for kc in range(8):
                        k.op("pe", lambda e, kc=kc, cols=cols, p=p: e.matmul(pbk[p][:, :], lhsT=Wb[:, kc, cols], rhs=self.hT[:, kc, toks],
                                                                              start=(kc == 0), stop=(kc == 7)), reads=[wab] + hb, writes=[pbb[p]])
                    k.op("act", lambda e, p=p: e.activation(out=sg[p][:], in_=pbk[p][:, :], func=AF.Sigmoid), reads=[pbb[p]], writes=[sgb[p]])
                    if G > 0:
                        k.op("act", lambda e, ct=ct: e.activation(out=ug[:, ct, 0:30], in_=u[(G - 1) % 2][:, ct, 512:542], func=AF.Copy),
                             reads=[ub[(G - 1) % 2][ct]], writes=[ugb[ct]])
                    k.op("dve", lambda e, ct=ct, p=p: e.tensor_tensor(out=ug[:, ct, 30:542], in0=pa[p][:, :], in1=sg[p][:], op=ALU.mult),
                         reads=[pab[p], sgb[p]], writes=[ugb[ct]])
                    en = "dve"
                    k.op(en, lambda e, ct=ct: e.tensor_scalar(out=acc[:, ct, :], in0=ug[:, ct, 0:512], scalar1=cw[:, ct, 0:1],
                                                              scalar2=cvec[:, 0, ct:ct + 1], op0=ALU.mult, op1=ALU.add),
                         reads=[ugb[ct], cc], writes=[accb[ct]])
                    for w in range(1, 31):
                        k.op(en, lambda e, ct=ct, w=w: e.scalar_tensor_tensor(out=acc[:, ct, :], in0=ug[:, ct, w:w + 512], scalar=cw[:, ct, w:w + 1],
                                                                               in1=acc[:, ct, :], op0=ALU.mult, op1=ALU.add),
                             reads=[ugb[ct], cc], writes=[accb[ct]])
                    k.op("act", lambda e, ct=ct, p=p: e.activation(out=ysq[p][:], in_=acc[:, ct, :], func=AF.Square), reads=[accb[ct]], writes=[ysqb[p]])
                    k.op("pe", lambda e, ct=ct: e.matmul(s1[:, :], lhsT=ones[:], rhs=acc[:, ct, :], start=(ct == 0), stop=(ct == 3)),
                         reads=[cc, accb[ct]], writes=[s1b])
                    k.op("pe", lambda e, ct=ct, p=p: e.matmul(s2[:, :], lhsT=ones[:], rhs=ysq[p][:], start=(ct == 0), stop=(ct == 3)),
                         reads=[cc, ysqb[p]], writes=[s2b])
                k.op("act", lambda e: e.activation(out=stt[:, 0, :], in_=s1[:, :], func=AF.Copy, scale=1.0 / 512), reads=[s1b], writes=[sttb])
                k.op("dve", lambda e: e.tensor_tensor(out=stt[:, 1, :], in0=stt[:, 0, :], in1=stt[:, 0, :], op=ALU.mult), reads=[sttb], writes=[sttb])
                k.op("dve", lambda e: e.scalar_tensor_tensor(out=stt[:, 1, :], in0=s2[:, :], scalar=1.0 / 512, in1=stt[:, 1, :],
                                                             op0=ALU.mult, op1=ALU.subtract), reads=[s2b, sttb], writes=[sttb])
                k.op("act", lambda e: e.activation(out=stt[:, 3, :], in_=stt[:, 1, :], func=AF.Sqrt, bias=self.cst[:, 0:1], scale=1.0),
                     reads=[sttb, self.cb], writes=[sttb])
                k.op("dve", lambda e: e.reciprocal(out=stt[:, 2, :], in_=stt[:, 3, :]), reads=[sttb], writes=[sttb])
                for ct in range(4):
                    p = ct % 2
                    k.op("dve", lambda e, ct=ct, p=p: e.tensor_tensor(out=yn[p][:], in0=acc[:, ct, :], in1=stt[:, 0, :], op=ALU.subtract),
                         reads=[accb[ct], sttb], writes=[ynb[p]])
                    k.op("dve", lambda e, p=p: e.tensor_tensor(out=yn[p][:], in0=yn[p][:], in1=stt[:, 2, :], op=ALU.mult),
                         reads=[sttb, ynb[p]], writes=[ynb[p]])
                    k.op("act", lambda e, ct=ct, p=p: e.activation(out=oc[p][:], in_=yn[p][:], func=AF.Silu, scale=cvec[:, 1, ct:ct + 1],
                                                                   bias=cvec[:, 2, ct:ct + 1]), reads=[ynb[p], cc], writes=[ocb[p]])
                    k.dma("sp", self.brT[2, ct, :, toks], oc[p][:], reads=[ocb[p]], writes=self.brT_b[2][G * 4:(G + 1) * 4])

    def phase_merge(self, l):
        k = self.k
        for dh in range(2):
            with ExitStack() as st:
                Wg = self.sb(st, "mWg", [128, 8, 3, 512], BF16)
                Wbr = self.sb(st, "mWbr", [128, 3, 4, 512], BF16)
                Wo = self.sb(st, "mWo", [128, 4, 1024], BF16)
                wb = Buf("mW")
                for b in range(3):
                    for kc in range(8):
                        c0 = C_MG + b * 1024 + dh * 512
                        k.dma("pool", Wg[:, kc, b, :], self.din["w_in"][l][kc * 128:(kc + 1) * 128, c0:c0 + 512], writes=[wb])
                    for c4 in range(4):
                        k.dma("pool", Wbr[:, b, c4, :], self.din["w_branch"][l][b][c4 * 128:(c4 + 1) * 128, dh * 512:(dh + 1) * 512], writes=[wb])
                for dc in range(4):
                    r0 = dh * 512 + dc * 128
                    k.dma("pool", Wo[:, dc, :], self.din["w_out"][l][r0:r0 + 128, :], writes=[wb])
                brg = [self.sb(st, "mbr%d" % i, [128, 3, 4, 512], BF16) for i in range(2)]
                brgb = [Buf("mbr%d" % i) for i in range(2)]
                mT = self.sb(st, "mmT", [128, 4, 512], BF16)
                mTb = [Buf("mT%d" % i) for i in range(4)]
                gate = [self.sb(st, "mgate%d" % i, [128, 512], F32) for i in range(2)]
                gateb = [Buf("mgate%d" % i) for i in range(2)]
                macc = self.sb(st, "mmacc", [128, 512], F32); maccb = Buf("macc")
                mtmp = self.sb(st, "mmtmp", [128, 512], F32); mtmpb = Buf("mtmp")
                ctx = self.upd_alloc(st, "m", self.din["nmlp"][l] if dh == 1 else None)
                pg = [self.ps(st, "mpg%d" % i, [128, 512], F32) for i in range(2)]
                pgb = [Buf("mpg%d" % i) for i in range(2)]
                pp = [self.ps(st, "mpp%d" % i, [128, 512], F32) for i in range(2)]
                ppb = [Buf("mpp%d" % i) for i in range(2)]
                po = [self.ps(st, "mpo%d" % i, [128, 512], F32) for i in range(2)]
                pob = [Buf("mpo%d" % i) for i in range(2)]
                if dh == 1:
                    ctx["ptr"] = self.ps(st, "mptr", [128, 1024], BF16)
                    ctx["ptr_b"] = Buf("mptr")
                it = 0
                for G in range(8):
                    toks = slice(G * 512, (G + 1) * 512)
                    hb = self.hT_b[G * 4:(G + 1) * 4]
                    bi = G % 2
                    for b in range(3):
                        k.dma("sp", brg[bi][:, b], self.brT[b, :, :, toks].rearrange("c p t -> p c t"),
                              reads=self.brT_b[b][G * 4:(G + 1) * 4], writes=[brgb[bi]])
                    for dc in range(4):
                        for b in range(3):
                            p = it % 2
                            it += 1
                            for kc in range(8):
                                k.op("pe", lambda e, kc=kc, b=b, dc=dc, p=p: e.matmul(pg[p][:, :], lhsT=Wg[:, kc, b, dc * 128:(dc + 1) * 128],
                                                                                     rhs=self.hT[:, kc, toks], start=(kc == 0), stop=(kc == 7)),
                                     reads=[wb] + hb, writes=[pgb[p]])
                            for c4 in range(4):
                                k.op("pe", lambda e, c4=c4, b=b, dc=dc, p=p: e.matmul(pp[p][:, :], lhsT=Wbr[:, b, c4, dc * 128:(dc + 1) * 128],
                                                                                     rhs=brg[bi][:, b, c4, :], start=(c4 == 0), stop=(c4 == 3)),
                                     reads=[wb, brgb[bi]], writes=[ppb[p]])
                            k.op("act", lambda e, p=p: e.activation(out=gate[p][:], in_=pg[p][:, :], func=AF.Sigmoid), reads=[pgb[p]], writes=[gateb[p]])
                            if b == 0:
                                k.op("dve", lambda e, p=p: e.tensor_tensor(out=macc[:], in0=pp[p][:, :], in1=gate[p][:], op=ALU.mult),
                                     reads=[ppb[p], gateb[p]], writes=[maccb])
                            else:
                                k.op("dve", lambda e, p=p: e.tensor_tensor(out=mtmp[:], in0=pp[p][:, :], in1=gate[p][:], op=ALU.mult),
                                     reads=[ppb[p], gateb[p]], writes=[mtmpb])
                                if b == 1:
                                    k.op("pool", lambda e: e.tensor_tensor(out=macc[:], in0=macc[:], in1=mtmp[:], op=ALU.add),
                                         reads=[mtmpb, maccb], writes=[maccb])
                                else:
                                    k.op("pool", lambda e, dc=dc: e.tensor_tensor(out=mT[:, dc, :], in0=macc[:], in1=mtmp[:], op=ALU.add),
                                         reads=[mtmpb, maccb], writes=[mTb[dc]])
                    for tt in range(4):
                        t = G * 4 + tt
                        for nh in range(2):
                            for dc in range(4):
                                k.op("pe", lambda e, nh=nh, dc=dc, tt=tt: e.matmul(po[nh][:, :], lhsT=mT[:, dc, tt * 128:(tt + 1) * 128],
                                                                                  rhs=Wo[:, dc, nh * 512:(nh + 1) * 512], start=(dc == 0), stop=(dc == 3)),
                                     reads=[wb, mTb[dc]], writes=[pob[nh]])
                        self.x_update(ctx, t, [po[0][:, :], po[1][:, :]], pob, "norm" if dh == 1 else None)
                k.barrier()


_PROG_CACHE = {}


def _get_prog(**kw):
    key = tuple(sorted((k, str(v)) for k, v in kw.items()))
    if key not in _PROG_CACHE:
        _PROG_CACHE[key] = Prog(**kw)
    return _PROG_CACHE[key]


def kernel(**inputs):
    inp = {k: np.asarray(v) for k, v in inputs.items()}
    x = np.ascontiguousarray(inp["x"], dtype=np.float32)
    shared = _host_inputs(inp)
    prog = _get_prog()
    in_maps = []
    for c in range(8):
        m = dict(shared)
        m["x"] = x[c]
        in_maps.append(m)
    res = run_bass_kernel_spmd(prog.nc, in_maps, core_ids=list(range(8)))
    return np.stack([np.asarray(r["out"], dtype=np.float32) for r in res.results], axis=0)
```

```python
import numpy as np
from contextlib import ExitStack
import concourse.bass as bass
import concourse.mybir as mybir
from concourse.bass_utils import run_bass_kernel_spmd

F32 = mybir.dt.float32
BF16 = mybir.dt.bfloat16
AF = mybir.ActivationFunctionType
ALU = mybir.AluOpType
AX = mybir.AxisListType

S = 4096
D = 1024
NT = 32
DEPTH = 2
IN_TOTAL = 6936
EPS = 1e-6
NEG = -30000.0
C_QN, C_KC, C_VC, C_KS, C_VS, C_KW, C_VW, C_GN = 0, 512, 640, 768, 896, 1024, 1152, 1280
C_QR, C_KR, C_VR, C_GR, C_CA, C_CB, C_MG = 1304, 1560, 1816, 2328, 2840, 3352, 3864

SAME_ENGINE_SYNC = True


class Buf:
    __slots__ = ("w", "r", "name", "wd")

    def __init__(self, name=""):
        self.w = None
        self.r = {}
        self.name = name
        self.wd = False


class _Sem:
    def __init__(self, sem, name):
        self.sem = sem
        self.count = 0
        self.name = name


class Eng(_Sem):
    def __init__(self, name, handle, sem):
        super().__init__(sem, name)
        self.h = handle
        self.waited = {}


class K:
    def __init__(self, nc, stack, n_dma_sems=64):
        self.nc = nc
        self.engs = {}
        for name, h in (("pe", nc.tensor), ("act", nc.scalar), ("dve", nc.vector),
                        ("pool", nc.gpsimd), ("sp", nc.sync)):
            sem = stack.enter_context(nc.semaphore("sem_" + name))
            self.engs[name] = Eng(name, h, sem)
        self.dsems = [_Sem(stack.enter_context(nc.semaphore("dsem%d" % i)), "d%d" % i)
                      for i in range(n_dma_sems)]
        self.dpool = {"pool": self.dsems[:n_dma_sems // 2], "sp": self.dsems[n_dma_sems // 2:]}
        self.dnext = {"pool": 0, "sp": 0}
        self.n_ops = 0
        self.muted = False

    def _wait_deps(self, E, reads, writes):
        deps = {}

        def need(tok):
            if tok is None:
                return
            s, v = tok
            if deps.get(s, 0) < v:
                deps[s] = v
        for b in reads:
            for t in b.w or ():
                need(t)
        for b in writes:
            for t in b.w or ():
                need(t)
            for t in b.r.values():
                need(t)
        for s, v in deps.items():
            if s is E and (E.name in ("pe", "sp") or not SAME_ENGINE_SYNC):
                continue
            if E.waited.get(s, 0) < v:
                E.h.wait_ge(s.sem, v)
                E.waited[s] = v

    def _mark(self, tok, reads, writes, is_dma=False):
        for b in reads:
            b.r[tok[0]] = tok
        for b in writes:
            if is_dma and b.w and getattr(b, "wd", False) and len(b.w) < 24:
                b.w = b.w + [tok]
            else:
                b.w = [tok]
            b.wd = is_dma
            b.r = {}

    def op(self, en, fn, reads=(), writes=()):
        if self.muted:
            return None
        E = self.engs[en]
        self._wait_deps(E, reads, writes)
        ins = fn(E.h)
        E.count += 1
        ins.then_inc(E.sem, 1)
        self._mark((E, E.count), reads, writes)
        self.n_ops += 1
        return ins

    def dma(self, en, out, in_, reads=(), writes=(), **kw):
        if self.muted:
            return None
        E = self.engs[en]
        self._wait_deps(E, reads, writes)
        pool_ = self.dpool[en]
        d = pool_[self.dnext[en]]
        self.dnext[en] = (self.dnext[en] + 1) % len(pool_)
        if d.count and E.waited.get(d, 0) < d.count:
            E.h.wait_ge(d.sem, d.count)
            E.waited[d] = d.count
        ins = E.h.dma_start(out=out, in_=in_, **kw)
        d.count += 16
        ins.then_inc(d.sem, 16)
        self._mark((d, d.count), reads, writes, is_dma=True)
        self.n_ops += 1
        return ins

    def barrier(self):
        allsems = list(self.engs.values()) + self.dsems
        for E in self.engs.values():
            for s in allsems:
                if s is E or s.count == 0:
                    continue
                if E.waited.get(s, 0) < s.count:
                    E.h.wait_ge(s.sem, s.count)
                    E.waited[s] = s.count


def _rel_bucket_np(dist):
    n = np.maximum(dist, 0)
    nf = np.maximum(n, 1).astype(np.float32)
    large = 16 + (np.log(nf / np.float32(16)) / np.float32(np.log(8.0)) * np.float32(16)).astype(np.int32)
    large = np.minimum(large, 31)
    return np.where(n < 16, n, large).astype(np.int64)


_CONST_CACHE = {}


def _host_consts():
    if _CONST_CACHE:
        return _CONST_CACHE
    c = {}
    i = np.arange(128)
    c["ident"] = np.eye(128, dtype=np.float32)
    c["i4"] = np.tile(np.eye(128, dtype=np.float32), (1, 4))
    mw = np.zeros((128, 5, 128), np.float32)
    jj, ii = np.meshgrid(i, i, indexing="ij")
    mw[:, 0, :] = (ii >= jj)
    mw[:, 1:4, :] = 1.0
    mw[:, 4, :] = (ii < jj)
    c["maskw"] = mw
    dist_w = 128 * np.arange(5)[None, :, None] + ii[:, None, :] - jj[:, None, :]
    c["_bucket_w"] = _rel_bucket_np(dist_w)
    m = np.arange(504)
    dist_c = i[None, :] - 16 * (m[:, None] - 248) - 31
    c["maskc"] = (dist_c >= 0).astype(np.float32)
    c["_bucket_c"] = _rel_bucket_np(dist_c)
    n = np.arange(256)
    s = np.arange(64)
    ov = ((16 * n[:, None] <= 64 * s[None, :] + 63) & (16 * n[:, None] + 31 >= 64 * s[None, :])).astype(np.float32)
    ov[255] = 0.0
    c["overlap"] = ov.reshape(2, 128, 64).transpose(1, 0, 2).copy()
    j = np.arange(126)
    sp = j[None, :] - 62
    cur = (i[:, None] >= 64).astype(np.int64)
    valid = sp <= cur
    forced = (sp == cur) | (sp == cur - 1)
    c["selvalid"] = valid.astype(np.float32)
    c["seladd"] = np.where(forced, 1e4, np.where(valid, 0.0, -1e4)).astype(np.float32)
    half = 32
    inv = (10000.0 ** (-np.arange(half, dtype=np.float32) / half)).astype(np.float32)
    pos = np.arange(S, dtype=np.float32)
    ang = (pos[:, None] * inv[None, :]).astype(np.float32)
    c["cos"] = np.cos(ang).astype(np.float32).reshape(NT, 128, 32).transpose(1, 0, 2).copy()
    c["sin"] = np.sin(ang).astype(np.float32).reshape(NT, 128, 32).transpose(1, 0, 2).copy()
    log_g = np.log1p(-np.exp2(-5.0 - np.arange(4, dtype=np.float32))).astype(np.float32)
    diff = i[None, :] - i[:, None]
    dec = np.where(diff[None] >= 0, np.exp(log_g[:, None, None] * np.maximum(diff[None], 0)), 0.0)
    c["decayT"] = dec.transpose(1, 0, 2).astype(np.float32).copy()
    xi = np.exp(log_g[:, None] * (i[None, :] + 1)).astype(np.float32)
    c["xi"] = np.ascontiguousarray(np.broadcast_to(xi[None, :, :], (64, 4, 128))).astype(np.float32)
    c["zeta"] = np.exp(log_g[None, :] * (127 - i[:, None])).astype(np.float32)
    gch = np.exp(log_g * 128).astype(np.float32)
    c["gch"] = np.ascontiguousarray(np.broadcast_to(gch[None, :], (64, 4))).astype(np.float32)
    c["ones"] = np.ones((128, 128), np.float32)
    c["rfull"] = (np.arange(S)[None, :] // 64 == np.arange(64)[:, None]).astype(np.float32)
    _CONST_CACHE.update(c)
    return c


CONST_SHAPES = {
    "ident": [128, 128], "i4": [128, 512], "maskw": [128, 5, 128], "maskc": [504, 128],
    "overlap": [128, 2, 64], "selvalid": [128, 126], "seladd": [128, 126],
    "cos": [128, NT, 32], "sin": [128, NT, 32], "decayT": [128, 4, 128], "xi": [64, 4, 128],
    "zeta": [128, 4], "gch": [64, 4], "ones": [128, 128], "rfull": [64, S],
    "bias_w": [128, 5, 8, 128], "bias_c": [504, 8, 128], "b31": [128, 8],
}

W_SHAPES = {
    "w_in": [DEPTH, D, IN_TOTAL], "w_branch": [DEPTH, 3, 512, D], "w_out": [DEPTH, D, D],
    "w_ff1": [DEPTH, D, 4 * D], "w_ff2": [DEPTH, 4 * D, D],
    "cmp_w1_k": [DEPTH, 32, 64, 128], "cmp_w1_v": [DEPTH, 32, 64, 128],
    "cmp_w2_k": [DEPTH, 128, 64], "cmp_w2_v": [DEPTH, 128, 64],
    "cmp_pe_kT": [DEPTH, 64, 32], "cmp_pe_vT": [DEPTH, 64, 32],
    "nmix": [DEPTH, 128, D], "nmlp": [DEPTH, 128, D], "nfin": [128, D],
    "retgn": [DEPTH, 128, 512],
    "convw": [DEPTH, 128, 4, 31], "convb": [DEPTH, 128, 4], "convg": [DEPTH, 128, 4], "convbb": [DEPTH, 128, 4],
}


def _host_inputs(inp):
    c = _host_consts()
    f = lambda a: np.ascontiguousarray(a, dtype=np.float32)
    out = {k: f(v) for k, v in c.items() if not k.startswith("_")}
    rt = f(inp["rel_table"])
    out["bias_w"] = f(rt[c["_bucket_w"]].transpose(0, 1, 3, 2))
    out["bias_c"] = f(rt[c["_bucket_c"]].transpose(0, 2, 1))
    out["b31"] = f(np.broadcast_to(rt[31][None, :], (128, 8)))
    for kname in ("w_in", "w_branch", "w_out", "w_ff1", "w_ff2", "cmp_w1_k", "cmp_w1_v", "cmp_w2_k", "cmp_w2_v"):
        out[kname] = f(inp[kname])
    out["cmp_pe_kT"] = f(np.transpose(inp["cmp_pe_k"], (0, 2, 1)))
    out["cmp_pe_vT"] = f(np.transpose(inp["cmp_pe_v"], (0, 2, 1)))
    out["nmix"] = f(np.broadcast_to(inp["norm_mix"][:, None, :], (DEPTH, 128, D)))
    out["nmlp"] = f(np.broadcast_to(inp["norm_mlp"][:, None, :], (DEPTH, 128, D)))
    out["nfin"] = f(np.broadcast_to(inp["norm_final"][None, :], (128, D)))
    out["retgn"] = f(np.broadcast_to(inp["ret_gn"][:, None, :], (DEPTH, 128, 512)))
    out["convw"] = f(np.transpose(inp["conv_w"].reshape(DEPTH, 31, 4, 128), (0, 3, 2, 1)))
    for a, b in (("convb", "conv_b"), ("convg", "conv_ln_g"), ("convbb", "conv_ln_b")):
        out[a] = f(np.transpose(inp[b].reshape(DEPTH, 4, 128), (0, 2, 1)))
    return out


class _Stop(Exception):
    pass


class Prog:
    def __init__(self, n_layers=DEPTH, phases=("nsa", "ret", "conv", "merge", "ffn"), dbg=False):
        self.n_layers = n_layers
        self.phases = phases
        self.dbg = dbg
        nc = self.nc = bass.Bass("TRN2", target_bir_lowering=False)
        self.din = {}
        self.din["x"] = nc.dram_tensor("x", [S, D], F32, kind="ExternalInput").ap()
        for name, shp in list(CONST_SHAPES.items()) + list(W_SHAPES.items()):
            self.din[name] = nc.dram_tensor(name, shp, F32, kind="ExternalInput").ap()
        self.out = nc.dram_tensor("out", [S, D], F32, kind="ExternalOutput").ap()
        skind = "ExternalOutput" if dbg else "Internal"
        self.xres = nc.dram_tensor("xres", [S, D], F32, kind=skind).ap()
        self.brT = nc.dram_tensor("brT", [3, 4, 128, S], BF16, kind=skind).ap()
        self.ebc_d = nc.dram_tensor("ebc_d", [504, 8, 128], BF16, kind=skind).ap()
        self.dbg_d = nc.dram_tensor("dbg_d", [128, 2048], F32, kind=skind).ap()
        self.xres_b = [Buf("xres%d" % t) for t in range(NT)]
        self.brT_b = [[Buf("brT%d_%d" % (b, t)) for t in range(NT)] for b in range(3)]
        self.ebc_db = Buf("ebc_d")
        self.out_b = Buf("out")
        with ExitStack() as st:
            self.st = st
            self.k = K(nc, st)
            self.build()

    def sb(self, st, name, shape, dt):
        self._uid = getattr(self, "_uid", 0) + 1
        return st.enter_context(self.nc.sbuf_tensor("s%d_%s" % (self._uid, name), shape, dt))

    def ps(self, st, name, shape, dt):
        self._uid = getattr(self, "_uid", 0) + 1
        return st.enter_context(self.nc.psum_tensor("p%d_%s" % (self._uid, name), shape, dt))

    def chk(self, n):
        import os
        v = os.environ.get("RSTOP")
        if v is not None and int(v) == n:
            self.k.muted = True

    def load_const(self, dst, src, buf, eng="sp"):
        self.k.dma(eng, dst, src, writes=[buf])

    def build(self):
        k, nc, st = self.k, self.nc, self.st
        self.hT = self.sb(st, "hT_all", [128, 8, S], BF16)
        self.hT_b = [Buf("hT%d" % t) for t in range(NT)]
        self.ident = self.sb(st, "ident", [128, 128], BF16)
        self.i4 = self.sb(st, "i4", [128, 512], BF16)
        self.identf = self.sb(st, "identf", [128, 128], F32)
        self.cst = self.sb(st, "cst", [128, 4], F32)
        self.EB = self.sb(st, "EB", [128, 5, 8, 128], BF16)
        self.cb = Buf("consts")
        k.dma("pool", self.ident[:], self.din["ident"][:, :], writes=[self.cb])
        k.dma("pool", self.i4[:], self.din["i4"][:, :], writes=[self.cb])
        k.dma("sp", self.identf[:], self.din["ident"][:, :], writes=[self.cb])
        k.op("dve", lambda e: e.memset(self.cst[:, 0:1], EPS), writes=[self.cb])
        k.op("dve", lambda e: e.memset(self.cst[:, 1:2], 0.0), writes=[self.cb])
        k.op("dve", lambda e: e.memset(self.cst[:, 2:3], 1.0), writes=[self.cb])
        self.phase_bias_tables()
        k.barrier()
        self.phase0()
        k.barrier()
        for l in range(self.n_layers):
            last = (l == DEPTH - 1)
            for ph, fn in (("nsa", lambda: self.phase_nsa(l)), ("ret", lambda: self.phase_ret(l)), ("conv", lambda: self.phase_conv(l)),
                           ("merge", lambda: self.phase_merge(l)), ("ffn", lambda: self.phase_ffn(l, last))):
                if ph in self.phases:
                    with nc.named_scope("%s%d" % (ph, l)):
                        fn()
                        k.muted = False
                        k.barrier()
        k.barrier()

    def rms_alloc(self, st, tag):
        r = {}
        r["junk"] = self.sb(st, "rjunk" + tag, [128, D], BF16)
        r["ss"] = self.sb(st, "rss" + tag, [128, 4], F32)
        r["hb"] = self.sb(st, "rhb" + tag, [128, D], BF16)
        r["b"] = Buf("rms" + tag)
        r["hbb"] = Buf("rmshb" + tag)
        return r

    def rms_stats(self, r, src, src_bufs):
        k = self.k
        ss = r["ss"]
        k.op("dve", lambda e: e.memset(ss[:, 0:1], 0.0), writes=[r["b"]])
        k.op("act", lambda e: e.activation(out=r["junk"][:], in_=src, func=AF.Square, accum_out=ss[:, 0:1]),
             reads=list(src_bufs) + [r["b"]], writes=[r["b"]])
        k.op("act", lambda e: e.activation(out=ss[:, 1:2], in_=ss[:, 0:1], func=AF.Sqrt, bias=self.cst[:, 0:1], scale=1.0 / D),
             reads=[r["b"], self.cb], writes=[r["b"]])
        k.op("dve", lambda e: e.reciprocal(out=ss[:, 2:3], in_=ss[:, 1:2]), reads=[r["b"]], writes=[r["b"]])
        return ss[:, 2:3]

    def rms_to_hT(self, r, src, src_bufs, gain, gain_buf, t, ptr, ptr_b):
        k = self.k
        rstd = self.rms_stats(r, src, src_bufs)
        hb = r["hb"]
        k.op("dve", lambda e: e.scalar_tensor_tensor(out=hb[:], in0=src, scalar=rstd, in1=gain, op0=ALU.mult, op1=ALU.mult),
             reads=list(src_bufs) + [r["b"], gain_buf], writes=[r["hbb"]])
        for c in range(8):
            k.op("pe", lambda e, c=c: e.transpose(ptr[:, c * 128:(c + 1) * 128], hb[:, c * 128:(c + 1) * 128], self.ident[:]),
                 reads=[r["hbb"], self.cb], writes=[ptr_b])
        k.op("act", lambda e: e.activation(out=self.hT[:, :, t * 128:(t + 1) * 128],
                                           in_=ptr[:, :].rearrange("p (c q) -> p c q", c=8), func=AF.Copy),
             reads=[ptr_b], writes=[self.hT_b[t]])

    def phase_bias_tables(self):
        k = self.k
        with ExitStack() as st:
            bw = self.sb(st, "bw", [128, 5, 8, 128], F32)
            mw = self.sb(st, "mw", [128, 5, 128], F32)
            b31 = self.sb(st, "b31", [128, 8], F32)
            bb = Buf("bw")
            k.dma("sp", bw[:], self.din["bias_w"][:, :, :, :], writes=[bb])
            k.dma("sp", mw[:], self.din["maskw"][:, :, :], writes=[bb])
            k.dma("sp", b31[:], self.din["b31"][:, :], writes=[bb])
            for off in range(5):
                k.op("dve", lambda e, off=off: e.tensor_tensor(out=bw[:, off], in0=bw[:, off],
                                                               in1=b31[:, :].unsqueeze(2).to_broadcast([128, 8, 128]), op=ALU.subtract),
                     reads=[bb], writes=[bb])
                k.op("act", lambda e, off=off: e.activation(out=bw[:, off], in_=bw[:, off], func=AF.Exp), reads=[bb], writes=[bb])
                k.op("dve", lambda e, off=off: e.tensor_tensor(out=self.EB[:, off], in0=bw[:, off],
                                                               in1=mw[:, off:off + 1, :].to_broadcast([128, 8, 128]), op=ALU.mult),
                     reads=[bb], writes=[self.cb])
            bc = self.sb(st, "bc", [126, 8, 128], F32)
            mc = self.sb(st, "mc", [126, 128], F32)
            bcb = self.sb(st, "bcb", [126, 8, 128], BF16)
            cbuf = Buf("bc")
            for r4 in range(4):
                rows = slice(r4 * 126, (r4 + 1) * 126)
                k.dma("sp", bc[:], self.din["bias_c"][rows, :, :], writes=[cbuf])
                k.dma("sp", mc[:], self.din["maskc"][rows, :], writes=[cbuf])
                k.op("dve", lambda e: e.tensor_tensor(out=bc[:], in0=bc[:], in1=b31[0:126, :].unsqueeze(2).to_broadcast([126, 8, 128]),
                                                      op=ALU.subtract), reads=[cbuf, bb], writes=[cbuf])
                k.op("act", lambda e: e.activation(out=bc[:], in_=bc[:], func=AF.Exp), reads=[cbuf], writes=[cbuf])
                k.op("dve", lambda e: e.tensor_tensor(out=bcb[:], in0=bc[:], in1=mc[:, :].unsqueeze(1).to_broadcast([126, 8, 128]),
                                                      op=ALU.mult), reads=[cbuf], writes=[cbuf])
                k.dma("sp", self.ebc_d[rows, :, :], bcb[:], reads=[cbuf], writes=[self.ebc_db])
            k.barrier()

    def phase0(self):
        k = self.k
        with ExitStack() as st:
            r = self.rms_alloc(st, "p0")
            gain = self.sb(st, "gain0", [128, D], F32)
            gb = Buf("gain0")
            k.dma("sp", gain[:], self.din["nmix"][0], writes=[gb])
            xt = [self.sb(st, "p0x%d" % i, [128, D], F32) for i in range(2)]
            xb = [Buf("p0x%d" % i) for i in range(2)]
            ptr = self.ps(st, "p0tr", [128, 1024], BF16)
            ptr_b = Buf("p0tr")
            for t in range(NT):
                i = t % 2
                k.dma("sp", xt[i][:], self.din["x"][t * 128:(t + 1) * 128, :], writes=[xb[i]])
                k.dma("pool", self.xres[t * 128:(t + 1) * 128, :], xt[i][:], reads=[xb[i]], writes=[self.xres_b[t]])
                self.rms_to_hT(r, xt[i][:], [xb[i]], gain[:], gb, t, ptr, ptr_b)

    def load_w(self, dst, src, buf, kc):
        for c in range(kc):
            self.k.dma("pool", dst[:, c, :], src[c * 128:(c + 1) * 128, :], writes=[buf])

    def x_update(self, ctx, t, psum_halves, psum_bufs, hook):
        k = self.k
        i = ctx["i"] = (ctx.get("i", 0) + 1) % 2
        xt, xb = ctx["xt"][i], ctx["xb"][i]
        k.dma("sp", xt[:], self.xres[t * 128:(t + 1) * 128, :], reads=[self.xres_b[t]], writes=[xb])
        for h in range(2):
            k.op("dve", lambda e, h=h: e.tensor_tensor(out=xt[:, h * 512:(h + 1) * 512], in0=psum_halves[h],
                                                       in1=xt[:, h * 512:(h + 1) * 512], op=ALU.add),
                 reads=[psum_bufs[h], xb], writes=[xb])
        if hook != "final":
            k.dma("pool", self.xres[t * 128:(t + 1) * 128, :], xt[:], reads=[xb], writes=[self.xres_b[t]])
        if hook is None:
            return
        r = ctx["rms"]
        if hook == "final":
            rstd = self.rms_stats(r, xt[:], [xb])
            ot = ctx["ot"]
            k.op("dve", lambda e: e.scalar_tensor_tensor(out=ot[:], in0=xt[:], scalar=rstd, in1=ctx["gain"][:], op0=ALU.mult, op1=ALU.mult),
                 reads=[xb, r["b"], ctx["gain_b"]], writes=[ctx["ot_b"]])
            k.dma("pool", self.out[t * 128:(t + 1) * 128, :], ot[:], reads=[ctx["ot_b"]], writes=[self.out_b])
        else:
            self.rms_to_hT(r, xt[:], [xb], ctx["gain"][:], ctx["gain_b"], t, ctx["ptr"], ctx["ptr_b"])

    def upd_alloc(self, st, tag, gain_src, final=False):
        ctx = {}
        ctx["xt"] = [self.sb(st, "ux%s%d" % (tag, i), [128, D], F32) for i in range(2)]
        ctx["xb"] = [Buf("ux%d" % i) for i in range(2)]
        if gain_src is not None:
            ctx["rms"] = self.rms_alloc(st, "u" + tag)
            ctx["gain"] = self.sb(st, "ug" + tag, [128, D], F32)
            ctx["gain_b"] = Buf("ug")
            self.k.dma("sp", ctx["gain"][:], gain_src, writes=[ctx["gain_b"]])
            if final:
                ctx["ot"] = self.sb(st, "uo" + tag, [128, D], F32)
                ctx["ot_b"] = Buf("uo")
        return ctx

    def phase_ffn(self, l, last):
        k = self.k
        for fh in range(2):
            with ExitStack() as st:
                W1 = self.sb(st, "W1", [128, 8, 2048], BF16)
                W2 = self.sb(st, "W2", [128, 16, 1024], BF16)
                w1b, w2b = Buf("W1"), Buf("W2")
                self.load_w(W1, self.din["w_ff1"][l][:, fh * 2048:(fh + 1) * 2048], w1b, 8)
                self.load_w(W2, self.din["w_ff2"][l][fh * 2048:(fh + 1) * 2048, :], w2b, 16)
                actT = self.sb(st, "actT", [128, 16, 512], BF16)
                act_b = [Buf("act%d" % i) for i in range(16)]
                rl = [self.sb(st, "rl%d" % i, [128, 512], F32) for i in range(2)]
                rl_b = [Buf("rl%d" % i) for i in range(2)]
                hook = None
                gain_src = None
                if fh == 1:
                    hook = "final" if last else "norm"
                    gain_src = self.din["nfin"][:, :] if last else self.din["nmix"][l + 1]
                ctx = self.upd_alloc(st, "f", gain_src, final=(fh == 1 and last))
                pb = [self.ps(st, "fpb%d" % i, [128, 512], F32) for i in range(6)]
                pbb = [Buf("fpb%d" % i) for i in range(6)]
                if hook == "norm":
                    ctx["ptr"] = self.ps(st, "fptr", [128, 1024], BF16)
                    ctx["ptr_b"] = Buf("fptr")
                for G in range(8):
                    toks = slice(G * 512, (G + 1) * 512)
                    hb = self.hT_b[G * 4:(G + 1) * 4]
                    for fc in range(16):
                        p = fc % 2
                        for kc in range(8):
                            k.op("pe", lambda e, kc=kc, fc=fc, p=p: e.matmul(pb[p][:, :], lhsT=W1[:, kc, fc * 128:(fc + 1) * 128],
                                                                               rhs=self.hT[:, kc, toks], start=(kc == 0), stop=(kc == 7)),
                                 reads=[w1b] + hb, writes=[pbb[p]])
                        k.op("act", lambda e, p=p: e.activation(out=rl[p][:], in_=pb[p][:, :], func=AF.Relu), reads=[pbb[p]], writes=[rl_b[p]])
                        k.op("dve", lambda e, p=p, fc=fc: e.tensor_tensor(out=actT[:, fc, :], in0=rl[p][:], in1=rl[p][:], op=ALU.mult),
                             reads=[rl_b[p]], writes=[act_b[fc]])
                    for tt in range(4):
                        t = G * 4 + tt
                        pp = [2 + 2 * (tt % 2), 3 + 2 * (tt % 2)]
                        for nh in range(2):
                            for fc in range(16):
                                k.op("pe", lambda e, nh=nh, fc=fc, tt=tt: e.matmul(pb[pp[nh]][:, :], lhsT=actT[:, fc, tt * 128:(tt + 1) * 128],
                                                                                  rhs=W2[:, fc, nh * 512:(nh + 1) * 512], start=(fc == 0), stop=(fc == 15)),
                                     reads=[w2b, act_b[fc]], writes=[pbb[pp[nh]]])
                        self.x_update(ctx, t, [pb[pp[0]][:, :], pb[pp[1]][:, :]], [pbb[pp[0]], pbb[pp[1]]], hook)
                k.barrier()

    def phase_nsa(self, l):
        k = self.k
        win = self.din["w_in"][l]
        with ExitStack() as st:
            Wq = self.sb(st, "nWq", [128, 8, 512], BF16); wqb = Buf("nWq")
            self.load_w(Wq, win[:, C_QN:C_QN + 512], wqb, 8)
            ksT = self.sb(st, "nksT", [128, 2, S], BF16); ksTb = Buf("ksT")
            kwT = self.sb(st, "nkwT", [64, 2, S], BF16); kwTb = Buf("kwT")
            vsa = self.sb(st, "nvsa", [128, NT, 2, 65], BF16); vsab = Buf("vsa")
            vwa = self.sb(st, "nvwa", [128, NT, 2, 65], BF16); vwab = Buf("vwa")
            gn = self.sb(st, "ngn", [128, NT, 24], F32); gnb = Buf("gn")
            kcmpT = self.sb(st, "nkcmpT", [64, 2, 256], BF16); kcmpb = Buf("kcmpT")
            VC = self.sb(st, "nVC", [128, 2, 2, 129], BF16); VCb = Buf("VC")
            selv = self.sb(st, "nselv", [128, 126], F32)
            sela = self.sb(st, "nsela", [128, 126], F32)
            ovl = self.sb(st, "novl", [128, 2, 64], F32)
            ncb = Buf("nconst")
            k.dma("sp", selv[:], self.din["selvalid"], writes=[ncb])
            k.dma("sp", sela[:], self.din["seladd"], writes=[ncb])
            k.dma("sp", ovl[:], self.din["overlap"], writes=[ncb])
            A = [self.ps(st, "nA%d" % i, [128, 512], F32) for i in range(4)]
            Ab = [Buf("nA%d" % i) for i in range(4)]
            for g in range(2):
                k.dma("pool", ksT[64:128, g, :], self.din["rfull"], writes=[ksTb])
            OC = [self.ps(st, "nOC%d" % i, [128, 512], F32) for i in range(2)]
            OCb = [Buf("nOC%d" % i) for i in range(2)]
            OS = self.ps(st, "nOS", [128, 512], F32); OSb = Buf("OS")
            OW = self.ps(st, "nOW", [128, 512], F32); OWb = Buf("OW")
            Q = OS; Qb = OSb
            R0bf = OS[:, :].bitcast(BF16)
            R1bf = OW[:, :].bitcast(BF16)
            k.op("dve", lambda e: e.memset(vsa[:], 1.0), writes=[vsab])
            k.op("dve", lambda e: e.memset(vwa[:], 1.0), writes=[vwab])
            k.op("dve", lambda e: e.memset(kcmpT[:], 0.0), writes=[kcmpb])
            k.op("dve", lambda e: e.memset(VC[:], 0.0), writes=[VCb])
            for j in range(2):
                for g in range(2):
                    k.op("dve", lambda e, j=j, g=g: e.memset(VC[:, j, g, 64:65], 1.0), writes=[VCb])
                    k.op("dve", lambda e, j=j, g=g: e.tensor_copy(out=VC[:, j, g, 65:129], in_=ovl[:, j, :]), reads=[ncb], writes=[VCb])
            with ExitStack() as st2:
                Wk = self.sb(st2, "nWk", [128, 8, 512], BF16); wkb = Buf("nWk")
                for i, c0 in enumerate((C_KC, C_VC, C_KS, C_KW)):
                    for kc in range(8):
                        k.dma("pool", Wk[:, kc, i * 128:(i + 1) * 128], win[kc * 128:(kc + 1) * 128, c0:c0 + 128], writes=[wkb])
                Wtm = self.sb(st2, "nWtm", [128, 8, 280], BF16); wtb = Buf("nWtm")
                for (o, c0, n) in ((0, C_VS, 128), (128, C_VW, 128), (256, C_GN, 24)):
                    for kc in range(8):
                        k.dma("pool", Wtm[:, kc, o:o + n], win[kc * 128:(kc + 1) * 128, c0:c0 + n], writes=[wtb])
                w1 = [self.sb(st2, "nw1%d" % i, [64, 32, 128], BF16) for i in range(2)]
                w2k = self.sb(st2, "nw2k", [128, 64], BF16)
                w2v = self.sb(st2, "nw2v", [128, 64], BF16)
                peT = [self.sb(st2, "npeT%d" % i, [64, 32], BF16) for i in range(2)]
                cwb = Buf("cmpw")
                for i, nm in enumerate(("cmp_w1_k", "cmp_w1_v")):
                    k.dma("pool", w1[i][:], self.din[nm][l].rearrange("l d f -> d l f"), writes=[cwb])
                k.dma("pool", w2k[:], self.din["cmp_w2_k"][l], writes=[cwb])
                k.dma("pool", w2v[:], self.din["cmp_w2_v"][l], writes=[cwb])
                k.dma("pool", peT[0][:], self.din["cmp_pe_kT"][l], writes=[cwb])
                k.dma("pool", peT[1][:], self.din["cmp_pe_vT"][l], writes=[cwb])
                for t in range(NT):
                    tk = slice(t * 128, (t + 1) * 128)
                    P = A[t % 2]; PB = Ab[t % 2]
                    for kc in range(8):
                        k.op("pe", lambda e, kc=kc, P=P: e.matmul(P[:, 0:280], lhsT=self.hT[:, kc, tk], rhs=Wtm[:, kc, :], start=(kc == 0), stop=(kc == 7)),
                             reads=[wtb, self.hT_b[t]], writes=[PB])
                    k.op("act", lambda e, P=P, t=t: e.activation(out=vsa[:, t, :, 0:64], in_=P[:, 0:128].rearrange("p (g d) -> p g d", g=2), func=AF.Copy),
                         reads=[PB], writes=[vsab])
                    k.op("act", lambda e, P=P, t=t: e.activation(out=vwa[:, t, :, 0:64], in_=P[:, 128:256].rearrange("p (g d) -> p g d", g=2), func=AF.Copy),
                         reads=[PB], writes=[vwab])
                    k.op("act", lambda e, P=P, t=t: e.activation(out=gn[:, t, :], in_=P[:, 256:280], func=AF.Sigmoid), reads=[PB], writes=[gnb])
                it = 0
                for (dst, dstb, wi) in ((ksT, ksTb, 2), (kwT, kwTb, 3)):
                    for g in range(2):
                        for G in range(8):
                            toks = slice(G * 512, (G + 1) * 512)
                            P = OC[it % 2]; PB = OCb[it % 2]; it += 1
                            for kc in range(8):
                                k.op("pe", lambda e, kc=kc, P=P, wi=wi, g=g: e.matmul(P[0:64, :], lhsT=Wk[:, kc, wi * 128 + g * 64:wi * 128 + g * 64 + 64],
                                                                                     rhs=self.hT[:, kc, toks], start=(kc == 0), stop=(kc == 7)),
                                     reads=[wkb] + self.hT_b[G * 4:(G + 1) * 4], writes=[PB])
                            k.op("act", lambda e, P=P, dst=dst, g=g: e.activation(out=dst[0:64, g, toks], in_=P[0:64, :], func=AF.Copy), reads=[PB], writes=[dstb])
                cT = [self.sb(st2, "ncT%d" % i, [64, S], BF16) for i in range(2)]
                cTb = [Buf("ncT%d" % i) for i in range(2)]
                hx = self.sb(st2, "nhx", [128, 4, 256], F32); hxb = Buf("nhx")
                hidb = self.sb(st2, "nhidb", [128, 256], BF16); hidbb = Buf("nhidb")
                cbias = self.sb(st2, "ncbias", [128, 2], F32); cbb = Buf("ncbias")
                for kv in range(2):
                    for lidx in range(32):
                        k.op("pe", lambda e, kv=kv, lidx=lidx: e.matmul(Q[:, kv:kv + 1], lhsT=w1[kv][:, lidx, :], rhs=peT[kv][:, lidx:lidx + 1],
                                                                       start=(lidx == 0), stop=(lidx == 31)), reads=[cwb], writes=[Qb])
                k.op("act", lambda e: e.activation(out=cbias[:], in_=Q[:, 0:2], func=AF.Copy), reads=[Qb], writes=[cbb])
                for g in range(2):
                    for kv in range(2):
                        for G in range(8):
                            toks = slice(G * 512, (G + 1) * 512)
                            P = OC[it % 2]; PB = OCb[it % 2]; it += 1
                            for kc in range(8):
                                k.op("pe", lambda e, kc=kc, P=P, kv=kv, g=g: e.matmul(P[0:64, :], lhsT=Wk[:, kc, kv * 128 + g * 64:kv * 128 + g * 64 + 64],
                                                                                     rhs=self.hT[:, kc, toks], start=(kc == 0), stop=(kc == 7)),
                                     reads=[wkb] + self.hT_b[G * 4:(G + 1) * 4], writes=[PB])
                            k.op("act", lambda e, P=P, kv=kv: e.activation(out=cT[kv][:, toks], in_=P[0:64, :], func=AF.Copy), reads=[PB], writes=[cTb[kv]])
                    for kv in range(2):
                        P = A[kv]; PB = Ab[kv]
                        for lidx in range(32):
                            k.op("pe", lambda e, kv=kv, lidx=lidx, P=P: e.matmul(P[:, 0:255], lhsT=w1[kv][:, lidx, :], rhs=cT[kv][:, lidx:lidx + 16 * 254 + 1:16],
                                                                                start=(lidx == 0), stop=(lidx == 31)), reads=[cwb, cTb[kv]], writes=[PB])
                        x_ = hx[:, 0, 0:255]
                        k.op("act", lambda e, P=P, kv=kv: e.activation(out=x_, in_=P[:, 0:255], func=AF.Identity, bias=cbias[:, kv:kv + 1], scale=1.0),
                             reads=[PB, cbb], writes=[hxb])
                        k.op("dve", lambda e: e.tensor_tensor(out=hx[:, 1, 0:255], in0=x_, in1=x_, op=ALU.mult), reads=[hxb], writes=[hxb])
                        k.op("dve", lambda e: e.tensor_scalar(out=hx[:, 1, 0:255], in0=hx[:, 1, 0:255], scalar1=0.044715, scalar2=1.0, op0=ALU.mult, op1=ALU.add),
                             reads=[hxb], writes=[hxb])
                        k.op("dve", lambda e: e.tensor_tensor(out=hx[:, 2, 0:255], in0=hx[:, 1, 0:255], in1=x_, op=ALU.mult), reads=[hxb], writes=[hxb])
                        k.op("act", lambda e: e.activation(out=hx[:, 3, 0:255], in_=hx[:, 2, 0:255], func=AF.Sigmoid, scale=1.5957691216057308),
                             reads=[hxb], writes=[hxb])
                        k.op("dve", lambda e: e.tensor_tensor(out=hidb[:, 0:255], in0=hx[:, 3, 0:255], in1=x_, op=ALU.mult), reads=[hxb], writes=[hidbb])
                        if kv == 0:
                            k.op("pe", lambda e: e.matmul(Q[0:64, 0:255], lhsT=w2k[:], rhs=hidb[:, 0:255], start=True, stop=True), reads=[cwb, hidbb], writes=[Qb])
                            k.op("act", lambda e, g=g: e.activation(out=kcmpT[:, g, 0:255], in_=Q[0:64, 0:255], func=AF.Copy), reads=[Qb], writes=[kcmpb])
                        else:
                            for j in range(2):
                                nn = 128 if j == 0 else 127
                                k.op("pe", lambda e, j=j, nn=nn: e.matmul(Q[0:nn, j * 64:(j + 1) * 64], lhsT=hidb[:, j * 128:j * 128 + nn], rhs=w2v[:],
                                                                         start=True, stop=True), reads=[cwb, hidbb], writes=[Qb])
                                k.op("act", lambda e, j=j, nn=nn, g=g: e.activation(out=VC[0:nn, j, g, 0:64], in_=Q[0:nn, j * 64:(j + 1) * 64], func=AF.Copy),
                                     reads=[Qb], writes=[VCb])
            k.barrier()
            qAll = self.sb(st, "nqAll", [128, 4, 2, 512], BF16)
            qTopb = Buf("qTop")
            qBotb = [[Buf("qBot%d_%d" % (b_, g_)) for g_ in range(2)] for b_ in range(4)]
            ebc = [self.sb(st, "nebc%d" % i, [128, 2, 8, 128], BF16) for i in range(2)]
            ebcb = [Buf("nebc%d" % i) for i in range(2)]
            NPT = 5
            pT = [self.sb(st, "npT%d" % i, [128, 512], BF16) for i in range(NPT)]
            pTb = [Buf("npT%d" % i) for i in range(NPT)]
            sm = self.sb(st, "nsm", [128, 3, 4], F32); smb = Buf("nsm")
            imp = self.sb(st, "nimp", [128, 64], F32); impb = Buf("nimp")
            m8 = self.sb(st, "nm8", [128, 8], F32)
            snegP = self.sb(st, "nsnegP", [128, 128], BF16); snegb = Buf("nsneg")
            k.op("dve", lambda e: e.memset(snegP[:], 0.0), writes=[snegb])
            coef = self.sb(st, "ncoef", [128, 4, 3], F32); coefb = Buf("ncoef")
            oacc = self.sb(st, "noacc", [128, 4, 64], F32); oaccb = Buf("noacc")
            onsa = self.sb(st, "nonsa", [128, 512], BF16); onsab = Buf("nonsa")
            onT = [self.sb(st, "nonT%d" % i, [128, 4, 128], BF16) for i in range(2)]
            onTb = [Buf("nonT%d" % i) for i in range(2)]
            sT = [self.sb(st, "nsT%d" % i, [65, 512], F32) for i in range(2)]
            sTb = [Buf("nsT%d" % i) for i in range(2)]
            T = OC; Tb = OCb
            R = [OS, OW]; Rb = [OSb, OWb]
            pti = 0
            LA = 3

            def finish(ti, ri, rows):
                k.op("act", lambda e: e.activation(out=sT[ti][0:rows, :], in_=T[ti][0:rows, :], func=AF.Copy), reads=[Tb[ti]], writes=[sTb[ti]])
                for h4 in range(4):
                    k.op("pe", lambda e, h4=h4: e.transpose(R[ri][:, h4 * 65:h4 * 65 + rows], sT[ti][0:rows, h4 * 128:(h4 + 1) * 128], self.identf[0:rows, 0:rows]),
                         reads=[sTb[ti], self.cb], writes=[Rb[ri]])

            for c in range(NT):
                bq = c % 4
                if bq == 0:
                    toks = slice(c * 128, (c + 4) * 128)
                    for h in range(8):
                        for kc in range(8):
                            k.op("pe", lambda e, kc=kc, h=h: e.matmul(T[0][0:64, :], lhsT=Wq[:, kc, h * 64:(h + 1) * 64], rhs=self.hT[:, kc, toks],
                                                                     start=(kc == 0), stop=(kc == 7)), reads=[wqb] + self.hT_b[c:c + 4], writes=[Tb[0]])
                        k.op("act", lambda e, h=h: e.activation(out=qAll[0:64, :, h // 4, (h % 4) * 128:(h % 4 + 1) * 128],
                                                                in_=T[0][0:64, :].rearrange("p (b q) -> p b q", b=4), func=AF.Copy, scale=0.125),
                             reads=[Tb[0]], writes=[qTopb])
                e_ = ebc[c % 2]; e_b = ebcb[c % 2]
                njt = 2 if c >= 16 else 1
                for j in range(njt):
                    r0 = 248 - 8 * c + 128 * j
                    k.dma("sp", e_[:, j], self.ebc_d[r0:r0 + 128, :, :], reads=[self.ebc_db], writes=[e_b])
                for g in range(2):
                    qg = qAll[0:64, bq, g, :]
                    qfull = qAll[:, bq, g, :]
                    for j in range(njt):
                        ai = pti % 4; pi = pti % NPT; pti += 1
                        k.op("pe", lambda e, j=j, ai=ai, g=g: e.matmul(A[ai][:, :], lhsT=kcmpT[:, g, j * 128:(j + 1) * 128], rhs=qg, start=True, stop=True),
                             reads=[kcmpb, qTopb], writes=[Ab[ai]])
                        k.op("act", lambda e, ai=ai, pi=pi: e.activation(out=pT[pi][:], in_=A[ai][:, :], func=AF.Exp), reads=[Ab[ai]], writes=[pTb[pi]])
                        k.op("dve", lambda e, pi=pi, j=j, g=g: e.tensor_tensor(out=pT[pi][:], in0=pT[pi][:],
                                                                               in1=e_[:, j, g * 4:(g + 1) * 4, :].rearrange("p h q -> p (h q)"), op=ALU.mult),
                             reads=[pTb[pi], e_b], writes=[pTb[pi]])
                        k.op("pe", lambda e, pi=pi, j=j, g=g: e.matmul(T[0][0:65, :], lhsT=VC[:, j, g, 0:65], rhs=pT[pi][:], start=(j == 0), stop=(j == njt - 1)),
                             reads=[pTb[pi], VCb], writes=[Tb[0]])
                        k.op("pe", lambda e, pi=pi, j=j, g=g: e.matmul(T[1][0:64, :], lhsT=VC[:, j, g, 65:129], rhs=pT[pi][:], start=(j == 0), stop=(j == njt - 1)),
                             reads=[pTb[pi], VCb], writes=[Tb[1]])
                    finish(0, 0, 65)
                    finish(1, 1, 64)
                    tiles = [("w", kb) for kb in range(max(0, c - 4), c + 1)] + [("s", kb) for kb in range(c + 1)]
                    nwin = len(tiles) - (c + 1)
                    slots = []

                    def emit_qk(idx):
                        nonlocal pti
                        kind, kb = tiles[idx]
                        ai = pti % 4; pi = pti % NPT; pti += 1
                        slots.append((ai, pi))
                        kt = slice(kb * 128, (kb + 1) * 128)
                        if kind == "s":
                            k.op("pe", lambda e: e.matmul(A[ai][:, :], lhsT=ksT[:, g, kt], rhs=qfull, start=True, stop=True),
                                 reads=[ksTb, qTopb, qBotb[bq][g]], writes=[Ab[ai]])
                        else:
                            k.op("pe", lambda e: e.matmul(A[ai][:, :], lhsT=kwT[:, g, kt], rhs=qg, start=True, stop=True), reads=[kwTb, qTopb], writes=[Ab[ai]])

                    def emit_pv(idx):
                        kind, kb = tiles[idx]
                        ai, pi = slots[idx]
                        off = c - kb
                        k.op("act", lambda e: e.activation(out=pT[pi][:], in_=A[ai][:, :], func=AF.Exp), reads=[Ab[ai]], writes=[pTb[pi]])
                        if kind == "w" or off <= 1:
                            k.op("dve", lambda e: e.tensor_tensor(out=pT[pi][:], in0=pT[pi][:],
                                                                  in1=self.EB[:, off, g * 4:(g + 1) * 4, :].rearrange("p h q -> p (h q)"), op=ALU.mult),
                                 reads=[pTb[pi], self.cb], writes=[pTb[pi]])
                        if kind == "s":
                            ti, V, Vb = 0, vsa, vsab
                            first, lastt = (idx == nwin), (idx == len(tiles) - 1)
                        else:
                            ti, V, Vb = 1, vwa, vwab
                            first, lastt = (idx == 0), (idx == nwin - 1)
                        k.op("pe", lambda e: e.matmul(T[ti][0:65, :], lhsT=V[:, kb, g, :], rhs=pT[pi][:], start=first, stop=lastt),
                             reads=[pTb[pi], Vb], writes=[Tb[ti]])

                    n_pre = min(LA, nwin)
                    for idx in range(n_pre):
                        emit_qk(idx)
                    k.op("dve", lambda e: e.tensor_scalar(out=sm[:, 0, :], in0=R[0][:, 64:64 + 260:65], scalar1=1e-30, scalar2=None, op0=ALU.max), reads=[Rb[0], smb], writes=[smb])
                    k.op("dve", lambda e: e.reciprocal(out=sm[:, 0, :], in_=sm[:, 0, :]), reads=[smb], writes=[smb])
                    for h4 in range(4):
                        src = R[1][:, h4 * 65:h4 * 65 + 64]
                        if h4 == 0:
                            k.op("dve", lambda e, src=src: e.tensor_scalar(out=imp[:], in0=src, scalar1=sm[:, 0, 0:1], scalar2=None, op0=ALU.mult),
                                 reads=[Rb[1], smb], writes=[impb])
                        else:
                            k.op("dve", lambda e, src=src, h4=h4: e.scalar_tensor_tensor(out=imp[:], in0=src, scalar=sm[:, 0, h4:h4 + 1], in1=imp[:], op0=ALU.mult, op1=ALU.add),
                                 reads=[Rb[1], smb, impb], writes=[impb])
                    sl = slice(62 - 2 * c, 62 - 2 * c + 64)
                    k.op("dve", lambda e: e.tensor_tensor(out=imp[:], in0=imp[:], in1=selv[:, sl], op=ALU.mult), reads=[impb, ncb], writes=[impb])
                    k.op("dve", lambda e: e.tensor_tensor(out=imp[:], in0=imp[:], in1=sela[:, sl], op=ALU.add), reads=[impb, ncb], writes=[impb])
                    if c >= 1:
                        k.op("dve", lambda e: e.tensor_scalar(out=imp[:, 0:1], in0=imp[:, 0:1], scalar1=1e4, scalar2=None, op0=ALU.add), reads=[impb], writes=[impb])
                    k.op("dve", lambda e: e.max(out=m8[:], in_=imp[:]), reads=[impb], writes=[impb])
                    k.op("dve", lambda e: e.tensor_scalar(out=snegP[:, 64:128], in0=imp[:], scalar1=m8[:, 7:8], scalar2=NEG, op0=ALU.is_lt, op1=ALU.mult),
                         reads=[impb], writes=[snegb])
                    k.op("pe", lambda e: e.transpose(R1bf[:, 0:128], snegP[:], self.ident[:]), reads=[snegb, self.cb], writes=[Rb[1]])
                    k.op("dve", lambda e: e.tensor_copy(out=qAll[64:128, bq, g, :].rearrange("p (h q) -> p h q", h=4),
                                                        in_=R1bf[64:128, 0:128].unsqueeze(1).to_broadcast([64, 4, 128])),
                         reads=[Rb[1]], writes=[qBotb[bq][g]])
                    gview = gn[:, c, g * 12:(g + 1) * 12].rearrange("p (h b) -> p h b", h=4)
                    k.op("dve", lambda e: e.tensor_tensor(out=coef[:, :, 0], in0=gview[:, :, 0], in1=sm[:, 0, :], op=ALU.mult), reads=[gnb, smb, coefb], writes=[coefb])
                    for h4 in range(4):
                        k.op("dve", lambda e, h4=h4: e.tensor_scalar(out=oacc[:, h4, :], in0=R[0][:, h4 * 65:h4 * 65 + 64], scalar1=coef[:, h4, 0:1], scalar2=None, op0=ALU.mult),
                             reads=[Rb[0], coefb, oaccb], writes=[oaccb])
                    for idx in range(n_pre, len(tiles) + LA):
                        if idx < len(tiles):
                            emit_qk(idx)
                        if idx >= LA:
                            emit_pv(idx - LA)
                    for idx in range(max(0, len(tiles) + LA - LA), len(tiles)):
                        pass
                    finish(0, 0, 65)
                    finish(1, 1, 65)
                    k.op("dve", lambda e: e.tensor_scalar(out=sm[:, 1, :], in0=R[0][:, 64:64 + 260:65], scalar1=1e-30, scalar2=None, op0=ALU.max), reads=[Rb[0], smb], writes=[smb])
                    k.op("dve", lambda e: e.tensor_scalar(out=sm[:, 2, :], in0=R[1][:, 64:64 + 260:65], scalar1=1e-30, scalar2=None, op0=ALU.max), reads=[Rb[1], smb], writes=[smb])
                    k.op("dve", lambda e: e.reciprocal(out=sm[:, 1:3, :], in_=sm[:, 1:3, :]), reads=[smb], writes=[smb])
                    k.op("dve", lambda e: e.tensor_tensor(out=coef[:, :, 1:3], in0=gview[:, :, 1:3],
                                                          in1=sm[:, 1:3, :].rearrange("p b h -> p h b"), op=ALU.mult), reads=[gnb, smb, coefb], writes=[coefb])
                    for h4 in range(4):
                        k.op("dve", lambda e, h4=h4: e.scalar_tensor_tensor(out=oacc[:, h4, :], in0=R[0][:, h4 * 65:h4 * 65 + 64], scalar=coef[:, h4, 1:2], in1=oacc[:, h4, :],
                                                                             op0=ALU.mult, op1=ALU.add), reads=[Rb[0], coefb, oaccb], writes=[oaccb])
                        hh = g * 4 + h4
                        k.op("dve", lambda e, h4=h4, hh=hh: e.scalar_tensor_tensor(out=onsa[:, hh * 64:(hh + 1) * 64], in0=R[1][:, h4 * 65:h4 * 65 + 64], scalar=coef[:, h4, 2:3],
                                                                                    in1=oacc[:, h4, :], op0=ALU.mult, op1=ALU.add), reads=[Rb[1], coefb, oaccb], writes=[onsab])
                import os
                if self.dbg and c == int(os.environ.get("DBG_C", "-1")):
                    dt_ = self.sb(st, "ndbg", [128, 2048], F32); dtb = Buf("ndbg")
                    k.op("dve", lambda e: e.memset(dt_[:], 0.0), writes=[dtb])
                    k.op("dve", lambda e: e.tensor_copy(out=dt_[:, 0:64], in_=imp[:]), reads=[impb], writes=[dtb])
                    k.op("dve", lambda e: e.tensor_copy(out=dt_[:, 64:128], in_=snegP[:, 64:128]), reads=[snegb], writes=[dtb])
                    k.op("dve", lambda e: e.tensor_copy(out=dt_[:, 128:140], in_=sm[:, :, :].rearrange("p a b -> p (a b)")), reads=[smb], writes=[dtb])
                    k.op("dve", lambda e: e.tensor_copy(out=dt_[:, 140:152], in_=coef[:, :, :].rearrange("p a b -> p (a b)")), reads=[coefb], writes=[dtb])
                    k.op("dve", lambda e: e.tensor_copy(out=dt_[:, 152:160], in_=m8[:]), reads=[impb], writes=[dtb])
                    k.op("dve", lambda e: e.tensor_copy(out=dt_[:, 256:512], in_=OC[0][:, 0:256]), reads=[OCb[0]], writes=[dtb])
                    k.op("dve", lambda e: e.tensor_copy(out=dt_[:, 512:1024], in_=OS[:, :]), reads=[OSb], writes=[dtb])
                    k.op("dve", lambda e: e.tensor_copy(out=dt_[:, 1024:1536], in_=OW[:, :]), reads=[OWb], writes=[dtb])
                    k.op("dve", lambda e: e.tensor_copy(out=dt_[:, 1536:2048], in_=onsa[:]), reads=[onsab], writes=[dtb])
                    k.dma("sp", self.dbg_d[:, :], dt_[:], reads=[dtb], writes=[Buf("dbgd")])
                for j in range(4):
                    k.op("pe", lambda e, j=j: e.transpose(R0bf[:, j * 128:(j + 1) * 128], onsa[:, j * 128:(j + 1) * 128], self.ident[:]),
                         reads=[onsab, self.cb], writes=[Rb[0]])
                i2 = c % 2
                k.op("act", lambda e, i2=i2: e.activation(out=onT[i2][:], in_=R0bf[:, 0:512].rearrange("p (j c) -> p j c", j=4), func=AF.Copy),
                     reads=[Rb[0]], writes=[onTb[i2]])
                tk = slice(c * 128, (c + 1) * 128)
                k.dma("sp", self.brT[0, :, :, tk].rearrange("c p q -> p c q"), onT[i2][:], reads=[onTb[i2]], writes=[self.brT_b[0][c]])

    def phase_ret(self, l):
        k = self.k
        with ExitStack() as st:
            Wqk = self.sb(st, "rWqk", [128, 8, 512], BF16)
            Wv = self.sb(st, "rWv", [128, 8, 512], BF16)
            Wg = self.sb(st, "rWg", [128, 8, 512], BF16)
            wb = Buf("rW")
            self.load_w(Wqk, self.din["w_in"][l][:, C_QR:C_QR + 512], wb, 8)
            self.load_w(Wv, self.din["w_in"][l][:, C_VR:C_VR + 512], wb, 8)
            self.load_w(Wg, self.din["w_in"][l][:, C_GR:C_GR + 512], wb, 8)
            cos = self.sb(st, "rcos", [128, NT, 32], F32)
            sin = self.sb(st, "rsin", [128, NT, 32], F32)
            dec = self.sb(st, "rdec", [128, 4, 128], F32)
            xi = self.sb(st, "rxi", [64, 4, 128], F32)
            zeta = self.sb(st, "rzeta", [128, 4], F32)
            gch = self.sb(st, "rgch", [64, 4], F32)
            gn = self.sb(st, "rgn", [128, 512], F32)
            rc = Buf("rconst")
            for dst, src in ((cos, "cos"), (sin, "sin"), (dec, "decayT"), (xi, "xi"), (zeta, "zeta"), (gch, "gch")):
                k.dma("sp", dst[:], self.din[src], writes=[rc])
            k.dma("sp", gn[:], self.din["retgn"][l], writes=[rc])
            Sf = self.sb(st, "rSf", [64, 4, 128], F32)
            Sb = self.sb(st, "rSb", [64, 4, 128], BF16)
            Sfb, Sbb = Buf("Sf"), Buf("Sb")
            k.op("dve", lambda e: e.memset(Sf[:], 0.0), writes=[Sfb])
            k.op("dve", lambda e: e.memset(Sb[:], 0.0), writes=[Sbb])
            qk = self.sb(st, "rqk", [128, 512], F32); qkb = Buf("rqk")
            tm = self.sb(st, "rtm", [128, 4, 8, 32], F32); tmb = Buf("rtm")
            rot = self.sb(st, "rrot", [128, 8, 2, 32], F32); rotb = Buf("rrot")
            qkbf = self.sb(st, "rqkbf", [128, 512], BF16); qkbfb = Buf("rqkbf")
            khat = self.sb(st, "rkhat", [128, 4, 64], BF16); khatb = Buf("rkhat")
            qkT = self.sb(st, "rqkT", [64, 8, 128], BF16); qkTb = Buf("rqkT")
            qxiT = self.sb(st, "rqxiT", [64, 4, 128], BF16); qxiTb = Buf("rqxiT")
            qf32 = self.sb(st, "rqf32", [64, 4, 128], F32); qf32b = Buf("rqf32")
            inT = self.sb(st, "rinT", [128, 4, 128], BF16); inTb = Buf("rinT")
            vbf = self.sb(st, "rvbf", [128, 512], BF16); vbfb = Buf("rvbf")
            osb = self.sb(st, "rosb", [128, 512], F32); osbb = Buf("rosb")
            osq = self.sb(st, "rosq", [128, 512], F32); osqb = Buf("rosq")
            sm = self.sb(st, "rsm", [128, 6, 4], F32); smb = Buf("rsm")
            yn = self.sb(st, "ryn", [128, 512], F32); ynb = Buf("ryn")
            gs = self.sb(st, "rgs", [128, 512], F32); gsb = Buf("rgs")
            orb = self.sb(st, "rorb", [128, 512], BF16); orbb = Buf("rorb")
            orT = [self.sb(st, "rorT%d" % i, [128, 4, 128], BF16) for i in range(2)]
            orTb = [Buf("rorT%d" % i) for i in range(2)]
            pqk = self.ps(st, "rpqk", [128, 512], F32); pqkb = Buf("pqk")
            pv = self.ps(st, "rpv", [128, 512], F32); pvb = Buf("pv")
            pg = self.ps(st, "rpg", [128, 512], F32); pgb = Buf("pg")
            pin = self.ps(st, "rpin", [128, 512], F32); pinb = Buf("pin")
            po = self.ps(st, "rpo", [128, 512], F32); pob = Buf("po")
            pkv = self.ps(st, "rpkv", [128, 512], F32); pkvb = Buf("pkv")
            ptr = self.ps(st, "rptr", [128, 1024], BF16); ptrb = Buf("ptr")
            ptr2 = self.ps(st, "rptr2", [128, 1024], BF16); ptr2b = Buf("ptr2")
            for t in range(NT):
                tk = slice(t * 128, (t + 1) * 128)
                for (W, P, PB) in ((Wqk, pqk, pqkb), (Wv, pv, pvb), (Wg, pg, pgb)):
                    for kc in range(8):
                        k.op("pe", lambda e, kc=kc, W=W, P=P: e.matmul(P[:, :], lhsT=self.hT[:, kc, tk], rhs=W[:, kc, :], start=(kc == 0), stop=(kc == 7)),
                             reads=[wb, self.hT_b[t]], writes=[PB])
                self.chk(1)
                k.op("act", lambda e: e.activation(out=qk[:, 0:256], in_=pqk[:, 0:256], func=AF.Copy), reads=[pqkb], writes=[qkb])
                k.op("act", lambda e: e.activation(out=qk[:, 256:512], in_=pqk[:, 256:512], func=AF.Copy, scale=0.125), reads=[pqkb], writes=[qkb])
                xv = qk[:, :].rearrange("p (h two d) -> p h two d", h=8, two=2)
                x1, x2 = xv[:, :, 0, :], xv[:, :, 1, :]
                cb_ = cos[:, t, :].unsqueeze(1).to_broadcast([128, 8, 32])
                sb_ = sin[:, t, :].unsqueeze(1).to_broadcast([128, 8, 32])
                k.op("dve", lambda e: e.tensor_tensor(out=tm[:, 0], in0=x1, in1=cb_, op=ALU.mult), reads=[qkb, rc], writes=[tmb])
                k.op("dve", lambda e: e.tensor_tensor(out=tm[:, 1], in0=x2, in1=sb_, op=ALU.mult), reads=[qkb, rc], writes=[tmb])
                k.op("dve", lambda e: e.tensor_tensor(out=tm[:, 2], in0=x1, in1=sb_, op=ALU.mult), reads=[qkb, rc], writes=[tmb])
                k.op("dve", lambda e: e.tensor_tensor(out=tm[:, 3], in0=x2, in1=cb_, op=ALU.mult), reads=[qkb, rc], writes=[tmb])
                k.op("dve", lambda e: e.tensor_tensor(out=rot[:, :, 0, :], in0=tm[:, 0], in1=tm[:, 1], op=ALU.subtract), reads=[tmb], writes=[rotb])
                k.op("dve", lambda e: e.tensor_tensor(out=rot[:, :, 1, :], in0=tm[:, 2], in1=tm[:, 3], op=ALU.add), reads=[tmb], writes=[rotb])
                self.chk(2)
                rflat = rot[:, :, :, :].rearrange("p h two d -> p (h two d)")
                k.op("act", lambda e: e.activation(out=qkbf[:], in_=rflat, func=AF.Copy), reads=[rotb], writes=[qkbfb])
                k.op("dve", lambda e: e.tensor_tensor(out=khat[:], in0=rflat[:, 256:512].rearrange("p (h d) -> p h d", h=4),
                                                      in1=zeta[:, :].unsqueeze(2).to_broadcast([128, 4, 64]), op=ALU.mult),
                     reads=[rotb, rc], writes=[khatb])
                self.chk(3)
                for j in range(8):
                    k.op("pe", lambda e, j=j: e.transpose(ptr[0:64, j * 128:(j + 1) * 128], qkbf[:, j * 64:(j + 1) * 64], self.ident[:]),
                         reads=[qkbfb, self.cb], writes=[ptrb])
                k.op("act", lambda e: e.activation(out=qkT[:], in_=ptr[0:64, 0:1024].rearrange("p (j c) -> p j c", j=8), func=AF.Copy),
                     reads=[ptrb], writes=[qkTb])
                k.op("act", lambda e: e.activation(out=qf32[:], in_=ptr[0:64, 0:512].rearrange("p (j c) -> p j c", j=4), func=AF.Copy),
                     reads=[ptrb], writes=[qf32b])
                k.op("dve", lambda e: e.tensor_tensor(out=qxiT[:], in0=qf32[:], in1=xi[:], op=ALU.mult),
                     reads=[qf32b, rc], writes=[qxiTb])
                self.chk(4)
                for h in range(4):
                    k.op("pe", lambda e, h=h: e.matmul(pin[:, h * 128:(h + 1) * 128], lhsT=qkT[:, 4 + h, :], rhs=qkT[:, h, :],
                                                       start=True, stop=True), reads=[qkTb], writes=[pinb])
                self.chk(45)
                k.op("dve", lambda e: e.tensor_tensor(out=inT[:], in0=pin[:, :].rearrange("p (h c) -> p h c", h=4), in1=dec[:], op=ALU.mult),
                     reads=[pinb, rc], writes=[inTb])
                self.chk(5)
                k.op("act", lambda e: e.activation(out=vbf[:], in_=pv[:, :], func=AF.Copy), reads=[pvb], writes=[vbfb])
                for h in range(4):
                    rows = slice((h % 2) * 64, (h % 2) * 64 + 64)
                    hc = slice(h * 128, (h + 1) * 128)
                    k.op("pe", lambda e, h=h, hc=hc: e.matmul(po[:, hc], lhsT=inT[:, h, :], rhs=vbf[:, hc], start=True, stop=False),
                         reads=[inTb, vbfb], writes=[pob])
                    k.op("pe", lambda e, h=h, hc=hc: e.matmul(po[:, hc], lhsT=qxiT[:, h, :], rhs=Sb[:, h, :], start=False, stop=True),
                         reads=[qxiTb, Sbb], writes=[pob])
                self.chk(6)
                for h in range(4):
                    hc = slice(h * 128, (h + 1) * 128)
                    k.op("pe", lambda e, h=h, hc=hc: e.matmul(pkv[0:64, hc], lhsT=khat[:, h, :],
                                                             rhs=vbf[:, hc], start=True, stop=True), reads=[khatb, vbfb], writes=[pkvb])
                self.chk(7)
                for h in range(4):
                    rows = slice((h % 2) * 64, (h % 2) * 64 + 64)
                    hc = slice(h * 128, (h + 1) * 128)
                    k.op("dve", lambda e, h=h, hc=hc: e.scalar_tensor_tensor(out=Sf[:, h, :], in0=Sf[:, h, :],
                                                                              scalar=gch[:, h:h + 1], in1=pkv[0:64, hc],
                                                                              op0=ALU.mult, op1=ALU.add),
                         reads=[pkvb, rc, Sfb], writes=[Sfb])
                k.op("act", lambda e: e.activation(out=Sb[:], in_=Sf[:], func=AF.Copy), reads=[Sfb], writes=[Sbb])
                self.chk(8)
                k.op("act", lambda e: e.activation(out=osb[:], in_=po[:, :], func=AF.Copy), reads=[pob], writes=[osbb])
                k.op("act", lambda e: e.activation(out=osq[:], in_=po[:, :], func=AF.Square), reads=[pob], writes=[osqb])
                k.op("dve", lambda e: e.reduce_sum(out=sm[:, 0, :], in_=osb[:, :].rearrange("p (h v) -> p h v", h=4), axis=AX.X), reads=[osbb], writes=[smb])
                k.op("dve", lambda e: e.reduce_sum(out=sm[:, 1, :], in_=osq[:, :].rearrange("p (h v) -> p h v", h=4), axis=AX.X), reads=[osqb, smb], writes=[smb])
                k.op("dve", lambda e: e.tensor_scalar(out=sm[:, 2, :], in0=sm[:, 0, :], scalar1=1.0 / 128, scalar2=None, op0=ALU.mult), reads=[smb], writes=[smb])
                k.op("dve", lambda e: e.tensor_tensor(out=sm[:, 3, :], in0=sm[:, 2, :], in1=sm[:, 2, :], op=ALU.mult), reads=[smb], writes=[smb])
                k.op("dve", lambda e: e.scalar_tensor_tensor(out=sm[:, 3, :], in0=sm[:, 1, :], scalar=1.0 / 128, in1=sm[:, 3, :], op0=ALU.mult, op1=ALU.subtract),
                     reads=[smb], writes=[smb])
                k.op("act", lambda e: e.activation(out=sm[:, 4, :], in_=sm[:, 3, :], func=AF.Sqrt, bias=self.cst[:, 0:1], scale=1.0), reads=[smb, self.cb], writes=[smb])
                k.op("dve", lambda e: e.reciprocal(out=sm[:, 5, :], in_=sm[:, 4, :]), reads=[smb], writes=[smb])
                self.chk(9)
                for h in range(4):
                    hc = slice(h * 128, (h + 1) * 128)
                    k.op("dve", lambda e, h=h, hc=hc: e.tensor_scalar(out=yn[:, hc], in0=osb[:, hc], scalar1=sm[:, 2, h:h + 1], scalar2=sm[:, 5, h:h + 1],
                                                                      op0=ALU.subtract, op1=ALU.mult), reads=[osbb, smb], writes=[ynb])
                k.op("dve", lambda e: e.tensor_tensor(out=yn[:], in0=yn[:], in1=gn[:], op=ALU.mult), reads=[ynb, rc], writes=[ynb])
                self.chk(10)
                k.op("act", lambda e: e.activation(out=gs[:], in_=pg[:, :], func=AF.Silu), reads=[pgb], writes=[gsb])
                k.op("dve", lambda e: e.tensor_tensor(out=orb[:], in0=yn[:], in1=gs[:], op=ALU.mult), reads=[ynb, gsb], writes=[orbb])
                self.chk(11)
                for j in range(4):
                    k.op("pe", lambda e, j=j: e.transpose(ptr2[:, j * 128:(j + 1) * 128], orb[:, j * 128:(j + 1) * 128], self.ident[:]),
                         reads=[orbb, self.cb], writes=[ptr2b])
                i = t % 2
                k.op("act", lambda e, i=i: e.activation(out=orT[i][:], in_=ptr2[:, 0:512].rearrange("p (j c) -> p j c", j=4), func=AF.Copy),
                     reads=[ptr2b], writes=[orTb[i]])
                self.chk(12)
                k.dma("sp", self.brT[1, :, :, tk].rearrange("c p q -> p c q"), orT[i][:], reads=[orTb[i]], writes=[self.brT_b[1][t]])

    def phase_conv(self, l):
        k = self.k
        with ExitStack() as st:
            Wa = self.sb(st, "cWa", [128, 8, 512], BF16)
            Wb = self.sb(st, "cWb", [128, 8, 512], BF16)
            wab = Buf("cW")
            self.load_w(Wa, self.din["w_in"][l][:, C_CA:C_CA + 512], wab, 8)
            self.load_w(Wb, self.din["w_in"][l][:, C_CB:C_CB + 512], wab, 8)
            cw = self.sb(st, "ccw", [128, 4, 31], F32)
            cvec = self.sb(st, "cvec", [128, 3, 4], F32)
            ones = self.sb(st, "cones", [128, 128], F32)
            cc = Buf("cconst")
            k.dma("sp", cw[:], self.din["convw"][l], writes=[cc])
            k.dma("sp", cvec[:, 0, :], self.din["convb"][l], writes=[cc])
            k.dma("sp", cvec[:, 1, :], self.din["convg"][l], writes=[cc])
            k.dma("sp", cvec[:, 2, :], self.din["convbb"][l], writes=[cc])
            k.dma("sp", ones[:], self.din["ones"][:, :], writes=[cc])
            Dg = self.sb(st, "cDg", [128, 4, 31, 128], BF16); dgb = Buf("cDg")
            for ct in range(4):
                for w in range(31):
                    en = "dve" if (w % 2 == 0) else "pool"
                    k.op(en, lambda e, ct=ct, w=w: e.tensor_scalar(out=Dg[:, ct, w, :], in0=self.identf[:], scalar1=cw[:, ct, w:w + 1], scalar2=None, op0=ALU.mult),
                         reads=[cc, self.cb], writes=[dgb])
            u = [self.sb(st, "cu%d" % i, [128, 4, 542], BF16) for i in range(2)]
            ub = [[Buf("cu%d_%d" % (i, ct)) for ct in range(4)] for i in range(2)]
            acc = self.sb(st, "cacc", [128, 4, 512], F32)
            accb = [Buf("cacc%d" % ct) for ct in range(4)]
            sg = [self.sb(st, "csg%d" % i, [128, 512], F32) for i in range(2)]
            sgb = [Buf("csg%d" % i) for i in range(2)]
            ysq = [self.sb(st, "cysq%d" % i, [128, 512], F32) for i in range(2)]
            ysqb = [Buf("cysq%d" % i) for i in range(2)]
            stt = self.sb(st, "cstt", [128, 4, 512], F32)
            sttb = Buf("cstt")
            yn = [self.sb(st, "cyn%d" % i, [128, 512], F32) for i in range(2)]
            ynb = [Buf("cyn%d" % i) for i in range(2)]
            oc = [self.sb(st, "coc%d" % i, [128, 512], BF16) for i in range(2)]
            ocb = [Buf("coc%d" % i) for i in range(2)]
            pa = [self.ps(st, "cpa%d" % i, [128, 512], F32) for i in range(2)]
            pab = [Buf("cpa%d" % i) for i in range(2)]
            pbk = [self.ps(st, "cpb%d" % i, [128, 512], F32) for i in range(2)]
            pbb = [Buf("cpb%d" % i) for i in range(2)]
            pcv = [self.ps(st, "cpc%d" % i, [128, 512], F32) for i in range(2)]
            pcb = [Buf("cpc%d" % i) for i in range(2)]
            s1 = self.ps(st, "cs1", [128, 512], F32)
            s2 = self.ps(st, "cs2", [128, 512], F32)
            s1b, s2b = Buf("cs1"), Buf("cs2")
            for ct in range(4):
                k.op("dve", lambda e, ct=ct: e.memset(u[0][:, ct, 0:30], 0.0), writes=[ub[0][ct]])
            for G in range(8):
                toks = slice(G * 512, (G + 1) * 512)
                hb = self.hT_b[G * 4:(G + 1) * 4]
                ug, ugb = u[G % 2], ub[G % 2]
                for ct in range(4):
                    p = ct % 2
                    cols = slice(ct * 128, (ct + 1) * 128)
                    for kc in range(8):
                        k.op("pe", lambda e, kc=kc, cols=cols, p=p: e.matmul(pa[p][:, :], lhsT=Wa[:, kc, cols], rhs=self.hT[:, kc, toks],
                                                                              start=(kc == 0), stop=(kc == 7)), reads=[wab] + hb, writes=[pab[p]])
                    for kc in range(8):
                        k.op("pe", lambda e, kc=kc, cols=cols, p=p: e.matmul(pbk[p][:, :], lhsT=Wb[:, kc, cols], rhs=self.hT[:, kc, toks],
                                                                              start=(kc == 0), stop=(kc == 7)), reads=[wab] + hb, writes=[pbb[p]])
                    k.op("act", lambda e, p=p: e.activation(out=sg[p][:], in_=pbk[p][:, :], func=AF.Sigmoid), reads=[pbb[p]], writes=[sgb[p]])
                    if G > 0:
                        k.op("pool", lambda e, ct=ct: e.tensor_copy(out=ug[:, ct, 0:30], in_=u[(G - 1) % 2][:, ct, 512:542]),
                             reads=[ub[(G - 1) % 2][ct]], writes=[ugb[ct]])
                    k.op("dve", lambda e, ct=ct, p=p: e.tensor_tensor(out=ug[:, ct, 30:542], in0=pa[p][:, :], in1=sg[p][:], op=ALU.mult),
                         reads=[pab[p], sgb[p]], writes=[ugb[ct]])
                for ct in range(4):
                    p = ct % 2
                    for w in range(31):
                        k.op("pe", lambda e, ct=ct, w=w, p=p: e.matmul(pcv[p][:, :], lhsT=Dg[:, ct, w, :], rhs=ug[:, ct, w:w + 512], start=(w == 0), stop=(w == 30)),
                             reads=[dgb, ugb[ct]], writes=[pcb[p]])
                    k.op("act", lambda e, ct=ct, p=p: e.activation(out=acc[:, ct, :], in_=pcv[p][:, :], func=AF.Identity, bias=cvec[:, 0, ct:ct + 1], scale=1.0),
                         reads=[pcb[p], cc], writes=[accb[ct]])
                    k.op("act", lambda e, ct=ct, p=p: e.activation(out=ysq[p][:], in_=acc[:, ct, :], func=AF.Square), reads=[accb[ct]], writes=[ysqb[p]])
                    k.op("pe", lambda e, ct=ct: e.matmul(s1[:, :], lhsT=ones[:], rhs=acc[:, ct, :], start=(ct == 0), stop=(ct == 3)),
                         reads=[cc, accb[ct]], writes=[s1b])
                    k.op("pe", lambda e, ct=ct, p=p: e.matmul(s2[:, :], lhsT=ones[:], rhs=ysq[p][:], start=(ct == 0), stop=(ct == 3)),
                         reads=[cc, ysqb[p]], writes=[s2b])
                k.op("act", lambda e: e.activation(out=stt[:, 0, :], in_=s1[:, :], func=AF.Copy, scale=1.0 / 512), reads=[s1b], writes=[sttb])
                k.op("dve", lambda e: e.tensor_tensor(out=stt[:, 1, :], in0=stt[:, 0, :], in1=stt[:, 0, :], op=ALU.mult), reads=[sttb], writes=[sttb])
                k.op("dve", lambda e: e.scalar_tensor_tensor(out=stt[:, 1, :], in0=s2[:, :], scalar=1.0 / 512, in1=stt[:, 1, :],
                                                             op0=ALU.mult, op1=ALU.subtract), reads=[s2b, sttb], writes=[sttb])
                k.op("act", lambda e: e.activation(out=stt[:, 3, :], in_=stt[:, 1, :], func=AF.Sqrt, bias=self.cst[:, 0:1], scale=1.0),
                     reads=[sttb, self.cb], writes=[sttb])
                k.op("dve", lambda e: e.reciprocal(out=stt[:, 2, :], in_=stt[:, 3, :]), reads=[sttb], writes=[sttb])
                for ct in range(4):
                    p = ct % 2
                    k.op("dve", lambda e, ct=ct, p=p: e.tensor_tensor(out=yn[p][:], in0=acc[:, ct, :], in1=stt[:, 0, :], op=ALU.subtract),
                         reads=[accb[ct], sttb], writes=[ynb[p]])
                    k.op("pool", lambda e, p=p: e.tensor_tensor(out=yn[p][:], in0=yn[p][:], in1=stt[:, 2, :], op=ALU.mult),
                         reads=[sttb, ynb[p]], writes=[ynb[p]])
                    k.op("act", lambda e, ct=ct, p=p: e.activation(out=oc[p][:], in_=yn[p][:], func=AF.Silu, scale=cvec[:, 1, ct:ct + 1],
                                                                   bias=cvec[:, 2, ct:ct + 1]), reads=[ynb[p], cc], writes=[ocb[p]])
                    k.dma("sp", self.brT[2, ct, :, toks], oc[p][:], reads=[ocb[p]], writes=self.brT_b[2][G * 4:(G + 1) * 4])

    def phase_merge(self, l):
        k = self.k
        for dh in range(2):
            with ExitStack() as st:
                Wg = self.sb(st, "mWg", [128, 8, 3, 512], BF16)
                Wbr = self.sb(st, "mWbr", [128, 3, 4, 512], BF16)
                Wo = self.sb(st, "mWo", [128, 4, 1024], BF16)
                wb = Buf("mW")
                for b in range(3):
                    for kc in range(8):
                        c0 = C_MG + b * 1024 + dh * 512
                        k.dma("pool", Wg[:, kc, b, :], self.din["w_in"][l][kc * 128:(kc + 1) * 128, c0:c0 + 512], writes=[wb])
                    for c4 in range(4):
                        k.dma("pool", Wbr[:, b, c4, :], self.din["w_branch"][l][b][c4 * 128:(c4 + 1) * 128, dh * 512:(dh + 1) * 512], writes=[wb])
                for dc in range(4):
                    r0 = dh * 512 + dc * 128
                    k.dma("pool", Wo[:, dc, :], self.din["w_out"][l][r0:r0 + 128, :], writes=[wb])
                brg = [self.sb(st, "mbr%d" % i, [128, 3, 4, 512], BF16) for i in range(2)]
                brgb = [Buf("mbr%d" % i) for i in range(2)]
                mT = self.sb(st, "mmT", [128, 4, 512], BF16)
                mTb = [Buf("mT%d" % i) for i in range(4)]
                gate = [self.sb(st, "mgate%d" % i, [128, 512], F32) for i in range(2)]
                gateb = [Buf("mgate%d" % i) for i in range(2)]
                macc = self.sb(st, "mmacc", [128, 512], F32); maccb = Buf("macc")
                mtmp = self.sb(st, "mmtmp", [128, 512], F32); mtmpb = Buf("mtmp")
                ctx = self.upd_alloc(st, "m", self.din["nmlp"][l] if dh == 1 else None)
                pg = [self.ps(st, "mpg%d" % i, [128, 512], F32) for i in range(2)]
                pgb = [Buf("mpg%d" % i) for i in range(2)]
                pp = [self.ps(st, "mpp%d" % i, [128, 512], F32) for i in range(2)]
                ppb = [Buf("mpp%d" % i) for i in range(2)]
                po = [self.ps(st, "mpo%d" % i, [128, 512], F32) for i in range(2)]
                pob = [Buf("mpo%d" % i) for i in range(2)]
                if dh == 1:
                    ctx["ptr"] = self.ps(st, "mptr", [128, 1024], BF16)
                    ctx["ptr_b"] = Buf("mptr")
                it = 0
                for G in range(8):
                    toks = slice(G * 512, (G + 1) * 512)
                    hb = self.hT_b[G * 4:(G + 1) * 4]
                    bi = G % 2
                    for b in range(3):
                        k.dma("sp", brg[bi][:, b], self.brT[b, :, :, toks].rearrange("c p t -> p c t"),
                              reads=self.brT_b[b][G * 4:(G + 1) * 4], writes=[brgb[bi]])
                    for dc in range(4):
                        for b in range(3):
                            p = it % 2
                            it += 1
                            for kc in range(8):
                                k.op("pe", lambda e, kc=kc, b=b, dc=dc, p=p: e.matmul(pg[p][:, :], lhsT=Wg[:, kc, b, dc * 128:(dc + 1) * 128],
                                                                                     rhs=self.hT[:, kc, toks], start=(kc == 0), stop=(kc == 7)),
                                     reads=[wb] + hb, writes=[pgb[p]])
                            for c4 in range(4):
                                k.op("pe", lambda e, c4=c4, b=b, dc=dc, p=p: e.matmul(pp[p][:, :], lhsT=Wbr[:, b, c4, dc * 128:(dc + 1) * 128],
                                                                                     rhs=brg[bi][:, b, c4, :], start=(c4 == 0), stop=(c4 == 3)),
                                     reads=[wb, brgb[bi]], writes=[ppb[p]])
                            k.op("act", lambda e, p=p: e.activation(out=gate[p][:], in_=pg[p][:, :], func=AF.Sigmoid), reads=[pgb[p]], writes=[gateb[p]])
                            if b == 0:
                                k.op("dve", lambda e, p=p: e.tensor_tensor(out=macc[:], in0=pp[p][:, :], in1=gate[p][:], op=ALU.mult),
                                     reads=[ppb[p], gateb[p]], writes=[maccb])
                            else:
                                k.op("dve", lambda e, p=p: e.tensor_tensor(out=mtmp[:], in0=pp[p][:, :], in1=gate[p][:], op=ALU.mult),
                                     reads=[ppb[p], gateb[p]], writes=[mtmpb])
                                if b == 1:
                                    k.op("pool", lambda e: e.tensor_tensor(out=macc[:], in0=macc[:], in1=mtmp[:], op=ALU.add),
                                         reads=[mtmpb, maccb], writes=[maccb])
                                else:
                                    k.op("pool", lambda e, dc=dc: e.tensor_tensor(out=mT[:, dc, :], in0=macc[:], in1=mtmp[:], op=ALU.add),
                                         reads=[mtmpb, maccb], writes=[mTb[dc]])
                    for tt in range(4):
                        t = G * 4 + tt
                        for nh in range(2):
                            for dc in range(4):
                                k.op("pe", lambda e, nh=nh, dc=dc, tt=tt: e.matmul(po[nh][:, :], lhsT=mT[:, dc, tt * 128:(tt + 1) * 128],
                                                                                  rhs=Wo[:, dc, nh * 512:(nh + 1) * 512], start=(dc == 0), stop=(dc == 3)),
                                     reads=[wb, mTb[dc]], writes=[pob[nh]])
                        self.x_update(ctx, t, [po[0][:, :], po[1][:, :]], pob, "norm" if dh == 1 else None)
                k.barrier()


_PROG_CACHE = {}


def _get_prog(**kw):
    key = tuple(sorted((k, str(v)) for k, v in kw.items()))
    if key not in _PROG_CACHE:
        _PROG_CACHE[key] = Prog(**kw)
    return _PROG_CACHE[key]


def kernel(**inputs):
    inp = {k: np.asarray(v) for k, v in inputs.items()}
    x = np.ascontiguousarray(inp["x"], dtype=np.float32)
    shared = _host_inputs(inp)
    prog = _get_prog()
    in_maps = []
    for c in range(8):
        m = dict(shared)
        m["x"] = x[c]
        in_maps.append(m)
    res = run_bass_kernel_spmd(prog.nc, in_maps, core_ids=list(range(8)))
    return np.stack([np.asarray(r["out"], dtype=np.float32) for r in res.results], axis=0)
```

```python
import numpy as np
from contextlib import ExitStack
import concourse.bass as bass
import concourse.mybir as mybir
from concourse.bass_utils import run_bass_kernel_spmd

F32 = mybir.dt.float32
BF16 = mybir.dt.bfloat16
AF = mybir.ActivationFunctionType
ALU = mybir.AluOpType
AX = mybir.AxisListType

S = 4096
D = 1024
NT = 32
DEPTH = 2
IN_TOTAL = 6936
EPS = 1e-6
NEG = -30000.0
C_QN, C_KC, C_VC, C_KS, C_VS, C_KW, C_VW, C_GN = 0, 512, 640, 768, 896, 1024, 1152, 1280
C_QR, C_KR, C_VR, C_GR, C_CA, C_CB, C_MG = 1304, 1560, 1816, 2328, 2840, 3352, 3864

SAME_ENGINE_SYNC = True


class Buf:
    __slots__ = ("w", "r", "name", "wd")

    def __init__(self, name=""):
        self.w = None
        self.r = {}
        self.name = name
        self.wd = False


class _Sem:
    def __init__(self, sem, name):
        self.sem = sem
        self.count = 0
        self.name = name


class Eng(_Sem):
    def __init__(self, name, handle, sem):
        super().__init__(sem, name)
        self.h = handle
        self.waited = {}


class K:
    def __init__(self, nc, stack, n_dma_sems=64):
        self.nc = nc
        self.engs = {}
        for name, h in (("pe", nc.tensor), ("act", nc.scalar), ("dve", nc.vector),
                        ("pool", nc.gpsimd), ("sp", nc.sync)):
            sem = stack.enter_context(nc.semaphore("sem_" + name))
            self.engs[name] = Eng(name, h, sem)
        self.dsems = [_Sem(stack.enter_context(nc.semaphore("dsem%d" % i)), "d%d" % i)
                      for i in range(n_dma_sems)]
        self.dpool = {"pool": self.dsems[:n_dma_sems // 2], "sp": self.dsems[n_dma_sems // 2:]}
        self.dnext = {"pool": 0, "sp": 0}
        self.n_ops = 0
        self.muted = False

    def _wait_deps(self, E, reads, writes):
        deps = {}

        def need(tok):
            if tok is None:
                return
            s, v = tok
            if deps.get(s, 0) < v:
                deps[s] = v
        for b in reads:
            for t in b.w or ():
                need(t)
        for b in writes:
            for t in b.w or ():
                need(t)
            for t in b.r.values():
                need(t)
        for s, v in deps.items():
            if s is E and (E.name in ("pe", "sp") or not SAME_ENGINE_SYNC):
                continue
            if E.waited.get(s, 0) < v:
                E.h.wait_ge(s.sem, v)
                E.waited[s] = v

    def _mark(self, tok, reads, writes, is_dma=False):
        for b in reads:
            b.r[tok[0]] = tok
        for b in writes:
            if is_dma and b.w and getattr(b, "wd", False) and len(b.w) < 24:
                b.w = b.w + [tok]
            else:
                b.w = [tok]
            b.wd = is_dma
            b.r = {}

    def op(self, en, fn, reads=(), writes=()):
        if self.muted:
            return None
        E = self.engs[en]
        self._wait_deps(E, reads, writes)
        ins = fn(E.h)
        E.count += 1
        ins.then_inc(E.sem, 1)
        self._mark((E, E.count), reads, writes)
        self.n_ops += 1
        return ins

    def dma(self, en, out, in_, reads=(), writes=(), **kw):
        if self.muted:
            return None
        E = self.engs[en]
        self._wait_deps(E, reads, writes)
        pool_ = self.dpool[en]
        d = pool_[self.dnext[en]]
        self.dnext[en] = (self.dnext[en] + 1) % len(pool_)
        if d.count and E.waited.get(d, 0) < d.count:
            E.h.wait_ge(d.sem, d.count)
            E.waited[d] = d.count
        ins = E.h.dma_start(out=out, in_=in_, **kw)
        d.count += 16
        ins.then_inc(d.sem, 16)
        self._mark((d, d.count), reads, writes, is_dma=True)
        self.n_ops += 1
        return ins

    def barrier(self):
        allsems = list(self.engs.values()) + self.dsems
        for E in self.engs.values():
            for s in allsems:
                if s is E or s.count == 0:
                    continue
                if E.waited.get(s, 0) < s.count:
                    E.h.wait_ge(s.sem, s.count)
                    E.waited[s] = s.count


def _rel_bucket_np(dist):
    n = np.maximum(dist, 0)
    nf = np.maximum(n, 1).astype(np.float32)
    large = 16 + (np.log(nf / np.float32(16)) / np.float32(np.log(8.0)) * np.float32(16)).astype(np.int32)
    large = np.minimum(large, 31)
    return np.where(n < 16, n, large).astype(np.int64)


_CONST_CACHE = {}


def _host_consts():
    if _CONST_CACHE:
        return _CONST_CACHE
    c = {}
    i = np.arange(128)
    c["ident"] = np.eye(128, dtype=np.float32)
    c["i4"] = np.tile(np.eye(128, dtype=np.float32), (1, 4))
    mw = np.zeros((128, 5, 128), np.float32)
    jj, ii = np.meshgrid(i, i, indexing="ij")
    mw[:, 0, :] = (ii >= jj)
    mw[:, 1:4, :] = 1.0
    mw[:, 4, :] = (ii < jj)
    c["maskw"] = mw
    dist_w = 128 * np.arange(5)[None, :, None] + ii[:, None, :] - jj[:, None, :]
    c["_bucket_w"] = _rel_bucket_np(dist_w)
    m = np.arange(504)
    dist_c = i[None, :] - 16 * (m[:, None] - 248) - 31
    c["maskc"] = (dist_c >= 0).astype(np.float32)
    c["_bucket_c"] = _rel_bucket_np(dist_c)
    n = np.arange(256)
    s = np.arange(64)
    ov = ((16 * n[:, None] <= 64 * s[None, :] + 63) & (16 * n[:, None] + 31 >= 64 * s[None, :])).astype(np.float32)
    ov[255] = 0.0
    c["overlap"] = ov.reshape(2, 128, 64).transpose(1, 0, 2).copy()
    j = np.arange(126)
    sp = j[None, :] - 62
    cur = (i[:, None] >= 64).astype(np.int64)
    valid = sp <= cur
    forced = (sp == cur) | (sp == cur - 1)
    c["selvalid"] = valid.astype(np.float32)
    c["seladd"] = np.where(forced, 1e4, np.where(valid, 0.0, -1e4)).astype(np.float32)
    half = 32
    inv = (10000.0 ** (-np.arange(half, dtype=np.float32) / half)).astype(np.float32)
    pos = np.arange(S, dtype=np.float32)
    ang = (pos[:, None] * inv[None, :]).astype(np.float32)
    c["cos"] = np.cos(ang).astype(np.float32).reshape(NT, 128, 32).transpose(1, 0, 2).copy()
    c["sin"] = np.sin(ang).astype(np.float32).reshape(NT, 128, 32).transpose(1, 0, 2).copy()
    log_g = np.log1p(-np.exp2(-5.0 - np.arange(4, dtype=np.float32))).astype(np.float32)
    diff = i[None, :] - i[:, None]
    dec = np.where(diff[None] >= 0, np.exp(log_g[:, None, None] * np.maximum(diff[None], 0)), 0.0)
    c["decayT"] = dec.transpose(1, 0, 2).astype(np.float32).copy()
    xi = np.exp(log_g[:, None] * (i[None, :] + 1)).astype(np.float32)
    c["xi"] = np.ascontiguousarray(np.broadcast_to(xi[None, :, :], (64, 4, 128))).astype(np.float32)
    c["zeta"] = np.exp(log_g[None, :] * (127 - i[:, None])).astype(np.float32)
    gch = np.exp(log_g * 128).astype(np.float32)
    c["gch"] = np.ascontiguousarray(np.broadcast_to(gch[None, :], (64, 4))).astype(np.float32)
    c["ones"] = np.ones((128, 128), np.float32)
    c["rfull"] = (np.arange(S)[None, :] // 64 == np.arange(64)[:, None]).astype(np.float32)
    _CONST_CACHE.update(c)
    return c


CONST_SHAPES = {
    "ident": [128, 128], "i4": [128, 512], "maskw": [128, 5, 128], "maskc": [504, 128],
    "overlap": [128, 2, 64], "selvalid": [128, 126], "seladd": [128, 126],
    "cos": [128, NT, 32], "sin": [128, NT, 32], "decayT": [128, 4, 128], "xi": [64, 4, 128],
    "zeta": [128, 4], "gch": [64, 4], "ones": [128, 128], "rfull": [64, S],
    "bias_w": [128, 5, 8, 128], "bias_c": [504, 8, 128], "b31": [128, 8],
}

W_SHAPES = {
    "w_in": [DEPTH, D, IN_TOTAL], "w_branch": [DEPTH, 3, 512, D], "w_out": [DEPTH, D, D],
    "w_ff1": [DEPTH, D, 4 * D], "w_ff2": [DEPTH, 4 * D, D],
    "cmp_w1_k": [DEPTH, 32, 64, 128], "cmp_w1_v": [DEPTH, 32, 64, 128],
    "cmp_w2_k": [DEPTH, 128, 64], "cmp_w2_v": [DEPTH, 128, 64],
    "cmp_pe_kT": [DEPTH, 64, 32], "cmp_pe_vT": [DEPTH, 64, 32],
    "nmix": [DEPTH, 128, D], "nmlp": [DEPTH, 128, D], "nfin": [128, D],
    "retgn": [DEPTH, 128, 512],
    "convw": [DEPTH, 128, 4, 31], "convb": [DEPTH, 128, 4], "convg": [DEPTH, 128, 4], "convbb": [DEPTH, 128, 4],
}


def _host_inputs(inp):
    c = _host_consts()
    f = lambda a: np.ascontiguousarray(a, dtype=np.float32)
    out = {k: f(v) for k, v in c.items() if not k.startswith("_")}
    rt = f(inp["rel_table"])
    out["bias_w"] = f(rt[c["_bucket_w"]].transpose(0, 1, 3, 2))
    out["bias_c"] = f(rt[c["_bucket_c"]].transpose(0, 2, 1))
    out["b31"] = f(np.broadcast_to(rt[31][None, :], (128, 8)))
    for kname in ("w_in", "w_branch", "w_out", "w_ff1", "w_ff2", "cmp_w1_k", "cmp_w1_v", "cmp_w2_k", "cmp_w2_v"):
        out[kname] = f(inp[kname])
    out["cmp_pe_kT"] = f(np.transpose(inp["cmp_pe_k"], (0, 2, 1)))
    out["cmp_pe_vT"] = f(np.transpose(inp["cmp_pe_v"], (0, 2, 1)))
    out["nmix"] = f(np.broadcast_to(inp["norm_mix"][:, None, :], (DEPTH, 128, D)))
    out["nmlp"] = f(np.broadcast_to(inp["norm_mlp"][:, None, :], (DEPTH, 128, D)))
    out["nfin"] = f(np.broadcast_to(inp["norm_final"][None, :], (128, D)))
    out["retgn"] = f(np.broadcast_to(inp["ret_gn"][:, None, :], (DEPTH, 128, 512)))
    out["convw"] = f(np.transpose(inp["conv_w"].reshape(DEPTH, 31, 4, 128), (0, 3, 2, 1)))
    for a, b in (("convb", "conv_b"), ("convg", "conv_ln_g"), ("convbb", "conv_ln_b")):
        out[a] = f(np.transpose(inp[b].reshape(DEPTH, 4, 128), (0, 2, 1)))
    return out


class _Stop(Exception):
    pass


class Prog:
    def __init__(self, n_layers=DEPTH, phases=("nsa", "ret", "conv", "merge", "ffn"), dbg=False):
        self.n_layers = n_layers
        self.phases = phases
        self.dbg = dbg
        nc = self.nc = bass.Bass("TRN2", target_bir_lowering=False)
        self.din = {}
        self.din["x"] = nc.dram_tensor("x", [S, D], F32, kind="ExternalInput").ap()
        for name, shp in list(CONST_SHAPES.items()) + list(W_SHAPES.items()):
            self.din[name] = nc.dram_tensor(name, shp, F32, kind="ExternalInput").ap()
        self.out = nc.dram_tensor("out", [S, D], F32, kind="ExternalOutput").ap()
        skind = "ExternalOutput" if dbg else "Internal"
        self.xres = nc.dram_tensor("xres", [S, D], F32, kind=skind).ap()
        self.brT = nc.dram_tensor("brT", [3, 4, 128, S], BF16, kind=skind).ap()
        self.ebc_d = nc.dram_tensor("ebc_d", [504, 8, 128], BF16, kind=skind).ap()
        self.dbg_d = nc.dram_tensor("dbg_d", [128, 2048], F32, kind=skind).ap()
        self.ocd = nc.dram_tensor("ocd", [NT, 128, 512], F32, kind="Internal").ap()
        self.ocd_b = [Buf("ocd%d" % t) for t in range(NT)]
        self.xres_b = [Buf("xres%d" % t) for t in range(NT)]
        self.brT_b = [[Buf("brT%d_%d" % (b, t)) for t in range(NT)] for b in range(3)]
        self.ebc_db = Buf("ebc_d")
        self.out_b = Buf("out")
        with ExitStack() as st:
            self.st = st
            self.k = K(nc, st)
            self.build()

    def sb(self, st, name, shape, dt):
        self._uid = getattr(self, "_uid", 0) + 1
        return st.enter_context(self.nc.sbuf_tensor("s%d_%s" % (self._uid, name), shape, dt))

    def ps(self, st, name, shape, dt):
        self._uid = getattr(self, "_uid", 0) + 1
        return st.enter_context(self.nc.psum_tensor("p%d_%s" % (self._uid, name), shape, dt))

    def chk(self, n):
        import os
        v = os.environ.get("RSTOP")
        if v is not None and int(v) == n:
            self.k.muted = True

    def load_const(self, dst, src, buf, eng="sp"):
        self.k.dma(eng, dst, src, writes=[buf])

    def build(self):
        k, nc, st = self.k, self.nc, self.st
        self.hT = self.sb(st, "hT_all", [128, 8, S], BF16)
        self.hT_b = [Buf("hT%d" % t) for t in range(NT)]
        self.ident = self.sb(st, "ident", [128, 128], BF16)
        self.i4 = self.sb(st, "i4", [128, 512], BF16)
        self.identf = self.sb(st, "identf", [128, 128], F32)
        self.cst = self.sb(st, "cst", [128, 4], F32)
        self.EB = self.sb(st, "EB", [128, 5, 8, 128], BF16)
        self.cb = Buf("consts")
        k.dma("pool", self.ident[:], self.din["ident"][:, :], writes=[self.cb])
        k.dma("pool", self.i4[:], self.din["i4"][:, :], writes=[self.cb])
        k.dma("sp", self.identf[:], self.din["ident"][:, :], writes=[self.cb])
        k.op("dve", lambda e: e.memset(self.cst[:, 0:1], EPS), writes=[self.cb])
        k.op("dve", lambda e: e.memset(self.cst[:, 1:2], 0.0), writes=[self.cb])
        k.op("dve", lambda e: e.memset(self.cst[:, 2:3], 1.0), writes=[self.cb])
        self.phase_bias_tables()
        k.barrier()
        self.phase0()
        k.barrier()
        for l in range(self.n_layers):
            last = (l == DEPTH - 1)
            for ph, fn in (("nsa", lambda: self.phase_nsa(l)), ("ret", lambda: self.phase_ret(l)), ("conv", lambda: self.phase_conv(l)),
                           ("merge", lambda: self.phase_merge(l)), ("ffn", lambda: self.phase_ffn(l, last))):
                if ph in self.phases:
                    with nc.named_scope("%s%d" % (ph, l)):
                        fn()
                        k.muted = False
                        k.barrier()
        k.barrier()

    def rms_alloc(self, st, tag):
        r = {}
        r["junk"] = self.sb(st, "rjunk" + tag, [128, D], BF16)
        r["ss"] = self.sb(st, "rss" + tag, [128, 4], F32)
        r["hb"] = self.sb(st, "rhb" + tag, [128, D], BF16)
        r["b"] = Buf("rms" + tag)
        r["hbb"] = Buf("rmshb" + tag)
        return r

    def rms_stats(self, r, src, src_bufs):
        k = self.k
        ss = r["ss"]
        k.op("dve", lambda e: e.memset(ss[:, 0:1], 0.0), writes=[r["b"]])
        k.op("act", lambda e: e.activation(out=r["junk"][:], in_=src, func=AF.Square, accum_out=ss[:, 0:1]),
             reads=list(src_bufs) + [r["b"]], writes=[r["b"]])
        k.op("act", lambda e: e.activation(out=ss[:, 1:2], in_=ss[:, 0:1], func=AF.Sqrt, bias=self.cst[:, 0:1], scale=1.0 / D),
             reads=[r["b"], self.cb], writes=[r["b"]])
        k.op("dve", lambda e: e.reciprocal(out=ss[:, 2:3], in_=ss[:, 1:2]), reads=[r["b"]], writes=[r["b"]])
        return ss[:, 2:3]

    def rms_to_hT(self, r, src, src_bufs, gain, gain_buf, t, ptr, ptr_b):
        k = self.k
        rstd = self.rms_stats(r, src, src_bufs)
        hb = r["hb"]
        k.op("dve", lambda e: e.scalar_tensor_tensor(out=hb[:], in0=src, scalar=rstd, in1=gain, op0=ALU.mult, op1=ALU.mult),
             reads=list(src_bufs) + [r["b"], gain_buf], writes=[r["hbb"]])
        for c in range(8):
            k.op("pe", lambda e, c=c: e.transpose(ptr[:, c * 128:(c + 1) * 128], hb[:, c * 128:(c + 1) * 128], self.ident[:]),
                 reads=[r["hbb"], self.cb], writes=[ptr_b])
        k.op("act", lambda e: e.activation(out=self.hT[:, :, t * 128:(t + 1) * 128],
                                           in_=ptr[:, :].rearrange("p (c q) -> p c q", c=8), func=AF.Copy),
             reads=[ptr_b], writes=[self.hT_b[t]])

    def phase_bias_tables(self):
        k = self.k
        with ExitStack() as st:
            bw = self.sb(st, "bw", [128, 5, 8, 128], F32)
            mw = self.sb(st, "mw", [128, 5, 128], F32)
            b31 = self.sb(st, "b31", [128, 8], F32)
            bb = Buf("bw")
            k.dma("sp", bw[:], self.din["bias_w"][:, :, :, :], writes=[bb])
            k.dma("sp", mw[:], self.din["maskw"][:, :, :], writes=[bb])
            k.dma("sp", b31[:], self.din["b31"][:, :], writes=[bb])
            for off in range(5):
                k.op("dve", lambda e, off=off: e.tensor_tensor(out=bw[:, off], in0=bw[:, off],
                                                               in1=b31[:, :].unsqueeze(2).to_broadcast([128, 8, 128]), op=ALU.subtract),
                     reads=[bb], writes=[bb])
                k.op("dve", lambda e, off=off: e.tensor_tensor(out=bw[:, off], in0=bw[:, off],
                                                               in1=mw[:, off:off + 1, :].to_broadcast([128, 8, 128]), op=ALU.mult),
                     reads=[bb], writes=[bb])
                k.op("dve", lambda e, off=off: e.tensor_scalar(out=mw[:, off, :], in0=mw[:, off, :], scalar1=-NEG, scalar2=NEG, op0=ALU.mult, op1=ALU.add),
                     reads=[bb], writes=[bb])
                k.op("dve", lambda e, off=off: e.tensor_tensor(out=self.EB[:, off], in0=bw[:, off],
                                                               in1=mw[:, off:off + 1, :].to_broadcast([128, 8, 128]), op=ALU.add),
                     reads=[bb], writes=[self.cb])
            bc = self.sb(st, "bc", [126, 8, 128], F32)
            mc = self.sb(st, "mc", [126, 128], F32)
            bcb = self.sb(st, "bcb", [126, 8, 128], BF16)
            cbuf = Buf("bc")
            for r4 in range(4):
                rows = slice(r4 * 126, (r4 + 1) * 126)
                k.dma("sp", bc[:], self.din["bias_c"][rows, :, :], writes=[cbuf])
                k.dma("sp", mc[:], self.din["maskc"][rows, :], writes=[cbuf])
                k.op("dve", lambda e: e.tensor_tensor(out=bc[:], in0=bc[:], in1=b31[0:126, :].unsqueeze(2).to_broadcast([126, 8, 128]),
                                                      op=ALU.subtract), reads=[cbuf, bb], writes=[cbuf])
                k.op("act", lambda e: e.activation(out=bc[:], in_=bc[:], func=AF.Exp), reads=[cbuf], writes=[cbuf])
                k.op("dve", lambda e: e.tensor_tensor(out=bcb[:], in0=bc[:], in1=mc[:, :].unsqueeze(1).to_broadcast([126, 8, 128]),
                                                      op=ALU.mult), reads=[cbuf], writes=[cbuf])
                k.dma("sp", self.ebc_d[rows, :, :], bcb[:], reads=[cbuf], writes=[self.ebc_db])
            k.barrier()

    def phase0(self):
        k = self.k
        with ExitStack() as st:
            r = self.rms_alloc(st, "p0")
            gain = self.sb(st, "gain0", [128, D], F32)
            gb = Buf("gain0")
            k.dma("sp", gain[:], self.din["nmix"][0], writes=[gb])
            xt = [self.sb(st, "p0x%d" % i, [128, D], F32) for i in range(2)]
            xb = [Buf("p0x%d" % i) for i in range(2)]
            ptr = self.ps(st, "p0tr", [128, 1024], BF16)
            ptr_b = Buf("p0tr")
            for t in range(NT):
                i = t % 2
                k.dma("sp", xt[i][:], self.din["x"][t * 128:(t + 1) * 128, :], writes=[xb[i]])
                k.dma("pool", self.xres[t * 128:(t + 1) * 128, :], xt[i][:], reads=[xb[i]], writes=[self.xres_b[t]])
                self.rms_to_hT(r, xt[i][:], [xb[i]], gain[:], gb, t, ptr, ptr_b)

    def load_w(self, dst, src, buf, kc):
        for c in range(kc):
            self.k.dma("pool", dst[:, c, :], src[c * 128:(c + 1) * 128, :], writes=[buf])

    def x_update(self, ctx, t, psum_halves, psum_bufs, hook):
        k = self.k
        i = ctx["i"] = (ctx.get("i", 0) + 1) % 2
        xt, xb = ctx["xt"][i], ctx["xb"][i]
        k.dma("sp", xt[:], self.xres[t * 128:(t + 1) * 128, :], reads=[self.xres_b[t]], writes=[xb])
        for h in range(2):
            k.op("dve", lambda e, h=h: e.tensor_tensor(out=xt[:, h * 512:(h + 1) * 512], in0=psum_halves[h],
                                                       in1=xt[:, h * 512:(h + 1) * 512], op=ALU.add),
                 reads=[psum_bufs[h], xb], writes=[xb])
        if hook != "final":
            k.dma("pool", self.xres[t * 128:(t + 1) * 128, :], xt[:], reads=[xb], writes=[self.xres_b[t]])
        if hook is None:
            return
        r = ctx["rms"]
        if hook == "final":
            rstd = self.rms_stats(r, xt[:], [xb])
            ot = ctx["ot"]
            k.op("dve", lambda e: e.scalar_tensor_tensor(out=ot[:], in0=xt[:], scalar=rstd, in1=ctx["gain"][:], op0=ALU.mult, op1=ALU.mult),
                 reads=[xb, r["b"], ctx["gain_b"]], writes=[ctx["ot_b"]])
            k.dma("pool", self.out[t * 128:(t + 1) * 128, :], ot[:], reads=[ctx["ot_b"]], writes=[self.out_b])
        else:
            self.rms_to_hT(r, xt[:], [xb], ctx["gain"][:], ctx["gain_b"], t, ctx["ptr"], ctx["ptr_b"])

    def upd_alloc(self, st, tag, gain_src, final=False):
        ctx = {}
        ctx["xt"] = [self.sb(st, "ux%s%d" % (tag, i), [128, D], F32) for i in range(2)]
        ctx["xb"] = [Buf("ux%d" % i) for i in range(2)]
        if gain_src is not None:
            ctx["rms"] = self.rms_alloc(st, "u" + tag)
            ctx["gain"] = self.sb(st, "ug" + tag, [128, D], F32)
            ctx["gain_b"] = Buf("ug")
            self.k.dma("sp", ctx["gain"][:], gain_src, writes=[ctx["gain_b"]])
            if final:
                ctx["ot"] = self.sb(st, "uo" + tag, [128, D], F32)
                ctx["ot_b"] = Buf("uo")
        return ctx

    def phase_ffn(self, l, last):
        k = self.k
        for fh in range(2):
            with ExitStack() as st:
                W1 = self.sb(st, "W1", [128, 8, 2048], BF16)
                W2 = self.sb(st, "W2", [128, 16, 1024], BF16)
                w1b, w2b = Buf("W1"), Buf("W2")
                self.load_w(W1, self.din["w_ff1"][l][:, fh * 2048:(fh + 1) * 2048], w1b, 8)
                self.load_w(W2, self.din["w_ff2"][l][fh * 2048:(fh + 1) * 2048, :], w2b, 16)
                actT = self.sb(st, "actT", [128, 16, 512], BF16)
                act_b = [Buf("act%d" % i) for i in range(16)]
                rl = [self.sb(st, "rl%d" % i, [128, 512], F32) for i in range(2)]
                rl_b = [Buf("rl%d" % i) for i in range(2)]
                hook = None
                gain_src = None
                if fh == 1:
                    hook = "final" if last else "norm"
                    gain_src = self.din["nfin"][:, :] if last else self.din["nmix"][l + 1]
                ctx = self.upd_alloc(st, "f", gain_src, final=(fh == 1 and last))
                pb = [self.ps(st, "fpb%d" % i, [128, 512], F32) for i in range(6)]
                pbb = [Buf("fpb%d" % i) for i in range(6)]
                if hook == "norm":
                    ctx["ptr"] = self.ps(st, "fptr", [128, 1024], BF16)
                    ctx["ptr_b"] = Buf("fptr")
                for G in range(8):
                    toks = slice(G * 512, (G + 1) * 512)
                    hb = self.hT_b[G * 4:(G + 1) * 4]
                    for fc in range(16):
                        p = fc % 2
                        for kc in range(8):
                            k.op("pe", lambda e, kc=kc, fc=fc, p=p: e.matmul(pb[p][:, :], lhsT=W1[:, kc, fc * 128:(fc + 1) * 128],
                                                                               rhs=self.hT[:, kc, toks], start=(kc == 0), stop=(kc == 7)),
                                 reads=[w1b] + hb, writes=[pbb[p]])
                        k.op("act", lambda e, p=p: e.activation(out=rl[p][:], in_=pb[p][:, :], func=AF.Relu), reads=[pbb[p]], writes=[rl_b[p]])
                        k.op("dve", lambda e, p=p, fc=fc: e.tensor_tensor(out=actT[:, fc, :], in0=rl[p][:], in1=rl[p][:], op=ALU.mult),
                             reads=[rl_b[p]], writes=[act_b[fc]])
                    for tt in range(4):
                        t = G * 4 + tt
                        pp = [2 + 2 * (tt % 2), 3 + 2 * (tt % 2)]
                        for nh in range(2):
                            for fc in range(16):
                                k.op("pe", lambda e, nh=nh, fc=fc, tt=tt: e.matmul(pb[pp[nh]][:, :], lhsT=actT[:, fc, tt * 128:(tt + 1) * 128],
                                                                                  rhs=W2[:, fc, nh * 512:(nh + 1) * 512], start=(fc == 0), stop=(fc == 15)),
                                     reads=[w2b, act_b[fc]], writes=[pbb[pp[nh]]])
                        self.x_update(ctx, t, [pb[pp[0]][:, :], pb[pp[1]][:, :]], [pbb[pp[0]], pbb[pp[1]]], hook)
                k.barrier()

    def phase_nsa(self, l):
        k = self.k
        win = self.din["w_in"][l]
        with ExitStack() as st:
            Wq = self.sb(st, "nWq", [128, 8, 512], BF16); wqb = Buf("nWq")
            self.load_w(Wq, win[:, C_QN:C_QN + 512], wqb, 8)
            ksT = self.sb(st, "nksT", [128, 2, S], BF16); ksTb = Buf("ksT")
            kwT = self.sb(st, "nkwT", [64, 2, S], BF16); kwTb = Buf("kwT")
            vsa = self.sb(st, "nvsa", [128, NT, 2, 65], BF16); vsab = Buf("vsa")
            vwa = self.sb(st, "nvwa", [128, NT, 2, 65], BF16); vwab = Buf("vwa")
            gn = self.sb(st, "ngn", [128, NT, 24], F32); gnb = Buf("gn")
            kcmpT = self.sb(st, "nkcmpT", [64, 2, 256], BF16); kcmpb = Buf("kcmpT")
            VC = self.sb(st, "nVC", [128, 2, 2, 129], BF16); VCb = Buf("VC")
            selv = self.sb(st, "nselv", [128, 126], F32)
            sela = self.sb(st, "nsela", [128, 126], F32)
            ovl = self.sb(st, "novl", [128, 2, 64], F32)
            ncb = Buf("nconst")
            k.dma("sp", selv[:], self.din["selvalid"], writes=[ncb])
            k.dma("sp", sela[:], self.din["seladd"], writes=[ncb])
            k.dma("sp", ovl[:], self.din["overlap"], writes=[ncb])
            A = [self.ps(st, "nA%d" % i, [128, 512], F32) for i in range(4)]
            Ab = [Buf("nA%d" % i) for i in range(4)]
            for g in range(2):
                k.dma("pool", ksT[64:128, g, :], self.din["rfull"], writes=[ksTb])
            OC = [self.ps(st, "nOC%d" % i, [128, 512], F32) for i in range(2)]
            OCb = [Buf("nOC%d" % i) for i in range(2)]
            OS = self.ps(st, "nOS", [128, 512], F32); OSb = Buf("OS")
            OW = self.ps(st, "nOW", [128, 512], F32); OWb = Buf("OW")
            Q = OS; Qb = OSb
            R0bf = OS[:, :].bitcast(BF16)
            R1bf = OW[:, :].bitcast(BF16)
            k.op("dve", lambda e: e.memset(vsa[:], 1.0), writes=[vsab])
            k.op("dve", lambda e: e.memset(vwa[:], 1.0), writes=[vwab])
            k.op("dve", lambda e: e.memset(kcmpT[:], 0.0), writes=[kcmpb])
            k.op("dve", lambda e: e.memset(VC[:], 0.0), writes=[VCb])
            for j in range(2):
                for g in range(2):
                    k.op("dve", lambda e, j=j, g=g: e.memset(VC[:, j, g, 64:65], 1.0), writes=[VCb])
                    k.op("dve", lambda e, j=j, g=g: e.tensor_copy(out=VC[:, j, g, 65:129], in_=ovl[:, j, :]), reads=[ncb], writes=[VCb])
            with ExitStack() as st2:
                Wk = self.sb(st2, "nWk", [128, 8, 512], BF16); wkb = Buf("nWk")
                for i, c0 in enumerate((C_KC, C_VC, C_KS, C_KW)):
                    for kc in range(8):
                        k.dma("pool", Wk[:, kc, i * 128:(i + 1) * 128], win[kc * 128:(kc + 1) * 128, c0:c0 + 128], writes=[wkb])
                Wtm = self.sb(st2, "nWtm", [128, 8, 280], BF16); wtb = Buf("nWtm")
                for (o, c0, n) in ((0, C_VS, 128), (128, C_VW, 128), (256, C_GN, 24)):
                    for kc in range(8):
                        k.dma("pool", Wtm[:, kc, o:o + n], win[kc * 128:(kc + 1) * 128, c0:c0 + n], writes=[wtb])
                w1 = [self.sb(st2, "nw1%d" % i, [64, 32, 128], BF16) for i in range(2)]
                w2k = self.sb(st2, "nw2k", [128, 64], BF16)
                w2v = self.sb(st2, "nw2v", [128, 64], BF16)
                peT = [self.sb(st2, "npeT%d" % i, [64, 32], BF16) for i in range(2)]
                cwb = Buf("cmpw")
                for i, nm in enumerate(("cmp_w1_k", "cmp_w1_v")):
                    k.dma("pool", w1[i][:], self.din[nm][l].rearrange("l d f -> d l f"), writes=[cwb])
                k.dma("pool", w2k[:], self.din["cmp_w2_k"][l], writes=[cwb])
                k.dma("pool", w2v[:], self.din["cmp_w2_v"][l], writes=[cwb])
                k.dma("pool", peT[0][:], self.din["cmp_pe_kT"][l], writes=[cwb])
                k.dma("pool", peT[1][:], self.din["cmp_pe_vT"][l], writes=[cwb])
                for t in range(NT):
                    tk = slice(t * 128, (t + 1) * 128)
                    P = A[t % 2]; PB = Ab[t % 2]
                    for kc in range(8):
                        k.op("pe", lambda e, kc=kc, P=P: e.matmul(P[:, 0:280], lhsT=self.hT[:, kc, tk], rhs=Wtm[:, kc, :], start=(kc == 0), stop=(kc == 7)),
                             reads=[wtb, self.hT_b[t]], writes=[PB])
                    k.op("act", lambda e, P=P, t=t: e.activation(out=vsa[:, t, :, 0:64], in_=P[:, 0:128].rearrange("p (g d) -> p g d", g=2), func=AF.Copy),
                         reads=[PB], writes=[vsab])
                    k.op("act", lambda e, P=P, t=t: e.activation(out=vwa[:, t, :, 0:64], in_=P[:, 128:256].rearrange("p (g d) -> p g d", g=2), func=AF.Copy),
                         reads=[PB], writes=[vwab])
                    k.op("act", lambda e, P=P, t=t: e.activation(out=gn[:, t, :], in_=P[:, 256:280], func=AF.Sigmoid), reads=[PB], writes=[gnb])
                it = 0
                for (dst, dstb, wi) in ((ksT, ksTb, 2), (kwT, kwTb, 3)):
                    for g in range(2):
                        for G in range(8):
                            toks = slice(G * 512, (G + 1) * 512)
                            P = OC[it % 2]; PB = OCb[it % 2]; it += 1
                            for kc in range(8):
                                k.op("pe", lambda e, kc=kc, P=P, wi=wi, g=g: e.matmul(P[0:64, :], lhsT=Wk[:, kc, wi * 128 + g * 64:wi * 128 + g * 64 + 64],
                                                                                     rhs=self.hT[:, kc, toks], start=(kc == 0), stop=(kc == 7)),
                                     reads=[wkb] + self.hT_b[G * 4:(G + 1) * 4], writes=[PB])
                            k.op("act", lambda e, P=P, dst=dst, g=g: e.activation(out=dst[0:64, g, toks], in_=P[0:64, :], func=AF.Copy), reads=[PB], writes=[dstb])
                cT = [self.sb(st2, "ncT%d" % i, [64, S], BF16) for i in range(2)]
                cTb = [Buf("ncT%d" % i) for i in range(2)]
                hx = self.sb(st2, "nhx", [128, 4, 256], F32); hxb = Buf("nhx")
                hidb = self.sb(st2, "nhidb", [128, 256], BF16); hidbb = Buf("nhidb")
                cbias = self.sb(st2, "ncbias", [128, 2], F32); cbb = Buf("ncbias")
                for kv in range(2):
                    for lidx in range(32):
                        k.op("pe", lambda e, kv=kv, lidx=lidx: e.matmul(Q[:, kv:kv + 1], lhsT=w1[kv][:, lidx, :], rhs=peT[kv][:, lidx:lidx + 1],
                                                                       start=(lidx == 0), stop=(lidx == 31)), reads=[cwb], writes=[Qb])
                k.op("act", lambda e: e.activation(out=cbias[:], in_=Q[:, 0:2], func=AF.Copy), reads=[Qb], writes=[cbb])
                for g in range(2):
                    for kv in range(2):
                        for G in range(8):
                            toks = slice(G * 512, (G + 1) * 512)
                            P = OC[it % 2]; PB = OCb[it % 2]; it += 1
                            for kc in range(8):
                                k.op("pe", lambda e, kc=kc, P=P, kv=kv, g=g: e.matmul(P[0:64, :], lhsT=Wk[:, kc, kv * 128 + g * 64:kv * 128 + g * 64 + 64],
                                                                                     rhs=self.hT[:, kc, toks], start=(kc == 0), stop=(kc == 7)),
                                     reads=[wkb] + self.hT_b[G * 4:(G + 1) * 4], writes=[PB])
                            k.op("act", lambda e, P=P, kv=kv: e.activation(out=cT[kv][:, toks], in_=P[0:64, :], func=AF.Copy), reads=[PB], writes=[cTb[kv]])
                    for kv in range(2):
                        P = A[kv]; PB = Ab[kv]
                        for lidx in range(32):
                            k.op("pe", lambda e, kv=kv, lidx=lidx, P=P: e.matmul(P[:, 0:255], lhsT=w1[kv][:, lidx, :], rhs=cT[kv][:, lidx:lidx + 16 * 254 + 1:16],
                                                                                start=(lidx == 0), stop=(lidx == 31)), reads=[cwb, cTb[kv]], writes=[PB])
                        x_ = hx[:, 0, 0:255]
                        k.op("act", lambda e, P=P, kv=kv: e.activation(out=x_, in_=P[:, 0:255], func=AF.Identity, bias=cbias[:, kv:kv + 1], scale=1.0),
                             reads=[PB, cbb], writes=[hxb])
                        k.op("dve", lambda e: e.tensor_tensor(out=hx[:, 1, 0:255], in0=x_, in1=x_, op=ALU.mult), reads=[hxb], writes=[hxb])
                        k.op("dve", lambda e: e.tensor_scalar(out=hx[:, 1, 0:255], in0=hx[:, 1, 0:255], scalar1=0.044715, scalar2=1.0, op0=ALU.mult, op1=ALU.add),
                             reads=[hxb], writes=[hxb])
                        k.op("dve", lambda e: e.tensor_tensor(out=hx[:, 2, 0:255], in0=hx[:, 1, 0:255], in1=x_, op=ALU.mult), reads=[hxb], writes=[hxb])
                        k.op("act", lambda e: e.activation(out=hx[:, 3, 0:255], in_=hx[:, 2, 0:255], func=AF.Sigmoid, scale=1.5957691216057308),
                             reads=[hxb], writes=[hxb])
                        k.op("dve", lambda e: e.tensor_tensor(out=hidb[:, 0:255], in0=hx[:, 3, 0:255], in1=x_, op=ALU.mult), reads=[hxb], writes=[hidbb])
                        if kv == 0:
                            k.op("pe", lambda e: e.matmul(Q[0:64, 0:255], lhsT=w2k[:], rhs=hidb[:, 0:255], start=True, stop=True), reads=[cwb, hidbb], writes=[Qb])
                            k.op("act", lambda e, g=g: e.activation(out=kcmpT[:, g, 0:255], in_=Q[0:64, 0:255], func=AF.Copy), reads=[Qb], writes=[kcmpb])
                        else:
                            for j in range(2):
                                nn = 128 if j == 0 else 127
                                k.op("pe", lambda e, j=j, nn=nn: e.matmul(Q[0:nn, j * 64:(j + 1) * 64], lhsT=hidb[:, j * 128:j * 128 + nn], rhs=w2v[:],
                                                                         start=True, stop=True), reads=[cwb, hidbb], writes=[Qb])
                                k.op("act", lambda e, j=j, nn=nn, g=g: e.activation(out=VC[0:nn, j, g, 0:64], in_=Q[0:nn, j * 64:(j + 1) * 64], func=AF.Copy),
                                     reads=[Qb], writes=[VCb])
            k.barrier()
            snegAll = self.sb(st, "nsnegAll", [128, NT * 2, 128], BF16); snegAllb = [Buf("sneg%d" % i) for i in range(NT * 2)]
            k.op("dve", lambda e: e.memset(snegAll[:], 0.0), writes=snegAllb)
            bank = {"A0": (A[0], Ab[0]), "A1": (A[1], Ab[1]), "A2": (A[2], Ab[2]), "A3": (A[3], Ab[3]),
                    "C0": (OC[0], OCb[0]), "C1": (OC[1], OCb[1]), "S": (OS, OSb), "W": (OW, OWb)}

            def finish(Tt, Tbb, sTt, sTbb, rows):
                k.op("act", lambda e: e.activation(out=sTt[0:rows, :], in_=Tt[0:rows, :], func=AF.Copy), reads=[Tbb], writes=[sTbb])
                for h4 in range(4):
                    k.op("pe", lambda e, h4=h4: e.transpose(Tt[:, h4 * 65:h4 * 65 + rows], sTt[0:rows, h4 * 128:(h4 + 1) * 128], self.identf[0:rows, 0:rows]),
                         reads=[sTbb, self.cb], writes=[Tbb])

            with ExitStack() as st3:
                qA = self.sb(st3, "nqA", [64, 4, 2, 512], BF16); qAb = Buf("nqA")
                ebc = [self.sb(st3, "nebc%d" % i, [128, 2, 8, 128], BF16) for i in range(2)]
                ebcb = [Buf("nebc%d" % i) for i in range(2)]
                pT = [self.sb(st3, "napT%d" % i, [128, 512], BF16) for i in range(4)]
                pTb = [Buf("napT%d" % i) for i in range(4)]
                sm = [self.sb(st3, "nasm%d" % i, [128, 4], F32) for i in range(2)]; smb = [Buf("nasm%d" % i) for i in range(2)]
                imp = [self.sb(st3, "naimp%d" % i, [128, 64], F32) for i in range(2)]; impb = [Buf("naimp%d" % i) for i in range(2)]
                m8 = [self.sb(st3, "nam8%d" % i, [128, 8], F32) for i in range(2)]
                cf = [self.sb(st3, "nacf%d" % i, [128, 4], F32) for i in range(2)]; cfb = [Buf("nacf%d" % i) for i in range(2)]
                occ = [self.sb(st3, "naocc%d" % i, [128, 512], F32) for i in range(2)]; occb = [Buf("naocc%d" % i) for i in range(2)]
                sTo = [self.sb(st3, "nasTo%d" % i, [65, 512], F32) for i in range(2)]; sTob = [Buf("nasTo%d" % i) for i in range(2)]
                sTi = [self.sb(st3, "nasTi%d" % i, [64, 512], F32) for i in range(2)]; sTib = [Buf("nasTi%d" % i) for i in range(2)]
                SA = [bank["A0"], bank["A1"]]
                CO = [bank["A2"], bank["C0"]]
                CI = [bank["A3"], bank["C1"]]
                QP, QPb = bank["S"]
                pti = 0
                it = 0
                for c in range(NT):
                    bq = c % 4
                    if bq == 0:
                        toks = slice(c * 128, (c + 4) * 128)
                        for h in range(8):
                            for kc in range(8):
                                k.op("pe", lambda e, kc=kc, h=h: e.matmul(QP[0:64, :], lhsT=Wq[:, kc, h * 64:(h + 1) * 64], rhs=self.hT[:, kc, toks],
                                                                         start=(kc == 0), stop=(kc == 7)), reads=[wqb] + self.hT_b[c:c + 4], writes=[QPb])
                            k.op("act", lambda e, h=h: e.activation(out=qA[:, :, h // 4, (h % 4) * 128:(h % 4 + 1) * 128],
                                                                    in_=QP[0:64, :].rearrange("p (b q) -> p b q", b=4), func=AF.Copy, scale=0.125),
                                 reads=[QPb], writes=[qAb])
                    e_ = ebc[c % 2]; e_b = ebcb[c % 2]
                    njt = 2 if c >= 16 else 1
                    for j in range(njt):
                        r0 = 248 - 8 * c + 128 * j
                        k.dma("sp", e_[:, j], self.ebc_d[r0:r0 + 128, :, :], reads=[self.ebc_db], writes=[e_b])
                    oc_ = occ[c % 2]; oc_b = occb[c % 2]
                    staged = []
                    for g in range(2):
                        qg = qA[:, bq, g, :]
                        for j in range(njt):
                            (Aa, Aab) = SA[pti % 2]; pi = pti % 4; pti += 1
                            k.op("pe", lambda e, j=j, g=g, Aa=Aa, qg=qg: e.matmul(Aa[:, :], lhsT=kcmpT[:, g, j * 128:(j + 1) * 128], rhs=qg, start=True, stop=True),
                                 reads=[kcmpb, qAb], writes=[Aab])
                            k.op("act", lambda e, Aa=Aa, pi=pi: e.activation(out=pT[pi][:], in_=Aa[:, :], func=AF.Exp), reads=[Aab], writes=[pTb[pi]])
                            k.op("dve", lambda e, pi=pi, j=j, g=g: e.tensor_tensor(out=pT[pi][:], in0=pT[pi][:],
                                                                                   in1=e_[:, j, g * 4:(g + 1) * 4, :].rearrange("p h q -> p (h q)"), op=ALU.mult),
                                 reads=[pTb[pi], e_b], writes=[pTb[pi]])
                            staged.append((g, j, pi))
                    for (g, j, pi) in staged:
                        (To, Tob), (Ti, Tib) = CO[g], CI[g]
                        k.op("pe", lambda e, pi=pi, j=j, g=g, To=To: e.matmul(To[0:65, :], lhsT=VC[:, j, g, 0:65], rhs=pT[pi][:], start=(j == 0), stop=(j == njt - 1)),
                             reads=[pTb[pi], VCb], writes=[Tob])
                        k.op("pe", lambda e, pi=pi, j=j, g=g, Ti=Ti: e.matmul(Ti[0:64, :], lhsT=VC[:, j, g, 65:129], rhs=pT[pi][:], start=(j == 0), stop=(j == njt - 1)),
                             reads=[pTb[pi], VCb], writes=[Tib])
                    for g in range(2):
                        (To, Tob), (Ti, Tib) = CO[g], CI[g]
                        k.op("act", lambda e, g=g, To=To: e.activation(out=sTo[g][0:65, :], in_=To[0:65, :], func=AF.Copy), reads=[Tob], writes=[sTob[g]])
                        k.op("act", lambda e, g=g, Ti=Ti: e.activation(out=sTi[g][0:64, :], in_=Ti[0:64, :], func=AF.Copy), reads=[Tib], writes=[sTib[g]])
                    for g in range(2):
                        (To, Tob), (Ti, Tib) = CO[g], CI[g]
                        for h4 in range(4):
                            k.op("pe", lambda e, h4=h4, g=g, To=To: e.transpose(To[:, h4 * 65:h4 * 65 + 65], sTo[g][0:65, h4 * 128:(h4 + 1) * 128], self.identf[0:65, 0:65]),
                                 reads=[sTob[g], self.cb], writes=[Tob])
                        for h4 in range(4):
                            k.op("pe", lambda e, h4=h4, g=g, Ti=Ti: e.transpose(Ti[:, h4 * 65:h4 * 65 + 64], sTi[g][0:64, h4 * 128:(h4 + 1) * 128], self.identf[0:64, 0:64]),
                                 reads=[sTib[g], self.cb], writes=[Tib])
                    chains = [[], []]
                    for g in range(2):
                        ch = chains[g]
                        (To, Tob), (Ti, Tib) = CO[g], CI[g]
                        sm_, smb_, imp_, impb_, cf_, cfb_, m8_ = sm[g], smb[g], imp[g], impb[g], cf[g], cfb[g], m8[g]
                        ch.append((lambda e, sm_=sm_, To=To: e.tensor_scalar(out=sm_[:], in0=To[:, 64:64 + 260:65], scalar1=1e-30, scalar2=None, op0=ALU.max), [Tob], [smb_]))
                        ch.append((lambda e, sm_=sm_: e.reciprocal(out=sm_[:], in_=sm_[:]), [smb_], [smb_]))
                        for h4 in range(4):
                            src = Ti[:, h4 * 65:h4 * 65 + 64]
                            if h4 == 0:
                                ch.append((lambda e, src=src, sm_=sm_, imp_=imp_: e.tensor_scalar(out=imp_[:], in0=src, scalar1=sm_[:, 0:1], scalar2=None, op0=ALU.mult),
                                           [Tib, smb_], [impb_]))
                            else:
                                ch.append((lambda e, src=src, h4=h4, sm_=sm_, imp_=imp_: e.scalar_tensor_tensor(out=imp_[:], in0=src, scalar=sm_[:, h4:h4 + 1], in1=imp_[:],
                                                                                                                 op0=ALU.mult, op1=ALU.add), [Tib, smb_, impb_], [impb_]))
                        sl = slice(62 - 2 * c, 62 - 2 * c + 64)
                        ch.append((lambda e, imp_=imp_, sl=sl: e.tensor_tensor(out=imp_[:], in0=imp_[:], in1=selv[:, sl], op=ALU.mult), [impb_, ncb], [impb_]))
                        ch.append((lambda e, imp_=imp_, sl=sl: e.tensor_tensor(out=imp_[:], in0=imp_[:], in1=sela[:, sl], op=ALU.add), [impb_, ncb], [impb_]))
                        if c >= 1:
                            ch.append((lambda e, imp_=imp_: e.tensor_scalar(out=imp_[:, 0:1], in0=imp_[:, 0:1], scalar1=1e4, scalar2=None, op0=ALU.add), [impb_], [impb_]))
                        ch.append((lambda e, imp_=imp_, m8_=m8_: e.max(out=m8_[:], in_=imp_[:]), [impb_], [impb_]))
                        si = c * 2 + g
                        ch.append((lambda e, si=si, imp_=imp_, m8_=m8_: e.tensor_scalar(out=snegAll[:, si, 64:128], in0=imp_[:], scalar1=m8_[:, 7:8], scalar2=NEG,
                                                                                       op0=ALU.is_lt, op1=ALU.mult), [impb_], [snegAllb[si]]))
                        gview = gn[:, c, g * 12:(g + 1) * 12].rearrange("p (h b) -> p h b", h=4)
                        ch.append((lambda e, cf_=cf_, gview=gview, sm_=sm_: e.tensor_tensor(out=cf_[:], in0=gview[:, :, 0], in1=sm_[:], op=ALU.mult), [gnb, smb_], [cfb_]))
                        for h4 in range(4):
                            hh = g * 4 + h4
                            ch.append((lambda e, h4=h4, hh=hh, To=To, cf_=cf_: e.tensor_scalar(out=oc_[:, hh * 64:(hh + 1) * 64], in0=To[:, h4 * 65:h4 * 65 + 64],
                                                                                               scalar1=cf_[:, h4:h4 + 1], scalar2=None, op0=ALU.mult), [Tob, cfb_], [oc_b]))
                    for i_ in range(max(len(chains[0]), len(chains[1]))):
                        for g in range(2):
                            if i_ < len(chains[g]):
                                fn_, rd_, wr_ = chains[g][i_]
                                k.op("dve", fn_, reads=rd_, writes=wr_)
                    k.dma("sp", self.ocd[c], oc_[:], reads=[oc_b], writes=[self.ocd_b[c]])
            k.barrier()
            qAll = self.sb(st, "nqAll", [128, 4, 2, 512], BF16)
            qTopb = Buf("qTop")
            qBotb = [[Buf("qBot%d_%d" % (b_, g_)) for g_ in range(2)] for b_ in range(4)]
            NPT = 5
            LA = 3
            pT = [self.sb(st, "npT%d" % i, [128, 512], BF16) for i in range(NPT)]
            pTb = [Buf("npT%d" % i) for i in range(NPT)]
            sm = self.sb(st, "nsm", [128, 2, 4], F32); smb = Buf("nsm")
            coef = self.sb(st, "ncoef", [128, 4, 2], F32); coefb = Buf("ncoef")
            oin = [self.sb(st, "noin%d" % i, [128, 512], F32) for i in range(2)]; oinb = [Buf("noin%d" % i) for i in range(2)]
            onsa = self.sb(st, "nonsa", [128, 512], BF16); onsab = Buf("nonsa")
            onT = [self.sb(st, "nonT%d" % i, [128, 4, 128], BF16) for i in range(2)]
            onTb = [Buf("nonT%d" % i) for i in range(2)]
            sTs = [self.sb(st, "nsTs%d" % i, [65, 512], F32) for i in range(2)]; sTsb = [Buf("nsTs%d" % i) for i in range(2)]
            sTw = [self.sb(st, "nsTw%d" % i, [65, 512], F32) for i in range(2)]; sTwb = [Buf("nsTw%d" % i) for i in range(2)]
            SA = [bank["A0"], bank["A1"], bank["A2"], bank["A3"]]
            (Ts, Tsb), (Tw, Twb) = bank["C0"], bank["C1"]
            (Rs, Rsb), (Rw, Rwb) = bank["S"], bank["W"]
            pti = 0
            it = 0

            def finish2(Tt, Tbb, sTt, sTbb, Rr, Rbb):
                k.op("act", lambda e: e.activation(out=sTt[0:65, :], in_=Tt[0:65, :], func=AF.Copy), reads=[Tbb], writes=[sTbb])
                for h4 in range(4):
                    k.op("pe", lambda e, h4=h4: e.transpose(Rr[:, h4 * 65:h4 * 65 + 65], sTt[0:65, h4 * 128:(h4 + 1) * 128], self.identf[0:65, 0:65]),
                         reads=[sTbb, self.cb], writes=[Rbb])

            for c in range(NT):
                bq = c % 4
                if bq == 0:
                    toks = slice(c * 128, (c + 4) * 128)
                    for h in range(8):
                        (X, Xb) = SA[pti % 4]; pti += 1
                        for kc in range(8):
                            k.op("pe", lambda e, kc=kc, h=h, X=X: e.matmul(X[0:64, :], lhsT=Wq[:, kc, h * 64:(h + 1) * 64], rhs=self.hT[:, kc, toks],
                                                                          start=(kc == 0), stop=(kc == 7)), reads=[wqb] + self.hT_b[c:c + 4], writes=[Xb])
                        k.op("act", lambda e, h=h, X=X: e.activation(out=qAll[0:64, :, h // 4, (h % 4) * 128:(h % 4 + 1) * 128],
                                                                     in_=X[0:64, :].rearrange("p (b q) -> p b q", b=4), func=AF.Copy, scale=0.125),
                             reads=[Xb], writes=[qTopb])
                oi = oin[c % 2]; oib = oinb[c % 2]
                k.dma("sp", oi[:], self.ocd[c], reads=[self.ocd_b[c]], writes=[oib])
                for g in range(2):
                    par = it % 2; it += 1
                    si = c * 2 + g
                    qg = qAll[0:64, bq, g, :]
                    qfull = qAll[:, bq, g, :]
                    (X, Xb) = SA[pti % 4]; pti += 1
                    Xbf = X[:, :].bitcast(BF16)
                    k.op("pe", lambda e, si=si: e.transpose(Xbf[:, 0:128], snegAll[:, si, :], self.ident[:]), reads=[snegAllb[si], self.cb], writes=[Xb])
                    k.op("dve", lambda e: e.tensor_copy(out=qAll[64:128, bq, g, :].rearrange("p (h q) -> p h q", h=4),
                                                        in_=Xbf[64:128, 0:128].unsqueeze(1).to_broadcast([64, 4, 128])),
                         reads=[Xb], writes=[qBotb[bq][g]])
                    tiles = [("w", kb) for kb in range(max(0, c - 4), c + 1)] + [("s", kb) for kb in range(c + 1)]
                    nwin = len(tiles) - (c + 1)
                    slots = []

                    def emit_qk(idx):
                        nonlocal pti
                        kind, kb = tiles[idx]
                        (Aa, Aab) = SA[pti % 4]; pi = pti % NPT; pti += 1
                        slots.append((Aa, Aab, pi))
                        kt = slice(kb * 128, (kb + 1) * 128)
                        off = c - kb
                        near = (kind == "w" or off <= 1)
                        if kind == "s":
                            k.op("pe", lambda e: e.matmul(Aa[:, :], lhsT=ksT[:, g, kt], rhs=qfull, start=True, stop=not near),
                                 reads=[ksTb, qTopb, qBotb[bq][g]], writes=[Aab])
                        else:
                            k.op("pe", lambda e: e.matmul(Aa[:, :], lhsT=kwT[:, g, kt], rhs=qg, start=True, stop=not near), reads=[kwTb, qTopb], writes=[Aab])
                        if near:
                            k.op("pe", lambda e: e.matmul(Aa[:, :], lhsT=self.ident[:], rhs=self.EB[:, off, g * 4:(g + 1) * 4, :].rearrange("p h q -> p (h q)"),
                                                          start=False, stop=True), reads=[self.cb], writes=[Aab])

                    def emit_pv(idx):
                        kind, kb = tiles[idx]
                        Aa, Aab, pi = slots[idx]
                        k.op("act", lambda e: e.activation(out=pT[pi][:], in_=Aa[:, :], func=AF.Exp), reads=[Aab], writes=[pTb[pi]])
                        if kind == "s":
                            Tt, Ttb, V, Vb = Ts, Tsb, vsa, vsab
                            first, lastt = (idx == nwin), (idx == len(tiles) - 1)
                        else:
                            Tt, Ttb, V, Vb = Tw, Twb, vwa, vwab
                            first, lastt = (idx == 0), (idx == nwin - 1)
                        k.op("pe", lambda e: e.matmul(Tt[0:65, :], lhsT=V[:, kb, g, :], rhs=pT[pi][:], start=first, stop=lastt),
                             reads=[pTb[pi], Vb], writes=[Ttb])

                    for idx in range(len(tiles) + LA):
                        if idx < len(tiles):
                            emit_qk(idx)
                        if idx >= LA:
                            emit_pv(idx - LA)
                    finish2(Tw, Twb, sTw[par], sTwb[par], Rw, Rwb)
                    finish2(Ts, Tsb, sTs[par], sTsb[par], Rs, Rsb)
                    gview = gn[:, c, g * 12:(g + 1) * 12].rearrange("p (h b) -> p h b", h=4)
                    k.op("dve", lambda e: e.tensor_scalar(out=sm[:, 0, :], in0=Rs[:, 64:64 + 260:65], scalar1=1e-30, scalar2=None, op0=ALU.max), reads=[Rsb, smb], writes=[smb])
                    k.op("dve", lambda e: e.tensor_scalar(out=sm[:, 1, :], in0=Rw[:, 64:64 + 260:65], scalar1=1e-30, scalar2=None, op0=ALU.max), reads=[Rwb, smb], writes=[smb])
                    k.op("dve", lambda e: e.reciprocal(out=sm[:, :, :], in_=sm[:, :, :]), reads=[smb], writes=[smb])
                    k.op("dve", lambda e: e.tensor_tensor(out=coef[:], in0=gview[:, :, 1:3], in1=sm[:, :, :].rearrange("p b h -> p h b"), op=ALU.mult),
                         reads=[gnb, smb, coefb], writes=[coefb])
                    for h4 in range(4):
                        hh = g * 4 + h4
                        k.op("dve", lambda e, h4=h4, hh=hh: e.scalar_tensor_tensor(out=oi[:, hh * 64:(hh + 1) * 64], in0=Rs[:, h4 * 65:h4 * 65 + 64], scalar=coef[:, h4, 0:1],
                                                                                    in1=oi[:, hh * 64:(hh + 1) * 64], op0=ALU.mult, op1=ALU.add), reads=[Rsb, coefb, oib], writes=[oib])
                        k.op("dve", lambda e, h4=h4, hh=hh: e.scalar_tensor_tensor(out=onsa[:, hh * 64:(hh + 1) * 64], in0=Rw[:, h4 * 65:h4 * 65 + 64], scalar=coef[:, h4, 1:2],
                                                                                    in1=oi[:, hh * 64:(hh + 1) * 64], op0=ALU.mult, op1=ALU.add), reads=[Rwb, coefb, oib], writes=[onsab])
                (X, Xb) = SA[pti % 4]; pti += 1
                Xbf = X[:, :].bitcast(BF16)
                for j in range(4):
                    k.op("pe", lambda e, j=j: e.transpose(Xbf[:, j * 128:(j + 1) * 128], onsa[:, j * 128:(j + 1) * 128], self.ident[:]),
                         reads=[onsab, self.cb], writes=[Xb])
                i2 = c % 2
                k.op("act", lambda e, i2=i2: e.activation(out=onT[i2][:], in_=Xbf[:, 0:512].rearrange("p (j c) -> p j c", j=4), func=AF.Copy),
                     reads=[Xb], writes=[onTb[i2]])
                tk = slice(c * 128, (c + 1) * 128)
                k.dma("sp", self.brT[0, :, :, tk].rearrange("c p q -> p c q"), onT[i2][:], reads=[onTb[i2]], writes=[self.brT_b[0][c]])

    def phase_ret(self, l):
        k = self.k
        with ExitStack() as st:
            Wqk = self.sb(st, "rWqk", [128, 8, 512], BF16)
            Wv = self.sb(st, "rWv", [128, 8, 512], BF16)
            Wg = self.sb(st, "rWg", [128, 8, 512], BF16)
            wb = Buf("rW")
            self.load_w(Wqk, self.din["w_in"][l][:, C_QR:C_QR + 512], wb, 8)
            self.load_w(Wv, self.din["w_in"][l][:, C_VR:C_VR + 512], wb, 8)
            self.load_w(Wg, self.din["w_in"][l][:, C_GR:C_GR + 512], wb, 8)
            cos = self.sb(st, "rcos", [128, NT, 32], F32)
            sin = self.sb(st, "rsin", [128, NT, 32], F32)
            dec = self.sb(st, "rdec", [128, 4, 128], F32)
            xi = self.sb(st, "rxi", [64, 4, 128], F32)
            zeta = self.sb(st, "rzeta", [128, 4], F32)
            gch = self.sb(st, "rgch", [64, 4], F32)
            gn = self.sb(st, "rgn", [128, 512], F32)
            rc = Buf("rconst")
            for dst, src in ((cos, "cos"), (sin, "sin"), (dec, "decayT"), (xi, "xi"), (zeta, "zeta"), (gch, "gch")):
                k.dma("sp", dst[:], self.din[src], writes=[rc])
            k.dma("sp", gn[:], self.din["retgn"][l], writes=[rc])
            Sf = self.sb(st, "rSf", [64, 4, 128], F32)
            Sb = self.sb(st, "rSb", [64, 4, 128], BF16)
            Sfb, Sbb = Buf("Sf"), Buf("Sb")
            k.op("dve", lambda e: e.memset(Sf[:], 0.0), writes=[Sfb])
            k.op("dve", lambda e: e.memset(Sb[:], 0.0), writes=[Sbb])
            qk = self.sb(st, "rqk", [128, 512], F32); qkb = Buf("rqk")
            tm = self.sb(st, "rtm", [128, 4, 8, 32], F32); tmb = Buf("rtm")
            rot = self.sb(st, "rrot", [128, 8, 2, 32], F32); rotb = Buf("rrot")
            qkbf = self.sb(st, "rqkbf", [128, 512], BF16); qkbfb = Buf("rqkbf")
            khat = self.sb(st, "rkhat", [128, 4, 64], BF16); khatb = Buf("rkhat")
            qkT = self.sb(st, "rqkT", [64, 8, 128], BF16); qkTb = Buf("rqkT")
            qxiT = self.sb(st, "rqxiT", [64, 4, 128], BF16); qxiTb = Buf("rqxiT")
            qf32 = self.sb(st, "rqf32", [64, 4, 128], F32); qf32b = Buf("rqf32")
            inT = self.sb(st, "rinT", [128, 4, 128], BF16); inTb = Buf("rinT")
            vbf = self.sb(st, "rvbf", [128, 512], BF16); vbfb = Buf("rvbf")
            osb = self.sb(st, "rosb", [128, 512], F32); osbb = Buf("rosb")
            osq = self.sb(st, "rosq", [128, 512], F32); osqb = Buf("rosq")
            sm = self.sb(st, "rsm", [128, 6, 4], F32); smb = Buf("rsm")
            yn = self.sb(st, "ryn", [128, 512], F32); ynb = Buf("ryn")
            gs = self.sb(st, "rgs", [128, 512], F32); gsb = Buf("rgs")
            orb = self.sb(st, "rorb", [128, 512], BF16); orbb = Buf("rorb")
            orT = [self.sb(st, "rorT%d" % i, [128, 4, 128], BF16) for i in range(2)]
            orTb = [Buf("rorT%d" % i) for i in range(2)]
            pqk = self.ps(st, "rpqk", [128, 512], F32); pqkb = Buf("pqk")
            pv = self.ps(st, "rpv", [128, 512], F32); pvb = Buf("pv")
            pg = self.ps(st, "rpg", [128, 512], F32); pgb = Buf("pg")
            pin = self.ps(st, "rpin", [128, 512], F32); pinb = Buf("pin")
            po = self.ps(st, "rpo", [128, 512], F32); pob = Buf("po")
            pkv = self.ps(st, "rpkv", [128, 512], F32); pkvb = Buf("pkv")
            ptr = self.ps(st, "rptr", [128, 1024], BF16); ptrb = Buf("ptr")
            ptr2 = self.ps(st, "rptr2", [128, 1024], BF16); ptr2b = Buf("ptr2")
            for t in range(NT):
                tk = slice(t * 128, (t + 1) * 128)
                for (W, P, PB) in ((Wqk, pqk, pqkb), (Wv, pv, pvb), (Wg, pg, pgb)):
                    for kc in range(8):
                        k.op("pe", lambda e, kc=kc, W=W, P=P: e.matmul(P[:, :], lhsT=self.hT[:, kc, tk], rhs=W[:, kc, :], start=(kc == 0), stop=(kc == 7)),
                             reads=[wb, self.hT_b[t]], writes=[PB])
                self.chk(1)
                k.op("act", lambda e: e.activation(out=qk[:, 0:256], in_=pqk[:, 0:256], func=AF.Copy), reads=[pqkb], writes=[qkb])
                k.op("act", lambda e: e.activation(out=qk[:, 256:512], in_=pqk[:, 256:512], func=AF.Copy, scale=0.125), reads=[pqkb], writes=[qkb])
                xv = qk[:, :].rearrange("p (h two d) -> p h two d", h=8, two=2)
                x1, x2 = xv[:, :, 0, :], xv[:, :, 1, :]
                cb_ = cos[:, t, :].unsqueeze(1).to_broadcast([128, 8, 32])
                sb_ = sin[:, t, :].unsqueeze(1).to_broadcast([128, 8, 32])
                k.op("dve", lambda e: e.tensor_tensor(out=tm[:, 0], in0=x1, in1=cb_, op=ALU.mult), reads=[qkb, rc], writes=[tmb])
                k.op("dve", lambda e: e.tensor_tensor(out=tm[:, 1], in0=x2, in1=sb_, op=ALU.mult), reads=[qkb, rc], writes=[tmb])
                k.op("dve", lambda e: e.tensor_tensor(out=tm[:, 2], in0=x1, in1=sb_, op=ALU.mult), reads=[qkb, rc], writes=[tmb])
                k.op("dve", lambda e: e.tensor_tensor(out=tm[:, 3], in0=x2, in1=cb_, op=ALU.mult), reads=[qkb, rc], writes=[tmb])
                k.op("dve", lambda e: e.tensor_tensor(out=rot[:, :, 0, :], in0=tm[:, 0], in1=tm[:, 1], op=ALU.subtract), reads=[tmb], writes=[rotb])
                k.op("dve", lambda e: e.tensor_tensor(out=rot[:, :, 1, :], in0=tm[:, 2], in1=tm[:, 3], op=ALU.add), reads=[tmb], writes=[rotb])
                self.chk(2)
                rflat = rot[:, :, :, :].rearrange("p h two d -> p (h two d)")
                k.op("act", lambda e: e.activation(out=qkbf[:], in_=rflat, func=AF.Copy), reads=[rotb], writes=[qkbfb])
                k.op("dve", lambda e: e.tensor_tensor(out=khat[:], in0=rflat[:, 256:512].rearrange("p (h d) -> p h d", h=4),
                                                      in1=zeta[:, :].unsqueeze(2).to_broadcast([128, 4, 64]), op=ALU.mult),
                     reads=[rotb, rc], writes=[khatb])
                self.chk(3)
                for j in range(8):
                    k.op("pe", lambda e, j=j: e.transpose(ptr[0:64, j * 128:(j + 1) * 128], qkbf[:, j * 64:(j + 1) * 64], self.ident[:]),
                         reads=[qkbfb, self.cb], writes=[ptrb])
                k.op("act", lambda e: e.activation(out=qkT[:], in_=ptr[0:64, 0:1024].rearrange("p (j c) -> p j c", j=8), func=AF.Copy),
                     reads=[ptrb], writes=[qkTb])
                k.op("act", lambda e: e.activation(out=qf32[:], in_=ptr[0:64, 0:512].rearrange("p (j c) -> p j c", j=4), func=AF.Copy),
                     reads=[ptrb], writes=[qf32b])
                k.op("dve", lambda e: e.tensor_tensor(out=qxiT[:], in0=qf32[:], in1=xi[:], op=ALU.mult),
                     reads=[qf32b, rc], writes=[qxiTb])
                self.chk(4)
                for h in range(4):
                    k.op("pe", lambda e, h=h: e.matmul(pin[:, h * 128:(h + 1) * 128], lhsT=qkT[:, 4 + h, :], rhs=qkT[:, h, :],
                                                       start=True, stop=True), reads=[qkTb], writes=[pinb])
                self.chk(45)
                k.op("dve", lambda e: e.tensor_tensor(out=inT[:], in0=pin[:, :].rearrange("p (h c) -> p h c", h=4), in1=dec[:], op=ALU.mult),
                     reads=[pinb, rc], writes=[inTb])
                self.chk(5)
                k.op("act", lambda e: e.activation(out=vbf[:], in_=pv[:, :], func=AF.Copy), reads=[pvb], writes=[vbfb])
                for h in range(4):
                    rows = slice((h % 2) * 64, (h % 2) * 64 + 64)
                    hc = slice(h * 128, (h + 1) * 128)
                    k.op("pe", lambda e, h=h, hc=hc: e.matmul(po[:, hc], lhsT=inT[:, h, :], rhs=vbf[:, hc], start=True, stop=False),
                         reads=[inTb, vbfb], writes=[pob])
                    k.op("pe", lambda e, h=h, hc=hc: e.matmul(po[:, hc], lhsT=qxiT[:, h, :], rhs=Sb[:, h, :], start=False, stop=True),
                         reads=[qxiTb, Sbb], writes=[pob])
                self.chk(6)
                for h in range(4):
                    hc = slice(h * 128, (h + 1) * 128)
                    k.op("pe", lambda e, h=h, hc=hc: e.matmul(pkv[0:64, hc], lhsT=khat[:, h, :],
                                                             rhs=vbf[:, hc], start=True, stop=True), reads=[khatb, vbfb], writes=[pkvb])
                self.chk(7)
                for h in range(4):
                    rows = slice((h % 2) * 64, (h % 2) * 64 + 64)
                    hc = slice(h * 128, (h + 1) * 128)
                    k.op("dve", lambda e, h=h, hc=hc: e.scalar_tensor_tensor(out=Sf[:, h, :], in0=Sf[:, h, :],
                                                                              scalar=gch[:, h:h + 1], in1=pkv[0:64, hc],
                                                                              op0=ALU.mult, op1=ALU.add),
                         reads=[pkvb, rc, Sfb], writes=[Sfb])
                k.op("act", lambda e: e.activation(out=Sb[:], in_=Sf[:], func=AF.Copy), reads=[Sfb], writes=[Sbb])
                self.chk(8)
                k.op("act", lambda e: e.activation(out=osb[:], in_=po[:, :], func=AF.Copy), reads=[pob], writes=[osbb])
                k.op("act", lambda e: e.activation(out=osq[:], in_=po[:, :], func=AF.Square), reads=[pob], writes=[osqb])
                k.op("dve", lambda e: e.reduce_sum(out=sm[:, 0, :], in_=osb[:, :].rearrange("p (h v) -> p h v", h=4), axis=AX.X), reads=[osbb], writes=[smb])
                k.op("dve", lambda e: e.reduce_sum(out=sm[:, 1, :], in_=osq[:, :].rearrange("p (h v) -> p h v", h=4), axis=AX.X), reads=[osqb, smb], writes=[smb])
                k.op("dve", lambda e: e.tensor_scalar(out=sm[:, 2, :], in0=sm[:, 0, :], scalar1=1.0 / 128, scalar2=None, op0=ALU.mult), reads=[smb], writes=[smb])
                k.op("dve", lambda e: e.tensor_tensor(out=sm[:, 3, :], in0=sm[:, 2, :], in1=sm[:, 2, :], op=ALU.mult), reads=[smb], writes=[smb])
                k.op("dve", lambda e: e.scalar_tensor_tensor(out=sm[:, 3, :], in0=sm[:, 1, :], scalar=1.0 / 128, in1=sm[:, 3, :], op0=ALU.mult, op1=ALU.subtract),
                     reads=[smb], writes=[smb])
                k.op("act", lambda e: e.activation(out=sm[:, 4, :], in_=sm[:, 3, :], func=AF.Sqrt, bias=self.cst[:, 0:1], scale=1.0), reads=[smb, self.cb], writes=[smb])
                k.op("dve", lambda e: e.reciprocal(out=sm[:, 5, :], in_=sm[:, 4, :]), reads=[smb], writes=[smb])
                self.chk(9)
                for h in range(4):
                    hc = slice(h * 128, (h + 1) * 128)
                    k.op("dve", lambda e, h=h, hc=hc: e.tensor_scalar(out=yn[:, hc], in0=osb[:, hc], scalar1=sm[:, 2, h:h + 1], scalar2=sm[:, 5, h:h + 1],
                                                                      op0=ALU.subtract, op1=ALU.mult), reads=[osbb, smb], writes=[ynb])
                k.op("dve", lambda e: e.tensor_tensor(out=yn[:], in0=yn[:], in1=gn[:], op=ALU.mult), reads=[ynb, rc], writes=[ynb])
                self.chk(10)
                k.op("act", lambda e: e.activation(out=gs[:], in_=pg[:, :], func=AF.Silu), reads=[pgb], writes=[gsb])
                k.op("dve", lambda e: e.tensor_tensor(out=orb[:], in0=yn[:], in1=gs[:], op=ALU.mult), reads=[ynb, gsb], writes=[orbb])
                self.chk(11)
                for j in range(4):
                    k.op("pe", lambda e, j=j: e.transpose(ptr2[:, j * 128:(j + 1) * 128], orb[:, j * 128:(j + 1) * 128], self.ident[:]),
                         reads=[orbb, self.cb], writes=[ptr2b])
                i = t % 2
                k.op("act", lambda e, i=i: e.activation(out=orT[i][:], in_=ptr2[:, 0:512].rearrange("p (j c) -> p j c", j=4), func=AF.Copy),
                     reads=[ptr2b], writes=[orTb[i]])
                self.chk(12)
                k.dma("sp", self.brT[1, :, :, tk].rearrange("c p q -> p c q"), orT[i][:], reads=[orTb[i]], writes=[self.brT_b[1][t]])

    def phase_conv(self, l):
        k = self.k
        with ExitStack() as st:
            Wa = self.sb(st, "cWa", [128, 8, 512], BF16)
            Wb = self.sb(st, "cWb", [128, 8, 512], BF16)
            wab = Buf("cW")
            self.load_w(Wa, self.din["w_in"][l][:, C_CA:C_CA + 512], wab, 8)
            self.load_w(Wb, self.din["w_in"][l][:, C_CB:C_CB + 512], wab, 8)
            cw = self.sb(st, "ccw", [128, 4, 31], F32)
            cvec = self.sb(st, "cvec", [128, 3, 4], F32)
            ones = self.sb(st, "cones", [128, 128], F32)
            cc = Buf("cconst")
            k.dma("sp", cw[:], self.din["convw"][l], writes=[cc])
            k.dma("sp", cvec[:, 0, :], self.din["convb"][l], writes=[cc])
            k.dma("sp", cvec[:, 1, :], self.din["convg"][l], writes=[cc])
            k.dma("sp", cvec[:, 2, :], self.din["convbb"][l], writes=[cc])
            k.dma("sp", ones[:], self.din["ones"][:, :], writes=[cc])
            Dg = self.sb(st, "cDg", [128, 4, 31, 128], BF16); dgb = Buf("cDg")
            for ct in range(4):
                for w in range(31):
                    en = "dve" if (w % 2 == 0) else "pool"
                    k.op(en, lambda e, ct=ct, w=w: e.tensor_scalar(out=Dg[:, ct, w, :], in0=self.identf[:], scalar1=cw[:, ct, w:w + 1], scalar2=None, op0=ALU.mult),
                         reads=[cc, self.cb], writes=[dgb])
            u = [self.sb(st, "cu%d" % i, [128, 4, 542], BF16) for i in range(2)]
            ub = [[Buf("cu%d_%d" % (i, ct)) for ct in range(4)] for i in range(2)]
            acc = self.sb(st, "cacc", [128, 4, 512], F32)
            accb = [Buf("cacc%d" % ct) for ct in range(4)]
            sg = [self.sb(st, "csg%d" % i, [128, 512], F32) for i in range(2)]
            sgb = [Buf("csg%d" % i) for i in range(2)]
            ysq = [self.sb(st, "cysq%d" % i, [128, 512], F32) for i in range(2)]
            ysqb = [Buf("cysq%d" % i) for i in range(2)]
            stt = self.sb(st, "cstt", [128, 4, 512], F32)
            sttb = Buf("cstt")
            yn = [self.sb(st, "cyn%d" % i, [128, 512], F32) for i in range(2)]
            ynb = [Buf("cyn%d" % i) for i in range(2)]
            oc = [self.sb(st, "coc%d" % i, [128, 512], BF16) for i in range(2)]
            ocb = [Buf("coc%d" % i) for i in range(2)]
            pa = [self.ps(st, "cpa%d" % i, [128, 512], F32) for i in range(2)]
            pab = [Buf("cpa%d" % i) for i in range(2)]
            pbk = [self.ps(st, "cpb%d" % i, [128, 512], F32) for i in range(2)]
            pbb = [Buf("cpb%d" % i) for i in range(2)]
            pcv = [self.ps(st, "cpc%d" % i, [128, 512], F32) for i in range(2)]
            pcb = [Buf("cpc%d" % i) for i in range(2)]
            s1 = self.ps(st, "cs1", [128, 512], F32)
            s2 = self.ps(st, "cs2", [128, 512], F32)
            s1b, s2b = Buf("cs1"), Buf("cs2")
            for ct in range(4):
                k.op("dve", lambda e, ct=ct: e.memset(u[0][:, ct, 0:30], 0.0), writes=[ub[0][ct]])
            for G in range(8):
                toks = slice(G * 512, (G + 1) * 512)
                hb = self.hT_b[G * 4:(G + 1) * 4]
                ug, ugb = u[G % 2], ub[G % 2]
                for ct in range(4):
                    p = ct % 2
                    cols = slice(ct * 128, (ct + 1) * 128)
                    for kc in range(8):
                        k.op("pe", lambda e, kc=kc, cols=cols, p=p: e.matmul(pa[p][:, :], lhsT=Wa[:, kc, cols], rhs=self.hT[:, kc, toks],
                                                                              start=(kc == 0), stop=(kc == 7)), reads=[wab] + hb, writes=[pab[p]])
                    for kc in range(8):
                        k.op("pe", lambda e, kc=kc, cols=cols, p=p: e.matmul(pbk[p][:, :], lhsT=Wb[:, kc, cols], rhs=self.hT[:, kc, toks],
                                                                              start=(kc == 0), stop=(kc == 7)), reads=[wab] + hb, writes=[pbb[p]])
                    k.op("act", lambda e, p=p: e.activation(out=sg[p][:], in_=pbk[p][:, :], func=AF.Sigmoid), reads=[pbb[p]], writes=[sgb[p]])
                    if G > 0:
                        k.op("pool", lambda e, ct=ct: e.tensor_copy(out=ug[:, ct, 0:30], in_=u[(G - 1) % 2][:, ct, 512:542]),
                             reads=[ub[(G - 1) % 2][ct]], writes=[ugb[ct]])
                    k.op("dve", lambda e, ct=ct, p=p: e.tensor_tensor(out=ug[:, ct, 30:542], in0=pa[p][:, :], in1=sg[p][:], op=ALU.mult),
                         reads=[pab[p], sgb[p]], writes=[ugb[ct]])
                for ct in range(4):
                    p = ct % 2
                    for w in range(31):
                        k.op("pe", lambda e, ct=ct, w=w, p=p: e.matmul(pcv[p][:, :], lhsT=Dg[:, ct, w, :], rhs=ug[:, ct, w:w + 512], start=(w == 0), stop=(w == 30)),
                             reads=[dgb, ugb[ct]], writes=[pcb[p]])
                    k.op("act", lambda e, ct=ct, p=p: e.activation(out=acc[:, ct, :], in_=pcv[p][:, :], func=AF.Identity, bias=cvec[:, 0, ct:ct + 1], scale=1.0),
                         reads=[pcb[p], cc], writes=[accb[ct]])
                    k.op("act", lambda e, ct=ct, p=p: e.activation(out=ysq[p][:], in_=acc[:, ct, :], func=AF.Square), reads=[accb[ct]], writes=[ysqb[p]])
                    k.op("pe", lambda e, ct=ct: e.matmul(s1[:, :], lhsT=ones[:], rhs=acc[:, ct, :], start=(ct == 0), stop=(ct == 3)),
                         reads=[cc, accb[ct]], writes=[s1b])
                    k.op("pe", lambda e, ct=ct, p=p: e.matmul(s2[:, :], lhsT=ones[:], rhs=ysq[p][:], start=(ct == 0), stop=(ct == 3)),
                         reads=[cc, ysqb[p]], writes=[s2b])
                k.op("act", lambda e: e.activation(out=stt[:, 0, :], in_=s1[:, :], func=AF.Copy, scale=1.0 / 512), reads=[s1b], writes=[sttb])
                k.op("dve", lambda e: e.tensor_tensor(out=stt[:, 1, :], in0=stt[:, 0, :], in1=stt[:, 0, :], op=ALU.mult), reads=[sttb], writes=[sttb])
                k.op("dve", lambda e: e.scalar_tensor_tensor(out=stt[:, 1, :], in0=s2[:, :], scalar=1.0 / 512, in1=stt[:, 1, :],
                                                             op0=ALU.mult, op1=ALU.subtract), reads=[s2b, sttb], writes=[sttb])
                k.op("act", lambda e: e.activation(out=stt[:, 3, :], in_=stt[:, 1, :], func=AF.Sqrt, bias=self.cst[:, 0:1], scale=1.0),
                     reads=[sttb, self.cb], writes=[sttb])
                k.op("dve", lambda e: e.reciprocal(out=stt[:, 2, :], in_=stt[:, 3, :]), reads=[sttb], writes=[sttb])
                for ct in range(4):
                    p = ct % 2
                    k.op("dve", lambda e, ct=ct, p=p: e.tensor_tensor(out=yn[p][:], in0=acc[:, ct, :], in1=stt[:, 0, :], op=ALU.subtract),
                         reads=[accb[ct], sttb], writes=[ynb[p]])
                    k.op("pool", lambda e, p=p: e.tensor_tensor(out=yn[p][:], in0=yn[p][:], in1=stt[:, 2, :], op=ALU.mult),
                         reads=[sttb, ynb[p]], writes=[ynb[p]])
                    k.op("act", lambda e, ct=ct, p=p: e.activation(out=oc[p][:], in_=yn[p][:], func=AF.Silu, scale=cvec[:, 1, ct:ct + 1],
                                                                   bias=cvec[:, 2, ct:ct + 1]), reads=[ynb[p], cc], writes=[ocb[p]])
                    k.dma("sp", self.brT[2, ct, :, toks], oc[p][:], reads=[ocb[p]], writes=self.brT_b[2][G * 4:(G + 1) * 4])

    def phase_merge(self, l):
        k = self.k
        for dh in range(2):
            with ExitStack() as st:
                Wg = self.sb(st, "mWg", [128, 8, 3, 512], BF16)
                Wbr = self.sb(st, "mWbr", [128, 3, 4, 512], BF16)
                Wo = self.sb(st, "mWo", [128, 4, 1024], BF16)
                wb = Buf("mW")
                for b in range(3):
                    for kc in range(8):
                        c0 = C_MG + b * 1024 + dh * 512
                        k.dma("pool", Wg[:, kc, b, :], self.din["w_in"][l][kc * 128:(kc + 1) * 128, c0:c0 + 512], writes=[wb])
                    for c4 in range(4):
                        k.dma("pool", Wbr[:, b, c4, :], self.din["w_branch"][l][b][c4 * 128:(c4 + 1) * 128, dh * 512:(dh + 1) * 512], writes=[wb])
                for dc in range(4):
                    r0 = dh * 512 + dc * 128
                    k.dma("pool", Wo[:, dc, :], self.din["w_out"][l][r0:r0 + 128, :], writes=[wb])
                brg = [self.sb(st, "mbr%d" % i, [128, 3, 4, 512], BF16) for i in range(2)]
                brgb = [Buf("mbr%d" % i) for i in range(2)]
                mT = self.sb(st, "mmT", [128, 4, 512], BF16)
                mTb = [Buf("mT%d" % i) for i in range(4)]
                gate = [self.sb(st, "mgate%d" % i, [128, 512], F32) for i in range(2)]
                gateb = [Buf("mgate%d" % i) for i in range(2)]
                macc = self.sb(st, "mmacc", [128, 512], F32); maccb = Buf("macc")
                mtmp = self.sb(st, "mmtmp", [128, 512], F32); mtmpb = Buf("mtmp")
                ctx = self.upd_alloc(st, "m", self.din["nmlp"][l] if dh == 1 else None)
                pg = [self.ps(st, "mpg%d" % i, [128, 512], F32) for i in range(2)]
                pgb = [Buf("mpg%d" % i) for i in range(2)]
                pp = [self.ps(st, "mpp%d" % i, [128, 512], F32) for i in range(2)]
                ppb = [Buf("mpp%d" % i) for i in range(2)]
                po = [self.ps(st, "mpo%d" % i, [128, 512], F32) for i in range(2)]
                pob = [Buf("mpo%d" % i) for i in range(2)]
                if dh == 1:
                    ctx["ptr"] = self.ps(st, "mptr", [128, 1024], BF16)
                    ctx["ptr_b"] = Buf("mptr")
                it = 0
                for G in range(8):
                    toks = slice(G * 512, (G + 1) * 512)
                    hb = self.hT_b[G * 4:(G + 1) * 4]
                    bi = G % 2
                    for b in range(3):
                        k.dma("sp", brg[bi][:, b], self.brT[b, :, :, toks].rearrange("c p t -> p c t"),
                              reads=self.brT_b[b][G * 4:(G + 1) * 4], writes=[brgb[bi]])
                    for dc in range(4):
                        for b in range(3):
                            p = it % 2
                            it += 1
                            for kc in range(8):
                                k.op("pe", lambda e, kc=kc, b=b, dc=dc, p=p: e.matmul(pg[p][:, :], lhsT=Wg[:, kc, b, dc * 128:(dc + 1) * 128],
                                                                                     rhs=self.hT[:, kc, toks], start=(kc == 0), stop=(kc == 7)),
                                     reads=[wb] + hb, writes=[pgb[p]])
                            for c4 in range(4):
                                k.op("pe", lambda e, c4=c4, b=b, dc=dc, p=p: e.matmul(pp[p][:, :], lhsT=Wbr[:, b, c4, dc * 128:(dc + 1) * 128],
                                                                                     rhs=brg[bi][:, b, c4, :], start=(c4 == 0), stop=(c4 == 3)),
                                     reads=[wb, brgb[bi]], writes=[ppb[p]])
                            k.op("act", lambda e, p=p: e.activation(out=gate[p][:], in_=pg[p][:, :], func=AF.Sigmoid), reads=[pgb[p]], writes=[gateb[p]])
                            if b == 0:
                                k.op("dve", lambda e, p=p: e.tensor_tensor(out=macc[:], in0=pp[p][:, :], in1=gate[p][:], op=ALU.mult),
                                     reads=[ppb[p], gateb[p]], writes=[maccb])
                            else:
                                k.op("dve", lambda e, p=p: e.tensor_tensor(out=mtmp[:], in0=pp[p][:, :], in1=gate[p][:], op=ALU.mult),
                                     reads=[ppb[p], gateb[p]], writes=[mtmpb])
                                if b == 1:
                                    k.op("pool", lambda e: e.tensor_tensor(out=macc[:], in0=macc[:], in1=mtmp[:], op=ALU.add),
                                         reads=[mtmpb, maccb], writes=[maccb])
                                else:
                                    k.op("pool", lambda e, dc=dc: e.tensor_tensor(out=mT[:, dc, :], in0=macc[:], in1=mtmp[:], op=ALU.add),
                                         reads=[mtmpb, maccb], writes=[mTb[dc]])
                    for tt in range(4):
                        t = G * 4 + tt
                        for nh in range(2):
                            for dc in range(4):
                                k.op("pe", lambda e, nh=nh, dc=dc, tt=tt: e.matmul(po[nh][:, :], lhsT=mT[:, dc, tt * 128:(tt + 1) * 128],
                                                                                  rhs=Wo[:, dc, nh * 512:(nh + 1) * 512], start=(dc == 0), stop=(dc == 3)),
                                     reads=[wb, mTb[dc]], writes=[pob[nh]])
                        self.x_update(ctx, t, [po[0][:, :], po[1][:, :]], pob, "norm" if dh == 1 else None)
                k.barrier()


_PROG_CACHE = {}


def _get_prog(**kw):
    key = tuple(sorted((k, str(v)) for k, v in kw.items()))
    if key not in _PROG_CACHE:
        _PROG_CACHE[key] = Prog(**kw)
    return _PROG_CACHE[key]


def kernel(**inputs):
    inp = {k: np.asarray(v) for k, v in inputs.items()}
    x = np.ascontiguousarray(inp["x"], dtype=np.float32)
    shared = _host_inputs(inp)
    prog = _get_prog()
    in_maps = []
    for c in range(8):
        m = dict(shared)
        m["x"] = x[c]
        in_maps.append(m)
    res = run_bass_kernel_spmd(prog.nc, in_maps, core_ids=list(range(8)))
    return np.stack([np.asarray(r["out"], dtype=np.float32) for r in res.results], axis=0)
```

```python
import numpy as np
from contextlib import ExitStack
import concourse.bass as bass
import concourse.mybir as mybir
from concourse.bass_utils import run_bass_kernel_spmd

F32 = mybir.dt.float32
BF16 = mybir.dt.bfloat16
AF = mybir.ActivationFunctionType
ALU = mybir.AluOpType
AX = mybir.AxisListType

S = 4096
D = 1024
NT = 32
DEPTH = 2
IN_TOTAL = 6936
EPS = 1e-6
NEG = -30000.0
C_QN, C_KC, C_VC, C_KS, C_VS, C_KW, C_VW, C_GN = 0, 512, 640, 768, 896, 1024, 1152, 1280
C_QR, C_KR, C_VR, C_GR, C_CA, C_CB, C_MG = 1304, 1560, 1816, 2328, 2840, 3352, 3864

SAME_ENGINE_SYNC = True


class Buf:
    __slots__ = ("w", "r", "name", "wd")

    def __init__(self, name=""):
        self.w = None
        self.r = {}
        self.name = name
        self.wd = False


class _Sem:
    def __init__(self, sem, name):
        self.sem = sem
        self.count = 0
        self.name = name


class Eng(_Sem):
    def __init__(self, name, handle, sem):
        super().__init__(sem, name)
        self.h = handle
        self.waited = {}


class K:
    def __init__(self, nc, stack, n_dma_sems=64):
        self.nc = nc
        self.engs = {}
        for name, h in (("pe", nc.tensor), ("act", nc.scalar), ("dve", nc.vector),
                        ("pool", nc.gpsimd), ("sp", nc.sync)):
            sem = stack.enter_context(nc.semaphore("sem_" + name))
            self.engs[name] = Eng(name, h, sem)
        self.dsems = [_Sem(stack.enter_context(nc.semaphore("dsem%d" % i)), "d%d" % i)
                      for i in range(n_dma_sems)]
        self.dpool = {"pool": self.dsems[:n_dma_sems // 2], "sp": self.dsems[n_dma_sems // 2:]}
        self.dnext = {"pool": 0, "sp": 0}
        self.n_ops = 0
        self.muted = False

    def _wait_deps(self, E, reads, writes):
        deps = {}

        def need(tok):
            if tok is None:
                return
            s, v = tok
            if deps.get(s, 0) < v:
                deps[s] = v
        for b in reads:
            for t in b.w or ():
                need(t)
        for b in writes:
            for t in b.w or ():
                need(t)
            for t in b.r.values():
                need(t)
        for s, v in deps.items():
            if s is E and (E.name in ("pe", "sp") or not SAME_ENGINE_SYNC):
                continue
            if E.waited.get(s, 0) < v:
                E.h.wait_ge(s.sem, v)
                E.waited[s] = v

    def _mark(self, tok, reads, writes, is_dma=False):
        for b in reads:
            b.r[tok[0]] = tok
        for b in writes:
            if is_dma and b.w and getattr(b, "wd", False) and len(b.w) < 24:
                b.w = b.w + [tok]
            else:
                b.w = [tok]
            b.wd = is_dma
            b.r = {}

    def op(self, en, fn, reads=(), writes=()):
        if self.muted:
            return None
        E = self.engs[en]
        self._wait_deps(E, reads, writes)
        ins = fn(E.h)
        E.count += 1
        ins.then_inc(E.sem, 1)
        self._mark((E, E.count), reads, writes)
        self.n_ops += 1
        return ins

    def dma(self, en, out, in_, reads=(), writes=(), **kw):
        if self.muted:
            return None
        E = self.engs[en]
        self._wait_deps(E, reads, writes)
        pool_ = self.dpool[en]
        d = pool_[self.dnext[en]]
        self.dnext[en] = (self.dnext[en] + 1) % len(pool_)
        if d.count and E.waited.get(d, 0) < d.count:
            E.h.wait_ge(d.sem, d.count)
            E.waited[d] = d.count
        ins = E.h.dma_start(out=out, in_=in_, **kw)
        d.count += 16
        ins.then_inc(d.sem, 16)
        self._mark((d, d.count), reads, writes, is_dma=True)
        self.n_ops += 1
        return ins

    def barrier(self):
        allsems = list(self.engs.values()) + self.dsems
        for E in self.engs.values():
            for s in allsems:
                if s is E or s.count == 0:
                    continue
                if E.waited.get(s, 0) < s.count:
                    E.h.wait_ge(s.sem, s.count)
                    E.waited[s] = s.count


def _rel_bucket_np(dist):
    n = np.maximum(dist, 0)
    nf = np.maximum(n, 1).astype(np.float32)
    large = 16 + (np.log(nf / np.float32(16)) / np.float32(np.log(8.0)) * np.float32(16)).astype(np.int32)
    large = np.minimum(large, 31)
    return np.where(n < 16, n, large).astype(np.int64)


_CONST_CACHE = {}


def _host_consts():
    if _CONST_CACHE:
        return _CONST_CACHE
    c = {}
    i = np.arange(128)
    c["ident"] = np.eye(128, dtype=np.float32)
    c["i4"] = np.tile(np.eye(128, dtype=np.float32), (1, 4))
    mw = np.zeros((128, 5, 128), np.float32)
    jj, ii = np.meshgrid(i, i, indexing="ij")
    mw[:, 0, :] = (ii >= jj)
    mw[:, 1:4, :] = 1.0
    mw[:, 4, :] = (ii < jj)
    c["maskw"] = mw
    dist_w = 128 * np.arange(5)[None, :, None] + ii[:, None, :] - jj[:, None, :]
    c["_bucket_w"] = _rel_bucket_np(dist_w)
    m = np.arange(504)
    dist_c = i[None, :] - 16 * (m[:, None] - 248) - 31
    c["maskc"] = (dist_c >= 0).astype(np.float32)
    c["_bucket_c"] = _rel_bucket_np(dist_c)
    n = np.arange(256)
    s = np.arange(64)
    ov = ((16 * n[:, None] <= 64 * s[None, :] + 63) & (16 * n[:, None] + 31 >= 64 * s[None, :])).astype(np.float32)
    ov[255] = 0.0
    c["overlap"] = ov.reshape(2, 128, 64).transpose(1, 0, 2).copy()
    j = np.arange(126)
    sp = j[None, :] - 62
    cur = (i[:, None] >= 64).astype(np.int64)
    valid = sp <= cur
    forced = (sp == cur) | (sp == cur - 1)
    c["selvalid"] = valid.astype(np.float32)
    c["seladd"] = np.where(forced, 1e4, np.where(valid, 0.0, -1e4)).astype(np.float32)
    half = 32
    inv = (10000.0 ** (-np.arange(half, dtype=np.float32) / half)).astype(np.float32)
    pos = np.arange(S, dtype=np.float32)
    ang = (pos[:, None] * inv[None, :]).astype(np.float32)
    c["cos"] = np.cos(ang).astype(np.float32).reshape(NT, 128, 32).transpose(1, 0, 2).copy()
    c["sin"] = np.sin(ang).astype(np.float32).reshape(NT, 128, 32).transpose(1, 0, 2).copy()
    log_g = np.log1p(-np.exp2(-5.0 - np.arange(4, dtype=np.float32))).astype(np.float32)
    diff = i[None, :] - i[:, None]
    dec = np.where(diff[None] >= 0, np.exp(log_g[:, None, None] * np.maximum(diff[None], 0)), 0.0)
    c["decayT"] = dec.transpose(1, 0, 2).astype(np.float32).copy()
    xi = np.exp(log_g[:, None] * (i[None, :] + 1)).astype(np.float32)
    c["xi"] = np.ascontiguousarray(np.broadcast_to(xi[None, :, :], (64, 4, 128))).astype(np.float32)
    c["zeta"] = np.exp(log_g[None, :] * (127 - i[:, None])).astype(np.float32)
    gch = np.exp(log_g * 128).astype(np.float32)
    c["gch"] = np.ascontiguousarray(np.broadcast_to(gch[None, :], (64, 4))).astype(np.float32)
    c["ones"] = np.ones((128, 128), np.float32)
    c["rfull"] = (np.arange(S)[None, :] // 64 == np.arange(64)[:, None]).astype(np.float32)
    _CONST_CACHE.update(c)
    return c


CONST_SHAPES = {
    "ident": [128, 128], "i4": [128, 512], "maskw": [128, 5, 128], "maskc": [504, 128],
    "overlap": [128, 2, 64], "selvalid": [128, 126], "seladd": [128, 126],
    "cos": [128, NT, 32], "sin": [128, NT, 32], "decayT": [128, 4, 128], "xi": [64, 4, 128],
    "zeta": [128, 4], "gch": [64, 4], "ones": [128, 128], "rfull": [64, S],
    "bias_w": [128, 5, 8, 128], "bias_c": [504, 8, 128], "b31": [128, 8],
}

W_SHAPES = {
    "w_in": [DEPTH, D, IN_TOTAL], "w_branch": [DEPTH, 3, 512, D], "w_out": [DEPTH, D, D],
    "w_ff1": [DEPTH, D, 4 * D], "w_ff2": [DEPTH, 4 * D, D],
    "cmp_w1_k": [DEPTH, 32, 64, 128], "cmp_w1_v": [DEPTH, 32, 64, 128],
    "cmp_w2_k": [DEPTH, 128, 64], "cmp_w2_v": [DEPTH, 128, 64],
    "cmp_pe_kT": [DEPTH, 64, 32], "cmp_pe_vT": [DEPTH, 64, 32],
    "nmix": [DEPTH, 128, D], "nmlp": [DEPTH, 128, D], "nfin": [128, D],
    "retgn": [DEPTH, 128, 512],
    "convw": [DEPTH, 128, 4, 31], "convb": [DEPTH, 128, 4], "convg": [DEPTH, 128, 4], "convbb": [DEPTH, 128, 4],
}


def _host_inputs(inp):
    c = _host_consts()
    f = lambda a: np.ascontiguousarray(a, dtype=np.float32)
    out = {k: f(v) for k, v in c.items() if not k.startswith("_")}
    rt = f(inp["rel_table"])
    out["bias_w"] = f(rt[c["_bucket_w"]].transpose(0, 1, 3, 2))
    out["bias_c"] = f(rt[c["_bucket_c"]].transpose(0, 2, 1))
    out["b31"] = f(np.broadcast_to(rt[31][None, :], (128, 8)))
    for kname in ("w_in", "w_branch", "w_out", "w_ff1", "w_ff2", "cmp_w1_k", "cmp_w1_v", "cmp_w2_k", "cmp_w2_v"):
        out[kname] = f(inp[kname])
    out["cmp_pe_kT"] = f(np.transpose(inp["cmp_pe_k"], (0, 2, 1)))
    out["cmp_pe_vT"] = f(np.transpose(inp["cmp_pe_v"], (0, 2, 1)))
    out["nmix"] = f(np.broadcast_to(inp["norm_mix"][:, None, :], (DEPTH, 128, D)))
    out["nmlp"] = f(np.broadcast_to(inp["norm_mlp"][:, None, :], (DEPTH, 128, D)))
    out["nfin"] = f(np.broadcast_to(inp["norm_final"][None, :], (128, D)))
    out["retgn"] = f(np.broadcast_to(inp["ret_gn"][:, None, :], (DEPTH, 128, 512)))
    out["convw"] = f(np.transpose(inp["conv_w"].reshape(DEPTH, 31, 4, 128), (0, 3, 2, 1)))
    for a, b in (("convb", "conv_b"), ("convg", "conv_ln_g"), ("convbb", "conv_ln_b")):
        out[a] = f(np.transpose(inp[b].reshape(DEPTH, 4, 128), (0, 2, 1)))
    return out


class _Stop(Exception):
    pass


class Prog:
    def __init__(self, n_layers=DEPTH, phases=("nsa", "ret", "conv", "merge", "ffn"), dbg=False):
        self.n_layers = n_layers
        self.phases = phases
        self.dbg = dbg
        nc = self.nc = bass.Bass("TRN2", target_bir_lowering=False)
        self.din = {}
        self.din["x"] = nc.dram_tensor("x", [S, D], F32, kind="ExternalInput").ap()
        for name, shp in list(CONST_SHAPES.items()) + list(W_SHAPES.items()):
            self.din[name] = nc.dram_tensor(name, shp, F32, kind="ExternalInput").ap()
        self.out = nc.dram_tensor("out", [S, D], F32, kind="ExternalOutput").ap()
        skind = "ExternalOutput" if dbg else "Internal"
        self.xres = nc.dram_tensor("xres", [S, D], F32, kind=skind).ap()
        self.brT = nc.dram_tensor("brT", [3, 4, 128, S], BF16, kind=skind).ap()
        self.ebc_d = nc.dram_tensor("ebc_d", [504, 8, 128], BF16, kind=skind).ap()
        self.dbg_d = nc.dram_tensor("dbg_d", [128, 2048], F32, kind=skind).ap()
        self.ocd = nc.dram_tensor("ocd", [NT, 128, 512], F32, kind="Internal").ap()
        self.ocd_b = [Buf("ocd%d" % t) for t in range(NT)]
        self.xres_b = [Buf("xres%d" % t) for t in range(NT)]
        self.brT_b = [[Buf("brT%d_%d" % (b, t)) for t in range(NT)] for b in range(3)]
        self.ebc_db = Buf("ebc_d")
        self.out_b = Buf("out")
        with ExitStack() as st:
            self.st = st
            self.k = K(nc, st)
            self.build()

    def sb(self, st, name, shape, dt):
        self._uid = getattr(self, "_uid", 0) + 1
        return st.enter_context(self.nc.sbuf_tensor("s%d_%s" % (self._uid, name), shape, dt))

    def ps(self, st, name, shape, dt):
        self._uid = getattr(self, "_uid", 0) + 1
        return st.enter_context(self.nc.psum_tensor("p%d_%s" % (self._uid, name), shape, dt))

    def chk(self, n):
        import os
        v = os.environ.get("RSTOP")
        if v is not None and int(v) == n:
            self.k.muted = True

    def load_const(self, dst, src, buf, eng="sp"):
        self.k.dma(eng, dst, src, writes=[buf])

    def build(self):
        k, nc, st = self.k, self.nc, self.st
        self.hT = self.sb(st, "hT_all", [128, 8, S], BF16)
        self.hT_b = [Buf("hT%d" % t) for t in range(NT)]
        self.ident = self.sb(st, "ident", [128, 128], BF16)
        self.i4 = self.sb(st, "i4", [128, 512], BF16)
        self.identf = self.sb(st, "identf", [128, 128], F32)
        self.cst = self.sb(st, "cst", [128, 4], F32)
        self.EB = self.sb(st, "EB", [128, 5, 8, 128], BF16)
        self.cb = Buf("consts")
        k.dma("pool", self.ident[:], self.din["ident"][:, :], writes=[self.cb])
        k.dma("pool", self.i4[:], self.din["i4"][:, :], writes=[self.cb])
        k.dma("sp", self.identf[:], self.din["ident"][:, :], writes=[self.cb])
        k.op("dve", lambda e: e.memset(self.cst[:, 0:1], EPS), writes=[self.cb])
        k.op("dve", lambda e: e.memset(self.cst[:, 1:2], 0.0), writes=[self.cb])
        k.op("dve", lambda e: e.memset(self.cst[:, 2:3], 1.0), writes=[self.cb])
        self.phase_bias_tables()
        k.barrier()
        self.phase0()
        k.barrier()
        for l in range(self.n_layers):
            last = (l == DEPTH - 1)
            for ph, fn in (("nsa", lambda: self.phase_nsa(l)), ("ret", lambda: self.phase_ret(l)), ("conv", lambda: self.phase_conv(l)),
                           ("merge", lambda: self.phase_merge(l)), ("ffn", lambda: self.phase_ffn(l, last))):
                if ph in self.phases:
                    with nc.named_scope("%s%d" % (ph, l)):
                        fn()
                        k.muted = False
                        k.barrier()
        k.barrier()

    def rms_alloc(self, st, tag):
        r = {}
        r["junk"] = self.sb(st, "rjunk" + tag, [128, D], BF16)
        r["ss"] = self.sb(st, "rss" + tag, [128, 4], F32)
        r["hb2"] = [self.sb(st, "rhb%d" % i + tag, [128, D], BF16) for i in range(2)]
        r["hbb2"] = [Buf("rmshb%d" % i + tag) for i in range(2)]
        r["i"] = 0
        r["b"] = Buf("rms" + tag)
        return r

    def rms_stats(self, r, src, src_bufs):
        k = self.k
        ss = r["ss"]
        k.op("dve", lambda e: e.memset(ss[:, 0:1], 0.0), writes=[r["b"]])
        k.op("act", lambda e: e.activation(out=r["junk"][:], in_=src, func=AF.Square, accum_out=ss[:, 0:1]),
             reads=list(src_bufs) + [r["b"]], writes=[r["b"]])
        k.op("act", lambda e: e.activation(out=ss[:, 1:2], in_=ss[:, 0:1], func=AF.Sqrt, bias=self.cst[:, 0:1], scale=1.0 / D),
             reads=[r["b"], self.cb], writes=[r["b"]])
        k.op("dve", lambda e: e.reciprocal(out=ss[:, 2:3], in_=ss[:, 1:2]), reads=[r["b"]], writes=[r["b"]])
        return ss[:, 2:3]

    def rms_to_hT(self, r, src, src_bufs, gain, gain_buf, t, ptr, ptr_b):
        i = self.rms_part1(r, src, src_bufs, gain, gain_buf)
        self.rms_part2(r, i, t, ptr, ptr_b)

    def rms_part1(self, r, src, src_bufs, gain, gain_buf):
        k = self.k
        rstd = self.rms_stats(r, src, src_bufs)
        i = r["i"] = (r["i"] + 1) % 2
        hb = r["hb2"][i]
        k.op("dve", lambda e: e.scalar_tensor_tensor(out=hb[:], in0=src, scalar=rstd, in1=gain, op0=ALU.mult, op1=ALU.mult),
             reads=list(src_bufs) + [r["b"], gain_buf], writes=[r["hbb2"][i]])
        return i

    def rms_part2(self, r, i, t, ptr, ptr_b):
        k = self.k
        hb = r["hb2"][i]
        for c in range(8):
            k.op("pe", lambda e, c=c: e.transpose(ptr[:, c * 128:(c + 1) * 128], hb[:, c * 128:(c + 1) * 128], self.ident[:]),
                 reads=[r["hbb2"][i], self.cb], writes=[ptr_b])
        k.op("act", lambda e: e.activation(out=self.hT[:, :, t * 128:(t + 1) * 128],
                                           in_=ptr[:, :].rearrange("p (c q) -> p c q", c=8), func=AF.Copy),
             reads=[ptr_b], writes=[self.hT_b[t]])

    def phase_bias_tables(self):
        k = self.k
        with ExitStack() as st:
            bw = self.sb(st, "bw", [128, 5, 8, 128], F32)
            mw = self.sb(st, "mw", [128, 5, 128], F32)
            b31 = self.sb(st, "b31", [128, 8], F32)
            bb = Buf("bw")
            k.dma("sp", bw[:], self.din["bias_w"][:, :, :, :], writes=[bb])
            k.dma("sp", mw[:], self.din["maskw"][:, :, :], writes=[bb])
            k.dma("sp", b31[:], self.din["b31"][:, :], writes=[bb])
            for off in range(5):
                k.op("dve", lambda e, off=off: e.tensor_tensor(out=bw[:, off], in0=bw[:, off],
                                                               in1=b31[:, :].unsqueeze(2).to_broadcast([128, 8, 128]), op=ALU.subtract),
                     reads=[bb], writes=[bb])
                k.op("dve", lambda e, off=off: e.tensor_tensor(out=bw[:, off], in0=bw[:, off],
                                                               in1=mw[:, off:off + 1, :].to_broadcast([128, 8, 128]), op=ALU.mult),
                     reads=[bb], writes=[bb])
                k.op("dve", lambda e, off=off: e.tensor_scalar(out=mw[:, off, :], in0=mw[:, off, :], scalar1=-NEG, scalar2=NEG, op0=ALU.mult, op1=ALU.add),
                     reads=[bb], writes=[bb])
                k.op("dve", lambda e, off=off: e.tensor_tensor(out=self.EB[:, off], in0=bw[:, off],
                                                               in1=mw[:, off:off + 1, :].to_broadcast([128, 8, 128]), op=ALU.add),
                     reads=[bb], writes=[self.cb])
            bc = self.sb(st, "bc", [126, 8, 128], F32)
            mc = self.sb(st, "mc", [126, 128], F32)
            bcb = self.sb(st, "bcb", [126, 8, 128], BF16)
            cbuf = Buf("bc")
            for r4 in range(4):
                rows = slice(r4 * 126, (r4 + 1) * 126)
                k.dma("sp", bc[:], self.din["bias_c"][rows, :, :], writes=[cbuf])
                k.dma("sp", mc[:], self.din["maskc"][rows, :], writes=[cbuf])
                k.op("dve", lambda e: e.tensor_tensor(out=bc[:], in0=bc[:], in1=b31[0:126, :].unsqueeze(2).to_broadcast([126, 8, 128]),
                                                      op=ALU.subtract), reads=[cbuf, bb], writes=[cbuf])
                k.op("act", lambda e: e.activation(out=bc[:], in_=bc[:], func=AF.Exp), reads=[cbuf], writes=[cbuf])
                k.op("dve", lambda e: e.tensor_tensor(out=bcb[:], in0=bc[:], in1=mc[:, :].unsqueeze(1).to_broadcast([126, 8, 128]),
                                                      op=ALU.mult), reads=[cbuf], writes=[cbuf])
                k.dma("sp", self.ebc_d[rows, :, :], bcb[:], reads=[cbuf], writes=[self.ebc_db])
            k.barrier()

    def phase0(self):
        k = self.k
        with ExitStack() as st:
            r = self.rms_alloc(st, "p0")
            gain = self.sb(st, "gain0", [128, D], F32)
            gb = Buf("gain0")
            k.dma("sp", gain[:], self.din["nmix"][0], writes=[gb])
            xt = [self.sb(st, "p0x%d" % i, [128, D], F32) for i in range(2)]
            xb = [Buf("p0x%d" % i) for i in range(2)]
            ptr = self.ps(st, "p0tr", [128, 1024], BF16)
            ptr_b = Buf("p0tr")
            for t in range(NT):
                i = t % 2
                k.dma("sp", xt[i][:], self.din["x"][t * 128:(t + 1) * 128, :], writes=[xb[i]])
                k.dma("pool", self.xres[t * 128:(t + 1) * 128, :], xt[i][:], reads=[xb[i]], writes=[self.xres_b[t]])
                self.rms_to_hT(r, xt[i][:], [xb[i]], gain[:], gb, t, ptr, ptr_b)

    def load_w(self, dst, src, buf, kc):
        for c in range(kc):
            self.k.dma("pool", dst[:, c, :], src[c * 128:(c + 1) * 128, :], writes=[buf])

    def x_update(self, ctx, t, psum_halves, psum_bufs, hook):
        k = self.k
        i = ctx["i"] = (ctx.get("i", 0) + 1) % 2
        xt, xb = ctx["xt"][i], ctx["xb"][i]
        k.dma("sp", xt[:], self.xres[t * 128:(t + 1) * 128, :], reads=[self.xres_b[t]], writes=[xb])
        for h in range(2):
            k.op("dve", lambda e, h=h: e.tensor_tensor(out=xt[:, h * 512:(h + 1) * 512], in0=psum_halves[h],
                                                       in1=xt[:, h * 512:(h + 1) * 512], op=ALU.add),
                 reads=[psum_bufs[h], xb], writes=[xb])
        if hook != "final":
            k.dma("pool", self.xres[t * 128:(t + 1) * 128, :], xt[:], reads=[xb], writes=[self.xres_b[t]])
        if hook is None:
            return
        r = ctx["rms"]
        if hook == "final":
            rstd = self.rms_stats(r, xt[:], [xb])
            ot = ctx["ot"]
            k.op("dve", lambda e: e.scalar_tensor_tensor(out=ot[:], in0=xt[:], scalar=rstd, in1=ctx["gain"][:], op0=ALU.mult, op1=ALU.mult),
                 reads=[xb, r["b"], ctx["gain_b"]], writes=[ctx["ot_b"]])
            k.dma("pool", self.out[t * 128:(t + 1) * 128, :], ot[:], reads=[ctx["ot_b"]], writes=[self.out_b])
        else:
            i2 = self.rms_part1(r, xt[:], [xb], ctx["gain"][:], ctx["gain_b"])
            self.upd_flush(ctx)
            ctx["pending"] = (i2, t)

    def upd_flush(self, ctx):
        p = ctx.pop("pending", None)
        if p is not None:
            self.rms_part2(ctx["rms"], p[0], p[1], ctx["ptr"], ctx["ptr_b"])

    def upd_alloc(self, st, tag, gain_src, final=False):
        ctx = {}
        ctx["xt"] = [self.sb(st, "ux%s%d" % (tag, i), [128, D], F32) for i in range(2)]
        ctx["xb"] = [Buf("ux%d" % i) for i in range(2)]
        if gain_src is not None:
            ctx["rms"] = self.rms_alloc(st, "u" + tag)
            ctx["gain"] = self.sb(st, "ug" + tag, [128, D], F32)
            ctx["gain_b"] = Buf("ug")
            self.k.dma("sp", ctx["gain"][:], gain_src, writes=[ctx["gain_b"]])
            if final:
                ctx["ot"] = self.sb(st, "uo" + tag, [128, D], F32)
                ctx["ot_b"] = Buf("uo")
        return ctx

    def phase_ffn(self, l, last):
        k = self.k
        for fh in range(2):
            with ExitStack() as st:
                W1 = self.sb(st, "W1", [128, 8, 2048], BF16)
                W2 = self.sb(st, "W2", [128, 16, 1024], BF16)
                w1b, w2b = Buf("W1"), Buf("W2")
                self.load_w(W1, self.din["w_ff1"][l][:, fh * 2048:(fh + 1) * 2048], w1b, 8)
                self.load_w(W2, self.din["w_ff2"][l][fh * 2048:(fh + 1) * 2048, :], w2b, 16)
                actT = self.sb(st, "actT", [128, 16, 512], BF16)
                act_b = [Buf("act%d" % i) for i in range(16)]
                rl = [self.sb(st, "rl%d" % i, [128, 512], F32) for i in range(2)]
                rl_b = [Buf("rl%d" % i) for i in range(2)]
                hook = None
                gain_src = None
                if fh == 1:
                    hook = "final" if last else "norm"
                    gain_src = self.din["nfin"][:, :] if last else self.din["nmix"][l + 1]
                ctx = self.upd_alloc(st, "f", gain_src, final=(fh == 1 and last))
                pb = [self.ps(st, "fpb%d" % i, [128, 512], F32) for i in range(6)]
                pbb = [Buf("fpb%d" % i) for i in range(6)]
                if hook == "norm":
                    ctx["ptr"] = self.ps(st, "fptr", [128, 1024], BF16)
                    ctx["ptr_b"] = Buf("fptr")
                for G in range(8):
                    toks = slice(G * 512, (G + 1) * 512)
                    hb = self.hT_b[G * 4:(G + 1) * 4]
                    for fc in range(16):
                        p = fc % 2
                        for kc in range(8):
                            k.op("pe", lambda e, kc=kc, fc=fc, p=p: e.matmul(pb[p][:, :], lhsT=W1[:, kc, fc * 128:(fc + 1) * 128],
                                                                               rhs=self.hT[:, kc, toks], start=(kc == 0), stop=(kc == 7)),
                                 reads=[w1b] + hb, writes=[pbb[p]])
                        k.op("act", lambda e, p=p: e.activation(out=rl[p][:], in_=pb[p][:, :], func=AF.Relu), reads=[pbb[p]], writes=[rl_b[p]])
                        k.op("dve", lambda e, p=p, fc=fc: e.tensor_tensor(out=actT[:, fc, :], in0=rl[p][:], in1=rl[p][:], op=ALU.mult),
                             reads=[rl_b[p]], writes=[act_b[fc]])
                    for tt in range(4):
                        t = G * 4 + tt
                        pp = [2 + 2 * (tt % 2), 3 + 2 * (tt % 2)]
                        for nh in range(2):
                            for fc in range(16):
                                k.op("pe", lambda e, nh=nh, fc=fc, tt=tt: e.matmul(pb[pp[nh]][:, :], lhsT=actT[:, fc, tt * 128:(tt + 1) * 128],
                                                                                  rhs=W2[:, fc, nh * 512:(nh + 1) * 512], start=(fc == 0), stop=(fc == 15)),
                                     reads=[w2b, act_b[fc]], writes=[pbb[pp[nh]]])
                        self.x_update(ctx, t, [pb[pp[0]][:, :], pb[pp[1]][:, :]], [pbb[pp[0]], pbb[pp[1]]], hook)
                self.upd_flush(ctx)
                k.barrier()

    def phase_nsa(self, l):
        k = self.k
        win = self.din["w_in"][l]
        with ExitStack() as st:
            Wq = self.sb(st, "nWq", [128, 8, 512], BF16); wqb = Buf("nWq")
            self.load_w(Wq, win[:, C_QN:C_QN + 512], wqb, 8)
            ksT = self.sb(st, "nksT", [128, 2, S], BF16); ksTb = Buf("ksT")
            kwT = self.sb(st, "nkwT", [64, 2, S], BF16); kwTb = Buf("kwT")
            vsa = self.sb(st, "nvsa", [128, NT, 2, 65], BF16); vsab = Buf("vsa")
            vwa = self.sb(st, "nvwa", [128, NT, 2, 65], BF16); vwab = Buf("vwa")
            gn = self.sb(st, "ngn", [128, NT, 24], F32); gnb = Buf("gn")
            kcmpT = self.sb(st, "nkcmpT", [64, 2, 256], BF16); kcmpb = Buf("kcmpT")
            VC = self.sb(st, "nVC", [128, 2, 2, 129], BF16); VCb = Buf("VC")
            selv = self.sb(st, "nselv", [128, 126], F32)
            sela = self.sb(st, "nsela", [128, 126], F32)
            ovl = self.sb(st, "novl", [128, 2, 64], F32)
            ncb = Buf("nconst")
            k.dma("sp", selv[:], self.din["selvalid"], writes=[ncb])
            k.dma("sp", sela[:], self.din["seladd"], writes=[ncb])
            k.dma("sp", ovl[:], self.din["overlap"], writes=[ncb])
            A = [self.ps(st, "nA%d" % i, [128, 512], F32) for i in range(4)]
            Ab = [Buf("nA%d" % i) for i in range(4)]
            for g in range(2):
                k.dma("pool", ksT[64:128, g, :], self.din["rfull"], writes=[ksTb])
            OC = [self.ps(st, "nOC%d" % i, [128, 512], F32) for i in range(2)]
            OCb = [Buf("nOC%d" % i) for i in range(2)]
            OS = self.ps(st, "nOS", [128, 512], F32); OSb = Buf("OS")
            OW = self.ps(st, "nOW", [128, 512], F32); OWb = Buf("OW")
            Q = OS; Qb = OSb
            R0bf = OS[:, :].bitcast(BF16)
            R1bf = OW[:, :].bitcast(BF16)
            k.op("dve", lambda e: e.memset(vsa[:], 1.0), writes=[vsab])
            k.op("dve", lambda e: e.memset(vwa[:], 1.0), writes=[vwab])
            k.op("dve", lambda e: e.memset(kcmpT[:], 0.0), writes=[kcmpb])
            k.op("dve", lambda e: e.memset(VC[:], 0.0), writes=[VCb])
            for j in range(2):
                for g in range(2):
                    k.op("dve", lambda e, j=j, g=g: e.memset(VC[:, j, g, 64:65], 1.0), writes=[VCb])
                    k.op("dve", lambda e, j=j, g=g: e.tensor_copy(out=VC[:, j, g, 65:129], in_=ovl[:, j, :]), reads=[ncb], writes=[VCb])
            with ExitStack() as st2:
                Wk = self.sb(st2, "nWk", [128, 8, 512], BF16); wkb = Buf("nWk")
                for i, c0 in enumerate((C_KC, C_VC, C_KS, C_KW)):
                    for kc in range(8):
                        k.dma("pool", Wk[:, kc, i * 128:(i + 1) * 128], win[kc * 128:(kc + 1) * 128, c0:c0 + 128], writes=[wkb])
                Wtm = self.sb(st2, "nWtm", [128, 8, 280], BF16); wtb = Buf("nWtm")
                for (o, c0, n) in ((0, C_VS, 128), (128, C_VW, 128), (256, C_GN, 24)):
                    for kc in range(8):
                        k.dma("pool", Wtm[:, kc, o:o + n], win[kc * 128:(kc + 1) * 128, c0:c0 + n], writes=[wtb])
                w1 = [self.sb(st2, "nw1%d" % i, [64, 32, 128], BF16) for i in range(2)]
                w2k = self.sb(st2, "nw2k", [128, 64], BF16)
                w2v = self.sb(st2, "nw2v", [128, 64], BF16)
                peT = [self.sb(st2, "npeT%d" % i, [64, 32], BF16) for i in range(2)]
                cwb = Buf("cmpw")
                for i, nm in enumerate(("cmp_w1_k", "cmp_w1_v")):
                    k.dma("pool", w1[i][:], self.din[nm][l].rearrange("l d f -> d l f"), writes=[cwb])
                k.dma("pool", w2k[:], self.din["cmp_w2_k"][l], writes=[cwb])
                k.dma("pool", w2v[:], self.din["cmp_w2_v"][l], writes=[cwb])
                k.dma("pool", peT[0][:], self.din["cmp_pe_kT"][l], writes=[cwb])
                k.dma("pool", peT[1][:], self.din["cmp_pe_vT"][l], writes=[cwb])
                for t in range(NT):
                    tk = slice(t * 128, (t + 1) * 128)
                    P = A[t % 2]; PB = Ab[t % 2]
                    for kc in range(8):
                        k.op("pe", lambda e, kc=kc, P=P: e.matmul(P[:, 0:280], lhsT=self.hT[:, kc, tk], rhs=Wtm[:, kc, :], start=(kc == 0), stop=(kc == 7)),
                             reads=[wtb, self.hT_b[t]], writes=[PB])
                    k.op("act", lambda e, P=P, t=t: e.activation(out=vsa[:, t, :, 0:64], in_=P[:, 0:128].rearrange("p (g d) -> p g d", g=2), func=AF.Copy),
                         reads=[PB], writes=[vsab])
                    k.op("act", lambda e, P=P, t=t: e.activation(out=vwa[:, t, :, 0:64], in_=P[:, 128:256].rearrange("p (g d) -> p g d", g=2), func=AF.Copy),
                         reads=[PB], writes=[vwab])
                    k.op("act", lambda e, P=P, t=t: e.activation(out=gn[:, t, :], in_=P[:, 256:280], func=AF.Sigmoid), reads=[PB], writes=[gnb])
                it = 0
                for (dst, dstb, wi) in ((ksT, ksTb, 2), (kwT, kwTb, 3)):
                    for g in range(2):
                        for G in range(8):
                            toks = slice(G * 512, (G + 1) * 512)
                            P = OC[it % 2]; PB = OCb[it % 2]; it += 1
                            for kc in range(8):
                                k.op("pe", lambda e, kc=kc, P=P, wi=wi, g=g: e.matmul(P[0:64, :], lhsT=Wk[:, kc, wi * 128 + g * 64:wi * 128 + g * 64 + 64],
                                                                                     rhs=self.hT[:, kc, toks], start=(kc == 0), stop=(kc == 7)),
                                     reads=[wkb] + self.hT_b[G * 4:(G + 1) * 4], writes=[PB])
                            k.op("act", lambda e, P=P, dst=dst, g=g: e.activation(out=dst[0:64, g, toks], in_=P[0:64, :], func=AF.Copy), reads=[PB], writes=[dstb])
                cT = [self.sb(st2, "ncT%d" % i, [64, S], BF16) for i in range(2)]
                cTb = [Buf("ncT%d" % i) for i in range(2)]
                hx = self.sb(st2, "nhx", [128, 4, 256], F32); hxb = Buf("nhx")
                hidb = self.sb(st2, "nhidb", [128, 256], BF16); hidbb = Buf("nhidb")
                cbias = self.sb(st2, "ncbias", [128, 2], F32); cbb = Buf("ncbias")
                for kv in range(2):
                    for lidx in range(32):
                        k.op("pe", lambda e, kv=kv, lidx=lidx: e.matmul(Q[:, kv:kv + 1], lhsT=w1[kv][:, lidx, :], rhs=peT[kv][:, lidx:lidx + 1],
                                                                       start=(lidx == 0), stop=(lidx == 31)), reads=[cwb], writes=[Qb])
                k.op("act", lambda e: e.activation(out=cbias[:], in_=Q[:, 0:2], func=AF.Copy), reads=[Qb], writes=[cbb])
                for g in range(2):
                    for kv in range(2):
                        for G in range(8):
                            toks = slice(G * 512, (G + 1) * 512)
                            P = OC[it % 2]; PB = OCb[it % 2]; it += 1
                            for kc in range(8):
                                k.op("pe", lambda e, kc=kc, P=P, kv=kv, g=g: e.matmul(P[0:64, :], lhsT=Wk[:, kc, kv * 128 + g * 64:kv * 128 + g * 64 + 64],
                                                                                     rhs=self.hT[:, kc, toks], start=(kc == 0), stop=(kc == 7)),
                                     reads=[wkb] + self.hT_b[G * 4:(G + 1) * 4], writes=[PB])
                            k.op("act", lambda e, P=P, kv=kv: e.activation(out=cT[kv][:, toks], in_=P[0:64, :], func=AF.Copy), reads=[PB], writes=[cTb[kv]])
                    for kv in range(2):
                        P = A[kv]; PB = Ab[kv]
                        for lidx in range(32):
                            k.op("pe", lambda e, kv=kv, lidx=lidx, P=P: e.matmul(P[:, 0:255], lhsT=w1[kv][:, lidx, :], rhs=cT[kv][:, lidx:lidx + 16 * 254 + 1:16],
                                                                                start=(lidx == 0), stop=(lidx == 31)), reads=[cwb, cTb[kv]], writes=[PB])
                        x_ = hx[:, 0, 0:255]
                        k.op("act", lambda e, P=P, kv=kv: e.activation(out=x_, in_=P[:, 0:255], func=AF.Identity, bias=cbias[:, kv:kv + 1], scale=1.0),
                             reads=[PB, cbb], writes=[hxb])
                        k.op("dve", lambda e: e.tensor_tensor(out=hx[:, 1, 0:255], in0=x_, in1=x_, op=ALU.mult), reads=[hxb], writes=[hxb])
                        k.op("dve", lambda e: e.tensor_scalar(out=hx[:, 1, 0:255], in0=hx[:, 1, 0:255], scalar1=0.044715, scalar2=1.0, op0=ALU.mult, op1=ALU.add),
                             reads=[hxb], writes=[hxb])
                        k.op("dve", lambda e: e.tensor_tensor(out=hx[:, 2, 0:255], in0=hx[:, 1, 0:255], in1=x_, op=ALU.mult), reads=[hxb], writes=[hxb])
                        k.op("act", lambda e: e.activation(out=hx[:, 3, 0:255], in_=hx[:, 2, 0:255], func=AF.Sigmoid, scale=1.5957691216057308),
                             reads=[hxb], writes=[hxb])
                        k.op("dve", lambda e: e.tensor_tensor(out=hidb[:, 0:255], in0=hx[:, 3, 0:255], in1=x_, op=ALU.mult), reads=[hxb], writes=[hidbb])
                        if kv == 0:
                            k.op("pe", lambda e: e.matmul(Q[0:64, 0:255], lhsT=w2k[:], rhs=hidb[:, 0:255], start=True, stop=True), reads=[cwb, hidbb], writes=[Qb])
                            k.op("act", lambda e, g=g: e.activation(out=kcmpT[:, g, 0:255], in_=Q[0:64, 0:255], func=AF.Copy), reads=[Qb], writes=[kcmpb])
                        else:
                            for j in range(2):
                                nn = 128 if j == 0 else 127
                                k.op("pe", lambda e, j=j, nn=nn: e.matmul(Q[0:nn, j * 64:(j + 1) * 64], lhsT=hidb[:, j * 128:j * 128 + nn], rhs=w2v[:],
                                                                         start=True, stop=True), reads=[cwb, hidbb], writes=[Qb])
                                k.op("act", lambda e, j=j, nn=nn, g=g: e.activation(out=VC[0:nn, j, g, 0:64], in_=Q[0:nn, j * 64:(j + 1) * 64], func=AF.Copy),
                                     reads=[Qb], writes=[VCb])
            k.barrier()
            snegAll = self.sb(st, "nsnegAll", [128, NT * 2, 128], BF16); snegAllb = [Buf("sneg%d" % i) for i in range(NT * 2)]
            k.op("dve", lambda e: e.memset(snegAll[:], 0.0), writes=snegAllb)
            bank = {"A0": (A[0], Ab[0]), "A1": (A[1], Ab[1]), "A2": (A[2], Ab[2]), "A3": (A[3], Ab[3]),
                    "C0": (OC[0], OCb[0]), "C1": (OC[1], OCb[1]), "S": (OS, OSb), "W": (OW, OWb)}

            def finish(Tt, Tbb, sTt, sTbb, rows):
                k.op("act", lambda e: e.activation(out=sTt[0:rows, :], in_=Tt[0:rows, :], func=AF.Copy), reads=[Tbb], writes=[sTbb])
                for h4 in range(4):
                    k.op("pe", lambda e, h4=h4: e.transpose(Tt[:, h4 * 65:h4 * 65 + rows], sTt[0:rows, h4 * 128:(h4 + 1) * 128], self.identf[0:rows, 0:rows]),
                         reads=[sTbb, self.cb], writes=[Tbb])

            with ExitStack() as st3:
                qA = self.sb(st3, "nqA", [64, 4, 2, 512], BF16); qAb = Buf("nqA")
                ebc = [self.sb(st3, "nebc%d" % i, [128, 2, 8, 128], BF16) for i in range(2)]
                ebcb = [Buf("nebc%d" % i) for i in range(2)]
                pT = [self.sb(st3, "napT%d" % i, [128, 512], BF16) for i in range(4)]
                pTb = [Buf("napT%d" % i) for i in range(4)]
                sm = [self.sb(st3, "nasm%d" % i, [128, 4], F32) for i in range(2)]; smb = [Buf("nasm%d" % i) for i in range(2)]
                imp = [self.sb(st3, "naimp%d" % i, [128, 64], F32) for i in range(2)]; impb = [Buf("naimp%d" % i) for i in range(2)]
                m8 = [self.sb(st3, "nam8%d" % i, [128, 8], F32) for i in range(2)]
                cf = [self.sb(st3, "nacf%d" % i, [128, 4], F32) for i in range(2)]; cfb = [Buf("nacf%d" % i) for i in range(2)]
                occ = [self.sb(st3, "naocc%d" % i, [128, 512], F32) for i in range(2)]; occb = [Buf("naocc%d" % i) for i in range(2)]
                sTo = [self.sb(st3, "nasTo%d" % i, [65, 512], F32) for i in range(2)]; sTob = [Buf("nasTo%d" % i) for i in range(2)]
                sTi = [self.sb(st3, "nasTi%d" % i, [64, 512], F32) for i in range(2)]; sTib = [Buf("nasTi%d" % i) for i in range(2)]
                SA = [bank["A0"], bank["A1"]]
                CO = [bank["A2"], bank["C0"]]
                CI = [bank["A3"], bank["C1"]]
                QP, QPb = bank["S"]
                pti = 0
                it = 0
                for c in range(NT):
                    bq = c % 4
                    if bq == 0:
                        toks = slice(c * 128, (c + 4) * 128)
                        for h in range(8):
                            for kc in range(8):
                                k.op("pe", lambda e, kc=kc, h=h: e.matmul(QP[0:64, :], lhsT=Wq[:, kc, h * 64:(h + 1) * 64], rhs=self.hT[:, kc, toks],
                                                                         start=(kc == 0), stop=(kc == 7)), reads=[wqb] + self.hT_b[c:c + 4], writes=[QPb])
                            k.op("act", lambda e, h=h: e.activation(out=qA[:, :, h // 4, (h % 4) * 128:(h % 4 + 1) * 128],
                                                                    in_=QP[0:64, :].rearrange("p (b q) -> p b q", b=4), func=AF.Copy, scale=0.125),
                                 reads=[QPb], writes=[qAb])
                    e_ = ebc[c % 2]; e_b = ebcb[c % 2]
                    njt = 2 if c >= 16 else 1
                    for j in range(njt):
                        r0 = 248 - 8 * c + 128 * j
                        k.dma("sp", e_[:, j], self.ebc_d[r0:r0 + 128, :, :], reads=[self.ebc_db], writes=[e_b])
                    oc_ = occ[c % 2]; oc_b = occb[c % 2]
                    staged = []
                    for g in range(2):
                        qg = qA[:, bq, g, :]
                        for j in range(njt):
                            (Aa, Aab) = SA[pti % 2]; pi = pti % 4; pti += 1
                            k.op("pe", lambda e, j=j, g=g, Aa=Aa, qg=qg: e.matmul(Aa[:, :], lhsT=kcmpT[:, g, j * 128:(j + 1) * 128], rhs=qg, start=True, stop=True),
                                 reads=[kcmpb, qAb], writes=[Aab])
                            k.op("act", lambda e, Aa=Aa, pi=pi: e.activation(out=pT[pi][:], in_=Aa[:, :], func=AF.Exp), reads=[Aab], writes=[pTb[pi]])
                            k.op("dve", lambda e, pi=pi, j=j, g=g: e.tensor_tensor(out=pT[pi][:], in0=pT[pi][:],
                                                                                   in1=e_[:, j, g * 4:(g + 1) * 4, :].rearrange("p h q -> p (h q)"), op=ALU.mult),
                                 reads=[pTb[pi], e_b], writes=[pTb[pi]])
                            staged.append((g, j, pi))
                    for (g, j, pi) in staged:
                        (To, Tob), (Ti, Tib) = CO[g], CI[g]
                        k.op("pe", lambda e, pi=pi, j=j, g=g, To=To: e.matmul(To[0:65, :], lhsT=VC[:, j, g, 0:65], rhs=pT[pi][:], start=(j == 0), stop=(j == njt - 1)),
                             reads=[pTb[pi], VCb], writes=[Tob])
                        k.op("pe", lambda e, pi=pi, j=j, g=g, Ti=Ti: e.matmul(Ti[0:64, :], lhsT=VC[:, j, g, 65:129], rhs=pT[pi][:], start=(j == 0), stop=(j == njt - 1)),
                             reads=[pTb[pi], VCb], writes=[Tib])
                    for g in range(2):
                        (To, Tob), (Ti, Tib) = CO[g], CI[g]
                        k.op("act", lambda e, g=g, To=To: e.activation(out=sTo[g][0:65, :], in_=To[0:65, :], func=AF.Copy), reads=[Tob], writes=[sTob[g]])
                        k.op("act", lambda e, g=g, Ti=Ti: e.activation(out=sTi[g][0:64, :], in_=Ti[0:64, :], func=AF.Copy), reads=[Tib], writes=[sTib[g]])
                    for g in range(2):
                        (To, Tob), (Ti, Tib) = CO[g], CI[g]
                        for h4 in range(4):
                            k.op("pe", lambda e, h4=h4, g=g, To=To: e.transpose(To[:, h4 * 65:h4 * 65 + 65], sTo[g][0:65, h4 * 128:(h4 + 1) * 128], self.identf[0:65, 0:65]),
                                 reads=[sTob[g], self.cb], writes=[Tob])
                        for h4 in range(4):
                            k.op("pe", lambda e, h4=h4, g=g, Ti=Ti: e.transpose(Ti[:, h4 * 65:h4 * 65 + 64], sTi[g][0:64, h4 * 128:(h4 + 1) * 128], self.identf[0:64, 0:64]),
                                 reads=[sTib[g], self.cb], writes=[Tib])
                    chains = [[], []]
                    for g in range(2):
                        ch = chains[g]
                        (To, Tob), (Ti, Tib) = CO[g], CI[g]
                        sm_, smb_, imp_, impb_, cf_, cfb_, m8_ = sm[g], smb[g], imp[g], impb[g], cf[g], cfb[g], m8[g]
                        ch.append((lambda e, sm_=sm_, To=To: e.tensor_scalar(out=sm_[:], in0=To[:, 64:64 + 260:65], scalar1=1e-30, scalar2=None, op0=ALU.max), [Tob], [smb_]))
                        ch.append((lambda e, sm_=sm_: e.reciprocal(out=sm_[:], in_=sm_[:]), [smb_], [smb_]))
                        for h4 in range(4):
                            src = Ti[:, h4 * 65:h4 * 65 + 64]
                            if h4 == 0:
                                ch.append((lambda e, src=src, sm_=sm_, imp_=imp_: e.tensor_scalar(out=imp_[:], in0=src, scalar1=sm_[:, 0:1], scalar2=None, op0=ALU.mult),
                                           [Tib, smb_], [impb_]))
                            else:
                                ch.append((lambda e, src=src, h4=h4, sm_=sm_, imp_=imp_: e.scalar_tensor_tensor(out=imp_[:], in0=src, scalar=sm_[:, h4:h4 + 1], in1=imp_[:],
                                                                                                                 op0=ALU.mult, op1=ALU.add), [Tib, smb_, impb_], [impb_]))
                        sl = slice(62 - 2 * c, 62 - 2 * c + 64)
                        ch.append((lambda e, imp_=imp_, sl=sl: e.tensor_tensor(out=imp_[:], in0=imp_[:], in1=selv[:, sl], op=ALU.mult), [impb_, ncb], [impb_]))
                        ch.append((lambda e, imp_=imp_, sl=sl: e.tensor_tensor(out=imp_[:], in0=imp_[:], in1=sela[:, sl], op=ALU.add), [impb_, ncb], [impb_]))
                        if c >= 1:
                            ch.append((lambda e, imp_=imp_: e.tensor_scalar(out=imp_[:, 0:1], in0=imp_[:, 0:1], scalar1=1e4, scalar2=None, op0=ALU.add), [impb_], [impb_]))
                        ch.append((lambda e, imp_=imp_, m8_=m8_: e.max(out=m8_[:], in_=imp_[:]), [impb_], [impb_]))
                        si = c * 2 + g
                        ch.append((lambda e, si=si, imp_=imp_, m8_=m8_: e.tensor_scalar(out=snegAll[:, si, 64:128], in0=imp_[:], scalar1=m8_[:, 7:8], scalar2=NEG,
                                                                                       op0=ALU.is_lt, op1=ALU.mult), [impb_], [snegAllb[si]]))
                        gview = gn[:, c, g * 12:(g + 1) * 12].rearrange("p (h b) -> p h b", h=4)
                        ch.append((lambda e, cf_=cf_, gview=gview, sm_=sm_: e.tensor_tensor(out=cf_[:], in0=gview[:, :, 0], in1=sm_[:], op=ALU.mult), [gnb, smb_], [cfb_]))
                        for h4 in range(4):
                            hh = g * 4 + h4
                            ch.append((lambda e, h4=h4, hh=hh, To=To, cf_=cf_: e.tensor_scalar(out=oc_[:, hh * 64:(hh + 1) * 64], in0=To[:, h4 * 65:h4 * 65 + 64],
                                                                                               scalar1=cf_[:, h4:h4 + 1], scalar2=None, op0=ALU.mult), [Tob, cfb_], [oc_b]))
                    for i_ in range(max(len(chains[0]), len(chains[1]))):
                        for g in range(2):
                            if i_ < len(chains[g]):
                                fn_, rd_, wr_ = chains[g][i_]
                                k.op("dve", fn_, reads=rd_, writes=wr_)
                    k.dma("sp", self.ocd[c], oc_[:], reads=[oc_b], writes=[self.ocd_b[c]])
            k.barrier()
            qAll = self.sb(st, "nqAll", [128, 4, 2, 512], BF16)
            qTopb = Buf("qTop")
            qBotb = [[Buf("qBot%d_%d" % (b_, g_)) for g_ in range(2)] for b_ in range(4)]
            NPT = 5
            LA = 3
            pT = [self.sb(st, "npT%d" % i, [128, 512], BF16) for i in range(NPT)]
            pTb = [Buf("npT%d" % i) for i in range(NPT)]
            sm = self.sb(st, "nsm", [128, 2, 4], F32); smb = Buf("nsm")
            coef = self.sb(st, "ncoef", [128, 4, 2], F32); coefb = Buf("ncoef")
            oin = [self.sb(st, "noin%d" % i, [128, 512], F32) for i in range(2)]; oinb = [Buf("noin%d" % i) for i in range(2)]
            onsa = self.sb(st, "nonsa", [128, 512], BF16); onsab = Buf("nonsa")
            onT = [self.sb(st, "nonT%d" % i, [128, 4, 128], BF16) for i in range(2)]
            onTb = [Buf("nonT%d" % i) for i in range(2)]
            sTs = [self.sb(st, "nsTs%d" % i, [65, 512], F32) for i in range(2)]; sTsb = [Buf("nsTs%d" % i) for i in range(2)]
            sTw = [self.sb(st, "nsTw%d" % i, [65, 512], F32) for i in range(2)]; sTwb = [Buf("nsTw%d" % i) for i in range(2)]
            SA = [bank["A0"], bank["A1"], bank["A2"], bank["A3"]]
            (Ts, Tsb), (Tw, Twb) = bank["C0"], bank["C1"]
            (Rs, Rsb), (Rw, Rwb) = bank["S"], bank["W"]
            pti = 0
            it = 0

            def finish2(Tt, Tbb, sTt, sTbb, Rr, Rbb):
                k.op("act", lambda e: e.activation(out=sTt[0:65, :], in_=Tt[0:65, :], func=AF.Copy), reads=[Tbb], writes=[sTbb])
                for h4 in range(4):
                    k.op("pe", lambda e, h4=h4: e.transpose(Rr[:, h4 * 65:h4 * 65 + 65], sTt[0:65, h4 * 128:(h4 + 1) * 128], self.identf[0:65, 0:65]),
                         reads=[sTbb, self.cb], writes=[Rbb])

            for c in range(NT):
                bq = c % 4
                if bq == 0:
                    toks = slice(c * 128, (c + 4) * 128)
                    for h in range(8):
                        (X, Xb) = SA[pti % 4]; pti += 1
                        for kc in range(8):
                            k.op("pe", lambda e, kc=kc, h=h, X=X: e.matmul(X[0:64, :], lhsT=Wq[:, kc, h * 64:(h + 1) * 64], rhs=self.hT[:, kc, toks],
                                                                          start=(kc == 0), stop=(kc == 7)), reads=[wqb] + self.hT_b[c:c + 4], writes=[Xb])
                        k.op("act", lambda e, h=h, X=X: e.activation(out=qAll[0:64, :, h // 4, (h % 4) * 128:(h % 4 + 1) * 128],
                                                                     in_=X[0:64, :].rearrange("p (b q) -> p b q", b=4), func=AF.Copy, scale=0.125),
                             reads=[Xb], writes=[qTopb])
                oi = oin[c % 2]; oib = oinb[c % 2]
                k.dma("sp", oi[:], self.ocd[c], reads=[self.ocd_b[c]], writes=[oib])
                for g in range(2):
                    par = it % 2; it += 1
                    si = c * 2 + g
                    qg = qAll[0:64, bq, g, :]
                    qfull = qAll[:, bq, g, :]
                    (X, Xb) = SA[pti % 4]; pti += 1
                    Xbf = X[:, :].bitcast(BF16)
                    k.op("pe", lambda e, si=si: e.transpose(Xbf[:, 0:128], snegAll[:, si, :], self.ident[:]), reads=[snegAllb[si], self.cb], writes=[Xb])
                    k.op("dve", lambda e: e.tensor_copy(out=qAll[64:128, bq, g, :].rearrange("p (h q) -> p h q", h=4),
                                                        in_=Xbf[64:128, 0:128].unsqueeze(1).to_broadcast([64, 4, 128])),
                         reads=[Xb], writes=[qBotb[bq][g]])
                    tiles = [("w", kb) for kb in range(max(0, c - 4), c + 1)] + [("s", kb) for kb in range(c + 1)]
                    nwin = len(tiles) - (c + 1)
                    slots = []

                    def emit_qk(idx):
                        nonlocal pti
                        kind, kb = tiles[idx]
                        (Aa, Aab) = SA[pti % 4]; pi = pti % NPT; pti += 1
                        slots.append((Aa, Aab, pi))
                        kt = slice(kb * 128, (kb + 1) * 128)
                        off = c - kb
                        near = (kind == "w" or off <= 1)
                        if kind == "s":
                            k.op("pe", lambda e: e.matmul(Aa[:, :], lhsT=ksT[:, g, kt], rhs=qfull, start=True, stop=not near),
                                 reads=[ksTb, qTopb, qBotb[bq][g]], writes=[Aab])
                        else:
                            k.op("pe", lambda e: e.matmul(Aa[:, :], lhsT=kwT[:, g, kt], rhs=qg, start=True, stop=not near), reads=[kwTb, qTopb], writes=[Aab])
                        if near:
                            k.op("pe", lambda e: e.matmul(Aa[:, :], lhsT=self.ident[:], rhs=self.EB[:, off, g * 4:(g + 1) * 4, :].rearrange("p h q -> p (h q)"),
                                                          start=False, stop=True), reads=[self.cb], writes=[Aab])

                    def emit_pv(idx):
                        kind, kb = tiles[idx]
                        Aa, Aab, pi = slots[idx]
                        k.op("act", lambda e: e.activation(out=pT[pi][:], in_=Aa[:, :], func=AF.Exp), reads=[Aab], writes=[pTb[pi]])
                        if kind == "s":
                            Tt, Ttb, V, Vb = Ts, Tsb, vsa, vsab
                            first, lastt = (idx == nwin), (idx == len(tiles) - 1)
                        else:
                            Tt, Ttb, V, Vb = Tw, Twb, vwa, vwab
                            first, lastt = (idx == 0), (idx == nwin - 1)
                        k.op("pe", lambda e: e.matmul(Tt[0:65, :], lhsT=V[:, kb, g, :], rhs=pT[pi][:], start=first, stop=lastt),
                             reads=[pTb[pi], Vb], writes=[Ttb])

                    for idx in range(len(tiles) + LA):
                        if idx < len(tiles):
                            emit_qk(idx)
                        if idx >= LA:
                            emit_pv(idx - LA)
                    finish2(Tw, Twb, sTw[par], sTwb[par], Rw, Rwb)
                    finish2(Ts, Tsb, sTs[par], sTsb[par], Rs, Rsb)
                    gview = gn[:, c, g * 12:(g + 1) * 12].rearrange("p (h b) -> p h b", h=4)
                    k.op("dve", lambda e: e.tensor_scalar(out=sm[:, 0, :], in0=Rs[:, 64:64 + 260:65], scalar1=1e-30, scalar2=None, op0=ALU.max), reads=[Rsb, smb], writes=[smb])
                    k.op("dve", lambda e: e.tensor_scalar(out=sm[:, 1, :], in0=Rw[:, 64:64 + 260:65], scalar1=1e-30, scalar2=None, op0=ALU.max), reads=[Rwb, smb], writes=[smb])
                    k.op("dve", lambda e: e.reciprocal(out=sm[:, :, :], in_=sm[:, :, :]), reads=[smb], writes=[smb])
                    k.op("dve", lambda e: e.tensor_tensor(out=coef[:], in0=gview[:, :, 1:3], in1=sm[:, :, :].rearrange("p b h -> p h b"), op=ALU.mult),
                         reads=[gnb, smb, coefb], writes=[coefb])
                    for h4 in range(4):
                        hh = g * 4 + h4
                        k.op("dve", lambda e, h4=h4, hh=hh: e.scalar_tensor_tensor(out=oi[:, hh * 64:(hh + 1) * 64], in0=Rs[:, h4 * 65:h4 * 65 + 64], scalar=coef[:, h4, 0:1],
                                                                                    in1=oi[:, hh * 64:(hh + 1) * 64], op0=ALU.mult, op1=ALU.add), reads=[Rsb, coefb, oib], writes=[oib])
                        k.op("dve", lambda e, h4=h4, hh=hh: e.scalar_tensor_tensor(out=onsa[:, hh * 64:(hh + 1) * 64], in0=Rw[:, h4 * 65:h4 * 65 + 64], scalar=coef[:, h4, 1:2],
                                                                                    in1=oi[:, hh * 64:(hh + 1) * 64], op0=ALU.mult, op1=ALU.add), reads=[Rwb, coefb, oib], writes=[onsab])
                (X, Xb) = SA[pti % 4]; pti += 1
                Xbf = X[:, :].bitcast(BF16)
                for j in range(4):
                    k.op("pe", lambda e, j=j: e.transpose(Xbf[:, j * 128:(j + 1) * 128], onsa[:, j * 128:(j + 1) * 128], self.ident[:]),
                         reads=[onsab, self.cb], writes=[Xb])
                i2 = c % 2
                k.op("act", lambda e, i2=i2: e.activation(out=onT[i2][:], in_=Xbf[:, 0:512].rearrange("p (j c) -> p j c", j=4), func=AF.Copy),
                     reads=[Xb], writes=[onTb[i2]])
                tk = slice(c * 128, (c + 1) * 128)
                k.dma("sp", self.brT[0, :, :, tk].rearrange("c p q -> p c q"), onT[i2][:], reads=[onTb[i2]], writes=[self.brT_b[0][c]])

    def phase_ret(self, l):
        k = self.k
        with ExitStack() as st:
            Wqk = self.sb(st, "rWqk", [128, 8, 512], BF16)
            Wv = self.sb(st, "rWv", [128, 8, 512], BF16)
            Wg = self.sb(st, "rWg", [128, 8, 512], BF16)
            wb = Buf("rW")
            self.load_w(Wqk, self.din["w_in"][l][:, C_QR:C_QR + 512], wb, 8)
            self.load_w(Wv, self.din["w_in"][l][:, C_VR:C_VR + 512], wb, 8)
            self.load_w(Wg, self.din["w_in"][l][:, C_GR:C_GR + 512], wb, 8)
            cos = self.sb(st, "rcos", [128, NT, 32], F32)
            sin = self.sb(st, "rsin", [128, NT, 32], F32)
            dec = self.sb(st, "rdec", [128, 4, 128], F32)
            xi = self.sb(st, "rxi", [64, 4, 128], F32)
            zeta = self.sb(st, "rzeta", [128, 4], F32)
            gch = self.sb(st, "rgch", [64, 4], F32)
            gn = self.sb(st, "rgn", [128, 512], F32)
            rc = Buf("rconst")
            for dst, src in ((cos, "cos"), (sin, "sin"), (dec, "decayT"), (xi, "xi"), (zeta, "zeta"), (gch, "gch")):
                k.dma("sp", dst[:], self.din[src], writes=[rc])
            k.dma("sp", gn[:], self.din["retgn"][l], writes=[rc])
            Sf = self.sb(st, "rSf", [64, 4, 128], F32)
            Sb = self.sb(st, "rSb", [64, 4, 128], BF16)
            Sfb, Sbb = Buf("Sf"), Buf("Sb")
            k.op("dve", lambda e: e.memset(Sf[:], 0.0), writes=[Sfb])
            k.op("dve", lambda e: e.memset(Sb[:], 0.0), writes=[Sbb])
            qk = self.sb(st, "rqk", [128, 512], F32); qkb = Buf("rqk")
            tm = self.sb(st, "rtm", [128, 4, 8, 32], F32); tmb = Buf("rtm")
            rot = self.sb(st, "rrot", [128, 8, 2, 32], F32); rotb = Buf("rrot")
            qkbf = self.sb(st, "rqkbf", [128, 512], BF16); qkbfb = Buf("rqkbf")
            khat = self.sb(st, "rkhat", [128, 4, 64], BF16); khatb = Buf("rkhat")
            qkT = self.sb(st, "rqkT", [64, 8, 128], BF16); qkTb = Buf("rqkT")
            qxiT = self.sb(st, "rqxiT", [64, 4, 128], BF16); qxiTb = Buf("rqxiT")
            qf32 = self.sb(st, "rqf32", [64, 4, 128], F32); qf32b = Buf("rqf32")
            inT = self.sb(st, "rinT", [128, 4, 128], BF16); inTb = Buf("rinT")
            vbf = self.sb(st, "rvbf", [128, 512], BF16); vbfb = Buf("rvbf")
            osb = self.sb(st, "rosb", [128, 512], F32); osbb = Buf("rosb")
            osq = self.sb(st, "rosq", [128, 512], F32); osqb = Buf("rosq")
            sm = self.sb(st, "rsm", [128, 6, 4], F32); smb = Buf("rsm")
            yn = self.sb(st, "ryn", [128, 512], F32); ynb = Buf("ryn")
            gs = self.sb(st, "rgs", [128, 512], F32); gsb = Buf("rgs")
            orb = self.sb(st, "rorb", [128, 512], BF16); orbb = Buf("rorb")
            orT = [self.sb(st, "rorT%d" % i, [128, 4, 128], BF16) for i in range(2)]
            orTb = [Buf("rorT%d" % i) for i in range(2)]
            pqk = self.ps(st, "rpqk", [128, 512], F32); pqkb = Buf("pqk")
            pv = self.ps(st, "rpv", [128, 512], F32); pvb = Buf("pv")
            pg = self.ps(st, "rpg", [128, 512], F32); pgb = Buf("pg")
            pin = self.ps(st, "rpin", [128, 512], F32); pinb = Buf("pin")
            po = self.ps(st, "rpo", [128, 512], F32); pob = Buf("po")
            pkv = self.ps(st, "rpkv", [128, 512], F32); pkvb = Buf("pkv")
            ptr = self.ps(st, "rptr", [128, 1024], BF16); ptrb = Buf("ptr")
            ptr2 = self.ps(st, "rptr2", [128, 1024], BF16); ptr2b = Buf("ptr2")
            for t in range(NT):
                tk = slice(t * 128, (t + 1) * 128)
                for (W, P, PB) in ((Wqk, pqk, pqkb), (Wv, pv, pvb), (Wg, pg, pgb)):
                    for kc in range(8):
                        k.op("pe", lambda e, kc=kc, W=W, P=P: e.matmul(P[:, :], lhsT=self.hT[:, kc, tk], rhs=W[:, kc, :], start=(kc == 0), stop=(kc == 7)),
                             reads=[wb, self.hT_b[t]], writes=[PB])
                self.chk(1)
                k.op("act", lambda e: e.activation(out=qk[:, 0:256], in_=pqk[:, 0:256], func=AF.Copy), reads=[pqkb], writes=[qkb])
                k.op("act", lambda e: e.activation(out=qk[:, 256:512], in_=pqk[:, 256:512], func=AF.Copy, scale=0.125), reads=[pqkb], writes=[qkb])
                xv = qk[:, :].rearrange("p (h two d) -> p h two d", h=8, two=2)
                x1, x2 = xv[:, :, 0, :], xv[:, :, 1, :]
                cb_ = cos[:, t, :].unsqueeze(1).to_broadcast([128, 8, 32])
                sb_ = sin[:, t, :].unsqueeze(1).to_broadcast([128, 8, 32])
                k.op("dve", lambda e: e.tensor_tensor(out=tm[:, 0], in0=x1, in1=cb_, op=ALU.mult), reads=[qkb, rc], writes=[tmb])
                k.op("dve", lambda e: e.tensor_tensor(out=tm[:, 1], in0=x2, in1=sb_, op=ALU.mult), reads=[qkb, rc], writes=[tmb])
                k.op("dve", lambda e: e.tensor_tensor(out=tm[:, 2], in0=x1, in1=sb_, op=ALU.mult), reads=[qkb, rc], writes=[tmb])
                k.op("dve", lambda e: e.tensor_tensor(out=tm[:, 3], in0=x2, in1=cb_, op=ALU.mult), reads=[qkb, rc], writes=[tmb])
                k.op("dve", lambda e: e.tensor_tensor(out=rot[:, :, 0, :], in0=tm[:, 0], in1=tm[:, 1], op=ALU.subtract), reads=[tmb], writes=[rotb])
                k.op("dve", lambda e: e.tensor_tensor(out=rot[:, :, 1, :], in0=tm[:, 2], in1=tm[:, 3], op=ALU.add), reads=[tmb], writes=[rotb])
                self.chk(2)
                rflat = rot[:, :, :, :].rearrange("p h two d -> p (h two d)")
                k.op("act", lambda e: e.activation(out=qkbf[:], in_=rflat, func=AF.Copy), reads=[rotb], writes=[qkbfb])
                k.op("dve", lambda e: e.tensor_tensor(out=khat[:], in0=rflat[:, 256:512].rearrange("p (h d) -> p h d", h=4),
                                                      in1=zeta[:, :].unsqueeze(2).to_broadcast([128, 4, 64]), op=ALU.mult),
                     reads=[rotb, rc], writes=[khatb])
                self.chk(3)
                for j in range(8):
                    k.op("pe", lambda e, j=j: e.transpose(ptr[0:64, j * 128:(j + 1) * 128], qkbf[:, j * 64:(j + 1) * 64], self.ident[:]),
                         reads=[qkbfb, self.cb], writes=[ptrb])
                k.op("act", lambda e: e.activation(out=qkT[:], in_=ptr[0:64, 0:1024].rearrange("p (j c) -> p j c", j=8), func=AF.Copy),
                     reads=[ptrb], writes=[qkTb])
                k.op("act", lambda e: e.activation(out=qf32[:], in_=ptr[0:64, 0:512].rearrange("p (j c) -> p j c", j=4), func=AF.Copy),
                     reads=[ptrb], writes=[qf32b])
                k.op("dve", lambda e: e.tensor_tensor(out=qxiT[:], in0=qf32[:], in1=xi[:], op=ALU.mult),
                     reads=[qf32b, rc], writes=[qxiTb])
                self.chk(4)
                for h in range(4):
                    k.op("pe", lambda e, h=h: e.matmul(pin[:, h * 128:(h + 1) * 128], lhsT=qkT[:, 4 + h, :], rhs=qkT[:, h, :],
                                                       start=True, stop=True), reads=[qkTb], writes=[pinb])
                self.chk(45)
                k.op("dve", lambda e: e.tensor_tensor(out=inT[:], in0=pin[:, :].rearrange("p (h c) -> p h c", h=4), in1=dec[:], op=ALU.mult),
                     reads=[pinb, rc], writes=[inTb])
                self.chk(5)
                k.op("act", lambda e: e.activation(out=vbf[:], in_=pv[:, :], func=AF.Copy), reads=[pvb], writes=[vbfb])
                for h in range(4):
                    rows = slice((h % 2) * 64, (h % 2) * 64 + 64)
                    hc = slice(h * 128, (h + 1) * 128)
                    k.op("pe", lambda e, h=h, hc=hc: e.matmul(po[:, hc], lhsT=inT[:, h, :], rhs=vbf[:, hc], start=True, stop=False),
                         reads=[inTb, vbfb], writes=[pob])
                    k.op("pe", lambda e, h=h, hc=hc: e.matmul(po[:, hc], lhsT=qxiT[:, h, :], rhs=Sb[:, h, :], start=False, stop=True),
                         reads=[qxiTb, Sbb], writes=[pob])
                self.chk(6)
                for h in range(4):
                    hc = slice(h * 128, (h + 1) * 128)
                    k.op("pe", lambda e, h=h, hc=hc: e.matmul(pkv[0:64, hc], lhsT=khat[:, h, :],
                                                             rhs=vbf[:, hc], start=True, stop=True), reads=[khatb, vbfb], writes=[pkvb])
                self.chk(7)
                for h in range(4):
                    rows = slice((h % 2) * 64, (h % 2) * 64 + 64)
                    hc = slice(h * 128, (h + 1) * 128)
                    k.op("dve", lambda e, h=h, hc=hc: e.scalar_tensor_tensor(out=Sf[:, h, :], in0=Sf[:, h, :],
                                                                              scalar=gch[:, h:h + 1], in1=pkv[0:64, hc],
                                                                              op0=ALU.mult, op1=ALU.add),
                         reads=[pkvb, rc, Sfb], writes=[Sfb])
                k.op("act", lambda e: e.activation(out=Sb[:], in_=Sf[:], func=AF.Copy), reads=[Sfb], writes=[Sbb])
                self.chk(8)
                k.op("act", lambda e: e.activation(out=osb[:], in_=po[:, :], func=AF.Copy), reads=[pob], writes=[osbb])
                k.op("act", lambda e: e.activation(out=osq[:], in_=po[:, :], func=AF.Square), reads=[pob], writes=[osqb])
                k.op("dve", lambda e: e.reduce_sum(out=sm[:, 0, :], in_=osb[:, :].rearrange("p (h v) -> p h v", h=4), axis=AX.X), reads=[osbb], writes=[smb])
                k.op("dve", lambda e: e.reduce_sum(out=sm[:, 1, :], in_=osq[:, :].rearrange("p (h v) -> p h v", h=4), axis=AX.X), reads=[osqb, smb], writes=[smb])
                k.op("dve", lambda e: e.tensor_scalar(out=sm[:, 2, :], in0=sm[:, 0, :], scalar1=1.0 / 128, scalar2=None, op0=ALU.mult), reads=[smb], writes=[smb])
                k.op("dve", lambda e: e.tensor_tensor(out=sm[:, 3, :], in0=sm[:, 2, :], in1=sm[:, 2, :], op=ALU.mult), reads=[smb], writes=[smb])
                k.op("dve", lambda e: e.scalar_tensor_tensor(out=sm[:, 3, :], in0=sm[:, 1, :], scalar=1.0 / 128, in1=sm[:, 3, :], op0=ALU.mult, op1=ALU.subtract),
                     reads=[smb], writes=[smb])
                k.op("act", lambda e: e.activation(out=sm[:, 4, :], in_=sm[:, 3, :], func=AF.Sqrt, bias=self.cst[:, 0:1], scale=1.0), reads=[smb, self.cb], writes=[smb])
                k.op("dve", lambda e: e.reciprocal(out=sm[:, 5, :], in_=sm[:, 4, :]), reads=[smb], writes=[smb])
                self.chk(9)
                for h in range(4):
                    hc = slice(h * 128, (h + 1) * 128)
                    k.op("dve", lambda e, h=h, hc=hc: e.tensor_scalar(out=yn[:, hc], in0=osb[:, hc], scalar1=sm[:, 2, h:h + 1], scalar2=sm[:, 5, h:h + 1],
                                                                      op0=ALU.subtract, op1=ALU.mult), reads=[osbb, smb], writes=[ynb])
                k.op("dve", lambda e: e.tensor_tensor(out=yn[:], in0=yn[:], in1=gn[:], op=ALU.mult), reads=[ynb, rc], writes=[ynb])
                self.chk(10)
                k.op("act", lambda e: e.activation(out=gs[:], in_=pg[:, :], func=AF.Silu), reads=[pgb], writes=[gsb])
                k.op("dve", lambda e: e.tensor_tensor(out=orb[:], in0=yn[:], in1=gs[:], op=ALU.mult), reads=[ynb, gsb], writes=[orbb])
                self.chk(11)
                for j in range(4):
                    k.op("pe", lambda e, j=j: e.transpose(ptr2[:, j * 128:(j + 1) * 128], orb[:, j * 128:(j + 1) * 128], self.ident[:]),
                         reads=[orbb, self.cb], writes=[ptr2b])
                i = t % 2
                k.op("act", lambda e, i=i: e.activation(out=orT[i][:], in_=ptr2[:, 0:512].rearrange("p (j c) -> p j c", j=4), func=AF.Copy),
                     reads=[ptr2b], writes=[orTb[i]])
                self.chk(12)
                k.dma("sp", self.brT[1, :, :, tk].rearrange("c p q -> p c q"), orT[i][:], reads=[orTb[i]], writes=[self.brT_b[1][t]])

    def phase_conv(self, l):
        k = self.k
        with ExitStack() as st:
            Wa = self.sb(st, "cWa", [128, 8, 512], BF16)
            Wb = self.sb(st, "cWb", [128, 8, 512], BF16)
            wab = Buf("cW")
            self.load_w(Wa, self.din["w_in"][l][:, C_CA:C_CA + 512], wab, 8)
            self.load_w(Wb, self.din["w_in"][l][:, C_CB:C_CB + 512], wab, 8)
            cw = self.sb(st, "ccw", [128, 4, 31], F32)
            cvec = self.sb(st, "cvec", [128, 3, 4], F32)
            ones = self.sb(st, "cones", [128, 128], F32)
            cc = Buf("cconst")
            k.dma("sp", cw[:], self.din["convw"][l], writes=[cc])
            k.dma("sp", cvec[:, 0, :], self.din["convb"][l], writes=[cc])
            k.dma("sp", cvec[:, 1, :], self.din["convg"][l], writes=[cc])
            k.dma("sp", cvec[:, 2, :], self.din["convbb"][l], writes=[cc])
            k.dma("sp", ones[:], self.din["ones"][:, :], writes=[cc])
            Dg = self.sb(st, "cDg", [128, 4, 31, 128], BF16); dgb = Buf("cDg")
            for ct in range(4):
                for w in range(31):
                    en = "dve" if (w % 2 == 0) else "pool"
                    k.op(en, lambda e, ct=ct, w=w: e.tensor_scalar(out=Dg[:, ct, w, :], in0=self.identf[:], scalar1=cw[:, ct, w:w + 1], scalar2=None, op0=ALU.mult),
                         reads=[cc, self.cb], writes=[dgb])
            u = [self.sb(st, "cu%d" % i, [128, 4, 542], BF16) for i in range(2)]
            ub = [[Buf("cu%d_%d" % (i, ct)) for ct in range(4)] for i in range(2)]
            acc = self.sb(st, "cacc", [128, 4, 512], F32)
            accb = [Buf("cacc%d" % ct) for ct in range(4)]
            sg = [self.sb(st, "csg%d" % i, [128, 512], F32) for i in range(2)]
            sgb = [Buf("csg%d" % i) for i in range(2)]
            ysq = [self.sb(st, "cysq%d" % i, [128, 512], F32) for i in range(2)]
            ysqb = [Buf("cysq%d" % i) for i in range(2)]
            stt = self.sb(st, "cstt", [128, 4, 512], F32)
            sttb = Buf("cstt")
            yn = [self.sb(st, "cyn%d" % i, [128, 512], F32) for i in range(2)]
            ynb = [Buf("cyn%d" % i) for i in range(2)]
            oc = [self.sb(st, "coc%d" % i, [128, 512], BF16) for i in range(2)]
            ocb = [Buf("coc%d" % i) for i in range(2)]
            pa = [self.ps(st, "cpa%d" % i, [128, 512], F32) for i in range(2)]
            pab = [Buf("cpa%d" % i) for i in range(2)]
            pbk = [self.ps(st, "cpb%d" % i, [128, 512], F32) for i in range(2)]
            pbb = [Buf("cpb%d" % i) for i in range(2)]
            pcv = [self.ps(st, "cpc%d" % i, [128, 512], F32) for i in range(2)]
            pcb = [Buf("cpc%d" % i) for i in range(2)]
            s1 = self.ps(st, "cs1", [128, 512], F32)
            s2 = self.ps(st, "cs2", [128, 512], F32)
            s1b, s2b = Buf("cs1"), Buf("cs2")
            for ct in range(4):
                k.op("dve", lambda e, ct=ct: e.memset(u[0][:, ct, 0:30], 0.0), writes=[ub[0][ct]])
            for G in range(8):
                toks = slice(G * 512, (G + 1) * 512)
                hb = self.hT_b[G * 4:(G + 1) * 4]
                ug, ugb = u[G % 2], ub[G % 2]
                for ct in range(4):
                    p = ct % 2
                    cols = slice(ct * 128, (ct + 1) * 128)
                    for kc in range(8):
                        k.op("pe", lambda e, kc=kc, cols=cols, p=p: e.matmul(pa[p][:, :], lhsT=Wa[:, kc, cols], rhs=self.hT[:, kc, toks],
                                                                              start=(kc == 0), stop=(kc == 7)), reads=[wab] + hb, writes=[pab[p]])
                    for kc in range(8):
                        k.op("pe", lambda e, kc=kc, cols=cols, p=p: e.matmul(pbk[p][:, :], lhsT=Wb[:, kc, cols], rhs=self.hT[:, kc, toks],
                                                                              start=(kc == 0), stop=(kc == 7)), reads=[wab] + hb, writes=[pbb[p]])
                    k.op("act", lambda e, p=p: e.activation(out=sg[p][:], in_=pbk[p][:, :], func=AF.Sigmoid), reads=[pbb[p]], writes=[sgb[p]])
                    if G > 0:
                        k.op("pool", lambda e, ct=ct: e.tensor_copy(out=ug[:, ct, 0:30], in_=u[(G - 1) % 2][:, ct, 512:542]),
                             reads=[ub[(G - 1) % 2][ct]], writes=[ugb[ct]])
                    k.op("dve", lambda e, ct=ct, p=p: e.tensor_tensor(out=ug[:, ct, 30:542], in0=pa[p][:, :], in1=sg[p][:], op=ALU.mult),
                         reads=[pab[p], sgb[p]], writes=[ugb[ct]])
                for ct in range(4):
                    p = ct % 2
                    for w in range(31):
                        k.op("pe", lambda e, ct=ct, w=w, p=p: e.matmul(pcv[p][:, :], lhsT=Dg[:, ct, w, :], rhs=ug[:, ct, w:w + 512], start=(w == 0), stop=(w == 30)),
                             reads=[dgb, ugb[ct]], writes=[pcb[p]])
                    k.op("act", lambda e, ct=ct, p=p: e.activation(out=acc[:, ct, :], in_=pcv[p][:, :], func=AF.Identity, bias=cvec[:, 0, ct:ct + 1], scale=1.0),
                         reads=[pcb[p], cc], writes=[accb[ct]])
                    k.op("act", lambda e, ct=ct, p=p: e.activation(out=ysq[p][:], in_=acc[:, ct, :], func=AF.Square), reads=[accb[ct]], writes=[ysqb[p]])
                    k.op("pe", lambda e, ct=ct: e.matmul(s1[:, :], lhsT=ones[:], rhs=acc[:, ct, :], start=(ct == 0), stop=(ct == 3)),
                         reads=[cc, accb[ct]], writes=[s1b])
                    k.op("pe", lambda e, ct=ct, p=p: e.matmul(s2[:, :], lhsT=ones[:], rhs=ysq[p][:], start=(ct == 0), stop=(ct == 3)),
                         reads=[cc, ysqb[p]], writes=[s2b])
                k.op("act", lambda e: e.activation(out=stt[:, 0, :], in_=s1[:, :], func=AF.Copy, scale=1.0 / 512), reads=[s1b], writes=[sttb])
                k.op("dve", lambda e: e.tensor_tensor(out=stt[:, 1, :], in0=stt[:, 0, :], in1=stt[:, 0, :], op=ALU.mult), reads=[sttb], writes=[sttb])
                k.op("dve", lambda e: e.scalar_tensor_tensor(out=stt[:, 1, :], in0=s2[:, :], scalar=1.0 / 512, in1=stt[:, 1, :],
                                                             op0=ALU.mult, op1=ALU.subtract), reads=[s2b, sttb], writes=[sttb])
                k.op("act", lambda e: e.activation(out=stt[:, 3, :], in_=stt[:, 1, :], func=AF.Sqrt, bias=self.cst[:, 0:1], scale=1.0),
                     reads=[sttb, self.cb], writes=[sttb])
                k.op("dve", lambda e: e.reciprocal(out=stt[:, 2, :], in_=stt[:, 3, :]), reads=[sttb], writes=[sttb])
                for ct in range(4):
                    p = ct % 2
                    k.op("dve", lambda e, ct=ct, p=p: e.tensor_tensor(out=yn[p][:], in0=acc[:, ct, :], in1=stt[:, 0, :], op=ALU.subtract),
                         reads=[accb[ct], sttb], writes=[ynb[p]])
                    k.op("pool", lambda e, p=p: e.tensor_tensor(out=yn[p][:], in0=yn[p][:], in1=stt[:, 2, :], op=ALU.mult),
                         reads=[sttb, ynb[p]], writes=[ynb[p]])
                    k.op("act", lambda e, ct=ct, p=p: e.activation(out=oc[p][:], in_=yn[p][:], func=AF.Silu, scale=cvec[:, 1, ct:ct + 1],
                                                                   bias=cvec[:, 2, ct:ct + 1]), reads=[ynb[p], cc], writes=[ocb[p]])
                    k.dma("sp", self.brT[2, ct, :, toks], oc[p][:], reads=[ocb[p]], writes=self.brT_b[2][G * 4:(G + 1) * 4])

    def phase_merge(self, l):
        k = self.k
        for dh in range(2):
            with ExitStack() as st:
                Wg = self.sb(st, "mWg", [128, 8, 3, 512], BF16)
                Wbr = self.sb(st, "mWbr", [128, 3, 4, 512], BF16)
                Wo = self.sb(st, "mWo", [128, 4, 1024], BF16)
                wb = Buf("mW")
                for b in range(3):
                    for kc in range(8):
                        c0 = C_MG + b * 1024 + dh * 512
                        k.dma("pool", Wg[:, kc, b, :], self.din["w_in"][l][kc * 128:(kc + 1) * 128, c0:c0 + 512], writes=[wb])
                    for c4 in range(4):
                        k.dma("pool", Wbr[:, b, c4, :], self.din["w_branch"][l][b][c4 * 128:(c4 + 1) * 128, dh * 512:(dh + 1) * 512], writes=[wb])
                for dc in range(4):
                    r0 = dh * 512 + dc * 128
                    k.dma("pool", Wo[:, dc, :], self.din["w_out"][l][r0:r0 + 128, :], writes=[wb])
                brg = [self.sb(st, "mbr%d" % i, [128, 3, 4, 512], BF16) for i in range(2)]
                brgb = [Buf("mbr%d" % i) for i in range(2)]
                mT = self.sb(st, "mmT", [128, 4, 512], BF16)
                mTb = [Buf("mT%d" % i) for i in range(4)]
                gate = [self.sb(st, "mgate%d" % i, [128, 512], F32) for i in range(2)]
                gateb = [Buf("mgate%d" % i) for i in range(2)]
                macc = self.sb(st, "mmacc", [128, 512], F32); maccb = Buf("macc")
                mtmp = self.sb(st, "mmtmp", [128, 512], F32); mtmpb = Buf("mtmp")
                ctx = self.upd_alloc(st, "m", self.din["nmlp"][l] if dh == 1 else None)
                pg = [self.ps(st, "mpg%d" % i, [128, 512], F32) for i in range(2)]
                pgb = [Buf("mpg%d" % i) for i in range(2)]
                pp = [self.ps(st, "mpp%d" % i, [128, 512], F32) for i in range(2)]
                ppb = [Buf("mpp%d" % i) for i in range(2)]
                po = [self.ps(st, "mpo%d" % i, [128, 512], F32) for i in range(2)]
                pob = [Buf("mpo%d" % i) for i in range(2)]
                if dh == 1:
                    ctx["ptr"] = self.ps(st, "mptr", [128, 1024], BF16)
                    ctx["ptr_b"] = Buf("mptr")
                it = 0
                for G in range(8):
                    toks = slice(G * 512, (G + 1) * 512)
                    hb = self.hT_b[G * 4:(G + 1) * 4]
                    bi = G % 2
                    for b in range(3):
                        k.dma("sp", brg[bi][:, b], self.brT[b, :, :, toks].rearrange("c p t -> p c t"),
                              reads=self.brT_b[b][G * 4:(G + 1) * 4], writes=[brgb[bi]])
                    for dc in range(4):
                        for b in range(3):
                            p = it % 2
                            it += 1
                            for kc in range(8):
                                k.op("pe", lambda e, kc=kc, b=b, dc=dc, p=p: e.matmul(pg[p][:, :], lhsT=Wg[:, kc, b, dc * 128:(dc + 1) * 128],
                                                                                     rhs=self.hT[:, kc, toks], start=(kc == 0), stop=(kc == 7)),
                                     reads=[wb] + hb, writes=[pgb[p]])
                            for c4 in range(4):
                                k.op("pe", lambda e, c4=c4, b=b, dc=dc, p=p: e.matmul(pp[p][:, :], lhsT=Wbr[:, b, c4, dc * 128:(dc + 1) * 128],
                                                                                     rhs=brg[bi][:, b, c4, :], start=(c4 == 0), stop=(c4 == 3)),
                                     reads=[wb, brgb[bi]], writes=[ppb[p]])
                            k.op("act", lambda e, p=p: e.activation(out=gate[p][:], in_=pg[p][:, :], func=AF.Sigmoid), reads=[pgb[p]], writes=[gateb[p]])
                            if b == 0:
                                k.op("dve", lambda e, p=p: e.tensor_tensor(out=macc[:], in0=pp[p][:, :], in1=gate[p][:], op=ALU.mult),
                                     reads=[ppb[p], gateb[p]], writes=[maccb])
                            else:
                                k.op("dve", lambda e, p=p: e.tensor_tensor(out=mtmp[:], in0=pp[p][:, :], in1=gate[p][:], op=ALU.mult),
                                     reads=[ppb[p], gateb[p]], writes=[mtmpb])
                                if b == 1:
                                    k.op("pool", lambda e: e.tensor_tensor(out=macc[:], in0=macc[:], in1=mtmp[:], op=ALU.add),
                                         reads=[mtmpb, maccb], writes=[maccb])
                                else:
                                    k.op("pool", lambda e, dc=dc: e.tensor_tensor(out=mT[:, dc, :], in0=macc[:], in1=mtmp[:], op=ALU.add),
                                         reads=[mtmpb, maccb], writes=[mTb[dc]])
                    for tt in range(4):
                        t = G * 4 + tt
                        for nh in range(2):
                            for dc in range(4):
                                k.op("pe", lambda e, nh=nh, dc=dc, tt=tt: e.matmul(po[nh][:, :], lhsT=mT[:, dc, tt * 128:(tt + 1) * 128],
                                                                                  rhs=Wo[:, dc, nh * 512:(nh + 1) * 512], start=(dc == 0), stop=(dc == 3)),
                                     reads=[wb, mTb[dc]], writes=[pob[nh]])
                        self.x_update(ctx, t, [po[0][:, :], po[1][:, :]], pob, "norm" if dh == 1 else None)
                self.upd_flush(ctx)
                k.barrier()


_PROG_CACHE = {}


def _get_prog(**kw):
    key = tuple(sorted((k, str(v)) for k, v in kw.items()))
    if key not in _PROG_CACHE:
        _PROG_CACHE[key] = Prog(**kw)
    return _PROG_CACHE[key]


def kernel(**inputs):
    inp = {k: np.asarray(v) for k, v in inputs.items()}
    x = np.ascontiguousarray(inp["x"], dtype=np.float32)
    shared = _host_inputs(inp)
    prog = _get_prog()
    in_maps = []
    for c in range(8):
        m = dict(shared)
        m["x"] = x[c]
        in_maps.append(m)
    res = run_bass_kernel_spmd(prog.nc, in_maps, core_ids=list(range(8)))
    return np.stack([np.asarray(r["out"], dtype=np.float32) for r in res.results], axis=0)
```

```python
import numpy as np
from contextlib import ExitStack
import concourse.bass as bass
import concourse.mybir as mybir
from concourse.bass_utils import run_bass_kernel_spmd

F32 = mybir.dt.float32
BF16 = mybir.dt.bfloat16
AF = mybir.ActivationFunctionType
ALU = mybir.AluOpType
AX = mybir.AxisListType

S = 4096
D = 1024
NT = 32
DEPTH = 2
IN_TOTAL = 6936
EPS = 1e-6
NEG = -30000.0
C_QN, C_KC, C_VC, C_KS, C_VS, C_KW, C_VW, C_GN = 0, 512, 640, 768, 896, 1024, 1152, 1280
C_QR, C_KR, C_VR, C_GR, C_CA, C_CB, C_MG = 1304, 1560, 1816, 2328, 2840, 3352, 3864

SAME_ENGINE_SYNC = True


class Buf:
    __slots__ = ("w", "r", "name", "wd")

    def __init__(self, name=""):
        self.w = None
        self.r = {}
        self.name = name
        self.wd = False


class _Sem:
    def __init__(self, sem, name):
        self.sem = sem
        self.count = 0
        self.name = name


class Eng(_Sem):
    def __init__(self, name, handle, sem):
        super().__init__(sem, name)
        self.h = handle
        self.waited = {}


class K:
    def __init__(self, nc, stack, n_dma_sems=64):
        self.nc = nc
        self.engs = {}
        for name, h in (("pe", nc.tensor), ("act", nc.scalar), ("dve", nc.vector),
                        ("pool", nc.gpsimd), ("sp", nc.sync)):
            sem = stack.enter_context(nc.semaphore("sem_" + name))
            self.engs[name] = Eng(name, h, sem)
        self.dsems = [_Sem(stack.enter_context(nc.semaphore("dsem%d" % i)), "d%d" % i)
                      for i in range(n_dma_sems)]
        self.dpool = {"pool": self.dsems[:n_dma_sems // 2], "sp": self.dsems[n_dma_sems // 2:]}
        self.dnext = {"pool": 0, "sp": 0}
        self.n_ops = 0
        self.muted = False

    def _wait_deps(self, E, reads, writes):
        deps = {}

        def need(tok):
            if tok is None:
                return
            s, v = tok
            if deps.get(s, 0) < v:
                deps[s] = v
        for b in reads:
            for t in b.w or ():
                need(t)
        for b in writes:
            for t in b.w or ():
                need(t)
            for t in b.r.values():
                need(t)
        for s, v in deps.items():
            if s is E and (E.name in ("pe", "sp") or not SAME_ENGINE_SYNC):
                continue
            if E.waited.get(s, 0) < v:
                E.h.wait_ge(s.sem, v)
                E.waited[s] = v

    def _mark(self, tok, reads, writes, is_dma=False):
        for b in reads:
            b.r[tok[0]] = tok
        for b in writes:
            if is_dma and b.w and getattr(b, "wd", False) and len(b.w) < 24:
                b.w = b.w + [tok]
            else:
                b.w = [tok]
            b.wd = is_dma
            b.r = {}

    def op(self, en, fn, reads=(), writes=()):
        if self.muted:
            return None
        E = self.engs[en]
        self._wait_deps(E, reads, writes)
        ins = fn(E.h)
        E.count += 1
        ins.then_inc(E.sem, 1)
        self._mark((E, E.count), reads, writes)
        self.n_ops += 1
        return ins

    def dma(self, en, out, in_, reads=(), writes=(), **kw):
        if self.muted:
            return None
        E = self.engs[en]
        self._wait_deps(E, reads, writes)
        pool_ = self.dpool[en]
        d = pool_[self.dnext[en]]
        self.dnext[en] = (self.dnext[en] + 1) % len(pool_)
        if d.count and E.waited.get(d, 0) < d.count:
            E.h.wait_ge(d.sem, d.count)
            E.waited[d] = d.count
        ins = E.h.dma_start(out=out, in_=in_, **kw)
        d.count += 16
        ins.then_inc(d.sem, 16)
        self._mark((d, d.count), reads, writes, is_dma=True)
        self.n_ops += 1
        return ins

    def barrier(self):
        allsems = list(self.engs.values()) + self.dsems
        for E in self.engs.values():
            for s in allsems:
                if s is E or s.count == 0:
                    continue
                if E.waited.get(s, 0) < s.count:
                    E.h.wait_ge(s.sem, s.count)
                    E.waited[s] = s.count


def _rel_bucket_np(dist):
    n = np.maximum(dist, 0)
    nf = np.maximum(n, 1).astype(np.float32)
    large = 16 + (np.log(nf / np.float32(16)) / np.float32(np.log(8.0)) * np.float32(16)).astype(np.int32)
    large = np.minimum(large, 31)
    return np.where(n < 16, n, large).astype(np.int64)


_CONST_CACHE = {}


def _host_consts():
    if _CONST_CACHE:
        return _CONST_CACHE
    c = {}
    i = np.arange(128)
    c["ident"] = np.eye(128, dtype=np.float32)
    c["i4"] = np.tile(np.eye(128, dtype=np.float32), (1, 4))
    mw = np.zeros((128, 5, 128), np.float32)
    jj, ii = np.meshgrid(i, i, indexing="ij")
    mw[:, 0, :] = (ii >= jj)
    mw[:, 1:4, :] = 1.0
    mw[:, 4, :] = (ii < jj)
    c["maskw"] = mw
    dist_w = 128 * np.arange(5)[None, :, None] + ii[:, None, :] - jj[:, None, :]
    c["_bucket_w"] = _rel_bucket_np(dist_w)
    m = np.arange(504)
    dist_c = i[None, :] - 16 * (m[:, None] - 248) - 31
    c["maskc"] = (dist_c >= 0).astype(np.float32)
    c["_bucket_c"] = _rel_bucket_np(dist_c)
    n = np.arange(256)
    s = np.arange(64)
    ov = ((16 * n[:, None] <= 64 * s[None, :] + 63) & (16 * n[:, None] + 31 >= 64 * s[None, :])).astype(np.float32)
    ov[255] = 0.0
    c["overlap"] = ov.reshape(2, 128, 64).transpose(1, 0, 2).copy()
    j = np.arange(126)
    sp = j[None, :] - 62
    cur = (i[:, None] >= 64).astype(np.int64)
    valid = sp <= cur
    forced = (sp == cur) | (sp == cur - 1)
    c["selvalid"] = valid.astype(np.float32)
    c["seladd"] = np.where(forced, 1e4, np.where(valid, 0.0, -1e4)).astype(np.float32)
    half = 32
    inv = (10000.0 ** (-np.arange(half, dtype=np.float32) / half)).astype(np.float32)
    pos = np.arange(S, dtype=np.float32)
    ang = (pos[:, None] * inv[None, :]).astype(np.float32)
    c["cos"] = np.cos(ang).astype(np.float32).reshape(NT, 128, 32).transpose(1, 0, 2).copy()
    c["sin"] = np.sin(ang).astype(np.float32).reshape(NT, 128, 32).transpose(1, 0, 2).copy()
    log_g = np.log1p(-np.exp2(-5.0 - np.arange(4, dtype=np.float32))).astype(np.float32)
    diff = i[None, :] - i[:, None]
    dec = np.where(diff[None] >= 0, np.exp(log_g[:, None, None] * np.maximum(diff[None], 0)), 0.0)
    c["decayT"] = dec.transpose(1, 0, 2).astype(np.float32).copy()
    xi = np.exp(log_g[:, None] * (i[None, :] + 1)).astype(np.float32)
    c["xi"] = np.ascontiguousarray(np.broadcast_to(xi[None, :, :], (64, 4, 128))).astype(np.float32)
    c["zeta"] = np.exp(log_g[None, :] * (127 - i[:, None])).astype(np.float32)
    gch = np.exp(log_g * 128).astype(np.float32)
    c["gch"] = np.ascontiguousarray(np.broadcast_to(gch[None, :], (64, 4))).astype(np.float32)
    c["ones"] = np.ones((128, 128), np.float32)
    c["rfull"] = (np.arange(S)[None, :] // 64 == np.arange(64)[:, None]).astype(np.float32)
    _CONST_CACHE.update(c)
    return c


CONST_SHAPES = {
    "ident": [128, 128], "i4": [128, 512], "maskw": [128, 5, 128], "maskc": [504, 128],
    "overlap": [128, 2, 64], "selvalid": [128, 126], "seladd": [128, 126],
    "cos": [128, NT, 32], "sin": [128, NT, 32], "decayT": [128, 4, 128], "xi": [64, 4, 128],
    "zeta": [128, 4], "gch": [64, 4], "ones": [128, 128], "rfull": [64, S],
    "bias_w": [128, 5, 8, 128], "bias_c": [504, 8, 128], "b31": [128, 8],
}

W_SHAPES = {
    "w_in": [DEPTH, D, IN_TOTAL], "w_branch": [DEPTH, 3, 512, D], "w_out": [DEPTH, D, D],
    "w_ff1": [DEPTH, D, 4 * D], "w_ff2": [DEPTH, 4 * D, D],
    "cmp_w1_k": [DEPTH, 32, 64, 128], "cmp_w1_v": [DEPTH, 32, 64, 128],
    "cmp_w2_k": [DEPTH, 128, 64], "cmp_w2_v": [DEPTH, 128, 64],
    "cmp_pe_kT": [DEPTH, 64, 32], "cmp_pe_vT": [DEPTH, 64, 32],
    "nmix": [DEPTH, 128, D], "nmlp": [DEPTH, 128, D], "nfin": [128, D],
    "retgn": [DEPTH, 128, 512],
    "convw": [DEPTH, 128, 4, 31], "convb": [DEPTH, 128, 4], "convg": [DEPTH, 128, 4], "convbb": [DEPTH, 128, 4],
}


def _host_inputs(inp):
    c = _host_consts()
    f = lambda a: np.ascontiguousarray(a, dtype=np.float32)
    out = {k: f(v) for k, v in c.items() if not k.startswith("_")}
    rt = f(inp["rel_table"])
    out["bias_w"] = f(rt[c["_bucket_w"]].transpose(0, 1, 3, 2))
    out["bias_c"] = f(rt[c["_bucket_c"]].transpose(0, 2, 1))
    out["b31"] = f(np.broadcast_to(rt[31][None, :], (128, 8)))
    for kname in ("w_in", "w_branch", "w_out", "w_ff1", "w_ff2", "cmp_w1_k", "cmp_w1_v", "cmp_w2_k", "cmp_w2_v"):
        out[kname] = f(inp[kname])
    out["cmp_pe_kT"] = f(np.transpose(inp["cmp_pe_k"], (0, 2, 1)))
    out["cmp_pe_vT"] = f(np.transpose(inp["cmp_pe_v"], (0, 2, 1)))
    out["nmix"] = f(np.broadcast_to(inp["norm_mix"][:, None, :], (DEPTH, 128, D)))
    out["nmlp"] = f(np.broadcast_to(inp["norm_mlp"][:, None, :], (DEPTH, 128, D)))
    out["nfin"] = f(np.broadcast_to(inp["norm_final"][None, :], (128, D)))
    out["retgn"] = f(np.broadcast_to(inp["ret_gn"][:, None, :], (DEPTH, 128, 512)))
    out["convw"] = f(np.transpose(inp["conv_w"].reshape(DEPTH, 31, 4, 128), (0, 3, 2, 1)))
    for a, b in (("convb", "conv_b"), ("convg", "conv_ln_g"), ("convbb", "conv_ln_b")):
        out[a] = f(np.transpose(inp[b].reshape(DEPTH, 4, 128), (0, 2, 1)))
    return out


class _Stop(Exception):
    pass


class Prog:
    def __init__(self, n_layers=DEPTH, phases=("nsa", "ret", "conv", "merge", "ffn"), dbg=False):
        self.n_layers = n_layers
        self.phases = phases
        self.dbg = dbg
        nc = self.nc = bass.Bass("TRN2", target_bir_lowering=False)
        self.din = {}
        self.din["x"] = nc.dram_tensor("x", [S, D], F32, kind="ExternalInput").ap()
        for name, shp in list(CONST_SHAPES.items()) + list(W_SHAPES.items()):
            self.din[name] = nc.dram_tensor(name, shp, F32, kind="ExternalInput").ap()
        self.out = nc.dram_tensor("out", [S, D], F32, kind="ExternalOutput").ap()
        skind = "ExternalOutput" if dbg else "Internal"
        self.xres = nc.dram_tensor("xres", [S, D], F32, kind=skind).ap()
        self.brT = nc.dram_tensor("brT", [3, 4, 128, S], BF16, kind=skind).ap()
        self.ebc_d = nc.dram_tensor("ebc_d", [504, 8, 128], BF16, kind=skind).ap()
        self.dbg_d = nc.dram_tensor("dbg_d", [128, 2048], F32, kind=skind).ap()
        self.ocd = nc.dram_tensor("ocd", [NT, 128, 512], F32, kind="Internal").ap()
        self.ocd_b = [Buf("ocd%d" % t) for t in range(NT)]
        self.xres_b = [Buf("xres%d" % t) for t in range(NT)]
        self.brT_b = [[Buf("brT%d_%d" % (b, t)) for t in range(NT)] for b in range(3)]
        self.ebc_db = Buf("ebc_d")
        self.out_b = Buf("out")
        with ExitStack() as st:
            self.st = st
            self.k = K(nc, st)
            self.build()

    def sb(self, st, name, shape, dt):
        self._uid = getattr(self, "_uid", 0) + 1
        return st.enter_context(self.nc.sbuf_tensor("s%d_%s" % (self._uid, name), shape, dt))

    def ps(self, st, name, shape, dt):
        self._uid = getattr(self, "_uid", 0) + 1
        return st.enter_context(self.nc.psum_tensor("p%d_%s" % (self._uid, name), shape, dt))

    def chk(self, n):
        import os
        v = os.environ.get("RSTOP")
        if v is not None and int(v) == n:
            self.k.muted = True

    def load_const(self, dst, src, buf, eng="sp"):
        self.k.dma(eng, dst, src, writes=[buf])

    def build(self):
        k, nc, st = self.k, self.nc, self.st
        self.hT = self.sb(st, "hT_all", [128, 8, S], BF16)
        self.hT_b = [Buf("hT%d" % t) for t in range(NT)]
        self.ident = self.sb(st, "ident", [128, 128], BF16)
        self.i4 = self.sb(st, "i4", [128, 512], BF16)
        self.identf = self.sb(st, "identf", [128, 128], F32)
        self.cst = self.sb(st, "cst", [128, 4], F32)
        self.EB = self.sb(st, "EB", [128, 5, 8, 128], BF16)
        self.cb = Buf("consts")
        k.dma("pool", self.ident[:], self.din["ident"][:, :], writes=[self.cb])
        k.dma("pool", self.i4[:], self.din["i4"][:, :], writes=[self.cb])
        k.dma("sp", self.identf[:], self.din["ident"][:, :], writes=[self.cb])
        k.op("dve", lambda e: e.memset(self.cst[:, 0:1], EPS), writes=[self.cb])
        k.op("dve", lambda e: e.memset(self.cst[:, 1:2], 0.0), writes=[self.cb])
        k.op("dve", lambda e: e.memset(self.cst[:, 2:3], 1.0), writes=[self.cb])
        self.phase_bias_tables()
        k.barrier()
        self.phase0()
        k.barrier()
        for l in range(self.n_layers):
            last = (l == DEPTH - 1)
            for ph, fn in (("nsa", lambda: self.phase_nsa(l)), ("ret", lambda: self.phase_ret(l)), ("conv", lambda: self.phase_conv(l)),
                           ("merge", lambda: self.phase_merge(l)), ("ffn", lambda: self.phase_ffn(l, last))):
                if ph in self.phases:
                    with nc.named_scope("%s%d" % (ph, l)):
                        fn()
                        k.muted = False
                        k.barrier()
        k.barrier()

    def rms_alloc(self, st, tag):
        r = {}
        r["junk"] = self.sb(st, "rjunk" + tag, [128, D], BF16)
        r["ss"] = self.sb(st, "rss" + tag, [128, 4], F32)
        r["hb2"] = [self.sb(st, "rhb%d" % i + tag, [128, D], BF16) for i in range(2)]
        r["hbb2"] = [Buf("rmshb%d" % i + tag) for i in range(2)]
        r["i"] = 0
        r["b"] = Buf("rms" + tag)
        return r

    def rms_stats(self, r, src, src_bufs):
        k = self.k
        ss = r["ss"]
        k.op("dve", lambda e: e.memset(ss[:, 0:1], 0.0), writes=[r["b"]])
        k.op("act", lambda e: e.activation(out=r["junk"][:], in_=src, func=AF.Square, accum_out=ss[:, 0:1]),
             reads=list(src_bufs) + [r["b"]], writes=[r["b"]])
        k.op("act", lambda e: e.activation(out=ss[:, 1:2], in_=ss[:, 0:1], func=AF.Sqrt, bias=self.cst[:, 0:1], scale=1.0 / D),
             reads=[r["b"], self.cb], writes=[r["b"]])
        k.op("dve", lambda e: e.reciprocal(out=ss[:, 2:3], in_=ss[:, 1:2]), reads=[r["b"]], writes=[r["b"]])
        return ss[:, 2:3]

    def rms_to_hT(self, r, src, src_bufs, gain, gain_buf, t, ptr, ptr_b):
        i = self.rms_part1(r, src, src_bufs, gain, gain_buf)
        self.rms_part2(r, i, t, ptr, ptr_b)

    def rms_part1(self, r, src, src_bufs, gain, gain_buf):
        k = self.k
        rstd = self.rms_stats(r, src, src_bufs)
        i = r["i"] = (r["i"] + 1) % 2
        hb = r["hb2"][i]
        k.op("dve", lambda e: e.scalar_tensor_tensor(out=hb[:], in0=src, scalar=rstd, in1=gain, op0=ALU.mult, op1=ALU.mult),
             reads=list(src_bufs) + [r["b"], gain_buf], writes=[r["hbb2"][i]])
        return i

    def rms_part2(self, r, i, t, ptr, ptr_b):
        k = self.k
        hb = r["hb2"][i]
        for c in range(8):
            k.op("pe", lambda e, c=c: e.transpose(ptr[:, c * 128:(c + 1) * 128], hb[:, c * 128:(c + 1) * 128], self.ident[:]),
                 reads=[r["hbb2"][i], self.cb], writes=[ptr_b])
        k.op("act", lambda e: e.activation(out=self.hT[:, :, t * 128:(t + 1) * 128],
                                           in_=ptr[:, :].rearrange("p (c q) -> p c q", c=8), func=AF.Copy),
             reads=[ptr_b], writes=[self.hT_b[t]])

    def phase_bias_tables(self):
        k = self.k
        with ExitStack() as st:
            bw = self.sb(st, "bw", [128, 5, 8, 128], F32)
            mw = self.sb(st, "mw", [128, 5, 128], F32)
            b31 = self.sb(st, "b31", [128, 8], F32)
            bb = Buf("bw")
            k.dma("sp", bw[:], self.din["bias_w"][:, :, :, :], writes=[bb])
            k.dma("sp", mw[:], self.din["maskw"][:, :, :], writes=[bb])
            k.dma("sp", b31[:], self.din["b31"][:, :], writes=[bb])
            for off in range(5):
                k.op("dve", lambda e, off=off: e.tensor_tensor(out=bw[:, off], in0=bw[:, off],
                                                               in1=b31[:, :].unsqueeze(2).to_broadcast([128, 8, 128]), op=ALU.subtract),
                     reads=[bb], writes=[bb])
                k.op("dve", lambda e, off=off: e.tensor_tensor(out=bw[:, off], in0=bw[:, off],
                                                               in1=mw[:, off:off + 1, :].to_broadcast([128, 8, 128]), op=ALU.mult),
                     reads=[bb], writes=[bb])
                k.op("dve", lambda e, off=off: e.tensor_scalar(out=mw[:, off, :], in0=mw[:, off, :], scalar1=-NEG, scalar2=NEG, op0=ALU.mult, op1=ALU.add),
                     reads=[bb], writes=[bb])
                k.op("dve", lambda e, off=off: e.tensor_tensor(out=self.EB[:, off], in0=bw[:, off],
                                                               in1=mw[:, off:off + 1, :].to_broadcast([128, 8, 128]), op=ALU.add),
                     reads=[bb], writes=[self.cb])
            bc = self.sb(st, "bc", [126, 8, 128], F32)
            mc = self.sb(st, "mc", [126, 128], F32)
            bcb = self.sb(st, "bcb", [126, 8, 128], BF16)
            cbuf = Buf("bc")
            for r4 in range(4):
                rows = slice(r4 * 126, (r4 + 1) * 126)
                k.dma("sp", bc[:], self.din["bias_c"][rows, :, :], writes=[cbuf])
                k.dma("sp", mc[:], self.din["maskc"][rows, :], writes=[cbuf])
                k.op("dve", lambda e: e.tensor_tensor(out=bc[:], in0=bc[:], in1=b31[0:126, :].unsqueeze(2).to_broadcast([126, 8, 128]),
                                                      op=ALU.subtract), reads=[cbuf, bb], writes=[cbuf])
                k.op("act", lambda e: e.activation(out=bc[:], in_=bc[:], func=AF.Exp), reads=[cbuf], writes=[cbuf])
                k.op("dve", lambda e: e.tensor_tensor(out=bcb[:], in0=bc[:], in1=mc[:, :].unsqueeze(1).to_broadcast([126, 8, 128]),
                                                      op=ALU.mult), reads=[cbuf], writes=[cbuf])
                k.dma("sp", self.ebc_d[rows, :, :], bcb[:], reads=[cbuf], writes=[self.ebc_db])
            k.barrier()

    def phase0(self):
        k = self.k
        with ExitStack() as st:
            r = self.rms_alloc(st, "p0")
            gain = self.sb(st, "gain0", [128, D], F32)
            gb = Buf("gain0")
            k.dma("sp", gain[:], self.din["nmix"][0], writes=[gb])
            xt = [self.sb(st, "p0x%d" % i, [128, D], F32) for i in range(2)]
            xb = [Buf("p0x%d" % i) for i in range(2)]
            ptr = self.ps(st, "p0tr", [128, 1024], BF16)
            ptr_b = Buf("p0tr")
            for t in range(NT):
                i = t % 2
                k.dma("sp", xt[i][:], self.din["x"][t * 128:(t + 1) * 128, :], writes=[xb[i]])
                k.dma("pool", self.xres[t * 128:(t + 1) * 128, :], xt[i][:], reads=[xb[i]], writes=[self.xres_b[t]])
                self.rms_to_hT(r, xt[i][:], [xb[i]], gain[:], gb, t, ptr, ptr_b)

    def load_w(self, dst, src, buf, kc):
        for c in range(kc):
            self.k.dma("pool", dst[:, c, :], src[c * 128:(c + 1) * 128, :], writes=[buf])

    def x_update(self, ctx, t, psum_halves, psum_bufs, hook):
        k = self.k
        i = ctx["i"] = (ctx.get("i", 0) + 1) % 2
        xt, xb = ctx["xt"][i], ctx["xb"][i]
        k.dma("sp", xt[:], self.xres[t * 128:(t + 1) * 128, :], reads=[self.xres_b[t]], writes=[xb])
        for h in range(2):
            k.op("dve", lambda e, h=h: e.tensor_tensor(out=xt[:, h * 512:(h + 1) * 512], in0=psum_halves[h],
                                                       in1=xt[:, h * 512:(h + 1) * 512], op=ALU.add),
                 reads=[psum_bufs[h], xb], writes=[xb])
        if hook != "final":
            k.dma("pool", self.xres[t * 128:(t + 1) * 128, :], xt[:], reads=[xb], writes=[self.xres_b[t]])
        if hook is None:
            return
        r = ctx["rms"]
        if hook == "final":
            rstd = self.rms_stats(r, xt[:], [xb])
            ot = ctx["ot"]
            k.op("dve", lambda e: e.scalar_tensor_tensor(out=ot[:], in0=xt[:], scalar=rstd, in1=ctx["gain"][:], op0=ALU.mult, op1=ALU.mult),
                 reads=[xb, r["b"], ctx["gain_b"]], writes=[ctx["ot_b"]])
            k.dma("pool", self.out[t * 128:(t + 1) * 128, :], ot[:], reads=[ctx["ot_b"]], writes=[self.out_b])
        else:
            i2 = self.rms_part1(r, xt[:], [xb], ctx["gain"][:], ctx["gain_b"])
            self.upd_flush(ctx)
            ctx["pending"] = (i2, t)

    def upd_flush(self, ctx):
        p = ctx.pop("pending", None)
        if p is not None:
            self.rms_part2(ctx["rms"], p[0], p[1], ctx["ptr"], ctx["ptr_b"])

    def upd_alloc(self, st, tag, gain_src, final=False):
        ctx = {}
        ctx["xt"] = [self.sb(st, "ux%s%d" % (tag, i), [128, D], F32) for i in range(2)]
        ctx["xb"] = [Buf("ux%d" % i) for i in range(2)]
        if gain_src is not None:
            ctx["rms"] = self.rms_alloc(st, "u" + tag)
            ctx["gain"] = self.sb(st, "ug" + tag, [128, D], F32)
            ctx["gain_b"] = Buf("ug")
            self.k.dma("sp", ctx["gain"][:], gain_src, writes=[ctx["gain_b"]])
            if final:
                ctx["ot"] = self.sb(st, "uo" + tag, [128, D], F32)
                ctx["ot_b"] = Buf("uo")
        return ctx

    def phase_ffn(self, l, last):
        k = self.k
        for fh in range(2):
            with ExitStack() as st:
                W1 = self.sb(st, "W1", [128, 8, 2048], BF16)
                W2 = self.sb(st, "W2", [128, 16, 1024], BF16)
                w1b = [Buf("W1_%d" % i) for i in range(4)]
                w2b = [Buf("W2_%d" % i) for i in range(4)]
                for kc in range(8):
                    k.dma("pool", W1[:, kc, :], self.din["w_ff1"][l][kc * 128:(kc + 1) * 128, fh * 2048:(fh + 1) * 2048], writes=w1b)
                for fc in range(16):
                    r0 = fh * 2048 + fc * 128
                    k.dma("pool", W2[:, fc, :], self.din["w_ff2"][l][r0:r0 + 128, :], writes=[w2b[fc // 4]])
                actT = self.sb(st, "actT", [128, 16, 512], BF16)
                act_b = [Buf("act%d" % i) for i in range(16)]
                rl = [self.sb(st, "rl%d" % i, [128, 512], F32) for i in range(2)]
                rl_b = [Buf("rl%d" % i) for i in range(2)]
                hook = None
                gain_src = None
                if fh == 1:
                    hook = "final" if last else "norm"
                    gain_src = self.din["nfin"][:, :] if last else self.din["nmix"][l + 1]
                ctx = self.upd_alloc(st, "f", gain_src, final=(fh == 1 and last))
                pb = [self.ps(st, "fpb%d" % i, [128, 512], F32) for i in range(6)]
                pbb = [Buf("fpb%d" % i) for i in range(6)]
                if hook == "norm":
                    ctx["ptr"] = self.ps(st, "fptr", [128, 1024], BF16)
                    ctx["ptr_b"] = Buf("fptr")
                for G in range(8):
                    toks = slice(G * 512, (G + 1) * 512)
                    hb = self.hT_b[G * 4:(G + 1) * 4]
                    for fc in range(16):
                        p = fc % 2
                        for kc in range(8):
                            k.op("pe", lambda e, kc=kc, fc=fc, p=p: e.matmul(pb[p][:, :], lhsT=W1[:, kc, fc * 128:(fc + 1) * 128],
                                                                               rhs=self.hT[:, kc, toks], start=(kc == 0), stop=(kc == 7)),
                                 reads=[w1b[fc // 4]] + hb, writes=[pbb[p]])
                        k.op("act", lambda e, p=p: e.activation(out=rl[p][:], in_=pb[p][:, :], func=AF.Relu), reads=[pbb[p]], writes=[rl_b[p]])
                        k.op("dve", lambda e, p=p, fc=fc: e.tensor_tensor(out=actT[:, fc, :], in0=rl[p][:], in1=rl[p][:], op=ALU.mult),
                             reads=[rl_b[p]], writes=[act_b[fc]])
                    for tt in range(4):
                        t = G * 4 + tt
                        pp = [2 + 2 * (tt % 2), 3 + 2 * (tt % 2)]
                        for nh in range(2):
                            for fc in range(16):
                                k.op("pe", lambda e, nh=nh, fc=fc, tt=tt: e.matmul(pb[pp[nh]][:, :], lhsT=actT[:, fc, tt * 128:(tt + 1) * 128],
                                                                                  rhs=W2[:, fc, nh * 512:(nh + 1) * 512], start=(fc == 0), stop=(fc == 15)),
                                     reads=[w2b[fc // 4], act_b[fc]], writes=[pbb[pp[nh]]])
                        self.x_update(ctx, t, [pb[pp[0]][:, :], pb[pp[1]][:, :]], [pbb[pp[0]], pbb[pp[1]]], hook)
                self.upd_flush(ctx)
                k.barrier()

    def phase_nsa(self, l):
        k = self.k
        win = self.din["w_in"][l]
        with ExitStack() as st:
            Wq = self.sb(st, "nWq", [128, 8, 512], BF16); wqb = Buf("nWq")
            ksT = self.sb(st, "nksT", [128, 2, S], BF16); ksTb = Buf("ksT")
            kwT = self.sb(st, "nkwT", [64, 2, S], BF16); kwTb = Buf("kwT")
            vsa = self.sb(st, "nvsa", [128, NT, 2, 65], BF16); vsab = Buf("vsa")
            vwa = self.sb(st, "nvwa", [128, NT, 2, 65], BF16); vwab = Buf("vwa")
            gn = self.sb(st, "ngn", [128, NT, 24], F32); gnb = Buf("gn")
            kcmpT = self.sb(st, "nkcmpT", [64, 2, 256], BF16); kcmpb = Buf("kcmpT")
            VC = self.sb(st, "nVC", [128, 2, 2, 129], BF16); VCb = Buf("VC")
            selv = self.sb(st, "nselv", [128, 126], F32)
            sela = self.sb(st, "nsela", [128, 126], F32)
            ovl = self.sb(st, "novl", [128, 2, 64], F32)
            ncb = Buf("nconst")
            k.dma("sp", selv[:], self.din["selvalid"], writes=[ncb])
            k.dma("sp", sela[:], self.din["seladd"], writes=[ncb])
            k.dma("sp", ovl[:], self.din["overlap"], writes=[ncb])
            A = [self.ps(st, "nA%d" % i, [128, 512], F32) for i in range(4)]
            Ab = [Buf("nA%d" % i) for i in range(4)]
            for g in range(2):
                k.dma("pool", ksT[64:128, g, :], self.din["rfull"], writes=[ksTb])
            OC = [self.ps(st, "nOC%d" % i, [128, 512], F32) for i in range(2)]
            OCb = [Buf("nOC%d" % i) for i in range(2)]
            OS = self.ps(st, "nOS", [128, 512], F32); OSb = Buf("OS")
            OW = self.ps(st, "nOW", [128, 512], F32); OWb = Buf("OW")
            Q = OS; Qb = OSb
            R0bf = OS[:, :].bitcast(BF16)
            R1bf = OW[:, :].bitcast(BF16)
            k.op("dve", lambda e: e.memset(vsa[:], 1.0), writes=[vsab])
            k.op("dve", lambda e: e.memset(vwa[:], 1.0), writes=[vwab])
            k.op("dve", lambda e: e.memset(kcmpT[:], 0.0), writes=[kcmpb])
            k.op("dve", lambda e: e.memset(VC[:], 0.0), writes=[VCb])
            for j in range(2):
                for g in range(2):
                    k.op("dve", lambda e, j=j, g=g: e.memset(VC[:, j, g, 64:65], 1.0), writes=[VCb])
                    k.op("dve", lambda e, j=j, g=g: e.tensor_copy(out=VC[:, j, g, 65:129], in_=ovl[:, j, :]), reads=[ncb], writes=[VCb])
            with ExitStack() as st2:
                Wk = self.sb(st2, "nWk", [128, 8, 512], BF16); wkb = Buf("nWk")
                for i, c0 in enumerate((C_KC, C_VC, C_KS, C_KW)):
                    for kc in range(8):
                        k.dma("pool", Wk[:, kc, i * 128:(i + 1) * 128], win[kc * 128:(kc + 1) * 128, c0:c0 + 128], writes=[wkb])
                Wtm = self.sb(st2, "nWtm", [128, 8, 280], BF16); wtb = Buf("nWtm")
                for (o, c0, n) in ((0, C_VS, 128), (128, C_VW, 128), (256, C_GN, 24)):
                    for kc in range(8):
                        k.dma("pool", Wtm[:, kc, o:o + n], win[kc * 128:(kc + 1) * 128, c0:c0 + n], writes=[wtb])
                w1 = [self.sb(st2, "nw1%d" % i, [64, 32, 128], BF16) for i in range(2)]
                w2k = self.sb(st2, "nw2k", [128, 64], BF16)
                w2v = self.sb(st2, "nw2v", [128, 64], BF16)
                peT = [self.sb(st2, "npeT%d" % i, [64, 32], BF16) for i in range(2)]
                cwb = Buf("cmpw")
                for i, nm in enumerate(("cmp_w1_k", "cmp_w1_v")):
                    k.dma("pool", w1[i][:], self.din[nm][l].rearrange("l d f -> d l f"), writes=[cwb])
                k.dma("pool", w2k[:], self.din["cmp_w2_k"][l], writes=[cwb])
                k.dma("pool", w2v[:], self.din["cmp_w2_v"][l], writes=[cwb])
                k.dma("pool", peT[0][:], self.din["cmp_pe_kT"][l], writes=[cwb])
                k.dma("pool", peT[1][:], self.din["cmp_pe_vT"][l], writes=[cwb])
                self.load_w(Wq, win[:, C_QN:C_QN + 512], wqb, 8)
                for t in range(NT):
                    tk = slice(t * 128, (t + 1) * 128)
                    P = A[t % 2]; PB = Ab[t % 2]
                    for kc in range(8):
                        k.op("pe", lambda e, kc=kc, P=P: e.matmul(P[:, 0:280], lhsT=self.hT[:, kc, tk], rhs=Wtm[:, kc, :], start=(kc == 0), stop=(kc == 7)),
                             reads=[wtb, self.hT_b[t]], writes=[PB])
                    k.op("act", lambda e, P=P, t=t: e.activation(out=vsa[:, t, :, 0:64], in_=P[:, 0:128].rearrange("p (g d) -> p g d", g=2), func=AF.Copy),
                         reads=[PB], writes=[vsab])
                    k.op("act", lambda e, P=P, t=t: e.activation(out=vwa[:, t, :, 0:64], in_=P[:, 128:256].rearrange("p (g d) -> p g d", g=2), func=AF.Copy),
                         reads=[PB], writes=[vwab])
                    k.op("act", lambda e, P=P, t=t: e.activation(out=gn[:, t, :], in_=P[:, 256:280], func=AF.Sigmoid), reads=[PB], writes=[gnb])
                it = 0
                for (dst, dstb, wi) in ((ksT, ksTb, 2), (kwT, kwTb, 3)):
                    for g in range(2):
                        for G in range(8):
                            toks = slice(G * 512, (G + 1) * 512)
                            P = OC[it % 2]; PB = OCb[it % 2]; it += 1
                            for kc in range(8):
                                k.op("pe", lambda e, kc=kc, P=P, wi=wi, g=g: e.matmul(P[0:64, :], lhsT=Wk[:, kc, wi * 128 + g * 64:wi * 128 + g * 64 + 64],
                                                                                     rhs=self.hT[:, kc, toks], start=(kc == 0), stop=(kc == 7)),
                                     reads=[wkb] + self.hT_b[G * 4:(G + 1) * 4], writes=[PB])
                            k.op("act", lambda e, P=P, dst=dst, g=g: e.activation(out=dst[0:64, g, toks], in_=P[0:64, :], func=AF.Copy), reads=[PB], writes=[dstb])
                cT = [self.sb(st2, "ncT%d" % i, [64, S], BF16) for i in range(2)]
                cTb = [Buf("ncT%d" % i) for i in range(2)]
                hx = self.sb(st2, "nhx", [128, 4, 256], F32); hxb = Buf("nhx")
                hidb = self.sb(st2, "nhidb", [128, 256], BF16); hidbb = Buf("nhidb")
                cbias = self.sb(st2, "ncbias", [128, 2], F32); cbb = Buf("ncbias")
                for kv in range(2):
                    for lidx in range(32):
                        k.op("pe", lambda e, kv=kv, lidx=lidx: e.matmul(Q[:, kv:kv + 1], lhsT=w1[kv][:, lidx, :], rhs=peT[kv][:, lidx:lidx + 1],
                                                                       start=(lidx == 0), stop=(lidx == 31)), reads=[cwb], writes=[Qb])
                k.op("act", lambda e: e.activation(out=cbias[:], in_=Q[:, 0:2], func=AF.Copy), reads=[Qb], writes=[cbb])
                for g in range(2):
                    for kv in range(2):
                        for G in range(8):
                            toks = slice(G * 512, (G + 1) * 512)
                            P = OC[it % 2]; PB = OCb[it % 2]; it += 1
                            for kc in range(8):
                                k.op("pe", lambda e, kc=kc, P=P, kv=kv, g=g: e.matmul(P[0:64, :], lhsT=Wk[:, kc, kv * 128 + g * 64:kv * 128 + g * 64 + 64],
                                                                                     rhs=self.hT[:, kc, toks], start=(kc == 0), stop=(kc == 7)),
                                     reads=[wkb] + self.hT_b[G * 4:(G + 1) * 4], writes=[PB])
                            k.op("act", lambda e, P=P, kv=kv: e.activation(out=cT[kv][:, toks], in_=P[0:64, :], func=AF.Copy), reads=[PB], writes=[cTb[kv]])
                    for kv in range(2):
                        P = A[kv]; PB = Ab[kv]
                        for lidx in range(32):
                            k.op("pe", lambda e, kv=kv, lidx=lidx, P=P: e.matmul(P[:, 0:255], lhsT=w1[kv][:, lidx, :], rhs=cT[kv][:, lidx:lidx + 16 * 254 + 1:16],
                                                                                start=(lidx == 0), stop=(lidx == 31)), reads=[cwb, cTb[kv]], writes=[PB])
                        x_ = hx[:, 0, 0:255]
                        k.op("act", lambda e, P=P, kv=kv: e.activation(out=x_, in_=P[:, 0:255], func=AF.Identity, bias=cbias[:, kv:kv + 1], scale=1.0),
                             reads=[PB, cbb], writes=[hxb])
                        k.op("dve", lambda e: e.tensor_tensor(out=hx[:, 1, 0:255], in0=x_, in1=x_, op=ALU.mult), reads=[hxb], writes=[hxb])
                        k.op("dve", lambda e: e.tensor_scalar(out=hx[:, 1, 0:255], in0=hx[:, 1, 0:255], scalar1=0.044715, scalar2=1.0, op0=ALU.mult, op1=ALU.add),
                             reads=[hxb], writes=[hxb])
                        k.op("dve", lambda e: e.tensor_tensor(out=hx[:, 2, 0:255], in0=hx[:, 1, 0:255], in1=x_, op=ALU.mult), reads=[hxb], writes=[hxb])
                        k.op("act", lambda e: e.activation(out=hx[:, 3, 0:255], in_=hx[:, 2, 0:255], func=AF.Sigmoid, scale=1.5957691216057308),
                             reads=[hxb], writes=[hxb])
                        k.op("dve", lambda e: e.tensor_tensor(out=hidb[:, 0:255], in0=hx[:, 3, 0:255], in1=x_, op=ALU.mult), reads=[hxb], writes=[hidbb])
                        if kv == 0:
                            k.op("pe", lambda e: e.matmul(Q[0:64, 0:255], lhsT=w2k[:], rhs=hidb[:, 0:255], start=True, stop=True), reads=[cwb, hidbb], writes=[Qb])
                            k.op("act", lambda e, g=g: e.activation(out=kcmpT[:, g, 0:255], in_=Q[0:64, 0:255], func=AF.Copy), reads=[Qb], writes=[kcmpb])
                        else:
                            for j in range(2):
                                nn = 128 if j == 0 else 127
                                k.op("pe", lambda e, j=j, nn=nn: e.matmul(Q[0:nn, j * 64:(j + 1) * 64], lhsT=hidb[:, j * 128:j * 128 + nn], rhs=w2v[:],
                                                                         start=True, stop=True), reads=[cwb, hidbb], writes=[Qb])
                                k.op("act", lambda e, j=j, nn=nn, g=g: e.activation(out=VC[0:nn, j, g, 0:64], in_=Q[0:nn, j * 64:(j + 1) * 64], func=AF.Copy),
                                     reads=[Qb], writes=[VCb])
            k.barrier()
            snegAll = self.sb(st, "nsnegAll", [128, NT * 2, 128], BF16); snegAllb = [Buf("sneg%d" % i) for i in range(NT * 2)]
            k.op("dve", lambda e: e.memset(snegAll[:], 0.0), writes=snegAllb)
            bank = {"A0": (A[0], Ab[0]), "A1": (A[1], Ab[1]), "A2": (A[2], Ab[2]), "A3": (A[3], Ab[3]),
                    "C0": (OC[0], OCb[0]), "C1": (OC[1], OCb[1]), "S": (OS, OSb), "W": (OW, OWb)}

            def finish(Tt, Tbb, sTt, sTbb, rows):
                k.op("act", lambda e: e.activation(out=sTt[0:rows, :], in_=Tt[0:rows, :], func=AF.Copy), reads=[Tbb], writes=[sTbb])
                for h4 in range(4):
                    k.op("pe", lambda e, h4=h4: e.transpose(Tt[:, h4 * 65:h4 * 65 + rows], sTt[0:rows, h4 * 128:(h4 + 1) * 128], self.identf[0:rows, 0:rows]),
                         reads=[sTbb, self.cb], writes=[Tbb])

            with ExitStack() as st3:
                qA = self.sb(st3, "nqA", [64, 4, 2, 512], BF16); qAb = Buf("nqA")
                ebc = [self.sb(st3, "nebc%d" % i, [128, 2, 8, 128], BF16) for i in range(2)]
                ebcb = [Buf("nebc%d" % i) for i in range(2)]
                pT = [self.sb(st3, "napT%d" % i, [128, 512], BF16) for i in range(4)]
                pTb = [Buf("napT%d" % i) for i in range(4)]
                sm = [self.sb(st3, "nasm%d" % i, [128, 4], F32) for i in range(2)]; smb = [Buf("nasm%d" % i) for i in range(2)]
                imp = [self.sb(st3, "naimp%d" % i, [128, 64], F32) for i in range(2)]; impb = [Buf("naimp%d" % i) for i in range(2)]
                m8 = [self.sb(st3, "nam8%d" % i, [128, 8], F32) for i in range(2)]
                cf = [self.sb(st3, "nacf%d" % i, [128, 4], F32) for i in range(2)]; cfb = [Buf("nacf%d" % i) for i in range(2)]
                occ = [self.sb(st3, "naocc%d" % i, [128, 512], F32) for i in range(2)]; occb = [Buf("naocc%d" % i) for i in range(2)]
                sTo = [self.sb(st3, "nasTo%d" % i, [65, 512], F32) for i in range(2)]; sTob = [Buf("nasTo%d" % i) for i in range(2)]
                sTi = [self.sb(st3, "nasTi%d" % i, [64, 512], F32) for i in range(2)]; sTib = [Buf("nasTi%d" % i) for i in range(2)]
                SA = [bank["A0"], bank["A1"]]
                CO = [bank["A2"], bank["C0"]]
                CI = [bank["A3"], bank["C1"]]
                QP, QPb = bank["S"]
                pti = 0
                it = 0
                for c in range(NT):
                    bq = c % 4
                    if bq == 0:
                        toks = slice(c * 128, (c + 4) * 128)
                        for h in range(8):
                            for kc in range(8):
                                k.op("pe", lambda e, kc=kc, h=h: e.matmul(QP[0:64, :], lhsT=Wq[:, kc, h * 64:(h + 1) * 64], rhs=self.hT[:, kc, toks],
                                                                         start=(kc == 0), stop=(kc == 7)), reads=[wqb] + self.hT_b[c:c + 4], writes=[QPb])
                            k.op("act", lambda e, h=h: e.activation(out=qA[:, :, h // 4, (h % 4) * 128:(h % 4 + 1) * 128],
                                                                    in_=QP[0:64, :].rearrange("p (b q) -> p b q", b=4), func=AF.Copy, scale=0.125),
                                 reads=[QPb], writes=[qAb])
                    e_ = ebc[c % 2]; e_b = ebcb[c % 2]
                    njt = 2 if c >= 16 else 1
                    for j in range(njt):
                        r0 = 248 - 8 * c + 128 * j
                        k.dma("sp", e_[:, j], self.ebc_d[r0:r0 + 128, :, :], reads=[self.ebc_db], writes=[e_b])
                    oc_ = occ[c % 2]; oc_b = occb[c % 2]
                    staged = []
                    for g in range(2):
                        qg = qA[:, bq, g, :]
                        for j in range(njt):
                            (Aa, Aab) = SA[pti % 2]; pi = pti % 4; pti += 1
                            k.op("pe", lambda e, j=j, g=g, Aa=Aa, qg=qg: e.matmul(Aa[:, :], lhsT=kcmpT[:, g, j * 128:(j + 1) * 128], rhs=qg, start=True, stop=True),
                                 reads=[kcmpb, qAb], writes=[Aab])
                            k.op("act", lambda e, Aa=Aa, pi=pi: e.activation(out=pT[pi][:], in_=Aa[:, :], func=AF.Exp), reads=[Aab], writes=[pTb[pi]])
                            k.op("dve", lambda e, pi=pi, j=j, g=g: e.tensor_tensor(out=pT[pi][:], in0=pT[pi][:],
                                                                                   in1=e_[:, j, g * 4:(g + 1) * 4, :].rearrange("p h q -> p (h q)"), op=ALU.mult),
                                 reads=[pTb[pi], e_b], writes=[pTb[pi]])
                            staged.append((g, j, pi))
                    for (g, j, pi) in staged:
                        (To, Tob), (Ti, Tib) = CO[g], CI[g]
                        k.op("pe", lambda e, pi=pi, j=j, g=g, To=To: e.matmul(To[0:65, :], lhsT=VC[:, j, g, 0:65], rhs=pT[pi][:], start=(j == 0), stop=(j == njt - 1)),
                             reads=[pTb[pi], VCb], writes=[Tob])
                        k.op("pe", lambda e, pi=pi, j=j, g=g, Ti=Ti: e.matmul(Ti[0:64, :], lhsT=VC[:, j, g, 65:129], rhs=pT[pi][:], start=(j == 0), stop=(j == njt - 1)),
                             reads=[pTb[pi], VCb], writes=[Tib])
                    for g in range(2):
                        (To, Tob), (Ti, Tib) = CO[g], CI[g]
                        k.op("act", lambda e, g=g, To=To: e.activation(out=sTo[g][0:65, :], in_=To[0:65, :], func=AF.Copy), reads=[Tob], writes=[sTob[g]])
                        k.op("act", lambda e, g=g, Ti=Ti: e.activation(out=sTi[g][0:64, :], in_=Ti[0:64, :], func=AF.Copy), reads=[Tib], writes=[sTib[g]])
                    for g in range(2):
                        (To, Tob), (Ti, Tib) = CO[g], CI[g]
                        for h4 in range(4):
                            k.op("pe", lambda e, h4=h4, g=g, To=To: e.transpose(To[:, h4 * 65:h4 * 65 + 65], sTo[g][0:65, h4 * 128:(h4 + 1) * 128], self.identf[0:65, 0:65]),
                                 reads=[sTob[g], self.cb], writes=[Tob])
                        for h4 in range(4):
                            k.op("pe", lambda e, h4=h4, g=g, Ti=Ti: e.transpose(Ti[:, h4 * 65:h4 * 65 + 64], sTi[g][0:64, h4 * 128:(h4 + 1) * 128], self.identf[0:64, 0:64]),
                                 reads=[sTib[g], self.cb], writes=[Tib])
                    chains = [[], []]
                    for g in range(2):
                        ch = chains[g]
                        (To, Tob), (Ti, Tib) = CO[g], CI[g]
                        sm_, smb_, imp_, impb_, cf_, cfb_, m8_ = sm[g], smb[g], imp[g], impb[g], cf[g], cfb[g], m8[g]
                        ch.append((lambda e, sm_=sm_, To=To: e.tensor_scalar(out=sm_[:], in0=To[:, 64:64 + 260:65], scalar1=1e-30, scalar2=None, op0=ALU.max), [Tob], [smb_]))
                        ch.append((lambda e, sm_=sm_: e.reciprocal(out=sm_[:], in_=sm_[:]), [smb_], [smb_]))
                        for h4 in range(4):
                            src = Ti[:, h4 * 65:h4 * 65 + 64]
                            if h4 == 0:
                                ch.append((lambda e, src=src, sm_=sm_, imp_=imp_: e.tensor_scalar(out=imp_[:], in0=src, scalar1=sm_[:, 0:1], scalar2=None, op0=ALU.mult),
                                           [Tib, smb_], [impb_]))
                            else:
                                ch.append((lambda e, src=src, h4=h4, sm_=sm_, imp_=imp_: e.scalar_tensor_tensor(out=imp_[:], in0=src, scalar=sm_[:, h4:h4 + 1], in1=imp_[:],
                                                                                                                 op0=ALU.mult, op1=ALU.add), [Tib, smb_, impb_], [impb_]))
                        sl = slice(62 - 2 * c, 62 - 2 * c + 64)
                        ch.append((lambda e, imp_=imp_, sl=sl: e.tensor_tensor(out=imp_[:], in0=imp_[:], in1=selv[:, sl], op=ALU.mult), [impb_, ncb], [impb_]))
                        ch.append((lambda e, imp_=imp_, sl=sl: e.tensor_tensor(out=imp_[:], in0=imp_[:], in1=sela[:, sl], op=ALU.add), [impb_, ncb], [impb_]))
                        if c >= 1:
                            ch.append((lambda e, imp_=imp_: e.tensor_scalar(out=imp_[:, 0:1], in0=imp_[:, 0:1], scalar1=1e4, scalar2=None, op0=ALU.add), [impb_], [impb_]))
                        ch.append((lambda e, imp_=imp_, m8_=m8_: e.max(out=m8_[:], in_=imp_[:]), [impb_], [impb_]))
                        si = c * 2 + g
                        ch.append((lambda e, si=si, imp_=imp_, m8_=m8_: e.tensor_scalar(out=snegAll[:, si, 64:128], in0=imp_[:], scalar1=m8_[:, 7:8], scalar2=NEG,
                                                                                       op0=ALU.is_lt, op1=ALU.mult), [impb_], [snegAllb[si]]))
                        gview = gn[:, c, g * 12:(g + 1) * 12].rearrange("p (h b) -> p h b", h=4)
                        ch.append((lambda e, cf_=cf_, gview=gview, sm_=sm_: e.tensor_tensor(out=cf_[:], in0=gview[:, :, 0], in1=sm_[:], op=ALU.mult), [gnb, smb_], [cfb_]))
                        for h4 in range(4):
                            hh = g * 4 + h4
                            ch.append((lambda e, h4=h4, hh=hh, To=To, cf_=cf_: e.tensor_scalar(out=oc_[:, hh * 64:(hh + 1) * 64], in0=To[:, h4 * 65:h4 * 65 + 64],
                                                                                               scalar1=cf_[:, h4:h4 + 1], scalar2=None, op0=ALU.mult), [Tob, cfb_], [oc_b]))
                    for i_ in range(max(len(chains[0]), len(chains[1]))):
                        for g in range(2):
                            if i_ < len(chains[g]):
                                fn_, rd_, wr_ = chains[g][i_]
                                k.op("dve", fn_, reads=rd_, writes=wr_)
                    k.dma("sp", self.ocd[c], oc_[:], reads=[oc_b], writes=[self.ocd_b[c]])
            k.barrier()
            qAll = self.sb(st, "nqAll", [128, 4, 2, 512], BF16)
            qTopb = Buf("qTop")
            qBotb = [[Buf("qBot%d_%d" % (b_, g_)) for g_ in range(2)] for b_ in range(4)]
            NPT = 5
            LA = 3
            pT = [self.sb(st, "npT%d" % i, [128, 512], BF16) for i in range(NPT)]
            pTb = [Buf("npT%d" % i) for i in range(NPT)]
            sm = self.sb(st, "nsm", [128, 2, 4], F32); smb = Buf("nsm")
            coef = self.sb(st, "ncoef", [128, 4, 2], F32); coefb = Buf("ncoef")
            oin = [self.sb(st, "noin%d" % i, [128, 512], F32) for i in range(2)]; oinb = [Buf("noin%d" % i) for i in range(2)]
            onsa = self.sb(st, "nonsa", [128, 512], BF16); onsab = Buf("nonsa")
            onT = [self.sb(st, "nonT%d" % i, [128, 4, 128], BF16) for i in range(2)]
            onTb = [Buf("nonT%d" % i) for i in range(2)]
            sTs = [self.sb(st, "nsTs%d" % i, [65, 512], F32) for i in range(2)]; sTsb = [Buf("nsTs%d" % i) for i in range(2)]
            sTw = [self.sb(st, "nsTw%d" % i, [65, 512], F32) for i in range(2)]; sTwb = [Buf("nsTw%d" % i) for i in range(2)]
            SA = [bank["A0"], bank["A1"], bank["A2"], bank["A3"]]
            (Ts, Tsb), (Tw, Twb) = bank["C0"], bank["C1"]
            (Rs, Rsb), (Rw, Rwb) = bank["S"], bank["W"]
            pti = 0
            it = 0

            def finish2(Tt, Tbb, sTt, sTbb, Rr, Rbb):
                k.op("act", lambda e: e.activation(out=sTt[0:65, :], in_=Tt[0:65, :], func=AF.Copy), reads=[Tbb], writes=[sTbb])
                for h4 in range(4):
                    k.op("pe", lambda e, h4=h4: e.transpose(Rr[:, h4 * 65:h4 * 65 + 65], sTt[0:65, h4 * 128:(h4 + 1) * 128], self.identf[0:65, 0:65]),
                         reads=[sTbb, self.cb], writes=[Rbb])

            for c in range(NT):
                bq = c % 4
                if bq == 0:
                    toks = slice(c * 128, (c + 4) * 128)
                    for h in range(8):
                        (X, Xb) = SA[pti % 4]; pti += 1
                        for kc in range(8):
                            k.op("pe", lambda e, kc=kc, h=h, X=X: e.matmul(X[0:64, :], lhsT=Wq[:, kc, h * 64:(h + 1) * 64], rhs=self.hT[:, kc, toks],
                                                                          start=(kc == 0), stop=(kc == 7)), reads=[wqb] + self.hT_b[c:c + 4], writes=[Xb])
                        k.op("act", lambda e, h=h, X=X: e.activation(out=qAll[0:64, :, h // 4, (h % 4) * 128:(h % 4 + 1) * 128],
                                                                     in_=X[0:64, :].rearrange("p (b q) -> p b q", b=4), func=AF.Copy, scale=0.125),
                             reads=[Xb], writes=[qTopb])
                oi = oin[c % 2]; oib = oinb[c % 2]
                k.dma("sp", oi[:], self.ocd[c], reads=[self.ocd_b[c]], writes=[oib])
                for g in range(2):
                    par = it % 2; it += 1
                    si = c * 2 + g
                    qg = qAll[0:64, bq, g, :]
                    qfull = qAll[:, bq, g, :]
                    (X, Xb) = SA[pti % 4]; pti += 1
                    Xbf = X[:, :].bitcast(BF16)
                    k.op("pe", lambda e, si=si: e.transpose(Xbf[:, 0:128], snegAll[:, si, :], self.ident[:]), reads=[snegAllb[si], self.cb], writes=[Xb])
                    k.op("dve", lambda e: e.tensor_copy(out=qAll[64:128, bq, g, :].rearrange("p (h q) -> p h q", h=4),
                                                        in_=Xbf[64:128, 0:128].unsqueeze(1).to_broadcast([64, 4, 128])),
                         reads=[Xb], writes=[qBotb[bq][g]])
                    tiles = [("w", kb) for kb in range(max(0, c - 4), c + 1)] + [("s", kb) for kb in range(c + 1)]
                    nwin = len(tiles) - (c + 1)
                    slots = []

                    def emit_qk(idx):
                        nonlocal pti
                        kind, kb = tiles[idx]
                        (Aa, Aab) = SA[pti % 4]; pi = pti % NPT; pti += 1
                        slots.append((Aa, Aab, pi))
                        kt = slice(kb * 128, (kb + 1) * 128)
                        off = c - kb
                        near = (kind == "w" or off <= 1)
                        if kind == "s":
                            k.op("pe", lambda e: e.matmul(Aa[:, :], lhsT=ksT[:, g, kt], rhs=qfull, start=True, stop=not near),
                                 reads=[ksTb, qTopb, qBotb[bq][g]], writes=[Aab])
                        else:
                            k.op("pe", lambda e: e.matmul(Aa[:, :], lhsT=kwT[:, g, kt], rhs=qg, start=True, stop=not near), reads=[kwTb, qTopb], writes=[Aab])
                        if near:
                            k.op("pe", lambda e: e.matmul(Aa[:, :], lhsT=self.ident[:], rhs=self.EB[:, off, g * 4:(g + 1) * 4, :].rearrange("p h q -> p (h q)"),
                                                          start=False, stop=True), reads=[self.cb], writes=[Aab])

                    def emit_pv(idx):
                        kind, kb = tiles[idx]
                        Aa, Aab, pi = slots[idx]
                        k.op("act", lambda e: e.activation(out=pT[pi][:], in_=Aa[:, :], func=AF.Exp), reads=[Aab], writes=[pTb[pi]])
                        if kind == "s":
                            Tt, Ttb, V, Vb = Ts, Tsb, vsa, vsab
                            first, lastt = (idx == nwin), (idx == len(tiles) - 1)
                        else:
                            Tt, Ttb, V, Vb = Tw, Twb, vwa, vwab
                            first, lastt = (idx == 0), (idx == nwin - 1)
                        k.op("pe", lambda e: e.matmul(Tt[0:65, :], lhsT=V[:, kb, g, :], rhs=pT[pi][:], start=first, stop=lastt),
                             reads=[pTb[pi], Vb], writes=[Ttb])

                    for idx in range(len(tiles) + LA):
                        if idx < len(tiles):
                            emit_qk(idx)
                        if idx >= LA:
                            emit_pv(idx - LA)
                    finish2(Tw, Twb, sTw[par], sTwb[par], Rw, Rwb)
                    finish2(Ts, Tsb, sTs[par], sTsb[par], Rs, Rsb)
                    gview = gn[:, c, g * 12:(g + 1) * 12].rearrange("p (h b) -> p h b", h=4)
                    k.op("dve", lambda e: e.tensor_scalar(out=sm[:, 0, :], in0=Rs[:, 64:64 + 260:65], scalar1=1e-30, scalar2=None, op0=ALU.max), reads=[Rsb, smb], writes=[smb])
                    k.op("dve", lambda e: e.tensor_scalar(out=sm[:, 1, :], in0=Rw[:, 64:64 + 260:65], scalar1=1e-30, scalar2=None, op0=ALU.max), reads=[Rwb, smb], writes=[smb])
                    k.op("dve", lambda e: e.reciprocal(out=sm[:, :, :], in_=sm[:, :, :]), reads=[smb], writes=[smb])
                    k.op("dve", lambda e: e.tensor_tensor(out=coef[:], in0=gview[:, :, 1:3], in1=sm[:, :, :].rearrange("p b h -> p h b"), op=ALU.mult),
                         reads=[gnb, smb, coefb], writes=[coefb])
                    for h4 in range(4):
                        hh = g * 4 + h4
                        k.op("dve", lambda e, h4=h4, hh=hh: e.scalar_tensor_tensor(out=oi[:, hh * 64:(hh + 1) * 64], in0=Rs[:, h4 * 65:h4 * 65 + 64], scalar=coef[:, h4, 0:1],
                                                                                    in1=oi[:, hh * 64:(hh + 1) * 64], op0=ALU.mult, op1=ALU.add), reads=[Rsb, coefb, oib], writes=[oib])
                        k.op("dve", lambda e, h4=h4, hh=hh: e.scalar_tensor_tensor(out=onsa[:, hh * 64:(hh + 1) * 64], in0=Rw[:, h4 * 65:h4 * 65 + 64], scalar=coef[:, h4, 1:2],
                                                                                    in1=oi[:, hh * 64:(hh + 1) * 64], op0=ALU.mult, op1=ALU.add), reads=[Rwb, coefb, oib], writes=[onsab])
                (X, Xb) = SA[pti % 4]; pti += 1
                Xbf = X[:, :].bitcast(BF16)
                for j in range(4):
                    k.op("pe", lambda e, j=j: e.transpose(Xbf[:, j * 128:(j + 1) * 128], onsa[:, j * 128:(j + 1) * 128], self.ident[:]),
                         reads=[onsab, self.cb], writes=[Xb])
                i2 = c % 2
                k.op("act", lambda e, i2=i2: e.activation(out=onT[i2][:], in_=Xbf[:, 0:512].rearrange("p (j c) -> p j c", j=4), func=AF.Copy),
                     reads=[Xb], writes=[onTb[i2]])
                tk = slice(c * 128, (c + 1) * 128)
                k.dma("sp", self.brT[0, :, :, tk].rearrange("c p q -> p c q"), onT[i2][:], reads=[onTb[i2]], writes=[self.brT_b[0][c]])

    def phase_ret(self, l):
        k = self.k
        with ExitStack() as st:
            Wqk = self.sb(st, "rWqk", [128, 8, 512], BF16)
            Wv = self.sb(st, "rWv", [128, 8, 512], BF16)
            Wg = self.sb(st, "rWg", [128, 8, 512], BF16)
            wb = Buf("rW")
            self.load_w(Wqk, self.din["w_in"][l][:, C_QR:C_QR + 512], wb, 8)
            self.load_w(Wv, self.din["w_in"][l][:, C_VR:C_VR + 512], wb, 8)
            self.load_w(Wg, self.din["w_in"][l][:, C_GR:C_GR + 512], wb, 8)
            cos = self.sb(st, "rcos", [128, NT, 32], F32)
            sin = self.sb(st, "rsin", [128, NT, 32], F32)
            dec = self.sb(st, "rdec", [128, 4, 128], F32)
            xi = self.sb(st, "rxi", [64, 4, 128], F32)
            zeta = self.sb(st, "rzeta", [128, 4], F32)
            gch = self.sb(st, "rgch", [64, 4], F32)
            gn = self.sb(st, "rgn", [128, 512], F32)
            rc = Buf("rconst")
            for dst, src in ((cos, "cos"), (sin, "sin"), (dec, "decayT"), (xi, "xi"), (zeta, "zeta"), (gch, "gch")):
                k.dma("sp", dst[:], self.din[src], writes=[rc])
            k.dma("sp", gn[:], self.din["retgn"][l], writes=[rc])
            Sf = self.sb(st, "rSf", [64, 4, 128], F32)
            Sb = self.sb(st, "rSb", [64, 4, 128], BF16)
            Sfb, Sbb = Buf("Sf"), Buf("Sb")
            k.op("dve", lambda e: e.memset(Sf[:], 0.0), writes=[Sfb])
            k.op("dve", lambda e: e.memset(Sb[:], 0.0), writes=[Sbb])
            qk = self.sb(st, "rqk", [128, 512], F32); qkb = Buf("rqk")
            tm = self.sb(st, "rtm", [128, 4, 8, 32], F32); tmb = Buf("rtm")
            rot = self.sb(st, "rrot", [128, 8, 2, 32], F32); rotb = Buf("rrot")
            qkbf = self.sb(st, "rqkbf", [128, 512], BF16); qkbfb = Buf("rqkbf")
            khat = self.sb(st, "rkhat", [128, 4, 64], BF16); khatb = Buf("rkhat")
            qkT = self.sb(st, "rqkT", [64, 8, 128], BF16); qkTb = Buf("rqkT")
            qxiT = self.sb(st, "rqxiT", [64, 4, 128], BF16); qxiTb = Buf("rqxiT")
            qf32 = self.sb(st, "rqf32", [64, 4, 128], F32); qf32b = Buf("rqf32")
            inT = self.sb(st, "rinT", [128, 4, 128], BF16); inTb = Buf("rinT")
            vbf = self.sb(st, "rvbf", [128, 512], BF16); vbfb = Buf("rvbf")
            osb = self.sb(st, "rosb", [128, 512], F32); osbb = Buf("rosb")
            osq = self.sb(st, "rosq", [128, 512], F32); osqb = Buf("rosq")
            sm = self.sb(st, "rsm", [128, 6, 4], F32); smb = Buf("rsm")
            yn = self.sb(st, "ryn", [128, 512], F32); ynb = Buf("ryn")
            gs = self.sb(st, "rgs", [128, 512], F32); gsb = Buf("rgs")
            orb = self.sb(st, "rorb", [128, 512], BF16); orbb = Buf("rorb")
            orT = [self.sb(st, "rorT%d" % i, [128, 4, 128], BF16) for i in range(2)]
            orTb = [Buf("rorT%d" % i) for i in range(2)]
            pqk = self.ps(st, "rpqk", [128, 512], F32); pqkb = Buf("pqk")
            pv = self.ps(st, "rpv", [128, 512], F32); pvb = Buf("pv")
            pg = self.ps(st, "rpg", [128, 512], F32); pgb = Buf("pg")
            pin = self.ps(st, "rpin", [128, 512], F32); pinb = Buf("pin")
            po = self.ps(st, "rpo", [128, 512], F32); pob = Buf("po")
            pkv = self.ps(st, "rpkv", [128, 512], F32); pkvb = Buf("pkv")
            ptr = self.ps(st, "rptr", [128, 1024], BF16); ptrb = Buf("ptr")
            ptr2 = self.ps(st, "rptr2", [128, 1024], BF16); ptr2b = Buf("ptr2")
            for t in range(NT):
                tk = slice(t * 128, (t + 1) * 128)
                for (W, P, PB) in ((Wqk, pqk, pqkb), (Wv, pv, pvb), (Wg, pg, pgb)):
                    for kc in range(8):
                        k.op("pe", lambda e, kc=kc, W=W, P=P: e.matmul(P[:, :], lhsT=self.hT[:, kc, tk], rhs=W[:, kc, :], start=(kc == 0), stop=(kc == 7)),
                             reads=[wb, self.hT_b[t]], writes=[PB])
                self.chk(1)
                k.op("act", lambda e: e.activation(out=qk[:, 0:256], in_=pqk[:, 0:256], func=AF.Copy), reads=[pqkb], writes=[qkb])
                k.op("act", lambda e: e.activation(out=qk[:, 256:512], in_=pqk[:, 256:512], func=AF.Copy, scale=0.125), reads=[pqkb], writes=[qkb])
                xv = qk[:, :].rearrange("p (h two d) -> p h two d", h=8, two=2)
                x1, x2 = xv[:, :, 0, :], xv[:, :, 1, :]
                cb_ = cos[:, t, :].unsqueeze(1).to_broadcast([128, 8, 32])
                sb_ = sin[:, t, :].unsqueeze(1).to_broadcast([128, 8, 32])
                k.op("dve", lambda e: e.tensor_tensor(out=tm[:, 0], in0=x1, in1=cb_, op=ALU.mult), reads=[qkb, rc], writes=[tmb])
                k.op("dve", lambda e: e.tensor_tensor(out=tm[:, 1], in0=x2, in1=sb_, op=ALU.mult), reads=[qkb, rc], writes=[tmb])
                k.op("dve", lambda e: e.tensor_tensor(out=tm[:, 2], in0=x1, in1=sb_, op=ALU.mult), reads=[qkb, rc], writes=[tmb])
                k.op("dve", lambda e: e.tensor_tensor(out=tm[:, 3], in0=x2, in1=cb_, op=ALU.mult), reads=[qkb, rc], writes=[tmb])
                k.op("dve", lambda e: e.tensor_tensor(out=rot[:, :, 0, :], in0=tm[:, 0], in1=tm[:, 1], op=ALU.subtract), reads=[tmb], writes=[rotb])
                k.op("dve", lambda e: e.tensor_tensor(out=rot[:, :, 1, :], in0=tm[:, 2], in1=tm[:, 3], op=ALU.add), reads=[tmb], writes=[rotb])
                self.chk(2)
                rflat = rot[:, :, :, :].rearrange("p h two d -> p (h two d)")
                k.op("act", lambda e: e.activation(out=qkbf[:], in_=rflat, func=AF.Copy), reads=[rotb], writes=[qkbfb])
                k.op("dve", lambda e: e.tensor_tensor(out=khat[:], in0=rflat[:, 256:512].rearrange("p (h d) -> p h d", h=4),
                                                      in1=zeta[:, :].unsqueeze(2).to_broadcast([128, 4, 64]), op=ALU.mult),
                     reads=[rotb, rc], writes=[khatb])
                self.chk(3)
                for j in range(8):
                    k.op("pe", lambda e, j=j: e.transpose(ptr[0:64, j * 128:(j + 1) * 128], qkbf[:, j * 64:(j + 1) * 64], self.ident[:]),
                         reads=[qkbfb, self.cb], writes=[ptrb])
                k.op("act", lambda e: e.activation(out=qkT[:], in_=ptr[0:64, 0:1024].rearrange("p (j c) -> p j c", j=8), func=AF.Copy),
                     reads=[ptrb], writes=[qkTb])
                k.op("act", lambda e: e.activation(out=qf32[:], in_=ptr[0:64, 0:512].rearrange("p (j c) -> p j c", j=4), func=AF.Copy),
                     reads=[ptrb], writes=[qf32b])
                k.op("dve", lambda e: e.tensor_tensor(out=qxiT[:], in0=qf32[:], in1=xi[:], op=ALU.mult),
                     reads=[qf32b, rc], writes=[qxiTb])
                self.chk(4)
                for h in range(4):
                    k.op("pe", lambda e, h=h: e.matmul(pin[:, h * 128:(h + 1) * 128], lhsT=qkT[:, 4 + h, :], rhs=qkT[:, h, :],
                                                       start=True, stop=True), reads=[qkTb], writes=[pinb])
                self.chk(45)
                k.op("dve", lambda e: e.tensor_tensor(out=inT[:], in0=pin[:, :].rearrange("p (h c) -> p h c", h=4), in1=dec[:], op=ALU.mult),
                     reads=[pinb, rc], writes=[inTb])
                self.chk(5)
                k.op("act", lambda e: e.activation(out=vbf[:], in_=pv[:, :], func=AF.Copy), reads=[pvb], writes=[vbfb])
                for h in range(4):
                    rows = slice((h % 2) * 64, (h % 2) * 64 + 64)
                    hc = slice(h * 128, (h + 1) * 128)
                    k.op("pe", lambda e, h=h, hc=hc: e.matmul(po[:, hc], lhsT=inT[:, h, :], rhs=vbf[:, hc], start=True, stop=False),
                         reads=[inTb, vbfb], writes=[pob])
                    k.op("pe", lambda e, h=h, hc=hc: e.matmul(po[:, hc], lhsT=qxiT[:, h, :], rhs=Sb[:, h, :], start=False, stop=True),
                         reads=[qxiTb, Sbb], writes=[pob])
                self.chk(6)
                for h in range(4):
                    hc = slice(h * 128, (h + 1) * 128)
                    k.op("pe", lambda e, h=h, hc=hc: e.matmul(pkv[0:64, hc], lhsT=khat[:, h, :],
                                                             rhs=vbf[:, hc], start=True, stop=True), reads=[khatb, vbfb], writes=[pkvb])
                self.chk(7)
                for h in range(4):
                    rows = slice((h % 2) * 64, (h % 2) * 64 + 64)
                    hc = slice(h * 128, (h + 1) * 128)
                    k.op("dve", lambda e, h=h, hc=hc: e.scalar_tensor_tensor(out=Sf[:, h, :], in0=Sf[:, h, :],
                                                                              scalar=gch[:, h:h + 1], in1=pkv[0:64, hc],
                                                                              op0=ALU.mult, op1=ALU.add),
                         reads=[pkvb, rc, Sfb], writes=[Sfb])
                k.op("act", lambda e: e.activation(out=Sb[:], in_=Sf[:], func=AF.Copy), reads=[Sfb], writes=[Sbb])
                self.chk(8)
                k.op("act", lambda e: e.activation(out=osb[:], in_=po[:, :], func=AF.Copy), reads=[pob], writes=[osbb])
                k.op("act", lambda e: e.activation(out=osq[:], in_=po[:, :], func=AF.Square), reads=[pob], writes=[osqb])
                k.op("dve", lambda e: e.reduce_sum(out=sm[:, 0, :], in_=osb[:, :].rearrange("p (h v) -> p h v", h=4), axis=AX.X), reads=[osbb], writes=[smb])
                k.op("dve", lambda e: e.reduce_sum(out=sm[:, 1, :], in_=osq[:, :].rearrange("p (h v) -> p h v", h=4), axis=AX.X), reads=[osqb, smb], writes=[smb])
                k.op("dve", lambda e: e.tensor_scalar(out=sm[:, 2, :], in0=sm[:, 0, :], scalar1=1.0 / 128, scalar2=None, op0=ALU.mult), reads=[smb], writes=[smb])
                k.op("dve", lambda e: e.tensor_tensor(out=sm[:, 3, :], in0=sm[:, 2, :], in1=sm[:, 2, :], op=ALU.mult), reads=[smb], writes=[smb])
                k.op("dve", lambda e: e.scalar_tensor_tensor(out=sm[:, 3, :], in0=sm[:, 1, :], scalar=1.0 / 128, in1=sm[:, 3, :], op0=ALU.mult, op1=ALU.subtract),
                     reads=[smb], writes=[smb])
                k.op("act", lambda e: e.activation(out=sm[:, 4, :], in_=sm[:, 3, :], func=AF.Sqrt, bias=self.cst[:, 0:1], scale=1.0), reads=[smb, self.cb], writes=[smb])
                k.op("dve", lambda e: e.reciprocal(out=sm[:, 5, :], in_=sm[:, 4, :]), reads=[smb], writes=[smb])
                self.chk(9)
                for h in range(4):
                    hc = slice(h * 128, (h + 1) * 128)
                    k.op("dve", lambda e, h=h, hc=hc: e.tensor_scalar(out=yn[:, hc], in0=osb[:, hc], scalar1=sm[:, 2, h:h + 1], scalar2=sm[:, 5, h:h + 1],
                                                                      op0=ALU.subtract, op1=ALU.mult), reads=[osbb, smb], writes=[ynb])
                k.op("dve", lambda e: e.tensor_tensor(out=yn[:], in0=yn[:], in1=gn[:], op=ALU.mult), reads=[ynb, rc], writes=[ynb])
                self.chk(10)
                k.op("act", lambda e: e.activation(out=gs[:], in_=pg[:, :], func=AF.Silu), reads=[pgb], writes=[gsb])
                k.op("dve", lambda e: e.tensor_tensor(out=orb[:], in0=yn[:], in1=gs[:], op=ALU.mult), reads=[ynb, gsb], writes=[orbb])
                self.chk(11)
                for j in range(4):
                    k.op("pe", lambda e, j=j: e.transpose(ptr2[:, j * 128:(j + 1) * 128], orb[:, j * 128:(j + 1) * 128], self.ident[:]),
                         reads=[orbb, self.cb], writes=[ptr2b])
                i = t % 2
                k.op("act", lambda e, i=i: e.activation(out=orT[i][:], in_=ptr2[:, 0:512].rearrange("p (j c) -> p j c", j=4), func=AF.Copy),
                     reads=[ptr2b], writes=[orTb[i]])
                self.chk(12)
                k.dma("sp", self.brT[1, :, :, tk].rearrange("c p q -> p c q"), orT[i][:], reads=[orTb[i]], writes=[self.brT_b[1][t]])

    def phase_conv(self, l):
        k = self.k
        with ExitStack() as st:
            Wa = self.sb(st, "cWa", [128, 8, 512], BF16)
            Wb = self.sb(st, "cWb", [128, 8, 512], BF16)
            wab = Buf("cW")
            self.load_w(Wa, self.din["w_in"][l][:, C_CA:C_CA + 512], wab, 8)
            self.load_w(Wb, self.din["w_in"][l][:, C_CB:C_CB + 512], wab, 8)
            cw = self.sb(st, "ccw", [128, 4, 31], F32)
            cvec = self.sb(st, "cvec", [128, 3, 4], F32)
            ones = self.sb(st, "cones", [128, 128], F32)
            cc = Buf("cconst")
            k.dma("sp", cw[:], self.din["convw"][l], writes=[cc])
            k.dma("sp", cvec[:, 0, :], self.din["convb"][l], writes=[cc])
            k.dma("sp", cvec[:, 1, :], self.din["convg"][l], writes=[cc])
            k.dma("sp", cvec[:, 2, :], self.din["convbb"][l], writes=[cc])
            k.dma("sp", ones[:], self.din["ones"][:, :], writes=[cc])
            Dg = self.sb(st, "cDg", [128, 4, 31, 128], BF16); dgb = Buf("cDg")
            for ct in range(4):
                for w in range(31):
                    en = "dve" if (w % 2 == 0) else "pool"
                    k.op(en, lambda e, ct=ct, w=w: e.tensor_scalar(out=Dg[:, ct, w, :], in0=self.identf[:], scalar1=cw[:, ct, w:w + 1], scalar2=None, op0=ALU.mult),
                         reads=[cc, self.cb], writes=[dgb])
            u = [self.sb(st, "cu%d" % i, [128, 4, 542], BF16) for i in range(2)]
            ub = [[Buf("cu%d_%d" % (i, ct)) for ct in range(4)] for i in range(2)]
            acc = self.sb(st, "cacc", [128, 4, 512], F32)
            accb = [Buf("cacc%d" % ct) for ct in range(4)]
            sg = [self.sb(st, "csg%d" % i, [128, 512], F32) for i in range(2)]
            sgb = [Buf("csg%d" % i) for i in range(2)]
            ysq = [self.sb(st, "cysq%d" % i, [128, 512], F32) for i in range(2)]
            ysqb = [Buf("cysq%d" % i) for i in range(2)]
            stt = self.sb(st, "cstt", [128, 4, 512], F32)
            sttb = Buf("cstt")
            yn = [self.sb(st, "cyn%d" % i, [128, 512], F32) for i in range(2)]
            ynb = [Buf("cyn%d" % i) for i in range(2)]
            oc = [self.sb(st, "coc%d" % i, [128, 512], BF16) for i in range(2)]
            ocb = [Buf("coc%d" % i) for i in range(2)]
            pa = [self.ps(st, "cpa%d" % i, [128, 512], F32) for i in range(2)]
            pab = [Buf("cpa%d" % i) for i in range(2)]
            pbk = [self.ps(st, "cpb%d" % i, [128, 512], F32) for i in range(2)]
            pbb = [Buf("cpb%d" % i) for i in range(2)]
            pcv = [self.ps(st, "cpc%d" % i, [128, 512], F32) for i in range(2)]
            pcb = [Buf("cpc%d" % i) for i in range(2)]
            s1 = self.ps(st, "cs1", [128, 512], F32)
            s2 = self.ps(st, "cs2", [128, 512], F32)
            s1b, s2b = Buf("cs1"), Buf("cs2")
            for ct in range(4):
                k.op("dve", lambda e, ct=ct: e.memset(u[0][:, ct, 0:30], 0.0), writes=[ub[0][ct]])
            for G in range(8):
                toks = slice(G * 512, (G + 1) * 512)
                hb = self.hT_b[G * 4:(G + 1) * 4]
                ug, ugb = u[G % 2], ub[G % 2]
                for ct in range(4):
                    p = ct % 2
                    cols = slice(ct * 128, (ct + 1) * 128)
                    for kc in range(8):
                        k.op("pe", lambda e, kc=kc, cols=cols, p=p: e.matmul(pa[p][:, :], lhsT=Wa[:, kc, cols], rhs=self.hT[:, kc, toks],
                                                                              start=(kc == 0), stop=(kc == 7)), reads=[wab] + hb, writes=[pab[p]])
                    for kc in range(8):
                        k.op("pe", lambda e, kc=kc, cols=cols, p=p: e.matmul(pbk[p][:, :], lhsT=Wb[:, kc, cols], rhs=self.hT[:, kc, toks],
                                                                              start=(kc == 0), stop=(kc == 7)), reads=[wab] + hb, writes=[pbb[p]])
                    k.op("act", lambda e, p=p: e.activation(out=sg[p][:], in_=pbk[p][:, :], func=AF.Sigmoid), reads=[pbb[p]], writes=[sgb[p]])
                    if G > 0:
                        k.op("pool", lambda e, ct=ct: e.tensor_copy(out=ug[:, ct, 0:30], in_=u[(G - 1) % 2][:, ct, 512:542]),
                             reads=[ub[(G - 1) % 2][ct]], writes=[ugb[ct]])
                    k.op("dve", lambda e, ct=ct, p=p: e.tensor_tensor(out=ug[:, ct, 30:542], in0=pa[p][:, :], in1=sg[p][:], op=ALU.mult),
                         reads=[pab[p], sgb[p]], writes=[ugb[ct]])
                for ct in range(4):
                    p = ct % 2
                    for w in range(31):
                        k.op("pe", lambda e, ct=ct, w=w, p=p: e.matmul(pcv[p][:, :], lhsT=Dg[:, ct, w, :], rhs=ug[:, ct, w:w + 512], start=(w == 0), stop=(w == 30)),
                             reads=[dgb, ugb[ct]], writes=[pcb[p]])
                    k.op("act", lambda e, ct=ct, p=p: e.activation(out=acc[:, ct, :], in_=pcv[p][:, :], func=AF.Identity, bias=cvec[:, 0, ct:ct + 1], scale=1.0),
                         reads=[pcb[p], cc], writes=[accb[ct]])
                    k.op("act", lambda e, ct=ct, p=p: e.activation(out=ysq[p][:], in_=acc[:, ct, :], func=AF.Square), reads=[accb[ct]], writes=[ysqb[p]])
                    k.op("pe", lambda e, ct=ct: e.matmul(s1[:, :], lhsT=ones[:], rhs=acc[:, ct, :], start=(ct == 0), stop=(ct == 3)),
                         reads=[cc, accb[ct]], writes=[s1b])
                    k.op("pe", lambda e, ct=ct, p=p: e.matmul(s2[:, :], lhsT=ones[:], rhs=ysq[p][:], start=(ct == 0), stop=(ct == 3)),
                         reads=[cc, ysqb[p]], writes=[s2b])
                k.op("act", lambda e: e.activation(out=stt[:, 0, :], in_=s1[:, :], func=AF.Copy, scale=1.0 / 512), reads=[s1b], writes=[sttb])
                k.op("dve", lambda e: e.tensor_tensor(out=stt[:, 1, :], in0=stt[:, 0, :], in1=stt[:, 0, :], op=ALU.mult), reads=[sttb], writes=[sttb])
                k.op("dve", lambda e: e.scalar_tensor_tensor(out=stt[:, 1, :], in0=s2[:, :], scalar=1.0 / 512, in1=stt[:, 1, :],
                                                             op0=ALU.mult, op1=ALU.subtract), reads=[s2b, sttb], writes=[sttb])
                k.op("act", lambda e: e.activation(out=stt[:, 3, :], in_=stt[:, 1, :], func=AF.Sqrt, bias=self.cst[:, 0:1], scale=1.0),
                     reads=[sttb, self.cb], writes=[sttb])
                k.op("dve", lambda e: e.reciprocal(out=stt[:, 2, :], in_=stt[:, 3, :]), reads=[sttb], writes=[sttb])
                for ct in range(4):
                    p = ct % 2
                    k.op("dve", lambda e, ct=ct, p=p: e.tensor_tensor(out=yn[p][:], in0=acc[:, ct, :], in1=stt[:, 0, :], op=ALU.subtract),
                         reads=[accb[ct], sttb], writes=[ynb[p]])
                    k.op("pool", lambda e, p=p: e.tensor_tensor(out=yn[p][:], in0=yn[p][:], in1=stt[:, 2, :], op=ALU.mult),
                         reads=[sttb, ynb[p]], writes=[ynb[p]])
                    k.op("act", lambda e, ct=ct, p=p: e.activation(out=oc[p][:], in_=yn[p][:], func=AF.Silu, scale=cvec[:, 1, ct:ct + 1],
                                                                   bias=cvec[:, 2, ct:ct + 1]), reads=[ynb[p], cc], writes=[ocb[p]])
                    k.dma("sp", self.brT[2, ct, :, toks], oc[p][:], reads=[ocb[p]], writes=self.brT_b[2][G * 4:(G + 1) * 4])

    def phase_merge(self, l):
        k = self.k
        for dh in range(2):
            with ExitStack() as st:
                Wg = self.sb(st, "mWg", [128, 8, 3, 512], BF16)
                Wbr = self.sb(st, "mWbr", [128, 3, 4, 512], BF16)
                Wo = self.sb(st, "mWo", [128, 4, 1024], BF16)
                wgb = [Buf("mWg%d" % i) for i in range(3)]
                wbrb = [Buf("mWbr%d" % i) for i in range(3)]
                wob = Buf("mWo")
                for b in range(3):
                    for kc in range(8):
                        c0 = C_MG + b * 1024 + dh * 512
                        k.dma("pool", Wg[:, kc, b, :], self.din["w_in"][l][kc * 128:(kc + 1) * 128, c0:c0 + 512], writes=[wgb[b]])
                    for c4 in range(4):
                        k.dma("pool", Wbr[:, b, c4, :], self.din["w_branch"][l][b][c4 * 128:(c4 + 1) * 128, dh * 512:(dh + 1) * 512], writes=[wbrb[b]])
                for dc in range(4):
                    r0 = dh * 512 + dc * 128
                    k.dma("pool", Wo[:, dc, :], self.din["w_out"][l][r0:r0 + 128, :], writes=[wob])
                brg = [self.sb(st, "mbr%d" % i, [128, 3, 4, 512], BF16) for i in range(2)]
                brgb = [Buf("mbr%d" % i) for i in range(2)]
                mT = self.sb(st, "mmT", [128, 4, 512], BF16)
                mTb = [Buf("mT%d" % i) for i in range(4)]
                gate = [self.sb(st, "mgate%d" % i, [128, 512], F32) for i in range(2)]
                gateb = [Buf("mgate%d" % i) for i in range(2)]
                macc = self.sb(st, "mmacc", [128, 512], F32); maccb = Buf("macc")
                mtmp = self.sb(st, "mmtmp", [128, 512], F32); mtmpb = Buf("mtmp")
                ctx = self.upd_alloc(st, "m", self.din["nmlp"][l] if dh == 1 else None)
                pg = [self.ps(st, "mpg%d" % i, [128, 512], F32) for i in range(2)]
                pgb = [Buf("mpg%d" % i) for i in range(2)]
                pp = [self.ps(st, "mpp%d" % i, [128, 512], F32) for i in range(2)]
                ppb = [Buf("mpp%d" % i) for i in range(2)]
                po = [self.ps(st, "mpo%d" % i, [128, 512], F32) for i in range(2)]
                pob = [Buf("mpo%d" % i) for i in range(2)]
                if dh == 1:
                    ctx["ptr"] = self.ps(st, "mptr", [128, 1024], BF16)
                    ctx["ptr_b"] = Buf("mptr")
                it = 0
                for G in range(8):
                    toks = slice(G * 512, (G + 1) * 512)
                    hb = self.hT_b[G * 4:(G + 1) * 4]
                    bi = G % 2
                    for b in range(3):
                        k.dma("sp", brg[bi][:, b], self.brT[b, :, :, toks].rearrange("c p t -> p c t"),
                              reads=self.brT_b[b][G * 4:(G + 1) * 4], writes=[brgb[bi]])
                    for dc in range(4):
                        for b in range(3):
                            p = it % 2
                            it += 1
                            for kc in range(8):
                                k.op("pe", lambda e, kc=kc, b=b, dc=dc, p=p: e.matmul(pg[p][:, :], lhsT=Wg[:, kc, b, dc * 128:(dc + 1) * 128],
                                                                                     rhs=self.hT[:, kc, toks], start=(kc == 0), stop=(kc == 7)),
                                     reads=[wgb[b]] + hb, writes=[pgb[p]])
                            for c4 in range(4):
                                k.op("pe", lambda e, c4=c4, b=b, dc=dc, p=p: e.matmul(pp[p][:, :], lhsT=Wbr[:, b, c4, dc * 128:(dc + 1) * 128],
                                                                                     rhs=brg[bi][:, b, c4, :], start=(c4 == 0), stop=(c4 == 3)),
                                     reads=[wbrb[b], brgb[bi]], writes=[ppb[p]])
                            k.op("act", lambda e, p=p: e.activation(out=gate[p][:], in_=pg[p][:, :], func=AF.Sigmoid), reads=[pgb[p]], writes=[gateb[p]])
                            if b == 0:
                                k.op("dve", lambda e, p=p: e.tensor_tensor(out=macc[:], in0=pp[p][:, :], in1=gate[p][:], op=ALU.mult),
                                     reads=[ppb[p], gateb[p]], writes=[maccb])
                            else:
                                k.op("dve", lambda e, p=p: e.tensor_tensor(out=mtmp[:], in0=pp[p][:, :], in1=gate[p][:], op=ALU.mult),
                                     reads=[ppb[p], gateb[p]], writes=[mtmpb])
                                if b == 1:
                                    k.op("pool", lambda e: e.tensor_tensor(out=macc[:], in0=macc[:], in1=mtmp[:], op=ALU.add),
                                         reads=[mtmpb, maccb], writes=[maccb])
                                else:
                                    k.op("pool", lambda e, dc=dc: e.tensor_tensor(out=mT[:, dc, :], in0=macc[:], in1=mtmp[:], op=ALU.add),
                                         reads=[mtmpb, maccb], writes=[mTb[dc]])
                    for tt in range(4):
                        t = G * 4 + tt
                        for nh in range(2):
                            for dc in range(4):
                                k.op("pe", lambda e, nh=nh, dc=dc, tt=tt: e.matmul(po[nh][:, :], lhsT=mT[:, dc, tt * 128:(tt + 1) * 128],
                                                                                  rhs=Wo[:, dc, nh * 512:(nh + 1) * 512], start=(dc == 0), stop=(dc == 3)),
                                     reads=[wob, mTb[dc]], writes=[pob[nh]])
                        self.x_update(ctx, t, [po[0][:, :], po[1][:, :]], pob, "norm" if dh == 1 else None)
                self.upd_flush(ctx)
                k.barrier()


_PROG_CACHE = {}


def _get_prog(**kw):
    key = tuple(sorted((k, str(v)) for k, v in kw.items()))
    if key not in _PROG_CACHE:
        _PROG_CACHE[key] = Prog(**kw)
    return _PROG_CACHE[key]


def kernel(**inputs):
    inp = {k: np.asarray(v) for k, v in inputs.items()}
    x = np.ascontiguousarray(inp["x"], dtype=np.float32)
    shared = _host_inputs(inp)
    prog = _get_prog()
    in_maps = []
    for c in range(8):
        m = dict(shared)
        m["x"] = x[c]
        in_maps.append(m)
    res = run_bass_kernel_spmd(prog.nc, in_maps, core_ids=list(range(8)))
    return np.stack([np.asarray(r["out"], dtype=np.float32) for r in res.results], axis=0)
```

```python
import numpy as np
from contextlib import ExitStack
import concourse.bass as bass
import concourse.mybir as mybir
from concourse.bass_utils import run_bass_kernel_spmd

F32 = mybir.dt.float32
BF16 = mybir.dt.bfloat16
AF = mybir.ActivationFunctionType
ALU = mybir.AluOpType
AX = mybir.AxisListType

S = 4096
D = 1024
NT = 32
DEPTH = 2
IN_TOTAL = 6936
EPS = 1e-6
NEG = -30000.0
C_QN, C_KC, C_VC, C_KS, C_VS, C_KW, C_VW, C_GN = 0, 512, 640, 768, 896, 1024, 1152, 1280
C_QR, C_KR, C_VR, C_GR, C_CA, C_CB, C_MG = 1304, 1560, 1816, 2328, 2840, 3352, 3864

SAME_ENGINE_SYNC = True


class Buf:
    __slots__ = ("w", "r", "name", "wd")

    def __init__(self, name=""):
        self.w = None
        self.r = {}
        self.name = name
        self.wd = False


class _Sem:
    def __init__(self, sem, name):
        self.sem = sem
        self.count = 0
        self.name = name


class Eng(_Sem):
    def __init__(self, name, handle, sem):
        super().__init__(sem, name)
        self.h = handle
        self.waited = {}


class K:
    def __init__(self, nc, stack, n_dma_sems=64):
        self.nc = nc
        self.engs = {}
        for name, h in (("pe", nc.tensor), ("act", nc.scalar), ("dve", nc.vector),
                        ("pool", nc.gpsimd), ("sp", nc.sync)):
            sem = stack.enter_context(nc.semaphore("sem_" + name))
            self.engs[name] = Eng(name, h, sem)
        self.dsems = [_Sem(stack.enter_context(nc.semaphore("dsem%d" % i)), "d%d" % i)
                      for i in range(n_dma_sems)]
        self.dpool = {"pool": self.dsems[:n_dma_sems // 2], "sp": self.dsems[n_dma_sems // 2:]}
        self.dnext = {"pool": 0, "sp": 0}
        self.n_ops = 0
        self.muted = False

    def _wait_deps(self, E, reads, writes):
        deps = {}

        def need(tok):
            if tok is None:
                return
            s, v = tok
            if deps.get(s, 0) < v:
                deps[s] = v
        for b in reads:
            for t in b.w or ():
                need(t)
        for b in writes:
            for t in b.w or ():
                need(t)
            for t in b.r.values():
                need(t)
        for s, v in deps.items():
            if s is E and (E.name in ("pe", "sp") or not SAME_ENGINE_SYNC):
                continue
            if E.waited.get(s, 0) < v:
                E.h.wait_ge(s.sem, v)
                E.waited[s] = v

    def _mark(self, tok, reads, writes, is_dma=False):
        for b in reads:
            b.r[tok[0]] = tok
        for b in writes:
            if is_dma and b.w and getattr(b, "wd", False) and len(b.w) < 24:
                b.w = b.w + [tok]
            else:
                b.w = [tok]
            b.wd = is_dma
            b.r = {}

    def op(self, en, fn, reads=(), writes=()):
        if self.muted:
            return None
        E = self.engs[en]
        self._wait_deps(E, reads, writes)
        ins = fn(E.h)
        E.count += 1
        ins.then_inc(E.sem, 1)
        self._mark((E, E.count), reads, writes)
        self.n_ops += 1
        return ins

    def dma(self, en, out, in_, reads=(), writes=(), **kw):
        if self.muted:
            return None
        E = self.engs[en]
        self._wait_deps(E, reads, writes)
        pool_ = self.dpool[en]
        d = pool_[self.dnext[en]]
        self.dnext[en] = (self.dnext[en] + 1) % len(pool_)
        if d.count and E.waited.get(d, 0) < d.count:
            E.h.wait_ge(d.sem, d.count)
            E.waited[d] = d.count
        ins = E.h.dma_start(out=out, in_=in_, **kw)
        d.count += 16
        ins.then_inc(d.sem, 16)
        self._mark((d, d.count), reads, writes, is_dma=True)
        self.n_ops += 1
        return ins

    def barrier(self):
        allsems = list(self.engs.values()) + self.dsems
        for E in self.engs.values():
            for s in allsems:
                if s is E or s.count == 0:
                    continue
                if E.waited.get(s, 0) < s.count:
                    E.h.wait_ge(s.sem, s.count)
                    E.waited[s] = s.count


def _rel_bucket_np(dist):
    n = np.maximum(dist, 0)
    nf = np.maximum(n, 1).astype(np.float32)
    large = 16 + (np.log(nf / np.float32(16)) / np.float32(np.log(8.0)) * np.float32(16)).astype(np.int32)
    large = np.minimum(large, 31)
    return np.where(n < 16, n, large).astype(np.int64)


_CONST_CACHE = {}


def _host_consts():
    if _CONST_CACHE:
        return _CONST_CACHE
    c = {}
    i = np.arange(128)
    c["ident"] = np.eye(128, dtype=np.float32)
    c["i4"] = np.tile(np.eye(128, dtype=np.float32), (1, 4))
    mw = np.zeros((128, 5, 128), np.float32)
    jj, ii = np.meshgrid(i, i, indexing="ij")
    mw[:, 0, :] = (ii >= jj)
    mw[:, 1:4, :] = 1.0
    mw[:, 4, :] = (ii < jj)
    c["maskw"] = mw
    dist_w = 128 * np.arange(5)[None, :, None] + ii[:, None, :] - jj[:, None, :]
    c["_bucket_w"] = _rel_bucket_np(dist_w)
    m = np.arange(504)
    dist_c = i[None, :] - 16 * (m[:, None] - 248) - 31
    c["maskc"] = (dist_c >= 0).astype(np.float32)
    c["_bucket_c"] = _rel_bucket_np(dist_c)
    n = np.arange(256)
    s = np.arange(64)
    ov = ((16 * n[:, None] <= 64 * s[None, :] + 63) & (16 * n[:, None] + 31 >= 64 * s[None, :])).astype(np.float32)
    ov[255] = 0.0
    c["overlap"] = ov.reshape(2, 128, 64).transpose(1, 0, 2).copy()
    j = np.arange(126)
    sp = j[None, :] - 62
    cur = (i[:, None] >= 64).astype(np.int64)
    valid = sp <= cur
    forced = (sp == cur) | (sp == cur - 1)
    c["selvalid"] = valid.astype(np.float32)
    c["seladd"] = np.where(forced, 1e4, np.where(valid, 0.0, -1e4)).astype(np.float32)
    half = 32
    inv = (10000.0 ** (-np.arange(half, dtype=np.float32) / half)).astype(np.float32)
    pos = np.arange(S, dtype=np.float32)
    ang = (pos[:, None] * inv[None, :]).astype(np.float32)
    c["cos"] = np.cos(ang).astype(np.float32).reshape(NT, 128, 32).transpose(1, 0, 2).copy()
    c["sin"] = np.sin(ang).astype(np.float32).reshape(NT, 128, 32).transpose(1, 0, 2).copy()
    log_g = np.log1p(-np.exp2(-5.0 - np.arange(4, dtype=np.float32))).astype(np.float32)
    diff = i[None, :] - i[:, None]
    dec = np.where(diff[None] >= 0, np.exp(log_g[:, None, None] * np.maximum(diff[None], 0)), 0.0)
    c["decayT"] = dec.transpose(1, 0, 2).astype(np.float32).copy()
    xi = np.exp(log_g[:, None] * (i[None, :] + 1)).astype(np.float32)
    c["xi"] = np.ascontiguousarray(np.broadcast_to(xi[None, :, :], (64, 4, 128))).astype(np.float32)
    c["zeta"] = np.exp(log_g[None, :] * (127 - i[:, None])).astype(np.float32)
    gch = np.exp(log_g * 128).astype(np.float32)
    c["gch"] = np.ascontiguousarray(np.broadcast_to(gch[None, :], (64, 4))).astype(np.float32)
    c["ones"] = np.ones((128, 128), np.float32)
    c["rfull"] = (np.arange(S)[None, :] // 64 == np.arange(64)[:, None]).astype(np.float32)
    _CONST_CACHE.update(c)
    return c


CONST_SHAPES = {
    "ident": [128, 128], "i4": [128, 512], "maskw": [128, 5, 128], "maskc": [504, 128],
    "overlap": [128, 2, 64], "selvalid": [128, 126], "seladd": [128, 126],
    "cos": [128, NT, 32], "sin": [128, NT, 32], "decayT": [128, 4, 128], "xi": [64, 4, 128],
    "zeta": [128, 4], "gch": [64, 4], "ones": [128, 128], "rfull": [64, S],
    "bias_w": [128, 5, 8, 128], "bias_c": [504, 8, 128], "b31": [128, 8],
}

W_SHAPES = {
    "w_in": [DEPTH, D, IN_TOTAL], "w_branch": [DEPTH, 3, 512, D], "w_out": [DEPTH, D, D],
    "w_ff1": [DEPTH, D, 4 * D], "w_ff2": [DEPTH, 4 * D, D],
    "cmp_w1_k": [DEPTH, 32, 64, 128], "cmp_w1_v": [DEPTH, 32, 64, 128],
    "cmp_w2_k": [DEPTH, 128, 64], "cmp_w2_v": [DEPTH, 128, 64],
    "cmp_pe_kT": [DEPTH, 64, 32], "cmp_pe_vT": [DEPTH, 64, 32],
    "nmix": [DEPTH, 128, D], "nmlp": [DEPTH, 128, D], "nfin": [128, D],
    "retgn": [DEPTH, 128, 512],
    "convw": [DEPTH, 128, 4, 31], "convb": [DEPTH, 128, 4], "convg": [DEPTH, 128, 4], "convbb": [DEPTH, 128, 4],
}


def _host_inputs(inp):
    c = _host_consts()
    f = lambda a: np.ascontiguousarray(a, dtype=np.float32)
    out = {k: f(v) for k, v in c.items() if not k.startswith("_")}
    rt = f(inp["rel_table"])
    out["bias_w"] = f(rt[c["_bucket_w"]].transpose(0, 1, 3, 2))
    out["bias_c"] = f(rt[c["_bucket_c"]].transpose(0, 2, 1))
    out["b31"] = f(np.broadcast_to(rt[31][None, :], (128, 8)))
    for kname in ("w_in", "w_branch", "w_out", "w_ff1", "w_ff2", "cmp_w1_k", "cmp_w1_v", "cmp_w2_k", "cmp_w2_v"):
        out[kname] = f(inp[kname])
    out["cmp_pe_kT"] = f(np.transpose(inp["cmp_pe_k"], (0, 2, 1)))
    out["cmp_pe_vT"] = f(np.transpose(inp["cmp_pe_v"], (0, 2, 1)))
    out["nmix"] = f(np.broadcast_to(inp["norm_mix"][:, None, :], (DEPTH, 128, D)))
    out["nmlp"] = f(np.broadcast_to(inp["norm_mlp"][:, None, :], (DEPTH, 128, D)))
    out["nfin"] = f(np.broadcast_to(inp["norm_final"][None, :], (128, D)))
    out["retgn"] = f(np.broadcast_to(inp["ret_gn"][:, None, :], (DEPTH, 128, 512)))
    out["convw"] = f(np.transpose(inp["conv_w"].reshape(DEPTH, 31, 4, 128), (0, 3, 2, 1)))
    for a, b in (("convb", "conv_b"), ("convg", "conv_ln_g"), ("convbb", "conv_ln_b")):
        out[a] = f(np.transpose(inp[b].reshape(DEPTH, 4, 128), (0, 2, 1)))
    return out


class _Stop(Exception):
    pass


class Prog:
    def __init__(self, n_layers=DEPTH, phases=("nsa", "ret", "conv", "merge", "ffn"), dbg=False):
        self.n_layers = n_layers
        self.phases = phases
        self.dbg = dbg
        nc = self.nc = bass.Bass("TRN2", target_bir_lowering=False)
        self.din = {}
        self.din["x"] = nc.dram_tensor("x", [S, D], F32, kind="ExternalInput").ap()
        for name, shp in list(CONST_SHAPES.items()) + list(W_SHAPES.items()):
            self.din[name] = nc.dram_tensor(name, shp, F32, kind="ExternalInput").ap()
        self.out = nc.dram_tensor("out", [S, D], F32, kind="ExternalOutput").ap()
        skind = "ExternalOutput" if dbg else "Internal"
        self.xres = nc.dram_tensor("xres", [S, D], F32, kind=skind).ap()
        self.brT = nc.dram_tensor("brT", [3, 4, 128, S], BF16, kind=skind).ap()
        self.ebc_d = nc.dram_tensor("ebc_d", [504, 8, 128], BF16, kind=skind).ap()
        self.dbg_d = nc.dram_tensor("dbg_d", [128, 2048], F32, kind=skind).ap()
        self.ocd = nc.dram_tensor("ocd", [NT, 128, 512], F32, kind="Internal").ap()
        self.ocd_b = [Buf("ocd%d" % t) for t in range(NT)]
        self.xres_b = [Buf("xres%d" % t) for t in range(NT)]
        self.brT_b = [[Buf("brT%d_%d" % (b, t)) for t in range(NT)] for b in range(3)]
        self.ebc_db = Buf("ebc_d")
        self.out_b = Buf("out")
        with ExitStack() as st:
            self.st = st
            self.k = K(nc, st)
            self.build()

    def sb(self, st, name, shape, dt):
        self._uid = getattr(self, "_uid", 0) + 1
        return st.enter_context(self.nc.sbuf_tensor("s%d_%s" % (self._uid, name), shape, dt))

    def ps(self, st, name, shape, dt):
        self._uid = getattr(self, "_uid", 0) + 1
        return st.enter_context(self.nc.psum_tensor("p%d_%s" % (self._uid, name), shape, dt))

    def chk(self, n):
        import os
        v = os.environ.get("RSTOP")
        if v is not None and int(v) == n:
            self.k.muted = True

    def load_const(self, dst, src, buf, eng="sp"):
        self.k.dma(eng, dst, src, writes=[buf])

    def build(self):
        k, nc, st = self.k, self.nc, self.st
        self.hT = self.sb(st, "hT_all", [128, 8, S], BF16)
        self.hT_b = [Buf("hT%d" % t) for t in range(NT)]
        self.ident = self.sb(st, "ident", [128, 128], BF16)
        self.i4 = self.sb(st, "i4", [128, 512], BF16)
        self.identf = self.sb(st, "identf", [128, 128], F32)
        self.cst = self.sb(st, "cst", [128, 4], F32)
        self.EB = self.sb(st, "EB", [128, 5, 8, 128], BF16)
        self.cb = Buf("consts")
        k.dma("pool", self.ident[:], self.din["ident"][:, :], writes=[self.cb])
        k.dma("pool", self.i4[:], self.din["i4"][:, :], writes=[self.cb])
        k.dma("sp", self.identf[:], self.din["ident"][:, :], writes=[self.cb])
        k.op("dve", lambda e: e.memset(self.cst[:, 0:1], EPS), writes=[self.cb])
        k.op("dve", lambda e: e.memset(self.cst[:, 1:2], 0.0), writes=[self.cb])
        k.op("dve", lambda e: e.memset(self.cst[:, 2:3], 1.0), writes=[self.cb])
        self.phase_bias_tables()
        k.barrier()
        self.phase0()
        k.barrier()
        for l in range(self.n_layers):
            last = (l == DEPTH - 1)
            for ph, fn in (("nsa", lambda: self.phase_nsa(l)), ("ret", lambda: self.phase_ret(l)), ("conv", lambda: self.phase_conv(l)),
                           ("merge", lambda: self.phase_merge(l)), ("ffn", lambda: self.phase_ffn(l, last))):
                if ph in self.phases:
                    with nc.named_scope("%s%d" % (ph, l)):
                        fn()
                        k.muted = False
                        k.barrier()
        k.barrier()

    def rms_alloc(self, st, tag):
        r = {}
        r["junk"] = self.sb(st, "rjunk" + tag, [128, D], BF16)
        r["ss"] = self.sb(st, "rss" + tag, [128, 4], F32)
        r["hb2"] = [self.sb(st, "rhb%d" % i + tag, [128, D], BF16) for i in range(2)]
        r["hbb2"] = [Buf("rmshb%d" % i + tag) for i in range(2)]
        r["i"] = 0
        r["b"] = Buf("rms" + tag)
        return r

    def rms_stats(self, r, src, src_bufs):
        k = self.k
        ss = r["ss"]
        k.op("dve", lambda e: e.memset(ss[:, 0:1], 0.0), writes=[r["b"]])
        k.op("act", lambda e: e.activation(out=r["junk"][:], in_=src, func=AF.Square, accum_out=ss[:, 0:1]),
             reads=list(src_bufs) + [r["b"]], writes=[r["b"]])
        k.op("act", lambda e: e.activation(out=ss[:, 1:2], in_=ss[:, 0:1], func=AF.Sqrt, bias=self.cst[:, 0:1], scale=1.0 / D),
             reads=[r["b"], self.cb], writes=[r["b"]])
        k.op("dve", lambda e: e.reciprocal(out=ss[:, 2:3], in_=ss[:, 1:2]), reads=[r["b"]], writes=[r["b"]])
        return ss[:, 2:3]

    def rms_to_hT(self, r, src, src_bufs, gain, gain_buf, t, ptr, ptr_b):
        i = self.rms_part1(r, src, src_bufs, gain, gain_buf)
        self.rms_part2(r, i, t, ptr, ptr_b)

    def rms_part1(self, r, src, src_bufs, gain, gain_buf):
        k = self.k
        rstd = self.rms_stats(r, src, src_bufs)
        i = r["i"] = (r["i"] + 1) % 2
        hb = r["hb2"][i]
        k.op("dve", lambda e: e.scalar_tensor_tensor(out=hb[:], in0=src, scalar=rstd, in1=gain, op0=ALU.mult, op1=ALU.mult),
             reads=list(src_bufs) + [r["b"], gain_buf], writes=[r["hbb2"][i]])
        return i

    def rms_part2(self, r, i, t, ptr, ptr_b):
        k = self.k
        hb = r["hb2"][i]
        for c in range(8):
            k.op("pe", lambda e, c=c: e.transpose(ptr[:, c * 128:(c + 1) * 128], hb[:, c * 128:(c + 1) * 128], self.ident[:]),
                 reads=[r["hbb2"][i], self.cb], writes=[ptr_b])
        k.op("act", lambda e: e.activation(out=self.hT[:, :, t * 128:(t + 1) * 128],
                                           in_=ptr[:, :].rearrange("p (c q) -> p c q", c=8), func=AF.Copy),
             reads=[ptr_b], writes=[self.hT_b[t]])

    def phase_bias_tables(self):
        k = self.k
        with ExitStack() as st:
            bw = self.sb(st, "bw", [128, 5, 8, 128], F32)
            mw = self.sb(st, "mw", [128, 5, 128], F32)
            b31 = self.sb(st, "b31", [128, 8], F32)
            bb = Buf("bw")
            k.dma("sp", bw[:], self.din["bias_w"][:, :, :, :], writes=[bb])
            k.dma("sp", mw[:], self.din["maskw"][:, :, :], writes=[bb])
            k.dma("sp", b31[:], self.din["b31"][:, :], writes=[bb])
            for off in range(5):
                k.op("dve", lambda e, off=off: e.tensor_tensor(out=bw[:, off], in0=bw[:, off],
                                                               in1=b31[:, :].unsqueeze(2).to_broadcast([128, 8, 128]), op=ALU.subtract),
                     reads=[bb], writes=[bb])
                k.op("dve", lambda e, off=off: e.tensor_tensor(out=bw[:, off], in0=bw[:, off],
                                                               in1=mw[:, off:off + 1, :].to_broadcast([128, 8, 128]), op=ALU.mult),
                     reads=[bb], writes=[bb])
                k.op("dve", lambda e, off=off: e.tensor_scalar(out=mw[:, off, :], in0=mw[:, off, :], scalar1=-NEG, scalar2=NEG, op0=ALU.mult, op1=ALU.add),
                     reads=[bb], writes=[bb])
                k.op("dve", lambda e, off=off: e.tensor_tensor(out=self.EB[:, off], in0=bw[:, off],
                                                               in1=mw[:, off:off + 1, :].to_broadcast([128, 8, 128]), op=ALU.add),
                     reads=[bb], writes=[self.cb])
            bc = self.sb(st, "bc", [126, 8, 128], F32)
            mc = self.sb(st, "mc", [126, 128], F32)
            bcb = self.sb(st, "bcb", [126, 8, 128], BF16)
            cbuf = Buf("bc")
            for r4 in range(4):
                rows = slice(r4 * 126, (r4 + 1) * 126)
                k.dma("sp", bc[:], self.din["bias_c"][rows, :, :], writes=[cbuf])
                k.dma("sp", mc[:], self.din["maskc"][rows, :], writes=[cbuf])
                k.op("dve", lambda e: e.tensor_tensor(out=bc[:], in0=bc[:], in1=b31[0:126, :].unsqueeze(2).to_broadcast([126, 8, 128]),
                                                      op=ALU.subtract), reads=[cbuf, bb], writes=[cbuf])
                k.op("act", lambda e: e.activation(out=bc[:], in_=bc[:], func=AF.Exp), reads=[cbuf], writes=[cbuf])
                k.op("dve", lambda e: e.tensor_tensor(out=bcb[:], in0=bc[:], in1=mc[:, :].unsqueeze(1).to_broadcast([126, 8, 128]),
                                                      op=ALU.mult), reads=[cbuf], writes=[cbuf])
                k.dma("sp", self.ebc_d[rows, :, :], bcb[:], reads=[cbuf], writes=[self.ebc_db])
            k.barrier()

    def phase0(self):
        k = self.k
        with ExitStack() as st:
            r = self.rms_alloc(st, "p0")
            gain = self.sb(st, "gain0", [128, D], F32)
            gb = Buf("gain0")
            k.dma("sp", gain[:], self.din["nmix"][0], writes=[gb])
            xt = [self.sb(st, "p0x%d" % i, [128, D], F32) for i in range(2)]
            xb = [Buf("p0x%d" % i) for i in range(2)]
            ptr = self.ps(st, "p0tr", [128, 1024], BF16)
            ptr_b = Buf("p0tr")
            for t in range(NT):
                i = t % 2
                k.dma("sp", xt[i][:], self.din["x"][t * 128:(t + 1) * 128, :], writes=[xb[i]])
                k.dma("pool", self.xres[t * 128:(t + 1) * 128, :], xt[i][:], reads=[xb[i]], writes=[self.xres_b[t]])
                self.rms_to_hT(r, xt[i][:], [xb[i]], gain[:], gb, t, ptr, ptr_b)

    def load_w(self, dst, src, buf, kc):
        for c in range(kc):
            self.k.dma("pool", dst[:, c, :], src[c * 128:(c + 1) * 128, :], writes=[buf])

    def x_update(self, ctx, t, psum_halves, psum_bufs, hook):
        k = self.k
        i = ctx["i"] = (ctx.get("i", 0) + 1) % 2
        xt, xb = ctx["xt"][i], ctx["xb"][i]
        k.dma("sp", xt[:], self.xres[t * 128:(t + 1) * 128, :], reads=[self.xres_b[t]], writes=[xb])
        for h in range(2):
            k.op("dve", lambda e, h=h: e.tensor_tensor(out=xt[:, h * 512:(h + 1) * 512], in0=psum_halves[h],
                                                       in1=xt[:, h * 512:(h + 1) * 512], op=ALU.add),
                 reads=[psum_bufs[h], xb], writes=[xb])
        if hook != "final":
            k.dma("pool", self.xres[t * 128:(t + 1) * 128, :], xt[:], reads=[xb], writes=[self.xres_b[t]])
        if hook is None:
            return
        r = ctx["rms"]
        if hook == "final":
            rstd = self.rms_stats(r, xt[:], [xb])
            ot = ctx["ot"]
            k.op("dve", lambda e: e.scalar_tensor_tensor(out=ot[:], in0=xt[:], scalar=rstd, in1=ctx["gain"][:], op0=ALU.mult, op1=ALU.mult),
                 reads=[xb, r["b"], ctx["gain_b"]], writes=[ctx["ot_b"]])
            k.dma("pool", self.out[t * 128:(t + 1) * 128, :], ot[:], reads=[ctx["ot_b"]], writes=[self.out_b])
        else:
            i2 = self.rms_part1(r, xt[:], [xb], ctx["gain"][:], ctx["gain_b"])
            self.upd_flush(ctx)
            ctx["pending"] = (i2, t)

    def upd_flush(self, ctx):
        p = ctx.pop("pending", None)
        if p is not None:
            self.rms_part2(ctx["rms"], p[0], p[1], ctx["ptr"], ctx["ptr_b"])

    def upd_alloc(self, st, tag, gain_src, final=False):
        ctx = {}
        ctx["xt"] = [self.sb(st, "ux%s%d" % (tag, i), [128, D], F32) for i in range(2)]
        ctx["xb"] = [Buf("ux%d" % i) for i in range(2)]
        if gain_src is not None:
            ctx["rms"] = self.rms_alloc(st, "u" + tag)
            ctx["gain"] = self.sb(st, "ug" + tag, [128, D], F32)
            ctx["gain_b"] = Buf("ug")
            self.k.dma("sp", ctx["gain"][:], gain_src, writes=[ctx["gain_b"]])
            if final:
                ctx["ot"] = self.sb(st, "uo" + tag, [128, D], F32)
                ctx["ot_b"] = Buf("uo")
        return ctx

    def phase_ffn(self, l, last):
        k = self.k
        for fh in range(2):
            with ExitStack() as st:
                W1 = self.sb(st, "W1", [128, 8, 2048], BF16)
                W2 = self.sb(st, "W2", [128, 16, 1024], BF16)
                w1b = [Buf("W1_%d" % i) for i in range(4)]
                w2b = [Buf("W2_%d" % i) for i in range(4)]
                for kc in range(8):
                    k.dma("pool", W1[:, kc, :], self.din["w_ff1"][l][kc * 128:(kc + 1) * 128, fh * 2048:(fh + 1) * 2048], writes=w1b)
                for fc in range(16):
                    r0 = fh * 2048 + fc * 128
                    k.dma("pool", W2[:, fc, :], self.din["w_ff2"][l][r0:r0 + 128, :], writes=[w2b[fc // 4]])
                actT = self.sb(st, "actT", [128, 16, 512], BF16)
                act_b = [Buf("act%d" % i) for i in range(16)]
                rl = [self.sb(st, "rl%d" % i, [128, 512], F32) for i in range(2)]
                rl_b = [Buf("rl%d" % i) for i in range(2)]
                hook = None
                gain_src = None
                if fh == 1:
                    hook = "final" if last else "norm"
                    gain_src = self.din["nfin"][:, :] if last else self.din["nmix"][l + 1]
                ctx = self.upd_alloc(st, "f", gain_src, final=(fh == 1 and last))
                pb = [self.ps(st, "fpb%d" % i, [128, 512], F32) for i in range(6)]
                pbb = [Buf("fpb%d" % i) for i in range(6)]
                if hook == "norm":
                    ctx["ptr"] = self.ps(st, "fptr", [128, 1024], BF16)
                    ctx["ptr_b"] = Buf("fptr")
                for G in range(8):
                    toks = slice(G * 512, (G + 1) * 512)
                    hb = self.hT_b[G * 4:(G + 1) * 4]
                    for fc in range(16):
                        p = fc % 2
                        for kc in range(8):
                            k.op("pe", lambda e, kc=kc, fc=fc, p=p: e.matmul(pb[p][:, :], lhsT=W1[:, kc, fc * 128:(fc + 1) * 128],
                                                                               rhs=self.hT[:, kc, toks], start=(kc == 0), stop=(kc == 7)),
                                 reads=[w1b[fc // 4]] + hb, writes=[pbb[p]])
                        k.op("act", lambda e, p=p: e.activation(out=rl[p][:], in_=pb[p][:, :], func=AF.Relu), reads=[pbb[p]], writes=[rl_b[p]])
                        k.op("dve", lambda e, p=p, fc=fc: e.tensor_tensor(out=actT[:, fc, :], in0=rl[p][:], in1=rl[p][:], op=ALU.mult),
                             reads=[rl_b[p]], writes=[act_b[fc]])
                    for tt in range(4):
                        t = G * 4 + tt
                        pp = [2 + 2 * (tt % 2), 3 + 2 * (tt % 2)]
                        for nh in range(2):
                            for fc in range(16):
                                k.op("pe", lambda e, nh=nh, fc=fc, tt=tt: e.matmul(pb[pp[nh]][:, :], lhsT=actT[:, fc, tt * 128:(tt + 1) * 128],
                                                                                  rhs=W2[:, fc, nh * 512:(nh + 1) * 512], start=(fc == 0), stop=(fc == 15)),
                                     reads=[w2b[fc // 4], act_b[fc]], writes=[pbb[pp[nh]]])
                        self.x_update(ctx, t, [pb[pp[0]][:, :], pb[pp[1]][:, :]], [pbb[pp[0]], pbb[pp[1]]], hook)
                self.upd_flush(ctx)
                k.barrier()

    def phase_nsa(self, l):
        k = self.k
        win = self.din["w_in"][l]
        with ExitStack() as st:
            Wq = self.sb(st, "nWq", [128, 8, 512], BF16); wqb = Buf("nWq")
            ksT = self.sb(st, "nksT", [128, 2, S], BF16); ksTb = Buf("ksT")
            kwT = self.sb(st, "nkwT", [64, 2, S], BF16); kwTb = Buf("kwT")
            vsa = self.sb(st, "nvsa", [128, NT, 2, 65], BF16); vsab = Buf("vsa")
            vwa = self.sb(st, "nvwa", [128, NT, 2, 65], BF16); vwab = Buf("vwa")
            gn = self.sb(st, "ngn", [128, NT, 24], F32); gnb = Buf("gn")
            kcmpT = self.sb(st, "nkcmpT", [64, 2, 256], BF16); kcmpb = Buf("kcmpT")
            VC = self.sb(st, "nVC", [128, 2, 2, 129], BF16); VCb = Buf("VC")
            selv = self.sb(st, "nselv", [128, 126], F32)
            sela = self.sb(st, "nsela", [128, 126], F32)
            ovl = self.sb(st, "novl", [128, 2, 64], F32)
            ncb = Buf("nconst")
            k.dma("sp", selv[:], self.din["selvalid"], writes=[ncb])
            k.dma("sp", sela[:], self.din["seladd"], writes=[ncb])
            k.dma("sp", ovl[:], self.din["overlap"], writes=[ncb])
            A = [self.ps(st, "nA%d" % i, [128, 512], F32) for i in range(4)]
            Ab = [Buf("nA%d" % i) for i in range(4)]
            for g in range(2):
                k.dma("pool", ksT[64:128, g, :], self.din["rfull"], writes=[ksTb])
            OC = [self.ps(st, "nOC%d" % i, [128, 512], F32) for i in range(2)]
            OCb = [Buf("nOC%d" % i) for i in range(2)]
            OS = self.ps(st, "nOS", [128, 512], F32); OSb = Buf("OS")
            OW = self.ps(st, "nOW", [128, 512], F32); OWb = Buf("OW")
            Q = OS; Qb = OSb
            R0bf = OS[:, :].bitcast(BF16)
            R1bf = OW[:, :].bitcast(BF16)
            k.op("dve", lambda e: e.memset(vsa[:], 1.0), writes=[vsab])
            k.op("dve", lambda e: e.memset(vwa[:], 1.0), writes=[vwab])
            k.op("dve", lambda e: e.memset(kcmpT[:], 0.0), writes=[kcmpb])
            k.op("dve", lambda e: e.memset(VC[:], 0.0), writes=[VCb])
            for j in range(2):
                for g in range(2):
                    k.op("dve", lambda e, j=j, g=g: e.memset(VC[:, j, g, 64:65], 1.0), writes=[VCb])
                    k.op("dve", lambda e, j=j, g=g: e.tensor_copy(out=VC[:, j, g, 65:129], in_=ovl[:, j, :]), reads=[ncb], writes=[VCb])
            with ExitStack() as st2:
                Wk = self.sb(st2, "nWk", [128, 8, 512], BF16); wkb = Buf("nWk")
                for i, c0 in enumerate((C_KC, C_VC, C_KS, C_KW)):
                    for kc in range(8):
                        k.dma("pool", Wk[:, kc, i * 128:(i + 1) * 128], win[kc * 128:(kc + 1) * 128, c0:c0 + 128], writes=[wkb])
                Wtm = self.sb(st2, "nWtm", [128, 8, 280], BF16); wtb = Buf("nWtm")
                for (o, c0, n) in ((0, C_VS, 128), (128, C_VW, 128), (256, C_GN, 24)):
                    for kc in range(8):
                        k.dma("pool", Wtm[:, kc, o:o + n], win[kc * 128:(kc + 1) * 128, c0:c0 + n], writes=[wtb])
                w1 = [self.sb(st2, "nw1%d" % i, [64, 32, 128], BF16) for i in range(2)]
                w2k = self.sb(st2, "nw2k", [128, 64], BF16)
                w2v = self.sb(st2, "nw2v", [128, 64], BF16)
                peT = [self.sb(st2, "npeT%d" % i, [64, 32], BF16) for i in range(2)]
                cwb = Buf("cmpw")
                for i, nm in enumerate(("cmp_w1_k", "cmp_w1_v")):
                    k.dma("pool", w1[i][:], self.din[nm][l].rearrange("l d f -> d l f"), writes=[cwb])
                k.dma("pool", w2k[:], self.din["cmp_w2_k"][l], writes=[cwb])
                k.dma("pool", w2v[:], self.din["cmp_w2_v"][l], writes=[cwb])
                k.dma("pool", peT[0][:], self.din["cmp_pe_kT"][l], writes=[cwb])
                k.dma("pool", peT[1][:], self.din["cmp_pe_vT"][l], writes=[cwb])
                self.load_w(Wq, win[:, C_QN:C_QN + 512], wqb, 8)
                for t in range(NT):
                    tk = slice(t * 128, (t + 1) * 128)
                    P = A[t % 2]; PB = Ab[t % 2]
                    for kc in range(8):
                        k.op("pe", lambda e, kc=kc, P=P: e.matmul(P[:, 0:280], lhsT=self.hT[:, kc, tk], rhs=Wtm[:, kc, :], start=(kc == 0), stop=(kc == 7)),
                             reads=[wtb, self.hT_b[t]], writes=[PB])
                    k.op("act", lambda e, P=P, t=t: e.activation(out=vsa[:, t, :, 0:64], in_=P[:, 0:128].rearrange("p (g d) -> p g d", g=2), func=AF.Copy),
                         reads=[PB], writes=[vsab])
                    k.op("act", lambda e, P=P, t=t: e.activation(out=vwa[:, t, :, 0:64], in_=P[:, 128:256].rearrange("p (g d) -> p g d", g=2), func=AF.Copy),
                         reads=[PB], writes=[vwab])
                    k.op("act", lambda e, P=P, t=t: e.activation(out=gn[:, t, :], in_=P[:, 256:280], func=AF.Sigmoid), reads=[PB], writes=[gnb])
                it = 0
                for (dst, dstb, wi) in ((ksT, ksTb, 2), (kwT, kwTb, 3)):
                    for g in range(2):
                        for G in range(8):
                            toks = slice(G * 512, (G + 1) * 512)
                            P = OC[it % 2]; PB = OCb[it % 2]; it += 1
                            for kc in range(8):
                                k.op("pe", lambda e, kc=kc, P=P, wi=wi, g=g: e.matmul(P[0:64, :], lhsT=Wk[:, kc, wi * 128 + g * 64:wi * 128 + g * 64 + 64],
                                                                                     rhs=self.hT[:, kc, toks], start=(kc == 0), stop=(kc == 7)),
                                     reads=[wkb] + self.hT_b[G * 4:(G + 1) * 4], writes=[PB])
                            k.op("act", lambda e, P=P, dst=dst, g=g: e.activation(out=dst[0:64, g, toks], in_=P[0:64, :], func=AF.Copy), reads=[PB], writes=[dstb])
                cT = [self.sb(st2, "ncT%d" % i, [64, S], BF16) for i in range(2)]
                cTb = [Buf("ncT%d" % i) for i in range(2)]
                hx = self.sb(st2, "nhx", [128, 4, 256], F32); hxb = Buf("nhx")
                hidb = self.sb(st2, "nhidb", [128, 256], BF16); hidbb = Buf("nhidb")
                cbias = self.sb(st2, "ncbias", [128, 2], F32); cbb = Buf("ncbias")
                for kv in range(2):
                    for lidx in range(32):
                        k.op("pe", lambda e, kv=kv, lidx=lidx: e.matmul(Q[:, kv:kv + 1], lhsT=w1[kv][:, lidx, :], rhs=peT[kv][:, lidx:lidx + 1],
                                                                       start=(lidx == 0), stop=(lidx == 31)), reads=[cwb], writes=[Qb])
                k.op("act", lambda e: e.activation(out=cbias[:], in_=Q[:, 0:2], func=AF.Copy), reads=[Qb], writes=[cbb])
                for g in range(2):
                    for kv in range(2):
                        for G in range(8):
                            toks = slice(G * 512, (G + 1) * 512)
                            P = OC[it % 2]; PB = OCb[it % 2]; it += 1
                            for kc in range(8):
                                k.op("pe", lambda e, kc=kc, P=P, kv=kv, g=g: e.matmul(P[0:64, :], lhsT=Wk[:, kc, kv * 128 + g * 64:kv * 128 + g * 64 + 64],
                                                                                     rhs=self.hT[:, kc, toks], start=(kc == 0), stop=(kc == 7)),
                                     reads=[wkb] + self.hT_b[G * 4:(G + 1) * 4], writes=[PB])
                            k.op("act", lambda e, P=P, kv=kv: e.activation(out=cT[kv][:, toks], in_=P[0:64, :], func=AF.Copy), reads=[PB], writes=[cTb[kv]])
                    for kv in range(2):
                        P = A[kv]; PB = Ab[kv]
                        for lidx in range(32):
                            k.op("pe", lambda e, kv=kv, lidx=lidx, P=P: e.matmul(P[:, 0:255], lhsT=w1[kv][:, lidx, :], rhs=cT[kv][:, lidx:lidx + 16 * 254 + 1:16],
                                                                                start=(lidx == 0), stop=(lidx == 31)), reads=[cwb, cTb[kv]], writes=[PB])
                        x_ = hx[:, 0, 0:255]
                        k.op("act", lambda e, P=P, kv=kv: e.activation(out=x_, in_=P[:, 0:255], func=AF.Identity, bias=cbias[:, kv:kv + 1], scale=1.0),
                             reads=[PB, cbb], writes=[hxb])
                        k.op("dve", lambda e: e.tensor_tensor(out=hx[:, 1, 0:255], in0=x_, in1=x_, op=ALU.mult), reads=[hxb], writes=[hxb])
                        k.op("dve", lambda e: e.tensor_scalar(out=hx[:, 1, 0:255], in0=hx[:, 1, 0:255], scalar1=0.044715, scalar2=1.0, op0=ALU.mult, op1=ALU.add),
                             reads=[hxb], writes=[hxb])
                        k.op("dve", lambda e: e.tensor_tensor(out=hx[:, 2, 0:255], in0=hx[:, 1, 0:255], in1=x_, op=ALU.mult), reads=[hxb], writes=[hxb])
                        k.op("act", lambda e: e.activation(out=hx[:, 3, 0:255], in_=hx[:, 2, 0:255], func=AF.Sigmoid, scale=1.5957691216057308),
                             reads=[hxb], writes=[hxb])
                        k.op("dve", lambda e: e.tensor_tensor(out=hidb[:, 0:255], in0=hx[:, 3, 0:255], in1=x_, op=ALU.mult), reads=[hxb], writes=[hidbb])
                        if kv == 0:
                            k.op("pe", lambda e: e.matmul(Q[0:64, 0:255], lhsT=w2k[:], rhs=hidb[:, 0:255], start=True, stop=True), reads=[cwb, hidbb], writes=[Qb])
                            k.op("act", lambda e, g=g: e.activation(out=kcmpT[:, g, 0:255], in_=Q[0:64, 0:255], func=AF.Copy), reads=[Qb], writes=[kcmpb])
                        else:
                            for j in range(2):
                                nn = 128 if j == 0 else 127
                                k.op("pe", lambda e, j=j, nn=nn: e.matmul(Q[0:nn, j * 64:(j + 1) * 64], lhsT=hidb[:, j * 128:j * 128 + nn], rhs=w2v[:],
                                                                         start=True, stop=True), reads=[cwb, hidbb], writes=[Qb])
                                k.op("act", lambda e, j=j, nn=nn, g=g: e.activation(out=VC[0:nn, j, g, 0:64], in_=Q[0:nn, j * 64:(j + 1) * 64], func=AF.Copy),
                                     reads=[Qb], writes=[VCb])
            k.barrier()
            snegAll = self.sb(st, "nsnegAll", [128, NT * 2, 128], BF16); snegAllb = [Buf("sneg%d" % i) for i in range(NT * 2)]
            k.op("dve", lambda e: e.memset(snegAll[:], 0.0), writes=snegAllb)
            bank = {"A0": (A[0], Ab[0]), "A1": (A[1], Ab[1]), "A2": (A[2], Ab[2]), "A3": (A[3], Ab[3]),
                    "C0": (OC[0], OCb[0]), "C1": (OC[1], OCb[1]), "S": (OS, OSb), "W": (OW, OWb)}

            def finish(Tt, Tbb, sTt, sTbb, rows):
                k.op("act", lambda e: e.activation(out=sTt[0:rows, :], in_=Tt[0:rows, :], func=AF.Copy), reads=[Tbb], writes=[sTbb])
                for h4 in range(4):
                    k.op("pe", lambda e, h4=h4: e.transpose(Tt[:, h4 * 65:h4 * 65 + rows], sTt[0:rows, h4 * 128:(h4 + 1) * 128], self.identf[0:rows, 0:rows]),
                         reads=[sTbb, self.cb], writes=[Tbb])

            with ExitStack() as st3:
                qA = self.sb(st3, "nqA", [64, 4, 2, 512], BF16); qAb = Buf("nqA")
                ebc = [self.sb(st3, "nebc%d" % i, [128, 2, 8, 128], BF16) for i in range(2)]
                ebcb = [Buf("nebc%d" % i) for i in range(2)]
                pT = [self.sb(st3, "napT%d" % i, [128, 512], BF16) for i in range(4)]
                pTb = [Buf("napT%d" % i) for i in range(4)]
                sm = [self.sb(st3, "nasm%d" % i, [128, 4], F32) for i in range(2)]; smb = [Buf("nasm%d" % i) for i in range(2)]
                imp = [self.sb(st3, "naimp%d" % i, [128, 64], F32) for i in range(2)]; impb = [Buf("naimp%d" % i) for i in range(2)]
                m8 = [self.sb(st3, "nam8%d" % i, [128, 8], F32) for i in range(2)]
                cf = [self.sb(st3, "nacf%d" % i, [128, 4], F32) for i in range(2)]; cfb = [Buf("nacf%d" % i) for i in range(2)]
                occ = [self.sb(st3, "naocc%d" % i, [128, 512], F32) for i in range(2)]; occb = [Buf("naocc%d" % i) for i in range(2)]
                sTo = [self.sb(st3, "nasTo%d" % i, [65, 512], F32) for i in range(2)]; sTob = [Buf("nasTo%d" % i) for i in range(2)]
                sTi = [self.sb(st3, "nasTi%d" % i, [64, 512], F32) for i in range(2)]; sTib = [Buf("nasTi%d" % i) for i in range(2)]
                SA = [bank["A0"], bank["A1"]]
                CO = [bank["A2"], bank["C0"]]
                CI = [bank["A3"], bank["C1"]]
                QP, QPb = bank["S"]
                pti = 0
                it = 0
                for c in range(NT):
                    bq = c % 4
                    if bq == 0:
                        toks = slice(c * 128, (c + 4) * 128)
                        for h in range(8):
                            for kc in range(8):
                                k.op("pe", lambda e, kc=kc, h=h: e.matmul(QP[0:64, :], lhsT=Wq[:, kc, h * 64:(h + 1) * 64], rhs=self.hT[:, kc, toks],
                                                                         start=(kc == 0), stop=(kc == 7)), reads=[wqb] + self.hT_b[c:c + 4], writes=[QPb])
                            k.op("act", lambda e, h=h: e.activation(out=qA[:, :, h // 4, (h % 4) * 128:(h % 4 + 1) * 128],
                                                                    in_=QP[0:64, :].rearrange("p (b q) -> p b q", b=4), func=AF.Copy, scale=0.125),
                                 reads=[QPb], writes=[qAb])
                    e_ = ebc[c % 2]; e_b = ebcb[c % 2]
                    njt = 2 if c >= 16 else 1
                    for j in range(njt):
                        r0 = 248 - 8 * c + 128 * j
                        k.dma("sp", e_[:, j], self.ebc_d[r0:r0 + 128, :, :], reads=[self.ebc_db], writes=[e_b])
                    oc_ = occ[c % 2]; oc_b = occb[c % 2]
                    staged = []
                    for g in range(2):
                        qg = qA[:, bq, g, :]
                        for j in range(njt):
                            (Aa, Aab) = SA[pti % 2]; pi = pti % 4; pti += 1
                            k.op("pe", lambda e, j=j, g=g, Aa=Aa, qg=qg: e.matmul(Aa[:, :], lhsT=kcmpT[:, g, j * 128:(j + 1) * 128], rhs=qg, start=True, stop=True),
                                 reads=[kcmpb, qAb], writes=[Aab])
                            k.op("act", lambda e, Aa=Aa, pi=pi: e.activation(out=pT[pi][:], in_=Aa[:, :], func=AF.Exp), reads=[Aab], writes=[pTb[pi]])
                            k.op("dve", lambda e, pi=pi, j=j, g=g: e.tensor_tensor(out=pT[pi][:], in0=pT[pi][:],
                                                                                   in1=e_[:, j, g * 4:(g + 1) * 4, :].rearrange("p h q -> p (h q)"), op=ALU.mult),
                                 reads=[pTb[pi], e_b], writes=[pTb[pi]])
                            staged.append((g, j, pi))
                    for (g, j, pi) in staged:
                        (To, Tob), (Ti, Tib) = CO[g], CI[g]
                        k.op("pe", lambda e, pi=pi, j=j, g=g, To=To: e.matmul(To[0:65, :], lhsT=VC[:, j, g, 0:65], rhs=pT[pi][:], start=(j == 0), stop=(j == njt - 1)),
                             reads=[pTb[pi], VCb], writes=[Tob])
                        k.op("pe", lambda e, pi=pi, j=j, g=g, Ti=Ti: e.matmul(Ti[0:64, :], lhsT=VC[:, j, g, 65:129], rhs=pT[pi][:], start=(j == 0), stop=(j == njt - 1)),
                             reads=[pTb[pi], VCb], writes=[Tib])
                    for g in range(2):
                        (To, Tob), (Ti, Tib) = CO[g], CI[g]
                        k.op("act", lambda e, g=g, To=To: e.activation(out=sTo[g][0:65, :], in_=To[0:65, :], func=AF.Copy), reads=[Tob], writes=[sTob[g]])
                        k.op("act", lambda e, g=g, Ti=Ti: e.activation(out=sTi[g][0:64, :], in_=Ti[0:64, :], func=AF.Copy), reads=[Tib], writes=[sTib[g]])
                    for g in range(2):
                        (To, Tob), (Ti, Tib) = CO[g], CI[g]
                        for h4 in range(4):
                            k.op("pe", lambda e, h4=h4, g=g, To=To: e.transpose(To[:, h4 * 65:h4 * 65 + 65], sTo[g][0:65, h4 * 128:(h4 + 1) * 128], self.identf[0:65, 0:65]),
                                 reads=[sTob[g], self.cb], writes=[Tob])
                        for h4 in range(4):
                            k.op("pe", lambda e, h4=h4, g=g, Ti=Ti: e.transpose(Ti[:, h4 * 65:h4 * 65 + 64], sTi[g][0:64, h4 * 128:(h4 + 1) * 128], self.identf[0:64, 0:64]),
                                 reads=[sTib[g], self.cb], writes=[Tib])
                    chains = [[], []]
                    for g in range(2):
                        ch = chains[g]
                        (To, Tob), (Ti, Tib) = CO[g], CI[g]
                        sm_, smb_, imp_, impb_, cf_, cfb_, m8_ = sm[g], smb[g], imp[g], impb[g], cf[g], cfb[g], m8[g]
                        ch.append((lambda e, sm_=sm_, To=To: e.tensor_scalar(out=sm_[:], in0=To[:, 64:64 + 260:65], scalar1=1e-30, scalar2=None, op0=ALU.max), [Tob], [smb_]))
                        ch.append((lambda e, sm_=sm_: e.reciprocal(out=sm_[:], in_=sm_[:]), [smb_], [smb_]))
                        for h4 in range(4):
                            src = Ti[:, h4 * 65:h4 * 65 + 64]
                            if h4 == 0:
                                ch.append((lambda e, src=src, sm_=sm_, imp_=imp_: e.tensor_scalar(out=imp_[:], in0=src, scalar1=sm_[:, 0:1], scalar2=None, op0=ALU.mult),
                                           [Tib, smb_], [impb_]))
                            else:
                                ch.append((lambda e, src=src, h4=h4, sm_=sm_, imp_=imp_: e.scalar_tensor_tensor(out=imp_[:], in0=src, scalar=sm_[:, h4:h4 + 1], in1=imp_[:],
                                                                                                                 op0=ALU.mult, op1=ALU.add), [Tib, smb_, impb_], [impb_]))
                        sl = slice(62 - 2 * c, 62 - 2 * c + 64)
                        ch.append((lambda e, imp_=imp_, sl=sl: e.tensor_tensor(out=imp_[:], in0=imp_[:], in1=selv[:, sl], op=ALU.mult), [impb_, ncb], [impb_]))
                        ch.append((lambda e, imp_=imp_, sl=sl: e.tensor_tensor(out=imp_[:], in0=imp_[:], in1=sela[:, sl], op=ALU.add), [impb_, ncb], [impb_]))
                        if c >= 1:
                            ch.append((lambda e, imp_=imp_: e.tensor_scalar(out=imp_[:, 0:1], in0=imp_[:, 0:1], scalar1=1e4, scalar2=None, op0=ALU.add), [impb_], [impb_]))
                        ch.append((lambda e, imp_=imp_, m8_=m8_: e.max(out=m8_[:], in_=imp_[:]), [impb_], [impb_]))
                        si = c * 2 + g
                        ch.append((lambda e, si=si, imp_=imp_, m8_=m8_: e.tensor_scalar(out=snegAll[:, si, 64:128], in0=imp_[:], scalar1=m8_[:, 7:8], scalar2=NEG,
                                                                                       op0=ALU.is_lt, op1=ALU.mult), [impb_], [snegAllb[si]]))
                        gview = gn[:, c, g * 12:(g + 1) * 12].rearrange("p (h b) -> p h b", h=4)
                        ch.append((lambda e, cf_=cf_, gview=gview, sm_=sm_: e.tensor_tensor(out=cf_[:], in0=gview[:, :, 0], in1=sm_[:], op=ALU.mult), [gnb, smb_], [cfb_]))
                        for h4 in range(4):
                            hh = g * 4 + h4
                            ch.append((lambda e, h4=h4, hh=hh, To=To, cf_=cf_: e.tensor_scalar(out=oc_[:, hh * 64:(hh + 1) * 64], in0=To[:, h4 * 65:h4 * 65 + 64],
                                                                                               scalar1=cf_[:, h4:h4 + 1], scalar2=None, op0=ALU.mult), [Tob, cfb_], [oc_b]))
                    for i_ in range(max(len(chains[0]), len(chains[1]))):
                        for g in range(2):
                            if i_ < len(chains[g]):
                                fn_, rd_, wr_ = chains[g][i_]
                                k.op("dve", fn_, reads=rd_, writes=wr_)
                    k.dma("sp", self.ocd[c], oc_[:], reads=[oc_b], writes=[self.ocd_b[c]])
            k.barrier()
            qAll = self.sb(st, "nqAll", [128, 4, 2, 512], BF16)
            qTopb = Buf("qTop")
            qBotb = [[Buf("qBot%d_%d" % (b_, g_)) for g_ in range(2)] for b_ in range(4)]
            NPT = 5
            LA = 3
            pT = [self.sb(st, "npT%d" % i, [128, 512], BF16) for i in range(NPT)]
            pTb = [Buf("npT%d" % i) for i in range(NPT)]
            sm = self.sb(st, "nsm", [128, 2, 4], F32); smb = Buf("nsm")
            coef = self.sb(st, "ncoef", [128, 4, 2], F32); coefb = Buf("ncoef")
            oin = [self.sb(st, "noin%d" % i, [128, 512], F32) for i in range(2)]; oinb = [Buf("noin%d" % i) for i in range(2)]
            onsa = self.sb(st, "nonsa", [128, 512], BF16); onsab = Buf("nonsa")
            onT = [self.sb(st, "nonT%d" % i, [128, 4, 128], BF16) for i in range(2)]
            onTb = [Buf("nonT%d" % i) for i in range(2)]
            sTs = [self.sb(st, "nsTs%d" % i, [65, 512], F32) for i in range(2)]; sTsb = [Buf("nsTs%d" % i) for i in range(2)]
            sTw = [self.sb(st, "nsTw%d" % i, [65, 512], F32) for i in range(2)]; sTwb = [Buf("nsTw%d" % i) for i in range(2)]
            SA = [bank["A0"], bank["A1"], bank["A2"], bank["A3"]]
            (Ts, Tsb), (Tw, Twb) = bank["C0"], bank["C1"]
            (Rs, Rsb), (Rw, Rwb) = bank["S"], bank["W"]
            pti = 0
            it = 0

            def finish2(Tt, Tbb, sTt, sTbb, Rr, Rbb):
                k.op("act", lambda e: e.activation(out=sTt[0:65, :], in_=Tt[0:65, :], func=AF.Copy), reads=[Tbb], writes=[sTbb])
                for h4 in range(4):
                    k.op("pe", lambda e, h4=h4: e.transpose(Rr[:, h4 * 65:h4 * 65 + 65], sTt[0:65, h4 * 128:(h4 + 1) * 128], self.identf[0:65, 0:65]),
                         reads=[sTbb, self.cb], writes=[Rbb])

            for c in range(NT):
                bq = c % 4
                if bq == 0:
                    toks = slice(c * 128, (c + 4) * 128)
                    for h in range(8):
                        (X, Xb) = SA[pti % 4]; pti += 1
                        for kc in range(8):
                            k.op("pe", lambda e, kc=kc, h=h, X=X: e.matmul(X[0:64, :], lhsT=Wq[:, kc, h * 64:(h + 1) * 64], rhs=self.hT[:, kc, toks],
                                                                          start=(kc == 0), stop=(kc == 7)), reads=[wqb] + self.hT_b[c:c + 4], writes=[Xb])
                        k.op("act", lambda e, h=h, X=X: e.activation(out=qAll[0:64, :, h // 4, (h % 4) * 128:(h % 4 + 1) * 128],
                                                                     in_=X[0:64, :].rearrange("p (b q) -> p b q", b=4), func=AF.Copy, scale=0.125),
                             reads=[Xb], writes=[qTopb])
                oi = oin[c % 2]; oib = oinb[c % 2]
                k.dma("sp", oi[:], self.ocd[c], reads=[self.ocd_b[c]], writes=[oib])
                for g in range(2):
                    par = it % 2; it += 1
                    si = c * 2 + g
                    qg = qAll[0:64, bq, g, :]
                    qfull = qAll[:, bq, g, :]
                    (X, Xb) = SA[pti % 4]; pti += 1
                    Xbf = X[:, :].bitcast(BF16)
                    k.op("pe", lambda e, si=si: e.transpose(Xbf[:, 0:128], snegAll[:, si, :], self.ident[:]), reads=[snegAllb[si], self.cb], writes=[Xb])
                    k.op("dve", lambda e: e.tensor_copy(out=qAll[64:128, bq, g, :].rearrange("p (h q) -> p h q", h=4),
                                                        in_=Xbf[64:128, 0:128].unsqueeze(1).to_broadcast([64, 4, 128])),
                         reads=[Xb], writes=[qBotb[bq][g]])
                    tiles = [("w", kb) for kb in range(max(0, c - 4), c + 1)] + [("s", kb) for kb in range(c + 1)]
                    nwin = len(tiles) - (c + 1)
                    slots = []

                    def emit_qk(idx):
                        nonlocal pti
                        kind, kb = tiles[idx]
                        (Aa, Aab) = SA[pti % 4]; pi = pti % NPT; pti += 1
                        slots.append((Aa, Aab, pi))
                        kt = slice(kb * 128, (kb + 1) * 128)
                        off = c - kb
                        near = (kind == "w" or off <= 1)
                        if kind == "s":
                            k.op("pe", lambda e: e.matmul(Aa[:, :], lhsT=ksT[:, g, kt], rhs=qfull, start=True, stop=not near),
                                 reads=[ksTb, qTopb, qBotb[bq][g]], writes=[Aab])
                        else:
                            k.op("pe", lambda e: e.matmul(Aa[:, :], lhsT=kwT[:, g, kt], rhs=qg, start=True, stop=not near), reads=[kwTb, qTopb], writes=[Aab])
                        if near:
                            k.op("pe", lambda e: e.matmul(Aa[:, :], lhsT=self.ident[:], rhs=self.EB[:, off, g * 4:(g + 1) * 4, :].rearrange("p h q -> p (h q)"),
                                                          start=False, stop=True), reads=[self.cb], writes=[Aab])

                    def emit_pv(idx):
                        kind, kb = tiles[idx]
                        Aa, Aab, pi = slots[idx]
                        k.op("act", lambda e: e.activation(out=pT[pi][:], in_=Aa[:, :], func=AF.Exp), reads=[Aab], writes=[pTb[pi]])
                        if kind == "s":
                            Tt, Ttb, V, Vb = Ts, Tsb, vsa, vsab
                            first, lastt = (idx == nwin), (idx == len(tiles) - 1)
                        else:
                            Tt, Ttb, V, Vb = Tw, Twb, vwa, vwab
                            first, lastt = (idx == 0), (idx == nwin - 1)
                        k.op("pe", lambda e: e.matmul(Tt[0:65, :], lhsT=V[:, kb, g, :], rhs=pT[pi][:], start=first, stop=lastt),
                             reads=[pTb[pi], Vb], writes=[Ttb])

                    for idx in range(len(tiles) + LA):
                        if idx < len(tiles):
                            emit_qk(idx)
                        if idx >= LA:
                            emit_pv(idx - LA)
                    finish2(Tw, Twb, sTw[par], sTwb[par], Rw, Rwb)
                    finish2(Ts, Tsb, sTs[par], sTsb[par], Rs, Rsb)
                    gview = gn[:, c, g * 12:(g + 1) * 12].rearrange("p (h b) -> p h b", h=4)
                    k.op("dve", lambda e: e.tensor_scalar(out=sm[:, 0, :], in0=Rs[:, 64:64 + 260:65], scalar1=1e-30, scalar2=None, op0=ALU.max), reads=[Rsb, smb], writes=[smb])
                    k.op("dve", lambda e: e.tensor_scalar(out=sm[:, 1, :], in0=Rw[:, 64:64 + 260:65], scalar1=1e-30, scalar2=None, op0=ALU.max), reads=[Rwb, smb], writes=[smb])
                    k.op("dve", lambda e: e.reciprocal(out=sm[:, :, :], in_=sm[:, :, :]), reads=[smb], writes=[smb])
                    k.op("dve", lambda e: e.tensor_tensor(out=coef[:], in0=gview[:, :, 1:3], in1=sm[:, :, :].rearrange("p b h -> p h b"), op=ALU.mult),
                         reads=[gnb, smb, coefb], writes=[coefb])
                    for h4 in range(4):
                        hh = g * 4 + h4
                        k.op("dve", lambda e, h4=h4, hh=hh: e.scalar_tensor_tensor(out=oi[:, hh * 64:(hh + 1) * 64], in0=Rs[:, h4 * 65:h4 * 65 + 64], scalar=coef[:, h4, 0:1],
                                                                                    in1=oi[:, hh * 64:(hh + 1) * 64], op0=ALU.mult, op1=ALU.add), reads=[Rsb, coefb, oib], writes=[oib])
                        k.op("dve", lambda e, h4=h4, hh=hh: e.scalar_tensor_tensor(out=onsa[:, hh * 64:(hh + 1) * 64], in0=Rw[:, h4 * 65:h4 * 65 + 64], scalar=coef[:, h4, 1:2],
                                                                                    in1=oi[:, hh * 64:(hh + 1) * 64], op0=ALU.mult, op1=ALU.add), reads=[Rwb, coefb, oib], writes=[onsab])
                (X, Xb) = SA[pti % 4]; pti += 1
                Xbf = X[:, :].bitcast(BF16)
                for j in range(4):
                    k.op("pe", lambda e, j=j: e.transpose(Xbf[:, j * 128:(j + 1) * 128], onsa[:, j * 128:(j + 1) * 128], self.ident[:]),
                         reads=[onsab, self.cb], writes=[Xb])
                i2 = c % 2
                k.op("act", lambda e, i2=i2: e.activation(out=onT[i2][:], in_=Xbf[:, 0:512].rearrange("p (j c) -> p j c", j=4), func=AF.Copy),
                     reads=[Xb], writes=[onTb[i2]])
                tk = slice(c * 128, (c + 1) * 128)
                k.dma("sp", self.brT[0, :, :, tk].rearrange("c p q -> p c q"), onT[i2][:], reads=[onTb[i2]], writes=[self.brT_b[0][c]])

    def phase_ret(self, l):
        k = self.k
        with ExitStack() as st:
            Wqk = self.sb(st, "rWqk", [128, 8, 512], BF16)
            Wv = self.sb(st, "rWv", [128, 8, 512], BF16)
            Wg = self.sb(st, "rWg", [128, 8, 512], BF16)
            wb = Buf("rW")
            self.load_w(Wqk, self.din["w_in"][l][:, C_QR:C_QR + 512], wb, 8)
            self.load_w(Wv, self.din["w_in"][l][:, C_VR:C_VR + 512], wb, 8)
            self.load_w(Wg, self.din["w_in"][l][:, C_GR:C_GR + 512], wb, 8)
            cos = self.sb(st, "rcos", [128, NT, 32], F32)
            sin = self.sb(st, "rsin", [128, NT, 32], F32)
            dec = self.sb(st, "rdec", [128, 4, 128], F32)
            xi = self.sb(st, "rxi", [64, 4, 128], F32)
            zeta = self.sb(st, "rzeta", [128, 4], F32)
            gch = self.sb(st, "rgch", [64, 4], F32)
            gn = self.sb(st, "rgn", [128, 512], F32)
            rc = Buf("rconst")
            for dst, src in ((cos, "cos"), (sin, "sin"), (dec, "decayT"), (xi, "xi"), (zeta, "zeta"), (gch, "gch")):
                k.dma("sp", dst[:], self.din[src], writes=[rc])
            k.dma("sp", gn[:], self.din["retgn"][l], writes=[rc])
            Sf = self.sb(st, "rSf", [64, 4, 128], F32)
            Sb = self.sb(st, "rSb", [64, 4, 128], BF16)
            Sfb, Sbb = Buf("Sf"), Buf("Sb")
            k.op("dve", lambda e: e.memset(Sf[:], 0.0), writes=[Sfb])
            k.op("dve", lambda e: e.memset(Sb[:], 0.0), writes=[Sbb])
            qk = self.sb(st, "rqk", [128, 512], F32); qkb = Buf("rqk")
            tm = self.sb(st, "rtm", [128, 4, 8, 32], F32); tmb = Buf("rtm")
            rot = self.sb(st, "rrot", [128, 8, 2, 32], F32); rotb = Buf("rrot")
            qkbf = self.sb(st, "rqkbf", [128, 512], BF16); qkbfb = Buf("rqkbf")
            khat = self.sb(st, "rkhat", [128, 4, 64], BF16); khatb = Buf("rkhat")
            qkT = self.sb(st, "rqkT", [64, 8, 128], BF16); qkTb = Buf("rqkT")
            qxiT = self.sb(st, "rqxiT", [64, 4, 128], BF16); qxiTb = Buf("rqxiT")
            qf32 = self.sb(st, "rqf32", [64, 4, 128], F32); qf32b = Buf("rqf32")
            inT = self.sb(st, "rinT", [128, 4, 128], BF16); inTb = Buf("rinT")
            vbf = self.sb(st, "rvbf", [128, 512], BF16); vbfb = Buf("rvbf")
            osb = self.sb(st, "rosb", [128, 512], F32); osbb = Buf("rosb")
            osq = self.sb(st, "rosq", [128, 512], F32); osqb = Buf("rosq")
            sm = self.sb(st, "rsm", [128, 6, 4], F32); smb = Buf("rsm")
            yn = self.sb(st, "ryn", [128, 512], F32); ynb = Buf("ryn")
            gs = self.sb(st, "rgs", [128, 512], F32); gsb = Buf("rgs")
            orb = self.sb(st, "rorb", [128, 512], BF16); orbb = Buf("rorb")
            orT = [self.sb(st, "rorT%d" % i, [128, 4, 128], BF16) for i in range(2)]
            orTb = [Buf("rorT%d" % i) for i in range(2)]
            pqk = self.ps(st, "rpqk", [128, 512], F32); pqkb = Buf("pqk")
            pv = self.ps(st, "rpv", [128, 512], F32); pvb = Buf("pv")
            pg = self.ps(st, "rpg", [128, 512], F32); pgb = Buf("pg")
            pin = self.ps(st, "rpin", [128, 512], F32); pinb = Buf("pin")
            po = self.ps(st, "rpo", [128, 512], F32); pob = Buf("po")
            pkv = self.ps(st, "rpkv", [128, 512], F32); pkvb = Buf("pkv")
            ptr = self.ps(st, "rptr", [128, 1024], BF16); ptrb = Buf("ptr")
            ptr2 = self.ps(st, "rptr2", [128, 1024], BF16); ptr2b = Buf("ptr2")
            gs2 = [gs, self.sb(st, "rgs_b", [128, 512], F32)]; gs2b = [gsb, Buf("rgs_b")]
            osb2 = [osb, self.sb(st, "rosb_b", [128, 512], F32)]; osb2b = [osbb, Buf("rosb_b")]
            osq2 = [osq, self.sb(st, "rosq_b", [128, 512], F32)]; osq2b = [osqb, Buf("rosq_b")]

            def front(t):
                tk = slice(t * 128, (t + 1) * 128)
                for (W, P, PB) in ((Wqk, pqk, pqkb), (Wv, pv, pvb), (Wg, pg, pgb)):
                    for kc in range(8):
                        k.op("pe", lambda e, kc=kc, W=W, P=P: e.matmul(P[:, :], lhsT=self.hT[:, kc, tk], rhs=W[:, kc, :], start=(kc == 0), stop=(kc == 7)),
                             reads=[wb, self.hT_b[t]], writes=[PB])
                k.op("act", lambda e: e.activation(out=qk[:, 0:256], in_=pqk[:, 0:256], func=AF.Copy), reads=[pqkb], writes=[qkb])
                k.op("act", lambda e: e.activation(out=qk[:, 256:512], in_=pqk[:, 256:512], func=AF.Copy, scale=0.125), reads=[pqkb], writes=[qkb])
                k.op("act", lambda e: e.activation(out=vbf[:], in_=pv[:, :], func=AF.Copy), reads=[pvb], writes=[vbfb])
                k.op("act", lambda e: e.activation(out=gs2[t % 2][:], in_=pg[:, :], func=AF.Silu), reads=[pgb], writes=[gs2b[t % 2]])
                xv = qk[:, :].rearrange("p (h two d) -> p h two d", h=8, two=2)
                x1, x2 = xv[:, :, 0, :], xv[:, :, 1, :]
                cb_ = cos[:, t, :].unsqueeze(1).to_broadcast([128, 8, 32])
                sb_ = sin[:, t, :].unsqueeze(1).to_broadcast([128, 8, 32])
                k.op("dve", lambda e: e.tensor_tensor(out=tm[:, 0], in0=x1, in1=cb_, op=ALU.mult), reads=[qkb, rc], writes=[tmb])
                k.op("dve", lambda e: e.tensor_tensor(out=tm[:, 1], in0=x2, in1=sb_, op=ALU.mult), reads=[qkb, rc], writes=[tmb])
                k.op("dve", lambda e: e.tensor_tensor(out=tm[:, 2], in0=x1, in1=sb_, op=ALU.mult), reads=[qkb, rc], writes=[tmb])
                k.op("dve", lambda e: e.tensor_tensor(out=tm[:, 3], in0=x2, in1=cb_, op=ALU.mult), reads=[qkb, rc], writes=[tmb])
                k.op("dve", lambda e: e.tensor_tensor(out=rot[:, :, 0, :], in0=tm[:, 0], in1=tm[:, 1], op=ALU.subtract), reads=[tmb], writes=[rotb])
                k.op("dve", lambda e: e.tensor_tensor(out=rot[:, :, 1, :], in0=tm[:, 2], in1=tm[:, 3], op=ALU.add), reads=[tmb], writes=[rotb])
                rflat = rot[:, :, :, :].rearrange("p h two d -> p (h two d)")
                k.op("act", lambda e: e.activation(out=qkbf[:], in_=rflat, func=AF.Copy), reads=[rotb], writes=[qkbfb])
                k.op("dve", lambda e: e.tensor_tensor(out=khat[:], in0=rflat[:, 256:512].rearrange("p (h d) -> p h d", h=4),
                                                      in1=zeta[:, :].unsqueeze(2).to_broadcast([128, 4, 64]), op=ALU.mult),
                     reads=[rotb, rc], writes=[khatb])
                for j in range(8):
                    k.op("pe", lambda e, j=j: e.transpose(ptr[0:64, j * 128:(j + 1) * 128], qkbf[:, j * 64:(j + 1) * 64], self.ident[:]),
                         reads=[qkbfb, self.cb], writes=[ptrb])
                k.op("act", lambda e: e.activation(out=qkT[:], in_=ptr[0:64, 0:1024].rearrange("p (j c) -> p j c", j=8), func=AF.Copy),
                     reads=[ptrb], writes=[qkTb])
                k.op("act", lambda e: e.activation(out=qf32[:], in_=ptr[0:64, 0:512].rearrange("p (j c) -> p j c", j=4), func=AF.Copy),
                     reads=[ptrb], writes=[qf32b])
                k.op("dve", lambda e: e.tensor_tensor(out=qxiT[:], in0=qf32[:], in1=xi[:], op=ALU.mult),
                     reads=[qf32b, rc], writes=[qxiTb])
                for h in range(4):
                    k.op("pe", lambda e, h=h: e.matmul(pin[:, h * 128:(h + 1) * 128], lhsT=qkT[:, 4 + h, :], rhs=qkT[:, h, :],
                                                       start=True, stop=True), reads=[qkTb], writes=[pinb])
                k.op("dve", lambda e: e.tensor_tensor(out=inT[:], in0=pin[:, :].rearrange("p (h c) -> p h c", h=4), in1=dec[:], op=ALU.mult),
                     reads=[pinb, rc], writes=[inTb])

            def mid(t):
                for h in range(4):
                    hc = slice(h * 128, (h + 1) * 128)
                    k.op("pe", lambda e, h=h, hc=hc: e.matmul(po[:, hc], lhsT=inT[:, h, :], rhs=vbf[:, hc], start=True, stop=False),
                         reads=[inTb, vbfb], writes=[pob])
                    k.op("pe", lambda e, h=h, hc=hc: e.matmul(po[:, hc], lhsT=qxiT[:, h, :], rhs=Sb[:, h, :], start=False, stop=True),
                         reads=[qxiTb, Sbb], writes=[pob])
                for h in range(4):
                    hc = slice(h * 128, (h + 1) * 128)
                    k.op("pe", lambda e, h=h, hc=hc: e.matmul(pkv[0:64, hc], lhsT=khat[:, h, :],
                                                             rhs=vbf[:, hc], start=True, stop=True), reads=[khatb, vbfb], writes=[pkvb])
                for h in range(4):
                    hc = slice(h * 128, (h + 1) * 128)
                    k.op("dve", lambda e, h=h, hc=hc: e.scalar_tensor_tensor(out=Sf[:, h, :], in0=Sf[:, h, :],
                                                                              scalar=gch[:, h:h + 1], in1=pkv[0:64, hc],
                                                                              op0=ALU.mult, op1=ALU.add),
                         reads=[pkvb, rc, Sfb], writes=[Sfb])
                k.op("act", lambda e: e.activation(out=Sb[:], in_=Sf[:], func=AF.Copy), reads=[Sfb], writes=[Sbb])
                k.op("act", lambda e: e.activation(out=osb2[t % 2][:], in_=po[:, :], func=AF.Copy), reads=[pob], writes=[osb2b[t % 2]])
                k.op("act", lambda e: e.activation(out=osq2[t % 2][:], in_=po[:, :], func=AF.Square), reads=[pob], writes=[osq2b[t % 2]])

            def tail(t):
                tk = slice(t * 128, (t + 1) * 128)
                osb_, osbb_, osq_, osqb_ = osb2[t % 2], osb2b[t % 2], osq2[t % 2], osq2b[t % 2]
                k.op("dve", lambda e: e.reduce_sum(out=sm[:, 0, :], in_=osb_[:, :].rearrange("p (h v) -> p h v", h=4), axis=AX.X), reads=[osbb_], writes=[smb])
                k.op("dve", lambda e: e.reduce_sum(out=sm[:, 1, :], in_=osq_[:, :].rearrange("p (h v) -> p h v", h=4), axis=AX.X), reads=[osqb_, smb], writes=[smb])
                k.op("dve", lambda e: e.tensor_scalar(out=sm[:, 2, :], in0=sm[:, 0, :], scalar1=1.0 / 128, scalar2=None, op0=ALU.mult), reads=[smb], writes=[smb])
                k.op("dve", lambda e: e.tensor_tensor(out=sm[:, 3, :], in0=sm[:, 2, :], in1=sm[:, 2, :], op=ALU.mult), reads=[smb], writes=[smb])
                k.op("dve", lambda e: e.scalar_tensor_tensor(out=sm[:, 3, :], in0=sm[:, 1, :], scalar=1.0 / 128, in1=sm[:, 3, :], op0=ALU.mult, op1=ALU.subtract),
                     reads=[smb], writes=[smb])
                k.op("act", lambda e: e.activation(out=sm[:, 4, :], in_=sm[:, 3, :], func=AF.Sqrt, bias=self.cst[:, 0:1], scale=1.0), reads=[smb, self.cb], writes=[smb])
                k.op("dve", lambda e: e.reciprocal(out=sm[:, 5, :], in_=sm[:, 4, :]), reads=[smb], writes=[smb])
                for h in range(4):
                    hc = slice(h * 128, (h + 1) * 128)
                    k.op("dve", lambda e, h=h, hc=hc: e.tensor_scalar(out=yn[:, hc], in0=osb_[:, hc], scalar1=sm[:, 2, h:h + 1], scalar2=sm[:, 5, h:h + 1],
                                                                      op0=ALU.subtract, op1=ALU.mult), reads=[osbb_, smb], writes=[ynb])
                k.op("dve", lambda e: e.tensor_tensor(out=yn[:], in0=yn[:], in1=gn[:], op=ALU.mult), reads=[ynb, rc], writes=[ynb])
                k.op("dve", lambda e: e.tensor_tensor(out=orb[:], in0=yn[:], in1=gs2[t % 2][:], op=ALU.mult), reads=[ynb, gs2b[t % 2]], writes=[orbb])
                for j in range(4):
                    k.op("pe", lambda e, j=j: e.transpose(ptr2[:, j * 128:(j + 1) * 128], orb[:, j * 128:(j + 1) * 128], self.ident[:]),
                         reads=[orbb, self.cb], writes=[ptr2b])
                i = t % 2
                k.op("act", lambda e, i=i: e.activation(out=orT[i][:], in_=ptr2[:, 0:512].rearrange("p (j c) -> p j c", j=4), func=AF.Copy),
                     reads=[ptr2b], writes=[orTb[i]])
                k.dma("sp", self.brT[1, :, :, tk].rearrange("c p q -> p c q"), orT[i][:], reads=[orTb[i]], writes=[self.brT_b[1][t]])

            front(0)
            for t in range(NT):
                mid(t)
                if t + 1 < NT:
                    front(t + 1)
                tail(t)

    def phase_conv(self, l):
        k = self.k
        with ExitStack() as st:
            Wa = self.sb(st, "cWa", [128, 8, 512], BF16)
            Wb = self.sb(st, "cWb", [128, 8, 512], BF16)
            wab = Buf("cW")
            self.load_w(Wa, self.din["w_in"][l][:, C_CA:C_CA + 512], wab, 8)
            self.load_w(Wb, self.din["w_in"][l][:, C_CB:C_CB + 512], wab, 8)
            cw = self.sb(st, "ccw", [128, 4, 31], F32)
            cvec = self.sb(st, "cvec", [128, 3, 4], F32)
            ones = self.sb(st, "cones", [128, 128], F32)
            cc = Buf("cconst")
            k.dma("sp", cw[:], self.din["convw"][l], writes=[cc])
            k.dma("sp", cvec[:, 0, :], self.din["convb"][l], writes=[cc])
            k.dma("sp", cvec[:, 1, :], self.din["convg"][l], writes=[cc])
            k.dma("sp", cvec[:, 2, :], self.din["convbb"][l], writes=[cc])
            k.dma("sp", ones[:], self.din["ones"][:, :], writes=[cc])
            Dg = self.sb(st, "cDg", [128, 4, 31, 128], BF16); dgb = Buf("cDg")
            for ct in range(4):
                for w in range(31):
                    en = "dve" if (w % 2 == 0) else "pool"
                    k.op(en, lambda e, ct=ct, w=w: e.tensor_scalar(out=Dg[:, ct, w, :], in0=self.identf[:], scalar1=cw[:, ct, w:w + 1], scalar2=None, op0=ALU.mult),
                         reads=[cc, self.cb], writes=[dgb])
            u = [self.sb(st, "cu%d" % i, [128, 4, 542], BF16) for i in range(2)]
            ub = [[Buf("cu%d_%d" % (i, ct)) for ct in range(4)] for i in range(2)]
            acc = self.sb(st, "cacc", [128, 4, 512], F32)
            accb = [Buf("cacc%d" % ct) for ct in range(4)]
            sg = [self.sb(st, "csg%d" % i, [128, 512], F32) for i in range(2)]
            sgb = [Buf("csg%d" % i) for i in range(2)]
            ysq = [self.sb(st, "cysq%d" % i, [128, 512], F32) for i in range(2)]
            ysqb = [Buf("cysq%d" % i) for i in range(2)]
            stt = self.sb(st, "cstt", [128, 4, 512], F32)
            sttb = Buf("cstt")
            yn = [self.sb(st, "cyn%d" % i, [128, 512], F32) for i in range(2)]
            ynb = [Buf("cyn%d" % i) for i in range(2)]
            oc = [self.sb(st, "coc%d" % i, [128, 512], BF16) for i in range(2)]
            ocb = [Buf("coc%d" % i) for i in range(2)]
            pa = [self.ps(st, "cpa%d" % i, [128, 512], F32) for i in range(2)]
            pab = [Buf("cpa%d" % i) for i in range(2)]
            pbk = [self.ps(st, "cpb%d" % i, [128, 512], F32) for i in range(2)]
            pbb = [Buf("cpb%d" % i) for i in range(2)]
            pcv = [self.ps(st, "cpc%d" % i, [128, 512], F32) for i in range(2)]
            pcb = [Buf("cpc%d" % i) for i in range(2)]
            s1 = self.ps(st, "cs1", [128, 512], F32)
            s2 = self.ps(st, "cs2", [128, 512], F32)
            s1b, s2b = Buf("cs1"), Buf("cs2")
            for ct in range(4):
                k.op("dve", lambda e, ct=ct: e.memset(u[0][:, ct, 0:30], 0.0), writes=[ub[0][ct]])
            for G in range(8):
                toks = slice(G * 512, (G + 1) * 512)
                hb = self.hT_b[G * 4:(G + 1) * 4]
                ug, ugb = u[G % 2], ub[G % 2]
                for ct in range(4):
                    p = ct % 2
                    cols = slice(ct * 128, (ct + 1) * 128)
                    for kc in range(8):
                        k.op("pe", lambda e, kc=kc, cols=cols, p=p: e.matmul(pa[p][:, :], lhsT=Wa[:, kc, cols], rhs=self.hT[:, kc, toks],
                                                                              start=(kc == 0), stop=(kc == 7)), reads=[wab] + hb, writes=[pab[p]])
                    for kc in range(8):
                        k.op("pe", lambda e, kc=kc, cols=cols, p=p: e.matmul(pbk[p][:, :], lhsT=Wb[:, kc, cols], rhs=self.hT[:, kc, toks],
                                                                              start=(kc == 0), stop=(kc == 7)), reads=[wab] + hb, writes=[pbb[p]])
                    k.op("act", lambda e, p=p: e.activation(out=sg[p][:], in_=pbk[p][:, :], func=AF.Sigmoid), reads=[pbb[p]], writes=[sgb[p]])
                    if G > 0:
                        k.op("pool", lambda e, ct=ct: e.tensor_copy(out=ug[:, ct, 0:30], in_=u[(G - 1) % 2][:, ct, 512:542]),
                             reads=[ub[(G - 1) % 2][ct]], writes=[ugb[ct]])
                    k.op("dve", lambda e, ct=ct, p=p: e.tensor_tensor(out=ug[:, ct, 30:542], in0=pa[p][:, :], in1=sg[p][:], op=ALU.mult),
                         reads=[pab[p], sgb[p]], writes=[ugb[ct]])
                for ct in range(4):
                    p = ct % 2
                    for w in range(31):
                        k.op("pe", lambda e, ct=ct, w=w, p=p: e.matmul(pcv[p][:, :], lhsT=Dg[:, ct, w, :], rhs=ug[:, ct, w:w + 512], start=(w == 0), stop=(w == 30)),
                             reads=[dgb, ugb[ct]], writes=[pcb[p]])
                    k.op("act", lambda e, ct=ct, p=p: e.activation(out=acc[:, ct, :], in_=pcv[p][:, :], func=AF.Identity, bias=cvec[:, 0, ct:ct + 1], scale=1.0),
                         reads=[pcb[p], cc], writes=[accb[ct]])
                    k.op("act", lambda e, ct=ct, p=p: e.activation(out=ysq[p][:], in_=acc[:, ct, :], func=AF.Square), reads=[accb[ct]], writes=[ysqb[p]])
                    k.op("pe", lambda e, ct=ct: e.matmul(s1[:, :], lhsT=ones[:], rhs=acc[:, ct, :], start=(ct == 0), stop=(ct == 3)),
                         reads=[cc, accb[ct]], writes=[s1b])
                    k.op("pe", lambda e, ct=ct, p=p: e.matmul(s2[:, :], lhsT=ones[:], rhs=ysq[p][:], start=(ct == 0), stop=(ct == 3)),
                         reads=[cc, ysqb[p]], writes=[s2b])
                k.op("act", lambda e: e.activation(out=stt[:, 0, :], in_=s1[:, :], func=AF.Copy, scale=1.0 / 512), reads=[s1b], writes=[sttb])
                k.op("dve", lambda e: e.tensor_tensor(out=stt[:, 1, :], in0=stt[:, 0, :], in1=stt[:, 0, :], op=ALU.mult), reads=[sttb], writes=[sttb])
                k.op("dve", lambda e: e.scalar_tensor_tensor(out=stt[:, 1, :], in0=s2[:, :], scalar=1.0 / 512, in1=stt[:, 1, :],
                                                             op0=ALU.mult, op1=ALU.subtract), reads=[s2b, sttb], writes=[sttb])
                k.op("act", lambda e: e.activation(out=stt[:, 3, :], in_=stt[:, 1, :], func=AF.Sqrt, bias=self.cst[:, 0:1], scale=1.0),
                     reads=[sttb, self.cb], writes=[sttb])
                k.op("dve", lambda e: e.reciprocal(out=stt[:, 2, :], in_=stt[:, 3, :]), reads=[sttb], writes=[sttb])
                for ct in range(4):
                    p = ct % 2
                    k.op("dve", lambda e, ct=ct, p=p: e.tensor_tensor(out=yn[p][:], in0=acc[:, ct, :], in1=stt[:, 0, :], op=ALU.subtract),
                         reads=[accb[ct], sttb], writes=[ynb[p]])
                    k.op("pool", lambda e, p=p: e.tensor_tensor(out=yn[p][:], in0=yn[p][:], in1=stt[:, 2, :], op=ALU.mult),
                         reads=[sttb, ynb[p]], writes=[ynb[p]])
                    k.op("act", lambda e, ct=ct, p=p: e.activation(out=oc[p][:], in_=yn[p][:], func=AF.Silu, scale=cvec[:, 1, ct:ct + 1],
                                                                   bias=cvec[:, 2, ct:ct + 1]), reads=[ynb[p], cc], writes=[ocb[p]])
                    k.dma("sp", self.brT[2, ct, :, toks], oc[p][:], reads=[ocb[p]], writes=self.brT_b[2][G * 4:(G + 1) * 4])

    def phase_merge(self, l):
        k = self.k
        for dh in range(2):
            with ExitStack() as st:
                Wg = self.sb(st, "mWg", [128, 8, 3, 512], BF16)
                Wbr = self.sb(st, "mWbr", [128, 3, 4, 512], BF16)
                Wo = self.sb(st, "mWo", [128, 4, 1024], BF16)
                wgb = [Buf("mWg%d" % i) for i in range(3)]
                wbrb = [Buf("mWbr%d" % i) for i in range(3)]
                wob = Buf("mWo")
                for b in range(3):
                    for kc in range(8):
                        c0 = C_MG + b * 1024 + dh * 512
                        k.dma("pool", Wg[:, kc, b, :], self.din["w_in"][l][kc * 128:(kc + 1) * 128, c0:c0 + 512], writes=[wgb[b]])
                    for c4 in range(4):
                        k.dma("pool", Wbr[:, b, c4, :], self.din["w_branch"][l][b][c4 * 128:(c4 + 1) * 128, dh * 512:(dh + 1) * 512], writes=[wbrb[b]])
                for dc in range(4):
                    r0 = dh * 512 + dc * 128
                    k.dma("pool", Wo[:, dc, :], self.din["w_out"][l][r0:r0 + 128, :], writes=[wob])
                brg = [self.sb(st, "mbr%d" % i, [128, 3, 4, 512], BF16) for i in range(2)]
                brgb = [Buf("mbr%d" % i) for i in range(2)]
                mT = self.sb(st, "mmT", [128, 4, 512], BF16)
                mTb = [Buf("mT%d" % i) for i in range(4)]
                gate = [self.sb(st, "mgate%d" % i, [128, 512], F32) for i in range(2)]
                gateb = [Buf("mgate%d" % i) for i in range(2)]
                macc = self.sb(st, "mmacc", [128, 512], F32); maccb = Buf("macc")
                mtmp = self.sb(st, "mmtmp", [128, 512], F32); mtmpb = Buf("mtmp")
                ctx = self.upd_alloc(st, "m", self.din["nmlp"][l] if dh == 1 else None)
                pg = [self.ps(st, "mpg%d" % i, [128, 512], F32) for i in range(2)]
                pgb = [Buf("mpg%d" % i) for i in range(2)]
                pp = [self.ps(st, "mpp%d" % i, [128, 512], F32) for i in range(2)]
                ppb = [Buf("mpp%d" % i) for i in range(2)]
                po = [self.ps(st, "mpo%d" % i, [128, 512], F32) for i in range(2)]
                pob = [Buf("mpo%d" % i) for i in range(2)]
                if dh == 1:
                    ctx["ptr"] = self.ps(st, "mptr", [128, 1024], BF16)
                    ctx["ptr_b"] = Buf("mptr")
                it = 0
                for G in range(8):
                    toks = slice(G * 512, (G + 1) * 512)
                    hb = self.hT_b[G * 4:(G + 1) * 4]
                    bi = G % 2
                    for b in range(3):
                        k.dma("sp", brg[bi][:, b], self.brT[b, :, :, toks].rearrange("c p t -> p c t"),
                              reads=self.brT_b[b][G * 4:(G + 1) * 4], writes=[brgb[bi]])
                    for dc in range(4):
                        for b in range(3):
                            p = it % 2
                            it += 1
                            for kc in range(8):
                                k.op("pe", lambda e, kc=kc, b=b, dc=dc, p=p: e.matmul(pg[p][:, :], lhsT=Wg[:, kc, b, dc * 128:(dc + 1) * 128],
                                                                                     rhs=self.hT[:, kc, toks], start=(kc == 0), stop=(kc == 7)),
                                     reads=[wgb[b]] + hb, writes=[pgb[p]])
                            for c4 in range(4):
                                k.op("pe", lambda e, c4=c4, b=b, dc=dc, p=p: e.matmul(pp[p][:, :], lhsT=Wbr[:, b, c4, dc * 128:(dc + 1) * 128],
                                                                                     rhs=brg[bi][:, b, c4, :], start=(c4 == 0), stop=(c4 == 3)),
                                     reads=[wbrb[b], brgb[bi]], writes=[ppb[p]])
                            k.op("act", lambda e, p=p: e.activation(out=gate[p][:], in_=pg[p][:, :], func=AF.Sigmoid), reads=[pgb[p]], writes=[gateb[p]])
                            if b == 0:
                                k.op("dve", lambda e, p=p: e.tensor_tensor(out=macc[:], in0=pp[p][:, :], in1=gate[p][:], op=ALU.mult),
                                     reads=[ppb[p], gateb[p]], writes=[maccb])
                            else:
                                k.op("dve", lambda e, p=p: e.tensor_tensor(out=mtmp[:], in0=pp[p][:, :], in1=gate[p][:], op=ALU.mult),
                                     reads=[ppb[p], gateb[p]], writes=[mtmpb])
                                if b == 1:
                                    k.op("pool", lambda e: e.tensor_tensor(out=macc[:], in0=macc[:], in1=mtmp[:], op=ALU.add),
                                         reads=[mtmpb, maccb], writes=[maccb])
                                else:
                                    k.op("pool", lambda e, dc=dc: e.tensor_tensor(out=mT[:, dc, :], in0=macc[:], in1=mtmp[:], op=ALU.add),
                                         reads=[mtmpb, maccb], writes=[mTb[dc]])
                    for tt in range(4):
                        t = G * 4 + tt
                        for nh in range(2):
                            for dc in range(4):
                                k.op("pe", lambda e, nh=nh, dc=dc, tt=tt: e.matmul(po[nh][:, :], lhsT=mT[:, dc, tt * 128:(tt + 1) * 128],
                                                                                  rhs=Wo[:, dc, nh * 512:(nh + 1) * 512], start=(dc == 0), stop=(dc == 3)),
                                     reads=[wob, mTb[dc]], writes=[pob[nh]])
                        self.x_update(ctx, t, [po[0][:, :], po[1][:, :]], pob, "norm" if dh == 1 else None)
                self.upd_flush(ctx)
                k.barrier()


_PROG_CACHE = {}


def _get_prog(**kw):
    key = tuple(sorted((k, str(v)) for k, v in kw.items()))
    if key not in _PROG_CACHE:
        _PROG_CACHE[key] = Prog(**kw)
    return _PROG_CACHE[key]


def kernel(**inputs):
    inp = {k: np.asarray(v) for k, v in inputs.items()}
    x = np.ascontiguousarray(inp["x"], dtype=np.float32)
    shared = _host_inputs(inp)
    prog = _get_prog()
    in_maps = []
    for c in range(8):
        m = dict(shared)
        m["x"] = x[c]
        in_maps.append(m)
    res = run_bass_kernel_spmd(prog.nc, in_maps, core_ids=list(range(8)))
    return np.stack([np.asarray(r["out"], dtype=np.float32) for r in res.results], axis=0)
```

```python
import numpy as np
from contextlib import ExitStack
import concourse.bass as bass
import concourse.mybir as mybir
from concourse.bass_utils import run_bass_kernel_spmd

F32 = mybir.dt.float32
BF16 = mybir.dt.bfloat16
AF = mybir.ActivationFunctionType
ALU = mybir.AluOpType
AX = mybir.AxisListType

S = 4096
D = 1024
NT = 32
DEPTH = 2
IN_TOTAL = 6936
EPS = 1e-6
NEG = -30000.0
C_QN, C_KC, C_VC, C_KS, C_VS, C_KW, C_VW, C_GN = 0, 512, 640, 768, 896, 1024, 1152, 1280
C_QR, C_KR, C_VR, C_GR, C_CA, C_CB, C_MG = 1304, 1560, 1816, 2328, 2840, 3352, 3864

SAME_ENGINE_SYNC = True


class Buf:
    __slots__ = ("w", "r", "name", "wd")

    def __init__(self, name=""):
        self.w = None
        self.r = {}
        self.name = name
        self.wd = False


class _Sem:
    def __init__(self, sem, name):
        self.sem = sem
        self.count = 0
        self.name = name


class Eng(_Sem):
    def __init__(self, name, handle, sem):
        super().__init__(sem, name)
        self.h = handle
        self.waited = {}


class K:
    def __init__(self, nc, stack, n_dma_sems=64):
        self.nc = nc
        self.engs = {}
        for name, h in (("pe", nc.tensor), ("act", nc.scalar), ("dve", nc.vector),
                        ("pool", nc.gpsimd), ("sp", nc.sync)):
            sem = stack.enter_context(nc.semaphore("sem_" + name))
            self.engs[name] = Eng(name, h, sem)
        self.dsems = [_Sem(stack.enter_context(nc.semaphore("dsem%d" % i)), "d%d" % i)
                      for i in range(n_dma_sems)]
        self.dpool = {"pool": self.dsems[:n_dma_sems // 2], "sp": self.dsems[n_dma_sems // 2:]}
        self.dnext = {"pool": 0, "sp": 0}
        self.n_ops = 0
        self.muted = False

    def _wait_deps(self, E, reads, writes):
        deps = {}

        def need(tok):
            if tok is None:
                return
            s, v = tok
            if deps.get(s, 0) < v:
                deps[s] = v
        for b in reads:
            for t in b.w or ():
                need(t)
        for b in writes:
            for t in b.w or ():
                need(t)
            for t in b.r.values():
                need(t)
        for s, v in deps.items():
            if s is E and (E.name in ("pe", "sp") or not SAME_ENGINE_SYNC):
                continue
            if E.waited.get(s, 0) < v:
                E.h.wait_ge(s.sem, v)
                E.waited[s] = v

    def _mark(self, tok, reads, writes, is_dma=False):
        for b in reads:
            b.r[tok[0]] = tok
        for b in writes:
            if is_dma and b.w and getattr(b, "wd", False) and len(b.w) < 24:
                b.w = b.w + [tok]
            else:
                b.w = [tok]
            b.wd = is_dma
            b.r = {}

    def op(self, en, fn, reads=(), writes=()):
        if self.muted:
            return None
        E = self.engs[en]
        self._wait_deps(E, reads, writes)
        ins = fn(E.h)
        E.count += 1
        ins.then_inc(E.sem, 1)
        self._mark((E, E.count), reads, writes)
        self.n_ops += 1
        return ins

    def dma(self, en, out, in_, reads=(), writes=(), **kw):
        if self.muted:
            return None
        E = self.engs[en]
        self._wait_deps(E, reads, writes)
        pool_ = self.dpool[en]
        d = pool_[self.dnext[en]]
        self.dnext[en] = (self.dnext[en] + 1) % len(pool_)
        if d.count and E.waited.get(d, 0) < d.count:
            E.h.wait_ge(d.sem, d.count)
            E.waited[d] = d.count
        ins = E.h.dma_start(out=out, in_=in_, **kw)
        d.count += 16
        ins.then_inc(d.sem, 16)
        self._mark((d, d.count), reads, writes, is_dma=True)
        self.n_ops += 1
        return ins

    def barrier(self):
        allsems = list(self.engs.values()) + self.dsems
        for E in self.engs.values():
            for s in allsems:
                if s is E or s.count == 0:
                    continue
                if E.waited.get(s, 0) < s.count:
                    E.h.wait_ge(s.sem, s.count)
                    E.waited[s] = s.count


def _rel_bucket_np(dist):
    n = np.maximum(dist, 0)
    nf = np.maximum(n, 1).astype(np.float32)
    large = 16 + (np.log(nf / np.float32(16)) / np.float32(np.log(8.0)) * np.float32(16)).astype(np.int32)
    large = np.minimum(large, 31)
    return np.where(n < 16, n, large).astype(np.int64)


_CONST_CACHE = {}


def _host_consts():
    if _CONST_CACHE:
        return _CONST_CACHE
    c = {}
    i = np.arange(128)
    c["ident"] = np.eye(128, dtype=np.float32)
    c["i4"] = np.tile(np.eye(128, dtype=np.float32), (1, 4))
    mw = np.zeros((128, 5, 128), np.float32)
    jj, ii = np.meshgrid(i, i, indexing="ij")
    mw[:, 0, :] = (ii >= jj)
    mw[:, 1:4, :] = 1.0
    mw[:, 4, :] = (ii < jj)
    c["maskw"] = mw
    dist_w = 128 * np.arange(5)[None, :, None] + ii[:, None, :] - jj[:, None, :]
    c["_bucket_w"] = _rel_bucket_np(dist_w)
    m = np.arange(504)
    dist_c = i[None, :] - 16 * (m[:, None] - 248) - 31
    c["maskc"] = (dist_c >= 0).astype(np.float32)
    c["_bucket_c"] = _rel_bucket_np(dist_c)
    n = np.arange(256)
    s = np.arange(64)
    ov = ((16 * n[:, None] <= 64 * s[None, :] + 63) & (16 * n[:, None] + 31 >= 64 * s[None, :])).astype(np.float32)
    ov[255] = 0.0
    c["overlap"] = ov.reshape(2, 128, 64).transpose(1, 0, 2).copy()
    j = np.arange(126)
    sp = j[None, :] - 62
    cur = (i[:, None] >= 64).astype(np.int64)
    valid = sp <= cur
    forced = (sp == cur) | (sp == cur - 1)
    c["selvalid"] = valid.astype(np.float32)
    c["seladd"] = np.where(forced, 1e4, np.where(valid, 0.0, -1e4)).astype(np.float32)
    half = 32
    inv = (10000.0 ** (-np.arange(half, dtype=np.float32) / half)).astype(np.float32)
    pos = np.arange(S, dtype=np.float32)
    ang = (pos[:, None] * inv[None, :]).astype(np.float32)
    c["cos"] = np.cos(ang).astype(np.float32).reshape(NT, 128, 32).transpose(1, 0, 2).copy()
    c["sin"] = np.sin(ang).astype(np.float32).reshape(NT, 128, 32).transpose(1, 0, 2).copy()
    log_g = np.log1p(-np.exp2(-5.0 - np.arange(4, dtype=np.float32))).astype(np.float32)
    diff = i[None, :] - i[:, None]
    dec = np.where(diff[None] >= 0, np.exp(log_g[:, None, None] * np.maximum(diff[None], 0)), 0.0)
    c["decayT"] = dec.transpose(1, 0, 2).astype(np.float32).copy()
    xi = np.exp(log_g[:, None] * (i[None, :] + 1)).astype(np.float32)
    c["xi"] = np.ascontiguousarray(np.broadcast_to(xi[None, :, :], (64, 4, 128))).astype(np.float32)
    c["zeta"] = np.exp(log_g[None, :] * (127 - i[:, None])).astype(np.float32)
    gch = np.exp(log_g * 128).astype(np.float32)
    c["gch"] = np.ascontiguousarray(np.broadcast_to(gch[None, :], (64, 4))).astype(np.float32)
    c["ones"] = np.ones((128, 128), np.float32)
    c["rfull"] = (np.arange(S)[None, :] // 64 == np.arange(64)[:, None]).astype(np.float32)
    _CONST_CACHE.update(c)
    return c


CONST_SHAPES = {
    "ident": [128, 128], "i4": [128, 512], "maskw": [128, 5, 128], "maskc": [504, 128],
    "overlap": [128, 2, 64], "selvalid": [128, 126], "seladd": [128, 126],
    "cos": [128, NT, 32], "sin": [128, NT, 32], "decayT": [128, 4, 128], "xi": [64, 4, 128],
    "zeta": [128, 4], "gch": [64, 4], "ones": [128, 128], "rfull": [64, S],
    "bias_w": [128, 5, 8, 128], "bias_c": [504, 8, 128], "b31": [128, 8],
}

W_SHAPES = {
    "w_in": [DEPTH, D, IN_TOTAL], "w_branch": [DEPTH, 3, 512, D], "w_out": [DEPTH, D, D],
    "w_ff1": [DEPTH, D, 4 * D], "w_ff2": [DEPTH, 4 * D, D],
    "cmp_w1_k": [DEPTH, 32, 64, 128], "cmp_w1_v": [DEPTH, 32, 64, 128],
    "cmp_w2_k": [DEPTH, 128, 64], "cmp_w2_v": [DEPTH, 128, 64],
    "cmp_pe_kT": [DEPTH, 64, 32], "cmp_pe_vT": [DEPTH, 64, 32],
    "nmix": [DEPTH, 128, D], "nmlp": [DEPTH, 128, D], "nfin": [128, D],
    "retgn": [DEPTH, 128, 512],
    "convw": [DEPTH, 128, 4, 31], "convb": [DEPTH, 128, 4], "convg": [DEPTH, 128, 4], "convbb": [DEPTH, 128, 4],
}


def _host_inputs(inp):
    c = _host_consts()
    f = lambda a: np.ascontiguousarray(a, dtype=np.float32)
    out = {k: f(v) for k, v in c.items() if not k.startswith("_")}
    rt = f(inp["rel_table"])
    out["bias_w"] = f(rt[c["_bucket_w"]].transpose(0, 1, 3, 2))
    out["bias_c"] = f(rt[c["_bucket_c"]].transpose(0, 2, 1))
    out["b31"] = f(np.broadcast_to(rt[31][None, :], (128, 8)))
    for kname in ("w_in", "w_branch", "w_out", "w_ff1", "w_ff2", "cmp_w1_k", "cmp_w1_v", "cmp_w2_k", "cmp_w2_v"):
        out[kname] = f(inp[kname])
    out["cmp_pe_kT"] = f(np.transpose(inp["cmp_pe_k"], (0, 2, 1)))
    out["cmp_pe_vT"] = f(np.transpose(inp["cmp_pe_v"], (0, 2, 1)))
    out["nmix"] = f(np.broadcast_to(inp["norm_mix"][:, None, :], (DEPTH, 128, D)))
    out["nmlp"] = f(np.broadcast_to(inp["norm_mlp"][:, None, :], (DEPTH, 128, D)))
    out["nfin"] = f(np.broadcast_to(inp["norm_final"][None, :], (128, D)))
    out["retgn"] = f(np.broadcast_to(inp["ret_gn"][:, None, :], (DEPTH, 128, 512)))
    out["convw"] = f(np.transpose(inp["conv_w"].reshape(DEPTH, 31, 4, 128), (0, 3, 2, 1)))
    for a, b in (("convb", "conv_b"), ("convg", "conv_ln_g"), ("convbb", "conv_ln_b")):
        out[a] = f(np.transpose(inp[b].reshape(DEPTH, 4, 128), (0, 2, 1)))
    return out


class _Stop(Exception):
    pass


class Prog:
    def __init__(self, n_layers=DEPTH, phases=("nsa", "ret", "conv", "merge", "ffn"), dbg=False):
        self.n_layers = n_layers
        self.phases = phases
        self.dbg = dbg
        nc = self.nc = bass.Bass("TRN2", target_bir_lowering=False)
        self.din = {}
        self.din["x"] = nc.dram_tensor("x", [S, D], F32, kind="ExternalInput").ap()
        for name, shp in list(CONST_SHAPES.items()) + list(W_SHAPES.items()):
            self.din[name] = nc.dram_tensor(name, shp, F32, kind="ExternalInput").ap()
        self.out = nc.dram_tensor("out", [S, D], F32, kind="ExternalOutput").ap()
        skind = "ExternalOutput" if dbg else "Internal"
        self.xres = nc.dram_tensor("xres", [S, D], F32, kind=skind).ap()
        self.brT = nc.dram_tensor("brT", [3, 4, 128, S], BF16, kind=skind).ap()
        self.ebc_d = nc.dram_tensor("ebc_d", [504, 8, 128], BF16, kind=skind).ap()
        self.dbg_d = nc.dram_tensor("dbg_d", [128, 2048], F32, kind=skind).ap()
        self.ocd = nc.dram_tensor("ocd", [NT, 128, 512], F32, kind="Internal").ap()
        self.ocd_b = [Buf("ocd%d" % t) for t in range(NT)]
        self.xres_b = [Buf("xres%d" % t) for t in range(NT)]
        self.brT_b = [[Buf("brT%d_%d" % (b, t)) for t in range(NT)] for b in range(3)]
        self.ebc_db = Buf("ebc_d")
        self.out_b = Buf("out")
        with ExitStack() as st:
            self.st = st
            self.k = K(nc, st)
            self.build()

    def sb(self, st, name, shape, dt):
        self._uid = getattr(self, "_uid", 0) + 1
        return st.enter_context(self.nc.sbuf_tensor("s%d_%s" % (self._uid, name), shape, dt))

    def ps(self, st, name, shape, dt):
        self._uid = getattr(self, "_uid", 0) + 1
        return st.enter_context(self.nc.psum_tensor("p%d_%s" % (self._uid, name), shape, dt))

    def chk(self, n):
        import os
        v = os.environ.get("RSTOP")
        if v is not None and int(v) == n:
            self.k.muted = True

    def load_const(self, dst, src, buf, eng="sp"):
        self.k.dma(eng, dst, src, writes=[buf])

    def build(self):
        k, nc, st = self.k, self.nc, self.st
        self.hT = self.sb(st, "hT_all", [128, 8, S], BF16)
        self.hT_b = [Buf("hT%d" % t) for t in range(NT)]
        self.ident = self.sb(st, "ident", [128, 128], BF16)
        self.i4 = self.sb(st, "i4", [128, 512], BF16)
        self.identf = self.sb(st, "identf", [128, 128], F32)
        self.cst = self.sb(st, "cst", [128, 4], F32)
        self.EB = self.sb(st, "EB", [128, 5, 8, 128], BF16)
        self.cb = Buf("consts")
        k.dma("pool", self.ident[:], self.din["ident"][:, :], writes=[self.cb])
        k.dma("pool", self.i4[:], self.din["i4"][:, :], writes=[self.cb])
        k.dma("sp", self.identf[:], self.din["ident"][:, :], writes=[self.cb])
        k.op("dve", lambda e: e.memset(self.cst[:, 0:1], EPS), writes=[self.cb])
        k.op("dve", lambda e: e.memset(self.cst[:, 1:2], 0.0), writes=[self.cb])
        k.op("dve", lambda e: e.memset(self.cst[:, 2:3], 1.0), writes=[self.cb])
        self.phase_bias_tables()
        k.barrier()
        self.phase0()
        k.barrier()
        for l in range(self.n_layers):
            last = (l == DEPTH - 1)
            for ph, fn in (("nsa", lambda: self.phase_nsa(l)), ("ret", lambda: self.phase_ret(l)), ("conv", lambda: self.phase_conv(l)),
                           ("merge", lambda: self.phase_merge(l)), ("ffn", lambda: self.phase_ffn(l, last))):
                if ph in self.phases:
                    with nc.named_scope("%s%d" % (ph, l)):
                        fn()
                        k.muted = False
                        k.barrier()
        k.barrier()

    def rms_alloc(self, st, tag):
        r = {}
        r["junk"] = self.sb(st, "rjunk" + tag, [128, D], BF16)
        r["ss"] = self.sb(st, "rss" + tag, [128, 4], F32)
        r["hb2"] = [self.sb(st, "rhb%d" % i + tag, [128, D], BF16) for i in range(2)]
        r["hbb2"] = [Buf("rmshb%d" % i + tag) for i in range(2)]
        r["i"] = 0
        r["b"] = Buf("rms" + tag)
        return r

    def rms_stats(self, r, src, src_bufs):
        k = self.k
        ss = r["ss"]
        k.op("dve", lambda e: e.memset(ss[:, 0:1], 0.0), writes=[r["b"]])
        k.op("act", lambda e: e.activation(out=r["junk"][:], in_=src, func=AF.Square, accum_out=ss[:, 0:1]),
             reads=list(src_bufs) + [r["b"]], writes=[r["b"]])
        k.op("act", lambda e: e.activation(out=ss[:, 1:2], in_=ss[:, 0:1], func=AF.Sqrt, bias=self.cst[:, 0:1], scale=1.0 / D),
             reads=[r["b"], self.cb], writes=[r["b"]])
        k.op("dve", lambda e: e.reciprocal(out=ss[:, 2:3], in_=ss[:, 1:2]), reads=[r["b"]], writes=[r["b"]])
        return ss[:, 2:3]

    def rms_to_hT(self, r, src, src_bufs, gain, gain_buf, t, ptr, ptr_b):
        i = self.rms_part1(r, src, src_bufs, gain, gain_buf)
        self.rms_part2(r, i, t, ptr, ptr_b)

    def rms_part1(self, r, src, src_bufs, gain, gain_buf):
        k = self.k
        rstd = self.rms_stats(r, src, src_bufs)
        i = r["i"] = (r["i"] + 1) % 2
        hb = r["hb2"][i]
        k.op("dve", lambda e: e.scalar_tensor_tensor(out=hb[:], in0=src, scalar=rstd, in1=gain, op0=ALU.mult, op1=ALU.mult),
             reads=list(src_bufs) + [r["b"], gain_buf], writes=[r["hbb2"][i]])
        return i

    def rms_part2(self, r, i, t, ptr, ptr_b):
        k = self.k
        hb = r["hb2"][i]
        for c in range(8):
            k.op("pe", lambda e, c=c: e.transpose(ptr[:, c * 128:(c + 1) * 128], hb[:, c * 128:(c + 1) * 128], self.ident[:]),
                 reads=[r["hbb2"][i], self.cb], writes=[ptr_b])
        k.op("act", lambda e: e.activation(out=self.hT[:, :, t * 128:(t + 1) * 128],
                                           in_=ptr[:, :].rearrange("p (c q) -> p c q", c=8), func=AF.Copy),
             reads=[ptr_b], writes=[self.hT_b[t]])

    def phase_bias_tables(self):
        k = self.k
        with ExitStack() as st:
            bw = self.sb(st, "bw", [128, 5, 8, 128], F32)
            mw = self.sb(st, "mw", [128, 5, 128], F32)
            b31 = self.sb(st, "b31", [128, 8], F32)
            bb = Buf("bw")
            k.dma("sp", bw[:], self.din["bias_w"][:, :, :, :], writes=[bb])
            k.dma("sp", mw[:], self.din["maskw"][:, :, :], writes=[bb])
            k.dma("sp", b31[:], self.din["b31"][:, :], writes=[bb])
            for off in range(5):
                k.op("dve", lambda e, off=off: e.tensor_tensor(out=bw[:, off], in0=bw[:, off],
                                                               in1=b31[:, :].unsqueeze(2).to_broadcast([128, 8, 128]), op=ALU.subtract),
                     reads=[bb], writes=[bb])
                k.op("dve", lambda e, off=off: e.tensor_tensor(out=bw[:, off], in0=bw[:, off],
                                                               in1=mw[:, off:off + 1, :].to_broadcast([128, 8, 128]), op=ALU.mult),
                     reads=[bb], writes=[bb])
                k.op("dve", lambda e, off=off: e.tensor_scalar(out=mw[:, off, :], in0=mw[:, off, :], scalar1=-NEG, scalar2=NEG, op0=ALU.mult, op1=ALU.add),
                     reads=[bb], writes=[bb])
                k.op("dve", lambda e, off=off: e.tensor_tensor(out=self.EB[:, off], in0=bw[:, off],
                                                               in1=mw[:, off:off + 1, :].to_broadcast([128, 8, 128]), op=ALU.add),
                     reads=[bb], writes=[self.cb])
            bc = self.sb(st, "bc", [126, 8, 128], F32)
            mc = self.sb(st, "mc", [126, 128], F32)
            bcb = self.sb(st, "bcb", [126, 8, 128], BF16)
            cbuf = Buf("bc")
            for r4 in range(4):
                rows = slice(r4 * 126, (r4 + 1) * 126)
                k.dma("sp", bc[:], self.din["bias_c"][rows, :, :], writes=[cbuf])
                k.dma("sp", mc[:], self.din["maskc"][rows, :], writes=[cbuf])
                k.op("dve", lambda e: e.tensor_tensor(out=bc[:], in0=bc[:], in1=b31[0:126, :].unsqueeze(2).to_broadcast([126, 8, 128]),
                                                      op=ALU.subtract), reads=[cbuf, bb], writes=[cbuf])
                k.op("act", lambda e: e.activation(out=bc[:], in_=bc[:], func=AF.Exp), reads=[cbuf], writes=[cbuf])
                k.op("dve", lambda e: e.tensor_tensor(out=bcb[:], in0=bc[:], in1=mc[:, :].unsqueeze(1).to_broadcast([126, 8, 128]),
                                                      op=ALU.mult), reads=[cbuf], writes=[cbuf])
                k.dma("sp", self.ebc_d[rows, :, :], bcb[:], reads=[cbuf], writes=[self.ebc_db])
            k.barrier()

    def phase0(self):
        k = self.k
        with ExitStack() as st:
            r = self.rms_alloc(st, "p0")
            gain = self.sb(st, "gain0", [128, D], F32)
            gb = Buf("gain0")
            k.dma("sp", gain[:], self.din["nmix"][0], writes=[gb])
            xt = [self.sb(st, "p0x%d" % i, [128, D], F32) for i in range(2)]
            xb = [Buf("p0x%d" % i) for i in range(2)]
            ptr = self.ps(st, "p0tr", [128, 1024], BF16)
            ptr_b = Buf("p0tr")
            for t in range(NT):
                i = t % 2
                k.dma("sp", xt[i][:], self.din["x"][t * 128:(t + 1) * 128, :], writes=[xb[i]])
                k.dma("pool", self.xres[t * 128:(t + 1) * 128, :], xt[i][:], reads=[xb[i]], writes=[self.xres_b[t]])
                self.rms_to_hT(r, xt[i][:], [xb[i]], gain[:], gb, t, ptr, ptr_b)

    def load_w(self, dst, src, buf, kc):
        for c in range(kc):
            self.k.dma("pool", dst[:, c, :], src[c * 128:(c + 1) * 128, :], writes=[buf])

    def x_update(self, ctx, t, psum_halves, psum_bufs, hook):
        k = self.k
        i = ctx["i"] = (ctx.get("i", 0) + 1) % 2
        xt, xb = ctx["xt"][i], ctx["xb"][i]
        k.dma("sp", xt[:], self.xres[t * 128:(t + 1) * 128, :], reads=[self.xres_b[t]], writes=[xb])
        for h in range(2):
            k.op("dve", lambda e, h=h: e.tensor_tensor(out=xt[:, h * 512:(h + 1) * 512], in0=psum_halves[h],
                                                       in1=xt[:, h * 512:(h + 1) * 512], op=ALU.add),
                 reads=[psum_bufs[h], xb], writes=[xb])
        if hook != "final":
            k.dma("pool", self.xres[t * 128:(t + 1) * 128, :], xt[:], reads=[xb], writes=[self.xres_b[t]])
        if hook is None:
            return
        r = ctx["rms"]
        if hook == "final":
            rstd = self.rms_stats(r, xt[:], [xb])
            ot = ctx["ot"]
            k.op("dve", lambda e: e.scalar_tensor_tensor(out=ot[:], in0=xt[:], scalar=rstd, in1=ctx["gain"][:], op0=ALU.mult, op1=ALU.mult),
                 reads=[xb, r["b"], ctx["gain_b"]], writes=[ctx["ot_b"]])
            k.dma("pool", self.out[t * 128:(t + 1) * 128, :], ot[:], reads=[ctx["ot_b"]], writes=[self.out_b])
        else:
            i2 = self.rms_part1(r, xt[:], [xb], ctx["gain"][:], ctx["gain_b"])
            self.upd_flush(ctx)
            ctx["pending"] = (i2, t)

    def upd_flush(self, ctx):
        p = ctx.pop("pending", None)
        if p is not None:
            self.rms_part2(ctx["rms"], p[0], p[1], ctx["ptr"], ctx["ptr_b"])

    def upd_alloc(self, st, tag, gain_src, final=False):
        ctx = {}
        ctx["xt"] = [self.sb(st, "ux%s%d" % (tag, i), [128, D], F32) for i in range(2)]
        ctx["xb"] = [Buf("ux%d" % i) for i in range(2)]
        if gain_src is not None:
            ctx["rms"] = self.rms_alloc(st, "u" + tag)
            ctx["gain"] = self.sb(st, "ug" + tag, [128, D], F32)
            ctx["gain_b"] = Buf("ug")
            self.k.dma("sp", ctx["gain"][:], gain_src, writes=[ctx["gain_b"]])
            if final:
                ctx["ot"] = self.sb(st, "uo" + tag, [128, D], F32)
                ctx["ot_b"] = Buf("uo")
        return ctx

    def phase_ffn(self, l, last):
        k = self.k
        for fh in range(2):
            with ExitStack() as st:
                W1 = self.sb(st, "W1", [128, 8, 2048], BF16)
                W2 = self.sb(st, "W2", [128, 16, 1024], BF16)
                w1b = [Buf("W1_%d" % i) for i in range(4)]
                w2b = [Buf("W2_%d" % i) for i in range(4)]
                for kc in range(8):
                    k.dma("pool", W1[:, kc, :], self.din["w_ff1"][l][kc * 128:(kc + 1) * 128, fh * 2048:(fh + 1) * 2048], writes=w1b)
                for fc in range(16):
                    r0 = fh * 2048 + fc * 128
                    k.dma("pool", W2[:, fc, :], self.din["w_ff2"][l][r0:r0 + 128, :], writes=[w2b[fc // 4]])
                actT = self.sb(st, "actT", [128, 16, 512], BF16)
                act_b = [Buf("act%d" % i) for i in range(16)]
                rl = [self.sb(st, "rl%d" % i, [128, 512], F32) for i in range(2)]
                rl_b = [Buf("rl%d" % i) for i in range(2)]
                hook = None
                gain_src = None
                if fh == 1:
                    hook = "final" if last else "norm"
                    gain_src = self.din["nfin"][:, :] if last else self.din["nmix"][l + 1]
                ctx = self.upd_alloc(st, "f", gain_src, final=(fh == 1 and last))
                pb = [self.ps(st, "fpb%d" % i, [128, 512], F32) for i in range(6)]
                pbb = [Buf("fpb%d" % i) for i in range(6)]
                if hook == "norm":
                    ctx["ptr"] = self.ps(st, "fptr", [128, 1024], BF16)
                    ctx["ptr_b"] = Buf("fptr")
                for G in range(8):
                    toks = slice(G * 512, (G + 1) * 512)
                    hb = self.hT_b[G * 4:(G + 1) * 4]
                    for fc in range(16):
                        p = fc % 2
                        for kc in range(8):
                            k.op("pe", lambda e, kc=kc, fc=fc, p=p: e.matmul(pb[p][:, :], lhsT=W1[:, kc, fc * 128:(fc + 1) * 128],
                                                                               rhs=self.hT[:, kc, toks], start=(kc == 0), stop=(kc == 7)),
                                 reads=[w1b[fc // 4]] + hb, writes=[pbb[p]])
                        k.op("act", lambda e, p=p: e.activation(out=rl[p][:], in_=pb[p][:, :], func=AF.Relu), reads=[pbb[p]], writes=[rl_b[p]])
                        k.op("dve", lambda e, p=p, fc=fc: e.tensor_tensor(out=actT[:, fc, :], in0=rl[p][:], in1=rl[p][:], op=ALU.mult),
                             reads=[rl_b[p]], writes=[act_b[fc]])
                    for tt in range(4):
                        t = G * 4 + tt
                        pp = [2 + 2 * (tt % 2), 3 + 2 * (tt % 2)]
                        for nh in range(2):
                            for fc in range(16):
                                k.op("pe", lambda e, nh=nh, fc=fc, tt=tt: e.matmul(pb[pp[nh]][:, :], lhsT=actT[:, fc, tt * 128:(tt + 1) * 128],
                                                                                  rhs=W2[:, fc, nh * 512:(nh + 1) * 512], start=(fc == 0), stop=(fc == 15)),
                                     reads=[w2b[fc // 4], act_b[fc]], writes=[pbb[pp[nh]]])
                        self.x_update(ctx, t, [pb[pp[0]][:, :], pb[pp[1]][:, :]], [pbb[pp[0]], pbb[pp[1]]], hook)
                self.upd_flush(ctx)
                k.barrier()

    def phase_nsa(self, l):
        k = self.k
        win = self.din["w_in"][l]
        with ExitStack() as st:
            Wq = self.sb(st, "nWq", [128, 8, 512], BF16); wqb = Buf("nWq")
            ksT = self.sb(st, "nksT", [128, 2, S], BF16); ksTb = Buf("ksT")
            kwT = self.sb(st, "nkwT", [64, 2, S], BF16); kwTb = Buf("kwT")
            vsa = self.sb(st, "nvsa", [128, NT, 2, 65], BF16); vsab = Buf("vsa")
            vwa = self.sb(st, "nvwa", [128, NT, 2, 65], BF16); vwab = Buf("vwa")
            gn = self.sb(st, "ngn", [128, NT, 24], F32); gnb = Buf("gn")
            kcmpT = self.sb(st, "nkcmpT", [64, 2, 256], BF16); kcmpb = Buf("kcmpT")
            VC = self.sb(st, "nVC", [128, 2, 2, 129], BF16); VCb = Buf("VC")
            selv = self.sb(st, "nselv", [128, 126], F32)
            sela = self.sb(st, "nsela", [128, 126], F32)
            ovl = self.sb(st, "novl", [128, 2, 64], F32)
            ncb = Buf("nconst")
            k.dma("sp", selv[:], self.din["selvalid"], writes=[ncb])
            k.dma("sp", sela[:], self.din["seladd"], writes=[ncb])
            k.dma("sp", ovl[:], self.din["overlap"], writes=[ncb])
            A = [self.ps(st, "nA%d" % i, [128, 512], F32) for i in range(4)]
            Ab = [Buf("nA%d" % i) for i in range(4)]
            for g in range(2):
                k.dma("pool", ksT[64:128, g, :], self.din["rfull"], writes=[ksTb])
            OC = [self.ps(st, "nOC%d" % i, [128, 512], F32) for i in range(2)]
            OCb = [Buf("nOC%d" % i) for i in range(2)]
            OS = self.ps(st, "nOS", [128, 512], F32); OSb = Buf("OS")
            OW = self.ps(st, "nOW", [128, 512], F32); OWb = Buf("OW")
            Q = OS; Qb = OSb
            R0bf = OS[:, :].bitcast(BF16)
            R1bf = OW[:, :].bitcast(BF16)
            k.op("dve", lambda e: e.memset(vsa[:], 1.0), writes=[vsab])
            k.op("dve", lambda e: e.memset(vwa[:], 1.0), writes=[vwab])
            k.op("dve", lambda e: e.memset(kcmpT[:], 0.0), writes=[kcmpb])
            k.op("dve", lambda e: e.memset(VC[:], 0.0), writes=[VCb])
            for j in range(2):
                for g in range(2):
                    k.op("dve", lambda e, j=j, g=g: e.memset(VC[:, j, g, 64:65], 1.0), writes=[VCb])
                    k.op("dve", lambda e, j=j, g=g: e.tensor_copy(out=VC[:, j, g, 65:129], in_=ovl[:, j, :]), reads=[ncb], writes=[VCb])
            with ExitStack() as st2:
                Wk = self.sb(st2, "nWk", [128, 8, 512], BF16); wkb = Buf("nWk")
                for i, c0 in enumerate((C_KC, C_VC, C_KS, C_KW)):
                    for kc in range(8):
                        k.dma("pool", Wk[:, kc, i * 128:(i + 1) * 128], win[kc * 128:(kc + 1) * 128, c0:c0 + 128], writes=[wkb])
                Wtm = self.sb(st2, "nWtm", [128, 8, 280], BF16); wtb = Buf("nWtm")
                for (o, c0, n) in ((0, C_VS, 128), (128, C_VW, 128), (256, C_GN, 24)):
                    for kc in range(8):
                        k.dma("pool", Wtm[:, kc, o:o + n], win[kc * 128:(kc + 1) * 128, c0:c0 + n], writes=[wtb])
                w1 = [self.sb(st2, "nw1%d" % i, [64, 32, 128], BF16) for i in range(2)]
                w2k = self.sb(st2, "nw2k", [128, 64], BF16)
                w2v = self.sb(st2, "nw2v", [128, 64], BF16)
                peT = [self.sb(st2, "npeT%d" % i, [64, 32], BF16) for i in range(2)]
                cwb = Buf("cmpw")
                for i, nm in enumerate(("cmp_w1_k", "cmp_w1_v")):
                    k.dma("pool", w1[i][:], self.din[nm][l].rearrange("l d f -> d l f"), writes=[cwb])
                k.dma("pool", w2k[:], self.din["cmp_w2_k"][l], writes=[cwb])
                k.dma("pool", w2v[:], self.din["cmp_w2_v"][l], writes=[cwb])
                k.dma("pool", peT[0][:], self.din["cmp_pe_kT"][l], writes=[cwb])
                k.dma("pool", peT[1][:], self.din["cmp_pe_vT"][l], writes=[cwb])
                self.load_w(Wq, win[:, C_QN:C_QN + 512], wqb, 8)
                for t in range(NT):
                    tk = slice(t * 128, (t + 1) * 128)
                    P = A[t % 2]; PB = Ab[t % 2]
                    for kc in range(8):
                        k.op("pe", lambda e, kc=kc, P=P: e.matmul(P[:, 0:280], lhsT=self.hT[:, kc, tk], rhs=Wtm[:, kc, :], start=(kc == 0), stop=(kc == 7)),
                             reads=[wtb, self.hT_b[t]], writes=[PB])
                    k.op("act", lambda e, P=P, t=t: e.activation(out=vsa[:, t, :, 0:64], in_=P[:, 0:128].rearrange("p (g d) -> p g d", g=2), func=AF.Copy),
                         reads=[PB], writes=[vsab])
                    k.op("act", lambda e, P=P, t=t: e.activation(out=vwa[:, t, :, 0:64], in_=P[:, 128:256].rearrange("p (g d) -> p g d", g=2), func=AF.Copy),
                         reads=[PB], writes=[vwab])
                    k.op("act", lambda e, P=P, t=t: e.activation(out=gn[:, t, :], in_=P[:, 256:280], func=AF.Sigmoid), reads=[PB], writes=[gnb])
                it = 0
                for (dst, dstb, wi) in ((ksT, ksTb, 2), (kwT, kwTb, 3)):
                    for g in range(2):
                        for G in range(8):
                            toks = slice(G * 512, (G + 1) * 512)
                            P = OC[it % 2]; PB = OCb[it % 2]; it += 1
                            for kc in range(8):
                                k.op("pe", lambda e, kc=kc, P=P, wi=wi, g=g: e.matmul(P[0:64, :], lhsT=Wk[:, kc, wi * 128 + g * 64:wi * 128 + g * 64 + 64],
                                                                                     rhs=self.hT[:, kc, toks], start=(kc == 0), stop=(kc == 7)),
                                     reads=[wkb] + self.hT_b[G * 4:(G + 1) * 4], writes=[PB])
                            k.op("act", lambda e, P=P, dst=dst, g=g: e.activation(out=dst[0:64, g, toks], in_=P[0:64, :], func=AF.Copy), reads=[PB], writes=[dstb])
                cT = [self.sb(st2, "ncT%d" % i, [64, S], BF16) for i in range(2)]
                cTb = [Buf("ncT%d" % i) for i in range(2)]
                hx = self.sb(st2, "nhx", [128, 4, 256], F32); hxb = Buf("nhx")
                hidb = self.sb(st2, "nhidb", [128, 256], BF16); hidbb = Buf("nhidb")
                cbias = self.sb(st2, "ncbias", [128, 2], F32); cbb = Buf("ncbias")
                for kv in range(2):
                    for lidx in range(32):
                        k.op("pe", lambda e, kv=kv, lidx=lidx: e.matmul(Q[:, kv:kv + 1], lhsT=w1[kv][:, lidx, :], rhs=peT[kv][:, lidx:lidx + 1],
                                                                       start=(lidx == 0), stop=(lidx == 31)), reads=[cwb], writes=[Qb])
                k.op("act", lambda e: e.activation(out=cbias[:], in_=Q[:, 0:2], func=AF.Copy), reads=[Qb], writes=[cbb])
                for g in range(2):
                    for kv in range(2):
                        for G in range(8):
                            toks = slice(G * 512, (G + 1) * 512)
                            P = OC[it % 2]; PB = OCb[it % 2]; it += 1
                            for kc in range(8):
                                k.op("pe", lambda e, kc=kc, P=P, kv=kv, g=g: e.matmul(P[0:64, :], lhsT=Wk[:, kc, kv * 128 + g * 64:kv * 128 + g * 64 + 64],
                                                                                     rhs=self.hT[:, kc, toks], start=(kc == 0), stop=(kc == 7)),
                                     reads=[wkb] + self.hT_b[G * 4:(G + 1) * 4], writes=[PB])
                            k.op("act", lambda e, P=P, kv=kv: e.activation(out=cT[kv][:, toks], in_=P[0:64, :], func=AF.Copy), reads=[PB], writes=[cTb[kv]])
                    for kv in range(2):
                        P = A[kv]; PB = Ab[kv]
                        for lidx in range(32):
                            k.op("pe", lambda e, kv=kv, lidx=lidx, P=P: e.matmul(P[:, 0:255], lhsT=w1[kv][:, lidx, :], rhs=cT[kv][:, lidx:lidx + 16 * 254 + 1:16],
                                                                                start=(lidx == 0), stop=(lidx == 31)), reads=[cwb, cTb[kv]], writes=[PB])
                        x_ = hx[:, 0, 0:255]
                        k.op("act", lambda e, P=P, kv=kv: e.activation(out=x_, in_=P[:, 0:255], func=AF.Identity, bias=cbias[:, kv:kv + 1], scale=1.0),
                             reads=[PB, cbb], writes=[hxb])
                        k.op("dve", lambda e: e.tensor_tensor(out=hx[:, 1, 0:255], in0=x_, in1=x_, op=ALU.mult), reads=[hxb], writes=[hxb])
                        k.op("dve", lambda e: e.tensor_scalar(out=hx[:, 1, 0:255], in0=hx[:, 1, 0:255], scalar1=0.044715, scalar2=1.0, op0=ALU.mult, op1=ALU.add),
                             reads=[hxb], writes=[hxb])
                        k.op("dve", lambda e: e.tensor_tensor(out=hx[:, 2, 0:255], in0=hx[:, 1, 0:255], in1=x_, op=ALU.mult), reads=[hxb], writes=[hxb])
                        k.op("act", lambda e: e.activation(out=hx[:, 3, 0:255], in_=hx[:, 2, 0:255], func=AF.Sigmoid, scale=1.5957691216057308),
                             reads=[hxb], writes=[hxb])
                        k.op("dve", lambda e: e.tensor_tensor(out=hidb[:, 0:255], in0=hx[:, 3, 0:255], in1=x_, op=ALU.mult), reads=[hxb], writes=[hidbb])
                        if kv == 0:
                            k.op("pe", lambda e: e.matmul(Q[0:64, 0:255], lhsT=w2k[:], rhs=hidb[:, 0:255], start=True, stop=True), reads=[cwb, hidbb], writes=[Qb])
                            k.op("act", lambda e, g=g: e.activation(out=kcmpT[:, g, 0:255], in_=Q[0:64, 0:255], func=AF.Copy), reads=[Qb], writes=[kcmpb])
                        else:
                            for j in range(2):
                                nn = 128 if j == 0 else 127
                                k.op("pe", lambda e, j=j, nn=nn: e.matmul(Q[0:nn, j * 64:(j + 1) * 64], lhsT=hidb[:, j * 128:j * 128 + nn], rhs=w2v[:],
                                                                         start=True, stop=True), reads=[cwb, hidbb], writes=[Qb])
                                k.op("act", lambda e, j=j, nn=nn, g=g: e.activation(out=VC[0:nn, j, g, 0:64], in_=Q[0:nn, j * 64:(j + 1) * 64], func=AF.Copy),
                                     reads=[Qb], writes=[VCb])
            k.barrier()
            snegAll = self.sb(st, "nsnegAll", [128, NT * 2, 128], BF16); snegAllb = [Buf("sneg%d" % i) for i in range(NT * 2)]
            k.op("dve", lambda e: e.memset(snegAll[:], 0.0), writes=snegAllb)
            bank = {"A0": (A[0], Ab[0]), "A1": (A[1], Ab[1]), "A2": (A[2], Ab[2]), "A3": (A[3], Ab[3]),
                    "C0": (OC[0], OCb[0]), "C1": (OC[1], OCb[1]), "S": (OS, OSb), "W": (OW, OWb)}

            def finish(Tt, Tbb, sTt, sTbb, rows):
                k.op("act", lambda e: e.activation(out=sTt[0:rows, :], in_=Tt[0:rows, :], func=AF.Copy), reads=[Tbb], writes=[sTbb])
                for h4 in range(4):
                    k.op("pe", lambda e, h4=h4: e.transpose(Tt[:, h4 * 65:h4 * 65 + rows], sTt[0:rows, h4 * 128:(h4 + 1) * 128], self.identf[0:rows, 0:rows]),
                         reads=[sTbb, self.cb], writes=[Tbb])

            with ExitStack() as st3:
                qA = self.sb(st3, "nqA", [64, 4, 2, 512], BF16); qAb = Buf("nqA")
                ebc = [self.sb(st3, "nebc%d" % i, [128, 2, 8, 128], BF16) for i in range(2)]
                ebcb = [Buf("nebc%d" % i) for i in range(2)]
                pT = [self.sb(st3, "napT%d" % i, [128, 512], BF16) for i in range(4)]
                pTb = [Buf("napT%d" % i) for i in range(4)]
                sm = [self.sb(st3, "nasm%d" % i, [128, 4], F32) for i in range(2)]; smb = [Buf("nasm%d" % i) for i in range(2)]
                imp = [self.sb(st3, "naimp%d" % i, [128, 64], F32) for i in range(2)]; impb = [Buf("naimp%d" % i) for i in range(2)]
                m8 = [self.sb(st3, "nam8%d" % i, [128, 8], F32) for i in range(2)]
                cf = [self.sb(st3, "nacf%d" % i, [128, 4], F32) for i in range(2)]; cfb = [Buf("nacf%d" % i) for i in range(2)]
                occ = [self.sb(st3, "naocc%d" % i, [128, 512], F32) for i in range(2)]; occb = [Buf("naocc%d" % i) for i in range(2)]
                sTo = [self.sb(st3, "nasTo%d" % i, [65, 512], F32) for i in range(2)]; sTob = [Buf("nasTo%d" % i) for i in range(2)]
                sTi = [self.sb(st3, "nasTi%d" % i, [64, 512], F32) for i in range(2)]; sTib = [Buf("nasTi%d" % i) for i in range(2)]
                SA = [bank["A0"], bank["A1"]]
                CO = [bank["A2"], bank["C0"]]
                CI = [bank["A3"], bank["C1"]]
                QP, QPb = bank["S"]
                pti = 0
                it = 0
                for c in range(NT):
                    bq = c % 4
                    if bq == 0:
                        toks = slice(c * 128, (c + 4) * 128)
                        for h in range(8):
                            for kc in range(8):
                                k.op("pe", lambda e, kc=kc, h=h: e.matmul(QP[0:64, :], lhsT=Wq[:, kc, h * 64:(h + 1) * 64], rhs=self.hT[:, kc, toks],
                                                                         start=(kc == 0), stop=(kc == 7)), reads=[wqb] + self.hT_b[c:c + 4], writes=[QPb])
                            k.op("act", lambda e, h=h: e.activation(out=qA[:, :, h // 4, (h % 4) * 128:(h % 4 + 1) * 128],
                                                                    in_=QP[0:64, :].rearrange("p (b q) -> p b q", b=4), func=AF.Copy, scale=0.125),
                                 reads=[QPb], writes=[qAb])
                    e_ = ebc[c % 2]; e_b = ebcb[c % 2]
                    njt = 2 if c >= 16 else 1
                    for j in range(njt):
                        r0 = 248 - 8 * c + 128 * j
                        k.dma("sp", e_[:, j], self.ebc_d[r0:r0 + 128, :, :], reads=[self.ebc_db], writes=[e_b])
                    oc_ = occ[c % 2]; oc_b = occb[c % 2]
                    staged = []
                    for g in range(2):
                        qg = qA[:, bq, g, :]
                        for j in range(njt):
                            (Aa, Aab) = SA[pti % 2]; pi = pti % 4; pti += 1
                            k.op("pe", lambda e, j=j, g=g, Aa=Aa, qg=qg: e.matmul(Aa[:, :], lhsT=kcmpT[:, g, j * 128:(j + 1) * 128], rhs=qg, start=True, stop=True),
                                 reads=[kcmpb, qAb], writes=[Aab])
                            k.op("act", lambda e, Aa=Aa, pi=pi: e.activation(out=pT[pi][:], in_=Aa[:, :], func=AF.Exp), reads=[Aab], writes=[pTb[pi]])
                            k.op("dve", lambda e, pi=pi, j=j, g=g: e.tensor_tensor(out=pT[pi][:], in0=pT[pi][:],
                                                                                   in1=e_[:, j, g * 4:(g + 1) * 4, :].rearrange("p h q -> p (h q)"), op=ALU.mult),
                                 reads=[pTb[pi], e_b], writes=[pTb[pi]])
                            staged.append((g, j, pi))
                    for (g, j, pi) in staged:
                        (To, Tob), (Ti, Tib) = CO[g], CI[g]
                        k.op("pe", lambda e, pi=pi, j=j, g=g, To=To: e.matmul(To[0:65, :], lhsT=VC[:, j, g, 0:65], rhs=pT[pi][:], start=(j == 0), stop=(j == njt - 1)),
                             reads=[pTb[pi], VCb], writes=[Tob])
                        k.op("pe", lambda e, pi=pi, j=j, g=g, Ti=Ti: e.matmul(Ti[0:64, :], lhsT=VC[:, j, g, 65:129], rhs=pT[pi][:], start=(j == 0), stop=(j == njt - 1)),
                             reads=[pTb[pi], VCb], writes=[Tib])
                    for g in range(2):
                        (To, Tob), (Ti, Tib) = CO[g], CI[g]
                        k.op("act", lambda e, g=g, To=To: e.activation(out=sTo[g][0:65, :], in_=To[0:65, :], func=AF.Copy), reads=[Tob], writes=[sTob[g]])
                        k.op("act", lambda e, g=g, Ti=Ti: e.activation(out=sTi[g][0:64, :], in_=Ti[0:64, :], func=AF.Copy), reads=[Tib], writes=[sTib[g]])
                    for g in range(2):
                        (To, Tob), (Ti, Tib) = CO[g], CI[g]
                        for h4 in range(4):
                            k.op("pe", lambda e, h4=h4, g=g, To=To: e.transpose(To[:, h4 * 65:h4 * 65 + 65], sTo[g][0:65, h4 * 128:(h4 + 1) * 128], self.identf[0:65, 0:65]),
                                 reads=[sTob[g], self.cb], writes=[Tob])
                        for h4 in range(4):
                            k.op("pe", lambda e, h4=h4, g=g, Ti=Ti: e.transpose(Ti[:, h4 * 65:h4 * 65 + 64], sTi[g][0:64, h4 * 128:(h4 + 1) * 128], self.identf[0:64, 0:64]),
                                 reads=[sTib[g], self.cb], writes=[Tib])
                    chains = [[], []]
                    for g in range(2):
                        ch = chains[g]
                        (To, Tob), (Ti, Tib) = CO[g], CI[g]
                        sm_, smb_, imp_, impb_, cf_, cfb_, m8_ = sm[g], smb[g], imp[g], impb[g], cf[g], cfb[g], m8[g]
                        ch.append((lambda e, sm_=sm_, To=To: e.tensor_scalar(out=sm_[:], in0=To[:, 64:64 + 260:65], scalar1=1e-30, scalar2=None, op0=ALU.max), [Tob], [smb_]))
                        ch.append((lambda e, sm_=sm_: e.reciprocal(out=sm_[:], in_=sm_[:]), [smb_], [smb_]))
                        for h4 in range(4):
                            src = Ti[:, h4 * 65:h4 * 65 + 64]
                            if h4 == 0:
                                ch.append((lambda e, src=src, sm_=sm_, imp_=imp_: e.tensor_scalar(out=imp_[:], in0=src, scalar1=sm_[:, 0:1], scalar2=None, op0=ALU.mult),
                                           [Tib, smb_], [impb_]))
                            else:
                                ch.append((lambda e, src=src, h4=h4, sm_=sm_, imp_=imp_: e.scalar_tensor_tensor(out=imp_[:], in0=src, scalar=sm_[:, h4:h4 + 1], in1=imp_[:],
                                                                                                                 op0=ALU.mult, op1=ALU.add), [Tib, smb_, impb_], [impb_]))
                        sl = slice(62 - 2 * c, 62 - 2 * c + 64)
                        ch.append((lambda e, imp_=imp_, sl=sl: e.tensor_tensor(out=imp_[:], in0=imp_[:], in1=selv[:, sl], op=ALU.mult), [impb_, ncb], [impb_]))
                        ch.append((lambda e, imp_=imp_, sl=sl: e.tensor_tensor(out=imp_[:], in0=imp_[:], in1=sela[:, sl], op=ALU.add), [impb_, ncb], [impb_]))
                        if c >= 1:
                            ch.append((lambda e, imp_=imp_: e.tensor_scalar(out=imp_[:, 0:1], in0=imp_[:, 0:1], scalar1=1e4, scalar2=None, op0=ALU.add), [impb_], [impb_]))
                        ch.append((lambda e, imp_=imp_, m8_=m8_: e.max(out=m8_[:], in_=imp_[:]), [impb_], [impb_]))
                        si = c * 2 + g
                        ch.append((lambda e, si=si, imp_=imp_, m8_=m8_: e.tensor_scalar(out=snegAll[:, si, 64:128], in0=imp_[:], scalar1=m8_[:, 7:8], scalar2=NEG,
                                                                                       op0=ALU.is_lt, op1=ALU.mult), [impb_], [snegAllb[si]]))
                        gview = gn[:, c, g * 12:(g + 1) * 12].rearrange("p (h b) -> p h b", h=4)
                        ch.append((lambda e, cf_=cf_, gview=gview, sm_=sm_: e.tensor_tensor(out=cf_[:], in0=gview[:, :, 0], in1=sm_[:], op=ALU.mult), [gnb, smb_], [cfb_]))
                        for h4 in range(4):
                            hh = g * 4 + h4
                            ch.append((lambda e, h4=h4, hh=hh, To=To, cf_=cf_: e.tensor_scalar(out=oc_[:, hh * 64:(hh + 1) * 64], in0=To[:, h4 * 65:h4 * 65 + 64],
                                                                                               scalar1=cf_[:, h4:h4 + 1], scalar2=None, op0=ALU.mult), [Tob, cfb_], [oc_b]))
                    for i_ in range(max(len(chains[0]), len(chains[1]))):
                        for g in range(2):
                            if i_ < len(chains[g]):
                                fn_, rd_, wr_ = chains[g][i_]
                                k.op("dve", fn_, reads=rd_, writes=wr_)
                    k.dma("sp", self.ocd[c], oc_[:], reads=[oc_b], writes=[self.ocd_b[c]])
            k.barrier()
            qAll = self.sb(st, "nqAll", [128, 4, 2, 512], BF16)
            qTopb = Buf("qTop")
            qBotb = [[Buf("qBot%d_%d" % (b_, g_)) for g_ in range(2)] for b_ in range(4)]
            NPT = 5
            LA = 3
            pT = [self.sb(st, "npT%d" % i, [128, 512], BF16) for i in range(NPT)]
            pTb = [Buf("npT%d" % i) for i in range(NPT)]
            sm = self.sb(st, "nsm", [128, 2, 4], F32); smb = Buf("nsm")
            coef = self.sb(st, "ncoef", [128, 4, 2], F32); coefb = Buf("ncoef")
            oin = [self.sb(st, "noin%d" % i, [128, 512], F32) for i in range(2)]; oinb = [Buf("noin%d" % i) for i in range(2)]
            onsa = self.sb(st, "nonsa", [128, 512], BF16); onsab = Buf("nonsa")
            onT = [self.sb(st, "nonT%d" % i, [128, 4, 128], BF16) for i in range(2)]
            onTb = [Buf("nonT%d" % i) for i in range(2)]
            sTs = [self.sb(st, "nsTs%d" % i, [65, 512], F32) for i in range(2)]; sTsb = [Buf("nsTs%d" % i) for i in range(2)]
            sTw = [self.sb(st, "nsTw%d" % i, [65, 512], F32) for i in range(2)]; sTwb = [Buf("nsTw%d" % i) for i in range(2)]
            SA = [bank["A0"], bank["A1"], bank["A2"], bank["A3"]]
            (Ts, Tsb), (Tw, Twb) = bank["C0"], bank["C1"]
            (Rs, Rsb), (Rw, Rwb) = bank["S"], bank["W"]
            pti = 0
            it = 0

            def finish2(Tt, Tbb, sTt, sTbb, Rr, Rbb):
                k.op("act", lambda e: e.activation(out=sTt[0:65, :], in_=Tt[0:65, :], func=AF.Copy), reads=[Tbb], writes=[sTbb])
                for h4 in range(4):
                    k.op("pe", lambda e, h4=h4: e.transpose(Rr[:, h4 * 65:h4 * 65 + 65], sTt[0:65, h4 * 128:(h4 + 1) * 128], self.identf[0:65, 0:65]),
                         reads=[sTbb, self.cb], writes=[Rbb])

            for c in range(NT):
                bq = c % 4
                if bq == 0:
                    toks = slice(c * 128, (c + 4) * 128)
                    for h in range(8):
                        (X, Xb) = SA[pti % 4]; pti += 1
                        for kc in range(8):
                            k.op("pe", lambda e, kc=kc, h=h, X=X: e.matmul(X[0:64, :], lhsT=Wq[:, kc, h * 64:(h + 1) * 64], rhs=self.hT[:, kc, toks],
                                                                          start=(kc == 0), stop=(kc == 7)), reads=[wqb] + self.hT_b[c:c + 4], writes=[Xb])
                        k.op("act", lambda e, h=h, X=X: e.activation(out=qAll[0:64, :, h // 4, (h % 4) * 128:(h % 4 + 1) * 128],
                                                                     in_=X[0:64, :].rearrange("p (b q) -> p b q", b=4), func=AF.Copy, scale=0.125),
                             reads=[Xb], writes=[qTopb])
                oi = oin[c % 2]; oib = oinb[c % 2]
                k.dma("sp", oi[:], self.ocd[c], reads=[self.ocd_b[c]], writes=[oib])
                for g in range(2):
                    par = it % 2; it += 1
                    si = c * 2 + g
                    qg = qAll[0:64, bq, g, :]
                    qfull = qAll[:, bq, g, :]
                    (X, Xb) = SA[pti % 4]; pti += 1
                    Xbf = X[:, :].bitcast(BF16)
                    k.op("pe", lambda e, si=si: e.transpose(Xbf[:, 0:128], snegAll[:, si, :], self.ident[:]), reads=[snegAllb[si], self.cb], writes=[Xb])
                    k.op("dve", lambda e: e.tensor_copy(out=qAll[64:128, bq, g, :].rearrange("p (h q) -> p h q", h=4),
                                                        in_=Xbf[64:128, 0:128].unsqueeze(1).to_broadcast([64, 4, 128])),
                         reads=[Xb], writes=[qBotb[bq][g]])
                    tiles = [("w", kb) for kb in range(max(0, c - 4), c + 1)] + [("s", kb) for kb in range(c + 1)]
                    nwin = len(tiles) - (c + 1)
                    slots = []

                    def emit_qk(idx):
                        nonlocal pti
                        kind, kb = tiles[idx]
                        (Aa, Aab) = SA[pti % 4]; pi = pti % NPT; pti += 1
                        slots.append((Aa, Aab, pi))
                        kt = slice(kb * 128, (kb + 1) * 128)
                        off = c - kb
                        near = (kind == "w" or off <= 1)
                        if kind == "s":
                            k.op("pe", lambda e: e.matmul(Aa[:, :], lhsT=ksT[:, g, kt], rhs=qfull, start=True, stop=not near),
                                 reads=[ksTb, qTopb, qBotb[bq][g]], writes=[Aab])
                        else:
                            k.op("pe", lambda e: e.matmul(Aa[:, :], lhsT=kwT[:, g, kt], rhs=qg, start=True, stop=not near), reads=[kwTb, qTopb], writes=[Aab])
                        if near:
                            k.op("pe", lambda e: e.matmul(Aa[:, :], lhsT=self.ident[:], rhs=self.EB[:, off, g * 4:(g + 1) * 4, :].rearrange("p h q -> p (h q)"),
                                                          start=False, stop=True), reads=[self.cb], writes=[Aab])

                    def emit_pv(idx):
                        kind, kb = tiles[idx]
                        Aa, Aab, pi = slots[idx]
                        k.op("act", lambda e: e.activation(out=pT[pi][:], in_=Aa[:, :], func=AF.Exp), reads=[Aab], writes=[pTb[pi]])
                        if kind == "s":
                            Tt, Ttb, V, Vb = Ts, Tsb, vsa, vsab
                            first, lastt = (idx == nwin), (idx == len(tiles) - 1)
                        else:
                            Tt, Ttb, V, Vb = Tw, Twb, vwa, vwab
                            first, lastt = (idx == 0), (idx == nwin - 1)
                        k.op("pe", lambda e: e.matmul(Tt[0:65, :], lhsT=V[:, kb, g, :], rhs=pT[pi][:], start=first, stop=lastt),
                             reads=[pTb[pi], Vb], writes=[Ttb])

                    for idx in range(len(tiles) + LA):
                        if idx < len(tiles):
                            emit_qk(idx)
                        if idx >= LA:
                            emit_pv(idx - LA)
                    finish2(Tw, Twb, sTw[par], sTwb[par], Rw, Rwb)
                    finish2(Ts, Tsb, sTs[par], sTsb[par], Rs, Rsb)
                    gview = gn[:, c, g * 12:(g + 1) * 12].rearrange("p (h b) -> p h b", h=4)
                    k.op("dve", lambda e: e.tensor_scalar(out=sm[:, 0, :], in0=Rs[:, 64:64 + 260:65], scalar1=1e-30, scalar2=None, op0=ALU.max), reads=[Rsb, smb], writes=[smb])
                    k.op("dve", lambda e: e.tensor_scalar(out=sm[:, 1, :], in0=Rw[:, 64:64 + 260:65], scalar1=1e-30, scalar2=None, op0=ALU.max), reads=[Rwb, smb], writes=[smb])
                    k.op("dve", lambda e: e.reciprocal(out=sm[:, :, :], in_=sm[:, :, :]), reads=[smb], writes=[smb])
                    k.op("dve", lambda e: e.tensor_tensor(out=coef[:], in0=gview[:, :, 1:3], in1=sm[:, :, :].rearrange("p b h -> p h b"), op=ALU.mult),
                         reads=[gnb, smb, coefb], writes=[coefb])
                    for h4 in range(4):
                        hh = g * 4 + h4
                        k.op("dve", lambda e, h4=h4, hh=hh: e.scalar_tensor_tensor(out=oi[:, hh * 64:(hh + 1) * 64], in0=Rs[:, h4 * 65:h4 * 65 + 64], scalar=coef[:, h4, 0:1],
                                                                                    in1=oi[:, hh * 64:(hh + 1) * 64], op0=ALU.mult, op1=ALU.add), reads=[Rsb, coefb, oib], writes=[oib])
                        k.op("dve", lambda e, h4=h4, hh=hh: e.scalar_tensor_tensor(out=onsa[:, hh * 64:(hh + 1) * 64], in0=Rw[:, h4 * 65:h4 * 65 + 64], scalar=coef[:, h4, 1:2],
                                                                                    in1=oi[:, hh * 64:(hh + 1) * 64], op0=ALU.mult, op1=ALU.add), reads=[Rwb, coefb, oib], writes=[onsab])
                (X, Xb) = SA[pti % 4]; pti += 1
                Xbf = X[:, :].bitcast(BF16)
                for j in range(4):
                    k.op("pe", lambda e, j=j: e.transpose(Xbf[:, j * 128:(j + 1) * 128], onsa[:, j * 128:(j + 1) * 128], self.ident[:]),
                         reads=[onsab, self.cb], writes=[Xb])
                i2 = c % 2
                k.op("act", lambda e, i2=i2: e.activation(out=onT[i2][:], in_=Xbf[:, 0:512].rearrange("p (j c) -> p j c", j=4), func=AF.Copy),
                     reads=[Xb], writes=[onTb[i2]])
                tk = slice(c * 128, (c + 1) * 128)
                k.dma("sp", self.brT[0, :, :, tk].rearrange("c p q -> p c q"), onT[i2][:], reads=[onTb[i2]], writes=[self.brT_b[0][c]])

    def phase_ret(self, l):
        k = self.k
        with ExitStack() as st:
            Wqk = self.sb(st, "rWqk", [128, 8, 512], BF16)
            Wv = self.sb(st, "rWv", [128, 8, 512], BF16)
            Wg = self.sb(st, "rWg", [128, 8, 512], BF16)
            wb = Buf("rW")
            self.load_w(Wqk, self.din["w_in"][l][:, C_QR:C_QR + 512], wb, 8)
            self.load_w(Wv, self.din["w_in"][l][:, C_VR:C_VR + 512], wb, 8)
            self.load_w(Wg, self.din["w_in"][l][:, C_GR:C_GR + 512], wb, 8)
            cos = self.sb(st, "rcos", [128, NT, 32], F32)
            sin = self.sb(st, "rsin", [128, NT, 32], F32)
            dec = self.sb(st, "rdec", [128, 4, 128], F32)
            xi = self.sb(st, "rxi", [64, 4, 128], F32)
            zeta = self.sb(st, "rzeta", [128, 4], F32)
            gch = self.sb(st, "rgch", [64, 4], F32)
            gn = self.sb(st, "rgn", [128, 512], F32)
            rc = Buf("rconst")
            for dst, src in ((cos, "cos"), (sin, "sin"), (dec, "decayT"), (xi, "xi"), (zeta, "zeta"), (gch, "gch")):
                k.dma("sp", dst[:], self.din[src], writes=[rc])
            k.dma("sp", gn[:], self.din["retgn"][l], writes=[rc])
            Sf = self.sb(st, "rSf", [64, 4, 128], F32)
            Sb = self.sb(st, "rSb", [64, 4, 128], BF16)
            Sfb, Sbb = Buf("Sf"), Buf("Sb")
            k.op("dve", lambda e: e.memset(Sf[:], 0.0), writes=[Sfb])
            k.op("dve", lambda e: e.memset(Sb[:], 0.0), writes=[Sbb])
            qk = self.sb(st, "rqk", [128, 512], F32); qkb = Buf("rqk")
            tm = self.sb(st, "rtm", [128, 4, 8, 32], F32); tmb = Buf("rtm")
            rot = self.sb(st, "rrot", [128, 8, 2, 32], F32); rotb = Buf("rrot")
            qkbf = self.sb(st, "rqkbf", [128, 512], BF16); qkbfb = Buf("rqkbf")
            khat = self.sb(st, "rkhat", [128, 4, 64], BF16); khatb = Buf("rkhat")
            qkT = self.sb(st, "rqkT", [64, 8, 128], BF16); qkTb = Buf("rqkT")
            qxiT = self.sb(st, "rqxiT", [64, 4, 128], BF16); qxiTb = Buf("rqxiT")
            qf32 = self.sb(st, "rqf32", [64, 4, 128], F32); qf32b = Buf("rqf32")
            inT = self.sb(st, "rinT", [128, 4, 128], BF16); inTb = Buf("rinT")
            vbf = self.sb(st, "rvbf", [128, 512], BF16); vbfb = Buf("rvbf")
            osb = self.sb(st, "rosb", [128, 512], F32); osbb = Buf("rosb")
            osq = self.sb(st, "rosq", [128, 512], F32); osqb = Buf("rosq")
            sm = self.sb(st, "rsm", [128, 6, 4], F32); smb = Buf("rsm")
            yn = self.sb(st, "ryn", [128, 512], F32); ynb = Buf("ryn")
            gs = self.sb(st, "rgs", [128, 512], F32); gsb = Buf("rgs")
            orb = self.sb(st, "rorb", [128, 512], BF16); orbb = Buf("rorb")
            orT = [self.sb(st, "rorT%d" % i, [128, 4, 128], BF16) for i in range(2)]
            orTb = [Buf("rorT%d" % i) for i in range(2)]
            pqk = self.ps(st, "rpqk", [128, 512], F32); pqkb = Buf("pqk")
            pv = self.ps(st, "rpv", [128, 512], F32); pvb = Buf("pv")
            pg = self.ps(st, "rpg", [128, 512], F32); pgb = Buf("pg")
            pin = self.ps(st, "rpin", [128, 512], F32); pinb = Buf("pin")
            po = self.ps(st, "rpo", [128, 512], F32); pob = Buf("po")
            pkv = self.ps(st, "rpkv", [128, 512], F32); pkvb = Buf("pkv")
            ptr = self.ps(st, "rptr", [128, 1024], BF16); ptrb = Buf("ptr")
            ptr2 = self.ps(st, "rptr2", [128, 1024], BF16); ptr2b = Buf("ptr2")
            gs2 = [gs, self.sb(st, "rgs_b", [128, 512], F32)]; gs2b = [gsb, Buf("rgs_b")]
            osb2 = [osb, self.sb(st, "rosb_b", [128, 512], F32)]; osb2b = [osbb, Buf("rosb_b")]
            osq2 = [osq, self.sb(st, "rosq_b", [128, 512], F32)]; osq2b = [osqb, Buf("rosq_b")]

            def front(t):
                tk = slice(t * 128, (t + 1) * 128)
                for (W, P, PB) in ((Wqk, pqk, pqkb), (Wv, pv, pvb), (Wg, pg, pgb)):
                    for kc in range(8):
                        k.op("pe", lambda e, kc=kc, W=W, P=P: e.matmul(P[:, :], lhsT=self.hT[:, kc, tk], rhs=W[:, kc, :], start=(kc == 0), stop=(kc == 7)),
                             reads=[wb, self.hT_b[t]], writes=[PB])
                k.op("act", lambda e: e.activation(out=qk[:, 0:256], in_=pqk[:, 0:256], func=AF.Copy), reads=[pqkb], writes=[qkb])
                k.op("act", lambda e: e.activation(out=qk[:, 256:512], in_=pqk[:, 256:512], func=AF.Copy, scale=0.125), reads=[pqkb], writes=[qkb])
                k.op("act", lambda e: e.activation(out=vbf[:], in_=pv[:, :], func=AF.Copy), reads=[pvb], writes=[vbfb])
                k.op("act", lambda e: e.activation(out=gs2[t % 2][:], in_=pg[:, :], func=AF.Silu), reads=[pgb], writes=[gs2b[t % 2]])
                xv = qk[:, :].rearrange("p (h two d) -> p h two d", h=8, two=2)
                x1, x2 = xv[:, :, 0, :], xv[:, :, 1, :]
                cb_ = cos[:, t, :].unsqueeze(1).to_broadcast([128, 8, 32])
                sb_ = sin[:, t, :].unsqueeze(1).to_broadcast([128, 8, 32])
                k.op("dve", lambda e: e.tensor_tensor(out=tm[:, 0], in0=x1, in1=cb_, op=ALU.mult), reads=[qkb, rc], writes=[tmb])
                k.op("dve", lambda e: e.tensor_tensor(out=tm[:, 1], in0=x2, in1=sb_, op=ALU.mult), reads=[qkb, rc], writes=[tmb])
                k.op("dve", lambda e: e.tensor_tensor(out=tm[:, 2], in0=x1, in1=sb_, op=ALU.mult), reads=[qkb, rc], writes=[tmb])
                k.op("dve", lambda e: e.tensor_tensor(out=tm[:, 3], in0=x2, in1=cb_, op=ALU.mult), reads=[qkb, rc], writes=[tmb])
                k.op("dve", lambda e: e.tensor_tensor(out=rot[:, :, 0, :], in0=tm[:, 0], in1=tm[:, 1], op=ALU.subtract), reads=[tmb], writes=[rotb])
                k.op("dve", lambda e: e.tensor_tensor(out=rot[:, :, 1, :], in0=tm[:, 2], in1=tm[:, 3], op=ALU.add), reads=[tmb], writes=[rotb])
                rflat = rot[:, :, :, :].rearrange("p h two d -> p (h two d)")
                k.op("act", lambda e: e.activation(out=qkbf[:], in_=rflat, func=AF.Copy), reads=[rotb], writes=[qkbfb])
                k.op("dve", lambda e: e.tensor_tensor(out=khat[:], in0=rflat[:, 256:512].rearrange("p (h d) -> p h d", h=4),
                                                      in1=zeta[:, :].unsqueeze(2).to_broadcast([128, 4, 64]), op=ALU.mult),
                     reads=[rotb, rc], writes=[khatb])
                for j in range(8):
                    k.op("pe", lambda e, j=j: e.transpose(ptr[0:64, j * 128:(j + 1) * 128], qkbf[:, j * 64:(j + 1) * 64], self.ident[:]),
                         reads=[qkbfb, self.cb], writes=[ptrb])
                k.op("act", lambda e: e.activation(out=qkT[:], in_=ptr[0:64, 0:1024].rearrange("p (j c) -> p j c", j=8), func=AF.Copy),
                     reads=[ptrb], writes=[qkTb])
                k.op("act", lambda e: e.activation(out=qf32[:], in_=ptr[0:64, 0:512].rearrange("p (j c) -> p j c", j=4), func=AF.Copy),
                     reads=[ptrb], writes=[qf32b])
                k.op("dve", lambda e: e.tensor_tensor(out=qxiT[:], in0=qf32[:], in1=xi[:], op=ALU.mult),
                     reads=[qf32b, rc], writes=[qxiTb])
                for h in range(4):
                    k.op("pe", lambda e, h=h: e.matmul(pin[:, h * 128:(h + 1) * 128], lhsT=qkT[:, 4 + h, :], rhs=qkT[:, h, :],
                                                       start=True, stop=True), reads=[qkTb], writes=[pinb])
                k.op("dve", lambda e: e.tensor_tensor(out=inT[:], in0=pin[:, :].rearrange("p (h c) -> p h c", h=4), in1=dec[:], op=ALU.mult),
                     reads=[pinb, rc], writes=[inTb])

            def mid(t):
                for h in range(4):
                    hc = slice(h * 128, (h + 1) * 128)
                    k.op("pe", lambda e, h=h, hc=hc: e.matmul(po[:, hc], lhsT=inT[:, h, :], rhs=vbf[:, hc], start=True, stop=False),
                         reads=[inTb, vbfb], writes=[pob])
                    k.op("pe", lambda e, h=h, hc=hc: e.matmul(po[:, hc], lhsT=qxiT[:, h, :], rhs=Sb[:, h, :], start=False, stop=True),
                         reads=[qxiTb, Sbb], writes=[pob])
                for h in range(4):
                    hc = slice(h * 128, (h + 1) * 128)
                    k.op("pe", lambda e, h=h, hc=hc: e.matmul(pkv[0:64, hc], lhsT=khat[:, h, :],
                                                             rhs=vbf[:, hc], start=True, stop=True), reads=[khatb, vbfb], writes=[pkvb])
                for h in range(4):
                    hc = slice(h * 128, (h + 1) * 128)
                    k.op("dve", lambda e, h=h, hc=hc: e.scalar_tensor_tensor(out=Sf[:, h, :], in0=Sf[:, h, :],
                                                                              scalar=gch[:, h:h + 1], in1=pkv[0:64, hc],
                                                                              op0=ALU.mult, op1=ALU.add),
                         reads=[pkvb, rc, Sfb], writes=[Sfb])
                k.op("act", lambda e: e.activation(out=Sb[:], in_=Sf[:], func=AF.Copy), reads=[Sfb], writes=[Sbb])
                k.op("act", lambda e: e.activation(out=osb2[t % 2][:], in_=po[:, :], func=AF.Copy), reads=[pob], writes=[osb2b[t % 2]])
                k.op("act", lambda e: e.activation(out=osq2[t % 2][:], in_=po[:, :], func=AF.Square), reads=[pob], writes=[osq2b[t % 2]])

            def tail(t):
                tk = slice(t * 128, (t + 1) * 128)
                osb_, osbb_, osq_, osqb_ = osb2[t % 2], osb2b[t % 2], osq2[t % 2], osq2b[t % 2]
                k.op("dve", lambda e: e.reduce_sum(out=sm[:, 0, :], in_=osb_[:, :].rearrange("p (h v) -> p h v", h=4), axis=AX.X), reads=[osbb_], writes=[smb])
                k.op("dve", lambda e: e.reduce_sum(out=sm[:, 1, :], in_=osq_[:, :].rearrange("p (h v) -> p h v", h=4), axis=AX.X), reads=[osqb_, smb], writes=[smb])
                k.op("dve", lambda e: e.tensor_scalar(out=sm[:, 2, :], in0=sm[:, 0, :], scalar1=1.0 / 128, scalar2=None, op0=ALU.mult), reads=[smb], writes=[smb])
                k.op("dve", lambda e: e.tensor_tensor(out=sm[:, 3, :], in0=sm[:, 2, :], in1=sm[:, 2, :], op=ALU.mult), reads=[smb], writes=[smb])
                k.op("dve", lambda e: e.scalar_tensor_tensor(out=sm[:, 3, :], in0=sm[:, 1, :], scalar=1.0 / 128, in1=sm[:, 3, :], op0=ALU.mult, op1=ALU.subtract),
                     reads=[smb], writes=[smb])
                k.op("act", lambda e: e.activation(out=sm[:, 4, :], in_=sm[:, 3, :], func=AF.Sqrt, bias=self.cst[:, 0:1], scale=1.0), reads=[smb, self.cb], writes=[smb])
                k.op("dve", lambda e: e.reciprocal(out=sm[:, 5, :], in_=sm[:, 4, :]), reads=[smb], writes=[smb])
                for h in range(4):
                    hc = slice(h * 128, (h + 1) * 128)
                    k.op("dve", lambda e, h=h, hc=hc: e.tensor_scalar(out=yn[:, hc], in0=osb_[:, hc], scalar1=sm[:, 2, h:h + 1], scalar2=sm[:, 5, h:h + 1],
                                                                      op0=ALU.subtract, op1=ALU.mult), reads=[osbb_, smb], writes=[ynb])
                k.op("dve", lambda e: e.tensor_tensor(out=yn[:], in0=yn[:], in1=gn[:], op=ALU.mult), reads=[ynb, rc], writes=[ynb])
                k.op("dve", lambda e: e.tensor_tensor(out=orb[:], in0=yn[:], in1=gs2[t % 2][:], op=ALU.mult), reads=[ynb, gs2b[t % 2]], writes=[orbb])
                for j in range(4):
                    k.op("pe", lambda e, j=j: e.transpose(ptr2[:, j * 128:(j + 1) * 128], orb[:, j * 128:(j + 1) * 128], self.ident[:]),
                         reads=[orbb, self.cb], writes=[ptr2b])
                i = t % 2
                k.op("act", lambda e, i=i: e.activation(out=orT[i][:], in_=ptr2[:, 0:512].rearrange("p (j c) -> p j c", j=4), func=AF.Copy),
                     reads=[ptr2b], writes=[orTb[i]])
                k.dma("sp", self.brT[1, :, :, tk].rearrange("c p q -> p c q"), orT[i][:], reads=[orTb[i]], writes=[self.brT_b[1][t]])

            front(0)
            mid(0)
            for t in range(NT):
                if t + 1 < NT:
                    front(t + 1)
                    mid(t + 1)
                tail(t)

    def phase_conv(self, l):
        k = self.k
        with ExitStack() as st:
            Wa = self.sb(st, "cWa", [128, 8, 512], BF16)
            Wb = self.sb(st, "cWb", [128, 8, 512], BF16)
            wab = Buf("cW")
            self.load_w(Wa, self.din["w_in"][l][:, C_CA:C_CA + 512], wab, 8)
            self.load_w(Wb, self.din["w_in"][l][:, C_CB:C_CB + 512], wab, 8)
            cw = self.sb(st, "ccw", [128, 4, 31], F32)
            cvec = self.sb(st, "cvec", [128, 3, 4], F32)
            ones = self.sb(st, "cones", [128, 128], F32)
            cc = Buf("cconst")
            k.dma("sp", cw[:], self.din["convw"][l], writes=[cc])
            k.dma("sp", cvec[:, 0, :], self.din["convb"][l], writes=[cc])
            k.dma("sp", cvec[:, 1, :], self.din["convg"][l], writes=[cc])
            k.dma("sp", cvec[:, 2, :], self.din["convbb"][l], writes=[cc])
            k.dma("sp", ones[:], self.din["ones"][:, :], writes=[cc])
            Dg = self.sb(st, "cDg", [128, 4, 31, 128], BF16); dgb = Buf("cDg")
            for ct in range(4):
                for w in range(31):
                    en = "dve" if (w % 2 == 0) else "pool"
                    k.op(en, lambda e, ct=ct, w=w: e.tensor_scalar(out=Dg[:, ct, w, :], in0=self.identf[:], scalar1=cw[:, ct, w:w + 1], scalar2=None, op0=ALU.mult),
                         reads=[cc, self.cb], writes=[dgb])
            u = [self.sb(st, "cu%d" % i, [128, 4, 542], BF16) for i in range(2)]
            ub = [[Buf("cu%d_%d" % (i, ct)) for ct in range(4)] for i in range(2)]
            acc = self.sb(st, "cacc", [128, 4, 512], F32)
            accb = [Buf("cacc%d" % ct) for ct in range(4)]
            sg = [self.sb(st, "csg%d" % i, [128, 512], F32) for i in range(2)]
            sgb = [Buf("csg%d" % i) for i in range(2)]
            ysq = [self.sb(st, "cysq%d" % i, [128, 512], F32) for i in range(2)]
            ysqb = [Buf("cysq%d" % i) for i in range(2)]
            stt = self.sb(st, "cstt", [128, 4, 512], F32)
            sttb = Buf("cstt")
            yn = [self.sb(st, "cyn%d" % i, [128, 512], F32) for i in range(2)]
            ynb = [Buf("cyn%d" % i) for i in range(2)]
            oc = [self.sb(st, "coc%d" % i, [128, 512], BF16) for i in range(2)]
            ocb = [Buf("coc%d" % i) for i in range(2)]
            pa = [self.ps(st, "cpa%d" % i, [128, 512], F32) for i in range(2)]
            pab = [Buf("cpa%d" % i) for i in range(2)]
            pbk = [self.ps(st, "cpb%d" % i, [128, 512], F32) for i in range(2)]
            pbb = [Buf("cpb%d" % i) for i in range(2)]
            pcv = [self.ps(st, "cpc%d" % i, [128, 512], F32) for i in range(2)]
            pcb = [Buf("cpc%d" % i) for i in range(2)]
            s1 = self.ps(st, "cs1", [128, 512], F32)
            s2 = self.ps(st, "cs2", [128, 512], F32)
            s1b, s2b = Buf("cs1"), Buf("cs2")
            for ct in range(4):
                k.op("dve", lambda e, ct=ct: e.memset(u[0][:, ct, 0:30], 0.0), writes=[ub[0][ct]])
            for G in range(8):
                toks = slice(G * 512, (G + 1) * 512)
                hb = self.hT_b[G * 4:(G + 1) * 4]
                ug, ugb = u[G % 2], ub[G % 2]
                for ct in range(4):
                    p = ct % 2
                    cols = slice(ct * 128, (ct + 1) * 128)
                    for kc in range(8):
                        k.op("pe", lambda e, kc=kc, cols=cols, p=p: e.matmul(pa[p][:, :], lhsT=Wa[:, kc, cols], rhs=self.hT[:, kc, toks],
                                                                              start=(kc == 0), stop=(kc == 7)), reads=[wab] + hb, writes=[pab[p]])
                    for kc in range(8):
                        k.op("pe", lambda e, kc=kc, cols=cols, p=p: e.matmul(pbk[p][:, :], lhsT=Wb[:, kc, cols], rhs=self.hT[:, kc, toks],
                                                                              start=(kc == 0), stop=(kc == 7)), reads=[wab] + hb, writes=[pbb[p]])
                    k.op("act", lambda e, p=p: e.activation(out=sg[p][:], in_=pbk[p][:, :], func=AF.Sigmoid), reads=[pbb[p]], writes=[sgb[p]])
                    if G > 0:
                        k.op("pool", lambda e, ct=ct: e.tensor_copy(out=ug[:, ct, 0:30], in_=u[(G - 1) % 2][:, ct, 512:542]),
                             reads=[ub[(G - 1) % 2][ct]], writes=[ugb[ct]])
                    k.op("dve", lambda e, ct=ct, p=p: e.tensor_tensor(out=ug[:, ct, 30:542], in0=pa[p][:, :], in1=sg[p][:], op=ALU.mult),
                         reads=[pab[p], sgb[p]], writes=[ugb[ct]])
                for ct in range(4):
                    p = ct % 2
                    for w in range(31):
                        k.op("pe", lambda e, ct=ct, w=w, p=p: e.matmul(pcv[p][:, :], lhsT=Dg[:, ct, w, :], rhs=ug[:, ct, w:w + 512], start=(w == 0), stop=(w == 30)),
                             reads=[dgb, ugb[ct]], writes=[pcb[p]])
                    k.op("act", lambda e, ct=ct, p=p: e.activation(out=acc[:, ct, :], in_=pcv[p][:, :], func=AF.Identity, bias=cvec[:, 0, ct:ct + 1], scale=1.0),
                         reads=[pcb[p], cc], writes=[accb[ct]])
                    k.op("act", lambda e, ct=ct, p=p: e.activation(out=ysq[p][:], in_=acc[:, ct, :], func=AF.Square), reads=[accb[ct]], writes=[ysqb[p]])
                    k.op("pe", lambda e, ct=ct: e.matmul(s1[:, :], lhsT=ones[:], rhs=acc[:, ct, :], start=(ct == 0), stop=(ct == 3)),
                         reads=[cc, accb[ct]], writes=[s1b])
                    k.op("pe", lambda e, ct=ct, p=p: e.matmul(s2[:, :], lhsT=ones[:], rhs=ysq[p][:], start=(ct == 0), stop=(ct == 3)),
                         reads=[cc, ysqb[p]], writes=[s2b])
                k.op("act", lambda e: e.activation(out=stt[:, 0, :], in_=s1[:, :], func=AF.Copy, scale=1.0 / 512), reads=[s1b], writes=[sttb])
                k.op("dve", lambda e: e.tensor_tensor(out=stt[:, 1, :], in0=stt[:, 0, :], in1=stt[:, 0, :], op=ALU.mult), reads=[sttb], writes=[sttb])
                k.op("dve", lambda e: e.scalar_tensor_tensor(out=stt[:, 1, :], in0=s2[:, :], scalar=1.0 / 512, in1=stt[:, 1, :],
                                                             op0=ALU.mult, op1=ALU.subtract), reads=[s2b, sttb], writes=[sttb])
                k.op("act", lambda e: e.activation(out=stt[:, 3, :], in_=stt[:, 1, :], func=AF.Sqrt, bias=self.cst[:, 0:1], scale=1.0),
                     reads=[sttb, self.cb], writes=[sttb])
                k.op("dve", lambda e: e.reciprocal(out=stt[:, 2, :], in_=stt[:, 3, :]), reads=[sttb], writes=[sttb])
                for ct in range(4):
                    p = ct % 2
                    k.op("dve", lambda e, ct=ct, p=p: e.tensor_tensor(out=yn[p][:], in0=acc[:, ct, :], in1=stt[:, 0, :], op=ALU.subtract),
                         reads=[accb[ct], sttb], writes=[ynb[p]])
                    k.op("pool", lambda e, p=p: e.tensor_tensor(out=yn[p][:], in0=yn[p][:], in1=stt[:, 2, :], op=ALU.mult),
                         reads=[sttb, ynb[p]], writes=[ynb[p]])
                    k.op("act", lambda e, ct=ct, p=p: e.activation(out=oc[p][:], in_=yn[p][:], func=AF.Silu, scale=cvec[:, 1, ct:ct + 1],
                                                                   bias=cvec[:, 2, ct:ct + 1]), reads=[ynb[p], cc], writes=[ocb[p]])
                    k.dma("sp", self.brT[2, ct, :, toks], oc[p][:], reads=[ocb[p]], writes=self.brT_b[2][G * 4:(G + 1) * 4])

    def phase_merge(self, l):
        k = self.k
        for dh in range(2):
            with ExitStack() as st:
                Wg = self.sb(st, "mWg", [128, 8, 3, 512], BF16)
                Wbr = self.sb(st, "mWbr", [128, 3, 4, 512], BF16)
                Wo = self.sb(st, "mWo", [128, 4, 1024], BF16)
                wgb = [Buf("mWg%d" % i) for i in range(3)]
                wbrb = [Buf("mWbr%d" % i) for i in range(3)]
                wob = Buf("mWo")
                for b in range(3):
                    for kc in range(8):
                        c0 = C_MG + b * 1024 + dh * 512
                        k.dma("pool", Wg[:, kc, b, :], self.din["w_in"][l][kc * 128:(kc + 1) * 128, c0:c0 + 512], writes=[wgb[b]])
                    for c4 in range(4):
                        k.dma("pool", Wbr[:, b, c4, :], self.din["w_branch"][l][b][c4 * 128:(c4 + 1) * 128, dh * 512:(dh + 1) * 512], writes=[wbrb[b]])
                for dc in range(4):
                    r0 = dh * 512 + dc * 128
                    k.dma("pool", Wo[:, dc, :], self.din["w_out"][l][r0:r0 + 128, :], writes=[wob])
                brg = [self.sb(st, "mbr%d" % i, [128, 3, 4, 512], BF16) for i in range(2)]
                brgb = [Buf("mbr%d" % i) for i in range(2)]
                mT = self.sb(st, "mmT", [128, 4, 512], BF16)
                mTb = [Buf("mT%d" % i) for i in range(4)]
                gate = [self.sb(st, "mgate%d" % i, [128, 512], F32) for i in range(2)]
                gateb = [Buf("mgate%d" % i) for i in range(2)]
                macc = self.sb(st, "mmacc", [128, 512], F32); maccb = Buf("macc")
                mtmp = self.sb(st, "mmtmp", [128, 512], F32); mtmpb = Buf("mtmp")
                ctx = self.upd_alloc(st, "m", self.din["nmlp"][l] if dh == 1 else None)
                pg = [self.ps(st, "mpg%d" % i, [128, 512], F32) for i in range(2)]
                pgb = [Buf("mpg%d" % i) for i in range(2)]
                pp = [self.ps(st, "mpp%d" % i, [128, 512], F32) for i in range(2)]
                ppb = [Buf("mpp%d" % i) for i in range(2)]
                po = [self.ps(st, "mpo%d" % i, [128, 512], F32) for i in range(2)]
                pob = [Buf("mpo%d" % i) for i in range(2)]
                if dh == 1:
                    ctx["ptr"] = self.ps(st, "mptr", [128, 1024], BF16)
                    ctx["ptr_b"] = Buf("mptr")
                it = 0
                for G in range(8):
                    toks = slice(G * 512, (G + 1) * 512)
                    hb = self.hT_b[G * 4:(G + 1) * 4]
                    bi = G % 2
                    for b in range(3):
                        k.dma("sp", brg[bi][:, b], self.brT[b, :, :, toks].rearrange("c p t -> p c t"),
                              reads=self.brT_b[b][G * 4:(G + 1) * 4], writes=[brgb[bi]])
                    for dc in range(4):
                        for b in range(3):
                            p = it % 2
                            it += 1
                            for kc in range(8):
                                k.op("pe", lambda e, kc=kc, b=b, dc=dc, p=p: e.matmul(pg[p][:, :], lhsT=Wg[:, kc, b, dc * 128:(dc + 1) * 128],
                                                                                     rhs=self.hT[:, kc, toks], start=(kc == 0), stop=(kc == 7)),
                                     reads=[wgb[b]] + hb, writes=[pgb[p]])
                            for c4 in range(4):
                                k.op("pe", lambda e, c4=c4, b=b, dc=dc, p=p: e.matmul(pp[p][:, :], lhsT=Wbr[:, b, c4, dc * 128:(dc + 1) * 128],
                                                                                     rhs=brg[bi][:, b, c4, :], start=(c4 == 0), stop=(c4 == 3)),
                                     reads=[wbrb[b], brgb[bi]], writes=[ppb[p]])
                            k.op("act", lambda e, p=p: e.activation(out=gate[p][:], in_=pg[p][:, :], func=AF.Sigmoid), reads=[pgb[p]], writes=[gateb[p]])
                            if b == 0:
                                k.op("dve", lambda e, p=p: e.tensor_tensor(out=macc[:], in0=pp[p][:, :], in1=gate[p][:], op=ALU.mult),
                                     reads=[ppb[p], gateb[p]], writes=[maccb])
                            else:
                                k.op("dve", lambda e, p=p: e.tensor_tensor(out=mtmp[:], in0=pp[p][:, :], in1=gate[p][:], op=ALU.mult),
                                     reads=[ppb[p], gateb[p]], writes=[mtmpb])
                                if b == 1:
                                    k.op("pool", lambda e: e.tensor_tensor(out=macc[:], in0=macc[:], in1=mtmp[:], op=ALU.add),
                                         reads=[mtmpb, maccb], writes=[maccb])
                                else:
                                    k.op("pool", lambda e, dc=dc: e.tensor_tensor(out=mT[:, dc, :], in0=macc[:], in1=mtmp[:], op=ALU.add),
                                         reads=[mtmpb, maccb], writes=[mTb[dc]])
                    for tt in range(4):
                        t = G * 4 + tt
                        for nh in range(2):
                            for dc in range(4):
                                k.op("pe", lambda e, nh=nh, dc=dc, tt=tt: e.matmul(po[nh][:, :], lhsT=mT[:, dc, tt * 128:(tt + 1) * 128],
                                                                                  rhs=Wo[:, dc, nh * 512:(nh + 1) * 512], start=(dc == 0), stop=(dc == 3)),
                                     reads=[wob, mTb[dc]], writes=[pob[nh]])
                        self.x_update(ctx, t, [po[0][:, :], po[1][:, :]], pob, "norm" if dh == 1 else None)
                self.upd_flush(ctx)
                k.barrier()


_PROG_CACHE = {}


def _get_prog(**kw):
    key = tuple(sorted((k, str(v)) for k, v in kw.items()))
    if key not in _PROG_CACHE:
        _PROG_CACHE[key] = Prog(**kw)
    return _PROG_CACHE[key]


def kernel(**inputs):
    inp = {k: np.asarray(v) for k, v in inputs.items()}
    x = np.ascontiguousarray(inp["x"], dtype=np.float32)
    shared = _host_inputs(inp)
    prog = _get_prog()
    in_maps = []
    for c in range(8):
        m = dict(shared)
        m["x"] = x[c]
        in_maps.append(m)
    res = run_bass_kernel_spmd(prog.nc, in_maps, core_ids=list(range(8)))
    return np.stack([np.asarray(r["out"], dtype=np.float32) for r in res.results], axis=0)
```
